# Optimizing a Trainium2 kernel written in Bass

```python
import math
import jax
import jax.numpy as jnp
from jax import lax
import numpy as np

D_MODEL = 1024
BATCH = 8
SEQ = 4096
DEPTH = 1

D_MIX = D_MODEL
DA_HEADS = 4
DA_HEAD_DIM = 64
DA_VAL_DIM = 2 * DA_HEAD_DIM
DA_QK = DA_HEADS * 2 * DA_HEAD_DIM
DA_WIDTH = DA_HEADS * DA_VAL_DIM
ML_HEADS = 4
ML_HEAD_DIM = (D_MIX - DA_WIDTH) // ML_HEADS
ML_WIDTH = ML_HEADS * ML_HEAD_DIM
ML_CONV = 4
ML_CHUNK = 64
IN_SPLITS = (DA_QK, DA_QK, DA_WIDTH, ML_WIDTH, ML_WIDTH, ML_WIDTH, ML_WIDTH, ML_HEADS, ML_HEADS)
N_IN = sum(IN_SPLITS)
MEM_LEN = 256
X_HEADS = 4
X_HEAD_DIM = D_MODEL // X_HEADS
N_GROUPS = 4
EXPERTS_PER_GROUP = 8
N_EXPERTS = N_GROUPS * EXPERTS_PER_GROUP
TOP_K = 2
D_EXPERT = 512
MOE_BLOCK = 128
Q_BLOCK = 128
ROPE_THETA = 10000.0
EPS = 1e-6

kernel_name = 'hybrid_diffattn_mlstm_hmoe_layer'


def rms_norm(x, g):
    xf = x.astype(jnp.float32)
    y = xf * lax.rsqrt(jnp.mean(xf * xf, axis=-1, keepdims=True) + EPS)
    return (y * g.astype(jnp.float32)).astype(x.dtype)


def rotary(t, pos):
    d = t.shape[-1]
    inv_freq = ROPE_THETA ** (-jnp.arange(0, d, 2, dtype=jnp.float32) / d)
    ang = pos.astype(jnp.float32)[:, None] * inv_freq[None, :]
    cos = jnp.cos(ang).astype(t.dtype)
    sin = jnp.sin(ang).astype(t.dtype)
    t1, t2 = t[..., : d // 2], t[..., d // 2:]
    return jnp.concatenate([t1 * cos - t2 * sin, t2 * cos + t1 * sin], axis=-1)


def diff_attention(q, k, v, lam, subln_g, lam_init):
    bsz, seq = q.shape[0], q.shape[1]
    pos = jnp.arange(seq)
    q = rotary(q.transpose(0, 2, 3, 1, 4), pos) * (DA_HEAD_DIM ** -0.5)
    k = rotary(k.transpose(0, 2, 3, 1, 4), pos)
    v = v.transpose(0, 2, 1, 3)
    n_blk = seq // Q_BLOCK
    q_blocks = q.reshape(bsz, DA_HEADS, 2, n_blk, Q_BLOCK, DA_HEAD_DIM).transpose(3, 0, 1, 2, 4, 5)
    k_pos = jnp.arange(seq)

    def one_block(args):
        q_blk, start = args
        s = jnp.einsum('bhmqd,bhmkd->bhmqk', q_blk, k).astype(jnp.float32)
        q_pos = start + jnp.arange(Q_BLOCK)
        causal = k_pos[None, :] <= q_pos[:, None]
        p = jax.nn.softmax(jnp.where(causal, s, -jnp.inf), axis=-1)
        a = p[:, :, 0] - lam * p[:, :, 1]
        return jnp.einsum('bhqk,bhkd->bhqd', a.astype(v.dtype), v)

    o = lax.map(one_block, (q_blocks, jnp.arange(n_blk) * Q_BLOCK))
    o = o.transpose(1, 0, 3, 2, 4).reshape(bsz, seq, DA_HEADS, DA_VAL_DIM)
    o = rms_norm(o, subln_g) * (1.0 - lam_init)
    return o.reshape(bsz, seq, DA_WIDTH)


def causal_depthwise_conv(u, w, b):
    c = u.shape[-1]
    y = lax.conv_general_dilated(u, w.astype(u.dtype), window_strides=(1,),
                                 padding=[(ML_CONV - 1, 0)],
                                 dimension_numbers=('NWC', 'WIO', 'NWC'),
                                 feature_group_count=c)
    return y + b.astype(u.dtype)


def mlstm_chunkwise(q, k, v, o_pre, i_pre, f_pre, norm_g):
    f32 = jnp.float32
    bsz, seq = q.shape[0], q.shape[1]
    n_ch = seq // ML_CHUNK

    def heads(t):
        return t.astype(f32).reshape(bsz, n_ch, ML_CHUNK, ML_HEADS, ML_HEAD_DIM).transpose(1, 0, 3, 2, 4)

    def gates(t):
        return t.astype(f32).reshape(bsz, n_ch, ML_CHUNK, ML_HEADS).transpose(1, 0, 3, 2)

    qc, kc, vc = heads(q), heads(k) * (ML_HEAD_DIM ** -0.5), heads(v)
    ic = gates(i_pre)
    lfc = jax.nn.log_sigmoid(gates(f_pre))
    tri = jnp.tril(jnp.ones((ML_CHUNK, ML_CHUNK), dtype=bool))

    def step(carry, xs):
        c_mat, n_vec, m_prev = carry
        qj, ks, vs, ig, lf = xs
        a = jnp.cumsum(lf, axis=-1)
        log_d = jnp.where(tri, a[..., :, None] - a[..., None, :] + ig[..., None, :], -jnp.inf)
        log_inter = a + m_prev[..., None]
        m_row = jnp.maximum(log_inter, jnp.max(log_d, axis=-1))
        w_intra = jnp.exp(log_d - m_row[..., None])
        w_inter = jnp.exp(log_inter - m_row)
        s = jnp.einsum('bhjd,bhsd->bhjs', qj, ks) * w_intra
        num = (w_inter[..., None] * jnp.einsum('bhjd,bhde->bhje', qj, c_mat)
               + jnp.einsum('bhjs,bhse->bhje', s, vs))
        den = w_inter * jnp.einsum('bhjd,bhd->bhj', qj, n_vec) + jnp.sum(s, axis=-1)
        h = num / jnp.maximum(jnp.abs(den), jnp.exp(-m_row))[..., None]
        g = a[..., -1]
        log_w = g[..., None] - a + ig
        m_new = jnp.maximum(g + m_prev, jnp.max(log_w, axis=-1))
        w_state = jnp.exp(log_w - m_new[..., None])
        decay = jnp.exp(g + m_prev - m_new)
        c_new = decay[..., None, None] * c_mat + jnp.einsum('bhs,bhsd,bhse->bhde', w_state, ks, vs)
        n_new = decay[..., None] * n_vec + jnp.einsum('bhs,bhsd->bhd', w_state, ks)
        return (c_new, n_new, m_new), h

    init = (jnp.zeros((bsz, ML_HEADS, ML_HEAD_DIM, ML_HEAD_DIM), f32),
            jnp.zeros((bsz, ML_HEADS, ML_HEAD_DIM), f32),
            jnp.zeros((bsz, ML_HEADS), f32))
    _, h = lax.scan(step, init, (qc, kc, vc, ic, lfc))
    h = h.transpose(1, 0, 3, 2, 4).reshape(bsz, seq, ML_HEADS, ML_HEAD_DIM)
    mu = jnp.mean(h, axis=-1, keepdims=True)
    var = jnp.mean(jnp.square(h - mu), axis=-1, keepdims=True)
    hn = (h - mu) * lax.rsqrt(var + EPS) * norm_g.astype(f32).reshape(ML_HEADS, ML_HEAD_DIM)
    o = jax.nn.sigmoid(o_pre.astype(f32)).reshape(bsz, seq, ML_HEADS, ML_HEAD_DIM)
    return (o * hn).reshape(bsz, seq, ML_WIDTH).astype(q.dtype)


def hybrid_mixer(h, w_in, w_out, da_lambda, da_subln_g, lam_init, ml_conv_w, ml_conv_b, ml_gate_b, ml_norm_g):
    bsz, seq = h.shape[0], h.shape[1]
    proj = h @ w_in
    dq, dk, dv, mq, mk, mv, mo, mi, mf = jnp.split(proj, np.cumsum(IN_SPLITS)[:-1].tolist(), axis=-1)
    lv = da_lambda.astype(jnp.float32)
    lam = jnp.exp(jnp.sum(lv[0] * lv[1])) - jnp.exp(jnp.sum(lv[2] * lv[3])) + lam_init
    y_da = diff_attention(dq.reshape(bsz, seq, DA_HEADS, 2, DA_HEAD_DIM),
                          dk.reshape(bsz, seq, DA_HEADS, 2, DA_HEAD_DIM),
                          dv.reshape(bsz, seq, DA_HEADS, DA_VAL_DIM), lam, da_subln_g, lam_init)
    qk = jax.nn.silu(causal_depthwise_conv(jnp.concatenate([mq, mk], axis=-1), ml_conv_w, ml_conv_b))
    mq_c, mk_c = jnp.split(qk, 2, axis=-1)
    y_ml = mlstm_chunkwise(mq_c, mk_c, mv, mo,
                           mi + ml_gate_b[0].astype(mi.dtype), mf + ml_gate_b[1].astype(mf.dtype), ml_norm_g)
    return jnp.concatenate([y_da, y_ml], axis=-1) @ w_out


def cross_attention(h, mem_n, wq, wk, wv, wo):
    bsz, seq = h.shape[0], h.shape[1]
    m_len = mem_n.shape[1]
    q = (h @ wq).reshape(bsz, seq, X_HEADS, X_HEAD_DIM)
    k = (mem_n @ wk).reshape(bsz, m_len, X_HEADS, X_HEAD_DIM)
    v = (mem_n @ wv).reshape(bsz, m_len, X_HEADS, X_HEAD_DIM)
    s = jnp.einsum('bqhd,bkhd->bhqk', q, k).astype(jnp.float32) * (X_HEAD_DIM ** -0.5)
    p = jax.nn.softmax(s, axis=-1)
    o = jnp.einsum('bhqk,bkhd->bqhd', p.astype(v.dtype), v).reshape(bsz, seq, D_MODEL)
    return o @ wo


def hierarchical_moe(h, w_rg, b_rg, w_re, b_re, w1, w3, w2):
    f32 = jnp.float32
    bsz, seq, d = h.shape
    n_tok = bsz * seq
    xt = h.reshape(n_tok, d)
    g_prob = jax.nn.softmax((xt @ w_rg).astype(f32) + b_rg.astype(f32), axis=-1)
    g_idx = jnp.argmax(g_prob, axis=-1).astype(jnp.int32)
    g_w = jnp.take_along_axis(g_prob, g_idx[:, None], axis=-1)[:, 0]
    e_logits = ((xt @ w_re).astype(f32) + b_re.astype(f32)).reshape(n_tok, N_GROUPS, EXPERTS_PER_GROUP)
    e_logits = jnp.take_along_axis(e_logits, g_idx[:, None, None], axis=1)[:, 0]
    top_p, top_i = lax.top_k(jax.nn.softmax(e_logits, axis=-1), TOP_K)
    comb = g_w[:, None] * top_p / jnp.sum(top_p, axis=-1, keepdims=True)
    expert = g_idx[:, None] * EXPERTS_PER_GROUP + top_i.astype(jnp.int32)
    n_asg = n_tok * TOP_K
    flat_e = expert.reshape(-1)
    flat_tok = jnp.repeat(jnp.arange(n_tok, dtype=jnp.int32), TOP_K)
    flat_w = comb.reshape(-1)
    order = jnp.argsort(flat_e)
    sorted_e, sorted_tok, sorted_w = flat_e[order], flat_tok[order], flat_w[order]
    counts = jnp.bincount(flat_e, length=N_EXPERTS).astype(jnp.int32)
    padded = (counts + MOE_BLOCK - 1) // MOE_BLOCK * MOE_BLOCK
    starts = jnp.cumsum(counts) - counts
    pad_ends = jnp.cumsum(padded)
    pad_starts = pad_ends - padded
    dest = pad_starts[sorted_e] + jnp.arange(n_asg, dtype=jnp.int32) - starts[sorted_e]
    n_blocks = -(-n_asg // MOE_BLOCK) + N_EXPERTS
    n_rows = n_blocks * MOE_BLOCK
    buf_tok = jnp.full((n_rows,), n_tok, jnp.int32).at[dest].set(sorted_tok)
    buf_w = jnp.zeros((n_rows,), f32).at[dest].set(sorted_w)
    block_e = jnp.clip(jnp.searchsorted(pad_ends, jnp.arange(n_blocks, dtype=jnp.int32) * MOE_BLOCK, side='right'),
                       0, N_EXPERTS - 1).astype(jnp.int32)
    xpad = jnp.concatenate([xt, jnp.zeros((1, d), xt.dtype)], axis=0)
    xb = xpad[buf_tok].reshape(n_blocks, MOE_BLOCK, d)

    def run_block(args):
        x_blk, e = args
        return (jax.nn.silu(x_blk @ w1[e]) * (x_blk @ w3[e])) @ w2[e]

    yb = lax.map(run_block, (xb, block_e)).reshape(n_rows, d)
    yb = yb * buf_w[:, None].astype(yb.dtype)
    out = jnp.zeros((n_tok + 1, d), yb.dtype).at[buf_tok].add(yb)[:n_tok]
    return out.reshape(bsz, seq, d)


def setup_inputs(seed: int = 0) -> dict:
    key = jax.random.key(seed)
    ks = jax.random.split(key, 32)
    f32 = jnp.float32
    L = DEPTH

    def nrm(k, shape, scale):
        return jax.random.normal(k, shape, f32) * scale

    def gain(k, shape):
        return 1.0 + 0.02 * jax.random.normal(k, shape, f32)

    f_bias = jnp.linspace(3.0, 6.0, ML_HEADS, dtype=f32)
    ml_gate_b = jnp.stack([nrm(ks[9], (L, ML_HEADS), 0.1),
                           f_bias[None, :] + nrm(ks[10], (L, ML_HEADS), 0.1)], axis=1)
    return {
        'x': nrm(ks[0], (BATCH, SEQ, D_MODEL), 1.0),
        'mem': nrm(ks[1], (BATCH, MEM_LEN, D_MODEL), 1.0),
        'norm_mix_g': gain(ks[2], (L, D_MODEL)),
        'w_in': nrm(ks[3], (L, D_MODEL, N_IN), D_MODEL ** -0.5),
        'w_out': nrm(ks[4], (L, D_MIX, D_MODEL), D_MIX ** -0.5),
        'da_lambda': nrm(ks[5], (L, 4, DA_HEAD_DIM), 0.1),
        'da_subln_g': gain(ks[6], (L, DA_VAL_DIM)),
        'ml_conv_w': nrm(ks[7], (L, ML_CONV, 1, 2 * ML_WIDTH), ML_CONV ** -0.5),
        'ml_conv_b': nrm(ks[8], (L, 2 * ML_WIDTH), 0.02),
        'ml_gate_b': ml_gate_b,
        'ml_norm_g': gain(ks[11], (L, ML_WIDTH)),
        'norm_x_g': gain(ks[12], (L, D_MODEL)),
        'norm_mem_g': gain(ks[13], (L, D_MODEL)),
        'w_xq': nrm(ks[14], (L, D_MODEL, D_MODEL), D_MODEL ** -0.5),
        'w_xk': nrm(ks[15], (L, D_MODEL, D_MODEL), D_MODEL ** -0.5),
        'w_xv': nrm(ks[16], (L, D_MODEL, D_MODEL), D_MODEL ** -0.5),
        'w_xo': nrm(ks[17], (L, D_MODEL, D_MODEL), D_MODEL ** -0.5),
        'norm_ffn_g': gain(ks[18], (L, D_MODEL)),
        'w_router_group': nrm(ks[19], (L, D_MODEL, N_GROUPS), D_MODEL ** -0.5),
        'b_router_group': nrm(ks[20], (L, N_GROUPS), 0.01),
        'w_router_expert': nrm(ks[21], (L, D_MODEL, N_EXPERTS), D_MODEL ** -0.5),
        'b_router_expert': nrm(ks[22], (L, N_EXPERTS), 0.01),
        'w1': nrm(ks[23], (L, N_EXPERTS, D_MODEL, D_EXPERT), D_MODEL ** -0.5),
        'w3': nrm(ks[24], (L, N_EXPERTS, D_MODEL, D_EXPERT), D_MODEL ** -0.5),
        'w2': nrm(ks[25], (L, N_EXPERTS, D_EXPERT, D_MODEL), D_EXPERT ** -0.5),
        'norm_final_g': gain(ks[26], (D_MODEL,)),
    }


def reference(x, mem, norm_mix_g, w_in, w_out, da_lambda, da_subln_g, ml_conv_w, ml_conv_b, ml_gate_b,
              ml_norm_g, norm_x_g, norm_mem_g, w_xq, w_xk, w_xv, w_xo, norm_ffn_g, w_router_group,
              b_router_group, w_router_expert, b_router_expert, w1, w3, w2, norm_final_g):
    for l in range(DEPTH):
        lam_init = 0.8 - 0.6 * math.exp(-0.3 * l)
        x = x + hybrid_mixer(rms_norm(x, norm_mix_g[l]), w_in[l], w_out[l], da_lambda[l], da_subln_g[l],
                             lam_init, ml_conv_w[l], ml_conv_b[l], ml_gate_b[l], ml_norm_g[l])
        x = x + cross_attention(rms_norm(x, norm_x_g[l]), rms_norm(mem, norm_mem_g[l]),
                                w_xq[l], w_xk[l], w_xv[l], w_xo[l])
        x = x + hierarchical_moe(rms_norm(x, norm_ffn_g[l]), w_router_group[l], b_router_group[l],
                                 w_router_expert[l], b_router_expert[l], w1[l], w3[l], w2[l])
    return rms_norm(x, norm_final_g)
```

```python
import numpy as np
from contextlib import ExitStack
import concourse.bass as bass
import concourse.mybir as mybir
from concourse.bass_utils import run_bass_kernel_spmd

F32 = mybir.dt.float32
BF16 = mybir.dt.bfloat16
I32 = mybir.dt.int32
AF = mybir.ActivationFunctionType
ALU = mybir.AluOpType
AX = mybir.AxisListType

S = 4096
D = 1024
NT = S // 128
NB = S // 512
EPS = 1e-6
MEM = 256
NEXP = 32
DEXP = 512
CAPB = 8
CAP = CAPB * 128
LAM_INIT = 0.2
NEG = -30000.0
STRICT = False

FM_COLS = 24 * 128
TM_COLS = 512 * 3 + 8
WIN_COLS = FM_COLS + TM_COLS


class Trk:
    def __init__(self, nc, needed=None):
        self.nc = nc
        self.needed_in = needed
        self.needed = {}
        self.phys = {}
        self.pcnt = {}
        self.eng = {'pe': nc.tensor, 'act': nc.scalar, 'dve': nc.vector, 'pool': nc.gpsimd, 'sp': nc.sync}
        self.sem = {}
        self.cnt = {}
        self.seen = {e: {} for e in self.eng}
        self.lastw = {}
        self.reads = {}
        self.stack = ExitStack()
        self.phase = 0
        self.nsem = 0
        self.pool = {'sw': [], 'hw': []}
        self.kind = {}

    def lane(self, name, eng='sp'):
        if name not in self.sem:
            kind = 'sw' if eng == 'pool' else 'hw'
            self.kind[name] = kind
            if self.pool[kind] and not name.startswith('eng_'):
                sh, c = self.pool[kind].pop()
                self.sem[name] = sh
                self.cnt[name] = c
            else:
                self.nsem += 1
                s = self.stack.enter_context(self.nc.semaphore("s%d" % self.nsem))
                self.sem[name] = s
                self.cnt[name] = 0
        return name

    def elane(self, eng):
        return "eng_%s" % eng

    def pval(self, ln, v):
        if not ln.startswith("eng_"):
            return v
        self.needed.setdefault(ln, set()).add(v)
        if self.needed_in is None:
            return v
        return self.phys[ln][v]

    def wait(self, eng, ticket):
        ln, v = ticket
        if self.seen[eng].get(ln, 0) < v:
            self.eng[eng].wait_ge(self.sem[ln], self.pval(ln, v))
            self.seen[eng][ln] = v

    def op(self, eng, fn, reads=(), writes=(), lane=None, inc=None, attach=None):
        deps = {}
        own = self.elane(eng)
        psr = [r for r in reads if isinstance(r, str) and r.startswith('ps:')]
        if psr:
            reads = [r for r in reads if r not in psr]
            writes = list(writes) + [r for r in psr if r not in writes]
        for r in reads:
            t = self.lastw.get(r)
            if t is not None:
                deps[t[0]] = max(deps.get(t[0], 0), t[1])
        for w in writes:
            t = self.lastw.get(w)
            if t is not None and (STRICT or t[0] != own):
                deps[t[0]] = max(deps.get(t[0], 0), t[1])
            for t in self.reads.get(w, ()):
                if STRICT or t[0] != own:
                    deps[t[0]] = max(deps.get(t[0], 0), t[1])
        for ln, v in deps.items():
            self.wait(eng, (ln, v))
        ins = fn(self.eng[eng])
        if attach is not None:
            t = self.lastw.get(attach)
            if t is not None:
                ins._wait_ge(self.sem[t[0]], self.pval(t[0], t[1]))
        if lane is None:
            lane = own
            inc = 1
        elif inc is None:
            inc = 16
        self.lane(lane, eng)
        self.cnt[lane] += inc
        t = (lane, self.cnt[lane])
        if lane.startswith("eng_"):
            if self.needed_in is None or t[1] in self.needed_in.get(lane, ()):
                ins.then_inc(self.sem[lane], 1)
                self.pcnt[lane] = self.pcnt.get(lane, 0) + 1
                self.phys.setdefault(lane, {})[t[1]] = self.pcnt[lane]
        else:
            ins.then_inc(self.sem[lane], inc)
        for r in reads:
            self.reads.setdefault(r, []).append(t)
        for w in writes:
            self.lastw[w] = t
            self.reads[w] = []
        return t

    def barrier(self):
        for e in self.eng:
            for ln, v in self.cnt.items():
                if v > 0:
                    self.wait(e, (ln, v))
        self.lastw = {}
        self.reads = {}
        self.phase += 1
        for ln in list(self.sem.keys()):
            if not ln.startswith("eng_"):
                self.pool[self.kind.pop(ln)].append((self.sem.pop(ln), self.cnt.pop(ln)))
                for e in self.eng:
                    self.seen[e].pop(ln, None)


def host_consts():
    c = {}
    c['ident_f'] = np.eye(128, dtype=np.float32)
    k = np.arange(128)[:, None]
    q = np.arange(128)[None, :]
    c['maskb'] = np.where(k <= q, 0.0, NEG).astype(np.float32)
    c['tri_incl'] = (k <= q).astype(np.float32)
    inv_freq = (10000.0 ** (-np.arange(0, 64, 2, dtype=np.float32) / np.float32(64))).astype(np.float32)
    pos = np.arange(S, dtype=np.float32)
    ang = (pos[:, None] * inv_freq[None, :]).astype(np.float32)
    cs = np.cos(ang).astype(np.float32).T
    sn = np.sin(ang).astype(np.float32).T
    cosT = np.zeros((128, S), np.float32)
    sinT = np.zeros((128, S), np.float32)
    for p in range(128):
        d = p % 64
        cosT[p] = cs[d % 32]
        sinT[p] = -sn[d % 32] if d < 32 else sn[d % 32]
    c['cosT'] = cosT
    c['sinT'] = sinT
    return c


def win_perm():
    idx = []
    off_q, off_k, off_v = 0, 512, 1024
    off_mq, off_mk, off_mv, off_mo, off_mi, off_mf = 1536, 2048, 2560, 3072, 3584, 3588

    def sw(base):
        out = []
        for m in range(2):
            b = base + m * 64
            out += list(range(b + 32, b + 64)) + list(range(b, b + 32))
        return out
    for h in range(4):
        idx += list(range(off_q + h * 128, off_q + (h + 1) * 128))
        idx += sw(off_q + h * 128)
        idx += list(range(off_k + h * 128, off_k + (h + 1) * 128))
        idx += sw(off_k + h * 128)
    idx += list(range(off_mq, off_mq + 512))
    idx += list(range(off_mk, off_mk + 512))
    idx += list(range(off_v, off_v + 512))
    idx += list(range(off_mv, off_mv + 512))
    idx += list(range(off_mo, off_mo + 512))
    idx += list(range(off_mi, off_mi + 4)) + list(range(off_mf, off_mf + 4))
    assert len(idx) == WIN_COLS
    return np.array(idx)


class K:
    def __init__(self, debug=None, needed=None):
        self.debug = debug or ()
        nc = bass.Bass("TRN2", target_bir_lowering=False)
        self.nc = nc
        self.T = Trk(nc, needed)
        self.inp = {}
        self.scr = {}
        self.dmaq = 0

    def din(self, name, shape, dt=F32):
        kb = self

        class Lazy:
            def _get(s_):
                if name not in kb.inp:
                    kb.inp[name] = kb.nc.dram_tensor(name, list(shape), dt, kind="ExternalInput").ap()
                return kb.inp[name]

            def __getitem__(s_, k):
                return s_._get()[k]

            def __getattr__(s_, a):
                return getattr(s_._get(), a)
        return Lazy()

    def dscr(self, name, shape, dt):
        kind = "ExternalOutput" if name in self.debug else "Internal"
        self.scr[name] = self.nc.dram_tensor(name, list(shape), dt, kind=kind).ap()
        return self.scr[name]

    def dma(self, eng, out, in_, reads, writes, lane, **kw):
        return self.T.op(eng, lambda e: e.dma_start(out=out, in_=in_, **kw), reads=reads, writes=writes, lane=lane)

    def op(self, eng, fn, reads=(), writes=(), attach=None):
        if eng == 'pe' and attach is None and reads:
            attach = reads[0]
        return self.T.op(eng, fn, reads=reads, writes=writes, attach=attach)


def build(debug=None, stop_after=None, skip12=False, p3_tiles=NT):
    rec = _build(debug, stop_after, skip12, p3_tiles, None)
    return _build(debug, stop_after, skip12, p3_tiles, rec.T.needed)


def _build(debug, stop_after, skip12, p3_tiles, needed):
    kb = K(debug, needed)
    nc, T = kb.nc, kb.T
    din, dscr = kb.din, kb.dscr
    x_d = din("x", [S, D])
    mem_d = din("mem", [MEM, D])
    win_d = din("w_in_ext", [D, WIN_COLS])
    wout_d = din("w_out", [D, D])
    gT_d = din("gT", [128, 4 * 8])
    gfin_d = din("g_final", [D])
    gffn_d = din("g_ffn_row", [D])
    tris_d = din("tri_strict", [128, 128])
    eoff_d = din("eoff", [NEXP])
    tokid_d = din("tokid", [128, NT], I32)
    cos_d = din("cosT", [128, S])
    sin_d = din("sinT", [128, S])
    ident_d = din("ident_f", [128, 128])
    maskb_d = din("maskb", [128, 128])
    tri_d = din("tri_incl", [128, 128])
    lam_d = din("da_lambda", [256])
    subln_d = din("da_subln_g", [128])
    convw_d = din("convT", [128, 8 * 5])
    gateb_d = din("ml_gate_b", [8])
    mlg_d = din("ml_norm_g", [512])
    wxq_d = din("w_xq", [D, D]); wxk_d = din("w_xk", [D, D]); wxv_d = din("w_xv", [D, D]); wxo_d = din("w_xo", [D, D])
    wr_d = din("w_router", [D, 36])
    br_d = din("b_router", [36])
    w1_d = din("w1", [NEXP, D, DEXP]); w3_d = din("w3", [NEXP, D, DEXP]); w2_d = din("w2", [NEXP, DEXP, D])
    out_d = nc.dram_tensor("out", [S, D], F32, kind="ExternalOutput").ap()

    qT_s = dscr("qT_s", [4, 128, S], BF16)
    kT_s = dscr("kT_s", [4, 128, S], BF16)
    mqT_s = dscr("mqT_s", [4, 128, S], BF16)
    mkT_s = dscr("mkT_s", [4, 128, S], BF16)
    tmb_s = dscr("tmb_s", [S, 1536], BF16)
    gate_s = dscr("gate_s", [128, NT, 8], F32)
    y_s = dscr("y_s", [S, D], BF16)
    x1_s = dscr("x1_s", [S, D], F32)
    x2_s = dscr("x2_s", [S, D], F32)
    Xs_d = dscr("Xs_d", [NEXP * CAP, D], BF16)
    Y_d = dscr("Y_d", [NEXP * CAP, D], BF16)

    es_all = ExitStack()

    def sbuf(es, name, shape, dt):
        return es.enter_context(nc.sbuf_tensor("sb_" + name, list(shape), dt))

    def psum(es, name, shape, dt):
        return es.enter_context(nc.psum_tensor("ps_" + name, list(shape), dt))

    ident_f = sbuf(es_all, "ident_f", [128, 128], F32)
    ident_b = sbuf(es_all, "ident_b", [128, 128], BF16)
    gT = sbuf(es_all, "gT", [128, 32], F32)
    kb.dma('sp', ident_f[:], ident_d[:, :], [], ['ident_f'], 'c0')
    kb.dma('sp', gT[:], gT_d[:, :], [], ['gT'], 'c1')
    kb.dma('pool', ident_b[:], ident_d[:, :], [], ['ident_b'], 'c2')
    slot_i = sbuf(es_all, "slot_i", [128, NT, 2], I32)
    comb_w = sbuf(es_all, "comb_w", [128, NT, 2], F32)
    ones_bb = sbuf(es_all, "ones_bb", [128, 128], BF16)
    kb.op('pool', lambda e: e.memset(ones_bb[:], 1.0), [], ['ones_bb'])
    es = ExitStack()
    if True:
        zt = sbuf(es, "zt", [128, 8192], BF16)
        kb.op('pool', lambda e: e.memset(zt[:], 0.0), [], ['zt'])
        for e_ in range(NEXP):
            kb.dma('pool', Xs_d[e_ * CAP:(e_ + 1) * CAP, :].rearrange("(p r) c -> p (r c)", p=128), zt[:, 0:CAP * D // 128], ['zt'], [('Xs0', e_)], 'zX%d' % (e_ % 4))

    hT = sbuf(es, "hT", [128, 8, S], BF16)
    cosT = sbuf(es, "cosT", [128, S], F32)
    sinT = sbuf(es, "sinT", [128, S], F32)
    convT = sbuf(es, "convT", [128, 40], F32)
    kb.dma('sp', cosT[:], cos_d[:, :], [], ['cosT'], 'c3')
    kb.dma('sp', sinT[:], sin_d[:, :], [], ['sinT'], 'c4')
    kb.dma('sp', convT[:], convw_d[:, :], [], ['convT'], 'c5')

    def norm_transpose(es_, tagp, x_src_tiles, gcol, dstT, ntiles, pT_tiles, dst_tag='hT_t'):
        xt = [sbuf(es_, "%s_xt%d" % (tagp, i), [128, D], F32) for i in range(3)]
        junk = sbuf(es_, tagp + "_junk", [128, D], BF16)
        ssq = [sbuf(es_, "%s_ssq%d" % (tagp, i), [128, 1], F32) for i in range(2)]
        rstd = [sbuf(es_, "%s_rstd%d" % (tagp, i), [128, 1], F32) for i in range(2)]
        xs = [sbuf(es_, "%s_xs%d" % (tagp, i), [128, D], BF16) for i in range(2)]
        for t in range(ntiles):
            a, b = t % 3, t % 2
            kb.dma('sp', xt[a][:], x_src_tiles(t), [], [tagp + 'xt%d' % a], tagp + 'ld%d' % a)
            kb.op('act', lambda e: e.activation(out=junk[:], in_=xt[a][:], func=AF.Square, accum_out=ssq[b][:]),
                  [tagp + 'xt%d' % a], [tagp + 'junk', tagp + 'ssq%d' % b])
            kb.op('act', lambda e: e.activation(out=rstd[b][:], in_=ssq[b][:], func=AF.Sqrt, scale=1.0 / D, bias=EPS),
                  [tagp + 'ssq%d' % b], [tagp + 'rstd%d' % b])
            kb.op('dve', lambda e: e.reciprocal(out=rstd[b][:], in_=rstd[b][:]),
                  [tagp + 'rstd%d' % b], [tagp + 'rstd%d' % b])
            kb.op('dve', lambda e: e.tensor_scalar(out=xs[b][:], in0=xt[a][:], scalar1=rstd[b][:], scalar2=None, op0=ALU.mult),
                  [tagp + 'xt%d' % a, tagp + 'rstd%d' % b], [tagp + 'xs%d' % b])
            pT = pT_tiles[b]
            for c in range(8):
                kb.op('pe', lambda e: e.transpose(out=pT[:, c, :], in_=xs[b][:, c * 128:(c + 1) * 128], identity=ident_b[:]),
                      [tagp + 'xs%d' % b, 'ident_b'], ['ps:' + tagp + 'pT%d' % b])
            kb.op('dve', lambda e: e.tensor_tensor(out=dstT[:, :, t * 128:(t + 1) * 128], in0=pT[:],
                                                    in1=gT[:, gcol * 8:(gcol + 1) * 8].unsqueeze(2).to_broadcast([128, 8, 128]),
                                                    op=ALU.mult),
                  ['ps:' + tagp + 'pT%d' % b, 'gT'], [dst_tag + '%d' % t])

    NT1 = 0 if skip12 else NT
    NB1 = 0 if skip12 else NB
    with ExitStack() as es1a:
        pTt = [psum(es1a, "p1a_pT%d" % i, [128, 8, 128], BF16) for i in range(2)]
        norm_transpose(es1a, "p1a", lambda t: x_d[t * 128:(t + 1) * 128, :], 0, hT, NT1, pTt)
    T.barrier()

    with ExitStack() as es1b:
        wq = [sbuf(es1b, "p1b_w%d" % i, [128, 8, 256], BF16) for i in range(2)]
        pq = [psum(es1b, "p1b_pq%d" % i, [128, 512], F32) for i in range(4)]
        r1 = [sbuf(es1b, "p1b_r1_%d" % i, [128, 512], F32) for i in range(2)]
        r2 = [sbuf(es1b, "p1b_r2_%d" % i, [128, 512], F32) for i in range(2)]
        ro = [sbuf(es1b, "p1b_ro%d" % i, [128, 512], BF16) for i in range(2)]
        it = 0
        for pair in range(0 if skip12 else 8):
            h, isk = pair // 2, pair % 2
            wbuf = wq[pair % 2]
            c0 = pair * 256
            kb.dma('pool', wbuf[:], win_d[:, c0:c0 + 256].rearrange("(c p) n -> p c n", p=128), [], ['p1b_w%d' % (pair % 2)], 'p1b_wl%d' % (pair % 2))
            dst = (kT_s if isk else qT_s)
            for blk in range(NB):
                pa, pb = pq[(it % 2) * 2], pq[(it % 2) * 2 + 1]
                na, nb_ = 'ps:p1b_pq%d' % ((it % 2) * 2), 'ps:p1b_pq%d' % ((it % 2) * 2 + 1)
                for kc in range(8):
                    kb.op('pe', lambda e: e.matmul(pa[:], lhsT=wbuf[:, kc, 0:128], rhs=hT[:, kc, blk * 512:(blk + 1) * 512], start=(kc == 0), stop=(kc == 7)),
                          ['p1b_w%d' % (pair % 2)] + ['hT_t%d' % (blk * 4 + j) for j in range(4)], [na])
                for kc in range(8):
                    kb.op('pe', lambda e: e.matmul(pb[:], lhsT=wbuf[:, kc, 128:256], rhs=hT[:, kc, blk * 512:(blk + 1) * 512], start=(kc == 0), stop=(kc == 7)),
                          ['p1b_w%d' % (pair % 2)] + ['hT_t%d' % (blk * 4 + j) for j in range(4)], [nb_])
                s_ = it % 2
                kb.op('dve', lambda e: e.tensor_tensor(out=r1[s_][:], in0=pa[:], in1=cosT[:, blk * 512:(blk + 1) * 512], op=ALU.mult),
                      [na, 'cosT'], ['p1b_r1_%d' % s_])
                kb.op('dve', lambda e: e.tensor_tensor(out=r2[s_][:], in0=pb[:], in1=sinT[:, blk * 512:(blk + 1) * 512], op=ALU.mult),
                      [nb_, 'sinT'], ['p1b_r2_%d' % s_])
                kb.op('pool', lambda e: e.tensor_tensor(out=ro[s_][:], in0=r1[s_][:], in1=r2[s_][:], op=ALU.add),
                      ['p1b_r1_%d' % s_, 'p1b_r2_%d' % s_], ['p1b_ro%d' % s_])
                kb.dma('sp', dst[h, :, blk * 512:(blk + 1) * 512], ro[s_][:], ['p1b_ro%d' % s_], [('qk', pair, blk)], 'p1b_st%d' % s_)
                it += 1

    if stop_after == '1b':
        return finish(kb, es, es_all, out_d)
    T.barrier()
    with ExitStack() as es1c:
        wm = [sbuf(es1c, "p1c_w%d" % i, [128, 8, 128], BF16) for i in range(2)]
        pm = [psum(es1c, "p1c_pm%d" % i, [128, 512], F32) for i in range(2)]
        ub = [sbuf(es1c, "p1c_ub%d" % i, [128, 515], F32) for i in range(2)]
        ca = [sbuf(es1c, "p1c_ca%d" % i, [128, 512], F32) for i in range(2)]
        co = [sbuf(es1c, "p1c_co%d" % i, [128, 512], BF16) for i in range(2)]
        it = 0
        for g in range(0 if skip12 else 8):
            wbuf = wm[g % 2]
            wn = 'p1c_w%d' % (g % 2)
            c0 = 16 * 128 + g * 128
            kb.dma('pool', wbuf[:], win_d[:, c0:c0 + 128].rearrange("(c p) n -> p c n", p=128), [], [wn], 'p1c_wl%d' % (g % 2))
            dst = (mqT_s if g < 4 else mkT_s)
            h = g % 4
            cw = lambda i: convT[:, g * 5 + i:g * 5 + i + 1]
            for blk in range(NB):
                s_ = it % 2
                p_, pn = pm[s_], 'ps:p1c_pm%d' % s_
                u_, un = ub[s_], 'p1c_ub%d' % s_
                for kc in range(8):
                    kb.op('pe', lambda e: e.matmul(p_[:], lhsT=wbuf[:, kc, :], rhs=hT[:, kc, blk * 512:(blk + 1) * 512], start=(kc == 0), stop=(kc == 7)),
                          [wn], [pn])
                if blk == 0:
                    kb.op('pool', lambda e: e.memset(u_[:, 0:3], 0.0), [], [un + 'h'])
                else:
                    up = ub[1 - s_]
                    kb.op('act', lambda e: e.copy(out=u_[:, 0:3], in_=up[:, 512:515]), ['p1c_ub%d' % (1 - s_)], [un + 'h'])
                kb.op('act', lambda e: e.copy(out=u_[:, 3:515], in_=p_[:]), [pn], [un])
                a_, an = ca[s_], 'p1c_ca%d' % s_
                kb.op('dve', lambda e: e.tensor_scalar(out=a_[:], in0=u_[:, 0:512], scalar1=cw(0), scalar2=None, op0=ALU.mult),
                      [un, un + 'h', 'convT'], [an])
                for i in (1, 2, 3):
                    kb.op('dve', lambda e: e.scalar_tensor_tensor(out=a_[:], in0=u_[:, i:i + 512], scalar=cw(i), in1=a_[:], op0=ALU.mult, op1=ALU.add),
                          [un, un + 'h', an, 'convT'], [an])
                o_, on = co[s_], 'p1c_co%d' % s_
                kb.op('act', lambda e: e.activation(out=o_[:], in_=a_[:], func=AF.Silu, bias=cw(4)), [an, 'convT'], [on])
                kb.dma('sp', dst[h, :, blk * 512:(blk + 1) * 512], o_[:], [on], [('mqk', g, blk)], 'p1c_st%d' % s_)
                it += 1
    T.barrier()
    with ExitStack() as es1d:
        wt = sbuf(es1d, "p1d_w", [128, 8, TM_COLS], BF16)
        for kc in range(8):
            kb.dma('pool', wt[:, kc, :], win_d[kc * 128:(kc + 1) * 128, FM_COLS:WIN_COLS], [], ['p1d_w'], 'p1d_wl')
        pt = [[psum(es1d, "p1d_p%d_%d" % (i, j), [128, 512], F32) for j in range(4)] for i in range(2)]
        ot = [sbuf(es1d, "p1d_o%d" % i, [128, 1536], BF16) for i in range(2)]
        og = [sbuf(es1d, "p1d_g%d" % i, [128, 8], F32) for i in range(2)]
        for t in range(NT1):
            s_ = t % 2
            for j in range(4):
                n0, n1 = (j * 512, (j + 1) * 512) if j < 3 else (1536, 1544)
                for kc in range(8):
                    kb.op('pe', lambda e: e.matmul(pt[s_][j][:, 0:n1 - n0], lhsT=hT[:, kc, t * 128:(t + 1) * 128], rhs=wt[:, kc, n0:n1], start=(kc == 0), stop=(kc == 7)),
                          ['p1d_w'], ['ps:p1d_p%d_%d' % (s_, j)], attach='p1d_w')
            for j in range(3):
                eng = 'act' if j != 1 else 'dve'
                if eng == 'act':
                    kb.op('act', lambda e: e.copy(out=ot[s_][:, j * 512:(j + 1) * 512], in_=pt[s_][j][:]), ['ps:p1d_p%d_%d' % (s_, j)], ['p1d_o%d_%d' % (s_, j)])
                else:
                    kb.op('dve', lambda e: e.tensor_copy(out=ot[s_][:, j * 512:(j + 1) * 512], in_=pt[s_][j][:]), ['ps:p1d_p%d_%d' % (s_, j)], ['p1d_o%d_%d' % (s_, j)])
            kb.op('dve', lambda e: e.tensor_copy(out=og[s_][:], in_=pt[s_][3][:, 0:8]), ['ps:p1d_p%d_3' % s_], ['p1d_g%d' % s_])
            kb.dma('sp', tmb_s[t * 128:(t + 1) * 128, :], ot[s_][:], ['p1d_o%d_%d' % (s_, j) for j in range(3)], [('tmb', t)], 'p1d_st%d' % s_)
            kb.dma('sp', gate_s[:, t, :], og[s_][:], ['p1d_g%d' % s_], [('gate', t)], 'p1d_sg%d' % s_)
    es.close()
    T.barrier()
    if stop_after == '1':
        return finish(kb, es, es_all, out_d)

    with ExitStack() as es2:
        lamb = sbuf(es2, "p2_lamb", [128, 256], F32)
        lj = sbuf(es2, "p2_lj", [128, 64], F32)
        ls = sbuf(es2, "p2_ls", [128, 2], F32)
        nlam = sbuf(es2, "p2_nlam", [128, 1], F32)
        sg = sbuf(es2, "p2_sg", [128, 128], F32)
        maskf = sbuf(es2, "p2_maskf", [128, 128], F32)
        maskb = sbuf(es2, "p2_maskb", [128, 128], BF16)
        kb.dma('sp', lamb[:], lam_d.partition_broadcast(128), [], ['lamb'], 'p2_c0')
        kb.dma('sp', sg[:], subln_d.partition_broadcast(128), [], ['sg'], 'p2_c1')
        kb.dma('sp', maskf[:], maskb_d[:, :], [], ['maskf'], 'p2_c2')
        kb.op('dve', lambda e: e.tensor_copy(out=maskb[:], in_=maskf[:]), ['maskf'], ['maskb'])
        for i in range(2):
            kb.op('dve', lambda e: e.scalar_tensor_tensor(out=lj[:], in0=lamb[:, i * 128:i * 128 + 64], scalar=1.0, in1=lamb[:, i * 128 + 64:i * 128 + 128],
                                                          op0=ALU.mult, op1=ALU.mult, accum_out=ls[:, i:i + 1]), ['lamb'], ['lj', 'ls'])
        kb.op('act', lambda e: e.activation(out=ls[:], in_=ls[:], func=AF.Exp), ['ls'], ['ls'])
        kb.op('dve', lambda e: e.tensor_tensor(out=nlam[:], in0=ls[:, 1:2], in1=ls[:, 0:1], op=ALU.subtract), ['ls'], ['nlam'])
        kb.op('dve', lambda e: e.tensor_scalar(out=nlam[:], in0=nlam[:], scalar1=-LAM_INIT, scalar2=None, op0=ALU.add), ['nlam'], ['nlam'])
        kb.op('dve', lambda e: e.tensor_scalar(out=sg[:], in0=sg[:], scalar1=1.0 - LAM_INIT, scalar2=None, op0=ALU.mult), ['sg'], ['sg'])

        kTh = [sbuf(es2, "p2_kT%d" % i, [128, S], BF16) for i in range(2)]
        Vh = [sbuf(es2, "p2_V%d" % i, [128, NT, 129], BF16) for i in range(2)]
        qTb = [sbuf(es2, "p2_q%d" % i, [128, 512], BF16) for i in range(2)]
        PT = [sbuf(es2, "p2_PT%d" % i, [128, 512], BF16) for i in range(3)]
        ps_s = [psum(es2, "p2_ps%d" % i, [128, 512], F32) for i in range(2)]
        accA_b = [psum(es2, "p2_accA%d" % i, [128, 512], F32) for i in range(2)]
        accB_b = [psum(es2, "p2_accB%d" % i, [128, 512], F32) for i in range(2)]
        accA = [b_[:, 0:387].rearrange("p (q c) -> p q c", c=129) for b_ in accA_b]
        accB = [b_[:, 0:129].rearrange("p (q c) -> p q c", c=129) for b_ in accB_b]
        rz = sbuf(es2, "p2_rz", [128, 2, 4], F32)
        o0 = [sbuf(es2, "p2_o0_%d" % i, [128, 4, 128], F32) for i in range(2)]
        oo = [sbuf(es2, "p2_oo_%d" % i, [128, 4, 128], F32) for i in range(2)]
        junk = sbuf(es2, "p2_junk", [128, 128], F32)
        ss = sbuf(es2, "p2_ss", [128, 4], F32)
        yo = [sbuf(es2, "p2_yo%d" % i, [128, 4, 128], BF16) for i in range(2)]
        for i in range(2):
            kb.op('pool', lambda e: e.memset(Vh[i][:, :, 128:129], 1.0), [], ['V%dones' % i])
        sc_it = 0
        hq = 0
        for h in range(0 if skip12 else 4):
            hs = h % 2
            kb.dma('sp', kTh[hs][:], kT_s[h, :, :], [], ['kT%d' % hs], 'p2_kl%d' % hs)
            kb.dma('sp', Vh[hs][:, :, 0:128], tmb_s[:, h * 128:(h + 1) * 128].rearrange("(t p) c -> p t c", p=128), [], ['V%d' % hs], 'p2_vl%d' % hs)
            for qb in range(NB):
                qs_ = hq % 2
                kb.dma('sp', qTb[qs_][:], qT_s[h, :, qb * 512:(qb + 1) * 512], [], ['q%d' % qs_], 'p2_ql%d' % qs_)
                nkt = 4 * qb + 4
                for m in range(2):
                    a_i = (hq * 2 + m) % 2
                    aA, aB = accA[a_i], accB[a_i]
                    an = 'ps:acc%d' % a_i
                    rows = slice(m * 64, (m + 1) * 64)
                    startedA = False
                    for kt in range(nkt):
                        j = kt - 4 * qb
                        c0 = max(j, 0) * 128
                        p_s, pn = ps_s[sc_it % 2], 'ps:s%d' % (sc_it % 2)
                        P_, Pn = PT[sc_it % 3], 'PT%d' % (sc_it % 3)
                        kb.op('pe', lambda e: e.matmul(p_s[:, c0:512], lhsT=kTh[hs][rows, kt * 128:(kt + 1) * 128], rhs=qTb[qs_][rows, c0:512],
                                                        start=True, stop=(j < 0)), ['kT%d' % hs, 'q%d' % qs_], [pn])
                        if j >= 0:
                            kb.op('pe', lambda e: e.matmul(p_s[:, c0:c0 + 128], lhsT=ident_b[:], rhs=maskb[:], start=False, stop=True),
                                  ['ident_b', 'maskb'], [pn])
                        kb.op('act', lambda e: e.activation(out=P_[:, c0:512], in_=p_s[:, c0:512], func=AF.Exp, scale=0.125), [pn], [Pn])
                        for qs in range(max(j, 0), 4):
                            last = (kt == 4 * qb + qs)
                            if qs < 3:
                                o_ap = aA[:, qs, :]
                                st = not startedA
                                startedA = True
                            else:
                                o_ap = aB[:, 0, :]
                                st = (kt == 0)
                            kb.op('pe', lambda e: e.matmul(o_ap, lhsT=P_[:, qs * 128:(qs + 1) * 128], rhs=Vh[hs][:, kt, :], start=st, stop=last),
                                  [Pn, 'V%d' % hs, 'V%dones' % hs], [an])
                        sc_it += 1
                    pq_ = hq % 2
                    kb.op('dve', lambda e: e.reciprocal(out=rz[:, m, 0:3], in_=aA[:, :, 128]), [an], ['rz%d' % m])
                    kb.op('dve', lambda e: e.reciprocal(out=rz[:, m, 3:4], in_=aB[:, :, 128]), [an], ['rz%d' % m])
                    if m == 0:
                        for qs in range(4):
                            src = aA[:, qs, 0:128] if qs < 3 else aB[:, 0, 0:128]
                            kb.op('dve', lambda e: e.tensor_scalar(out=o0[pq_][:, qs, :], in0=src, scalar1=rz[:, 0, qs:qs + 1], scalar2=None, op0=ALU.mult),
                                  [an, 'rz0'], ['o0_%d' % pq_])
                    else:
                        kb.op('dve', lambda e: e.tensor_scalar(out=rz[:, 1, :], in0=rz[:, 1, :], scalar1=nlam[:, 0:1], scalar2=None, op0=ALU.mult),
                              ['rz1', 'nlam'], ['rz1'])
                        for qs in range(4):
                            src = aA[:, qs, 0:128] if qs < 3 else aB[:, 0, 0:128]
                            kb.op('dve', lambda e: e.scalar_tensor_tensor(out=oo[pq_][:, qs, :], in0=src, scalar=rz[:, 1, qs:qs + 1], in1=o0[pq_][:, qs, :],
                                                                          op0=ALU.mult, op1=ALU.add), [an, 'rz1', 'o0_%d' % pq_], ['oo_%d' % pq_])
                pq_ = hq % 2
                for qs in range(4):
                    kb.op('dve', lambda e: e.scalar_tensor_tensor(out=junk[:], in0=oo[pq_][:, qs, :], scalar=1.0, in1=oo[pq_][:, qs, :], op0=ALU.mult, op1=ALU.mult,
                                                                  accum_out=ss[:, qs:qs + 1]), ['oo_%d' % pq_], ['junk', 'ss'])
                kb.op('act', lambda e: e.activation(out=ss[:], in_=ss[:], func=AF.Ln, scale=1.0 / 128, bias=EPS), ['ss'], ['ss'])
                kb.op('act', lambda e: e.activation(out=ss[:], in_=ss[:], func=AF.Exp, scale=-0.5), ['ss'], ['ss'])
                for qs in range(4):
                    kb.op('dve', lambda e: e.scalar_tensor_tensor(out=yo[pq_][:, qs, :], in0=oo[pq_][:, qs, :], scalar=ss[:, qs:qs + 1], in1=sg[:], op0=ALU.mult, op1=ALU.mult),
                          ['oo_%d' % pq_, 'ss', 'sg'], ['yo%d' % pq_])
                kb.dma('sp', y_s[qb * 512:(qb + 1) * 512, h * 128:(h + 1) * 128].rearrange("(qs p) c -> p qs c", p=128), yo[pq_][:],
                       ['yo%d' % pq_], [('yda', h, qb)], 'p2_st%d' % pq_)
                hq += 1
    T.barrier()
    if stop_after == '2':
        return finish(kb, es, es_all, out_d)

    with ExitStack() as es3:
        DH = 128
        KS = float(DH) ** -0.5
        tri = sbuf(es3, "p3_tri", [128, 128], F32)
        maskf4 = sbuf(es3, "p3_maskf4", [128, 4, 128], F32)
        ones_f = sbuf(es3, "p3_ones", [128, 128], F32)
        gb = sbuf(es3, "p3_gb", [128, 8], F32)
        mlg = sbuf(es3, "p3_mlg", [128, 512], F32)
        G = sbuf(es3, "p3_G", [128, NT, 8], F32)
        ig = sbuf(es3, "p3_ig", [128, NT, 4], F32)
        lf = sbuf(es3, "p3_lf", [128, NT, 4], F32)
        bcol = sbuf(es3, "p3_bcol", [128, NT, 4], F32)
        ea = sbuf(es3, "p3_ea", [128, NT, 4], F32)
        wcol = sbuf(es3, "p3_wcol", [128, NT, 4], F32)
        eaL = sbuf(es3, "p3_eaL", [128, NT, 4], F32)
        mqT = sbuf(es3, "p3_mqT", [128, 4, S], BF16)
        mkT = sbuf(es3, "p3_mkT", [128, 4, S], BF16)
        MV = sbuf(es3, "p3_MV", [128, NT, 4, 129], BF16)
        kb.dma('sp', tri[:], tri_d[:, :], [], ['tri'], 'p3_c0')
        for hh in range(4):
            kb.dma('sp', maskf4[:, hh, :], maskb_d[:, :], [], ['maskf4'], 'p3_c1')
        kb.dma('sp', gb[:], gateb_d.partition_broadcast(128), [], ['gb'], 'p3_c2')
        kb.dma('sp', mlg[:], mlg_d.partition_broadcast(128), [], ['mlg'], 'p3_c3')
        kb.dma('sp', G[:], gate_s, [], ['G'], 'p3_c4')
        for hh in range(4):
            kb.dma('sp', mqT[:, hh, :], mqT_s[hh, :, :], [], ['mqT'], 'p3_c5')
            kb.dma('sp', mkT[:, hh, :], mkT_s[hh, :, :], [], ['mkT'], 'p3_c6')
        kb.op('pool', lambda e: e.memset(MV[:, :, :, 128:129], 1.0), [], ['MVones'])
        for hh in range(4):
            kb.dma('sp', MV[:, :, hh, 0:128], tmb_s[:, 512 + hh * 128:512 + (hh + 1) * 128].rearrange("(t p) c -> p t c", p=128), [], ['MV'], 'p3_c7')
        kb.op('pool', lambda e: e.memset(ones_f[:], 1.0), [], ['ones_f'])
        if stop_after == '3a':
            T.barrier()
            return finish(kb, es, es_all, out_d)
        kb.op('dve', lambda e: e.tensor_tensor(out=ig[:], in0=G[:, :, 0:4], in1=gb[:, 0:4].unsqueeze(1).to_broadcast([128, NT, 4]), op=ALU.add), ['G', 'gb'], ['ig'])
        kb.op('dve', lambda e: e.tensor_tensor(out=lf[:], in0=G[:, :, 4:8], in1=gb[:, 4:8].unsqueeze(1).to_broadcast([128, NT, 4]), op=ALU.add), ['G', 'gb'], ['lf'])
        kb.op('act', lambda e: e.activation(out=lf[:], in_=lf[:], func=AF.Exp, scale=-1.0), ['lf'], ['lf'])
        kb.op('act', lambda e: e.activation(out=lf[:], in_=lf[:], func=AF.Ln, bias=1.0), ['lf'], ['lf'])
        kb.op('dve', lambda e: e.tensor_scalar(out=lf[:], in0=lf[:], scalar1=-1.0, scalar2=None, op0=ALU.mult), ['lf'], ['lf'])
        if stop_after == '3b':
            T.barrier()
            return finish(kb, es, es_all, out_d)
        pg_b = psum(es3, "p3_pg", [128, 512], F32)
        pg = pg_b[:, 0:256].rearrange("p (a b) -> p a b", a=2)
        tri_b = sbuf(es3, "p3_tri_b", [128, 128], BF16)
        ones_b = sbuf(es3, "p3_ones_b", [128, 128], BF16)
        maskb4 = sbuf(es3, "p3_maskb4", [128, 4, 128], BF16)
        lf_hi = sbuf(es3, "p3_lf_hi", [128, NT, 4], BF16)
        lf_lo = sbuf(es3, "p3_lf_lo", [128, NT, 4], BF16)
        kb.op('dve', lambda e: e.tensor_copy(out=tri_b[:], in_=tri[:]), ['tri'], ['tri_b'])
        kb.op('dve', lambda e: e.tensor_copy(out=ones_b[:], in_=ones_f[:]), ['ones_f'], ['ones_b'])
        kb.op('dve', lambda e: e.tensor_copy(out=maskb4[:], in_=maskf4[:]), ['maskf4'], ['maskb4'])
        kb.op('dve', lambda e: e.tensor_copy(out=lf_hi[:], in_=lf[:]), ['lf'], ['lf_hi'])
        kb.op('dve', lambda e: e.tensor_tensor(out=lf_lo[:], in0=lf[:], in1=lf_hi[:], op=ALU.subtract), ['lf', 'lf_hi'], ['lf_lo'])
        if stop_after == '3b1':
            T.barrier()
            return finish(kb, es, es_all, out_d)
        lfh2 = lf_hi[:].rearrange("p t h -> p (t h)")
        lfl2 = lf_lo[:].rearrange("p t h -> p (t h)")
        kb.op('pe', lambda e: e.matmul(pg[:, 0, :], lhsT=tri_b[:], rhs=lfh2, start=True, stop=False), ['tri_b', 'lf_hi'], ['ps:pg'])
        kb.op('pe', lambda e: e.matmul(pg[:, 0, :], lhsT=tri_b[:], rhs=lfl2, start=False, stop=True), ['tri_b', 'lf_lo'], ['ps:pg'])
        kb.op('pe', lambda e: e.matmul(pg[:, 1, :], lhsT=ones_b[:], rhs=lfh2, start=True, stop=False), ['ones_b', 'lf_hi'], ['ps:pg'])
        kb.op('pe', lambda e: e.matmul(pg[:, 1, :], lhsT=ones_b[:], rhs=lfl2, start=False, stop=True), ['ones_b', 'lf_lo'], ['ps:pg'])
        if stop_after == '3b2':
            T.barrier()
            return finish(kb, es, es_all, out_d)
        f2 = lambda t_: t_[:].rearrange("p t h -> p (t h)")
        kb.op('dve', lambda e: e.scalar_tensor_tensor(out=f2(bcol), in0=pg[:, 0, :], scalar=-1.0, in1=f2(ig), op0=ALU.mult, op1=ALU.add), ['ig', 'ps:pg'], ['bcol'])
        kb.op('act', lambda e: e.activation(out=f2(ea), in_=pg[:, 0, :], func=AF.Exp), ['ps:pg'], ['ea'])
        if stop_after == '3b3':
            T.barrier()
            return finish(kb, es, es_all, out_d)
        kb.op('dve', lambda e: e.tensor_tensor(out=f2(wcol), in0=pg[:, 1, :], in1=f2(bcol), op=ALU.add), ['bcol', 'ps:pg'], ['wcol'])
        kb.op('act', lambda e: e.activation(out=f2(wcol), in_=f2(wcol), func=AF.Exp), ['wcol'], ['wcol'])
        kb.op('act', lambda e: e.activation(out=f2(eaL), in_=pg[:, 1, :], func=AF.Exp), ['ps:pg'], ['eaL'])

        if stop_after == '3c':
            T.barrier()
            return finish(kb, es, es_all, out_d)
        pX = [psum(es3, "p3_pX%d" % i, [128, 4, 128], F32) for i in range(2)]
        pP = [psum(es3, "p3_pP%d" % i, [128, 512], F32) for i in range(2)]
        pU_b = psum(es3, "p3_pU", [128, 512], F32)
        pU = pU_b[:, 0:258].rearrange("p (a b) -> p a b", a=2)
        pTk_b = psum(es3, "p3_pTk", [128, 1024], BF16)
        pTk = pTk_b[:, 0:256].rearrange("p (a b) -> p a b", a=2)
        Rc = [sbuf(es3, "p3_Rc%d" % i, [128, 4, 128], BF16) for i in range(2)]
        Rl = [sbuf(es3, "p3_Rl%d" % i, [128, 4, 128], BF16) for i in range(2)]
        DT = [sbuf(es3, "p3_DT%d" % i, [128, 4, 128], F32) for i in range(2)]
        SD = [sbuf(es3, "p3_SD%d" % i, [128, 128], BF16) for i in range(2)]
        KW = [sbuf(es3, "p3_KW%d" % i, [128, 128], BF16) for i in range(2)]
        Cf = [sbuf(es3, "p3_Cf%d" % i, [128, 129], F32) for i in range(4)]
        Cb = [sbuf(es3, "p3_Cb%d" % i, [128, 129], BF16) for i in range(4)]
        intra_sb = [sbuf(es3, "p3_intra%d" % i, [128, 129], F32) for i in range(2)]
        num = [sbuf(es3, "p3_num%d" % i, [128, 4, 129], F32) for i in range(2)]
        rdn = sbuf(es3, "p3_rdn", [128, 4], F32)
        hsc = [sbuf(es3, "p3_hsc%d" % i, [128, 4, 128], F32) for i in range(2)]
        stats = sbuf(es3, "p3_stats", [128, 4, 6], F32)
        mvv = sbuf(es3, "p3_mvv", [128, 4, 2], F32)
        rstd3 = sbuf(es3, "p3_rstd", [128, 4], F32)
        mo_t = [sbuf(es3, "p3_mo%d" % i, [128, 512], BF16) for i in range(2)]
        gg = [sbuf(es3, "p3_gg%d" % i, [128, 512], F32) for i in range(2)]
        yo3 = [sbuf(es3, "p3_yo%d" % i, [128, 512], BF16) for i in range(2)]
        for hh in range(4):
            kb.op('pool', lambda e: e.memset(Cf[hh][:], 0.0), [], ['Cf%d' % hh])
            kb.op('pool', lambda e: e.memset(Cb[hh][:], 0.0), [], ['Cb%d' % hh])
        u_it = 0
        for c in range(p3_tiles):
            cs = c % 2
            tsl = slice(c * 128, (c + 1) * 128)
            kb.dma('sp', mo_t[cs][:], tmb_s[tsl, 1024:1536], [], ['mo%d' % cs], 'p3_mol%d' % cs)
            kb.op('act', lambda e: e.activation(out=gg[cs][:], in_=mo_t[cs][:], func=AF.Exp, scale=-1.0), ['mo%d' % cs], ['gg%d' % cs])
            kb.op('pool', lambda e: e.tensor_scalar(out=gg[cs][:], in0=gg[cs][:], scalar1=1.0, scalar2=None, op0=ALU.add), ['gg%d' % cs], ['gg%d' % cs])
            kb.op('dve', lambda e: e.reciprocal(out=gg[cs][:], in_=gg[cs][:]), ['gg%d' % cs], ['gg%d' % cs])
            kb.op('pool', lambda e: e.tensor_tensor(out=gg[cs][:], in0=gg[cs][:], in1=mlg[:], op=ALU.mult), ['gg%d' % cs, 'mlg'], ['gg%d' % cs])
            kb.op('dve', lambda e: e.tensor_tensor(out=Rc[cs][:], in0=tri_b[:].unsqueeze(1).to_broadcast([128, 4, 128]),
                                                    in1=lf_hi[:, c, :].unsqueeze(2).to_broadcast([128, 4, 128]), op=ALU.mult), ['tri_b', 'lf_hi'], ['Rc%d' % cs])
            kb.op('dve', lambda e: e.tensor_tensor(out=Rl[cs][:], in0=tri_b[:].unsqueeze(1).to_broadcast([128, 4, 128]),
                                                    in1=lf_lo[:, c, :].unsqueeze(2).to_broadcast([128, 4, 128]), op=ALU.mult), ['tri_b', 'lf_lo'], ['Rl%d' % cs])
            pXf = pX[cs][:].rearrange("p h j -> p (h j)")
            kb.op('pe', lambda e: e.matmul(pXf, lhsT=ones_b[:], rhs=Rc[cs][:].rearrange("p h j -> p (h j)"), start=True, stop=False),
                  ['ones_b', 'Rc%d' % cs], ['ps:pX%d' % cs])
            kb.op('pe', lambda e: e.matmul(pXf, lhsT=ones_b[:], rhs=Rl[cs][:].rearrange("p h j -> p (h j)"), start=False, stop=False),
                  ['ones_b', 'Rl%d' % cs], ['ps:pX%d' % cs])
            kb.op('pe', lambda e: e.matmul(pXf, lhsT=ident_b[:], rhs=maskb4[:].rearrange("p h j -> p (h j)"), start=False, stop=True),
                  ['ident_b', 'maskb4'], ['ps:pX%d' % cs])
            for hh in range(4):
                kb.op('act', lambda e: e.activation(out=DT[cs][:, hh, :], in_=pX[cs][:, hh, :], func=AF.Exp, bias=bcol[:, c, hh:hh + 1]),
                      ['ps:pX%d' % cs, 'bcol'], ['DT%d_%d' % (cs, hh)])
            for hh in range(4):
                us = u_it % 2
                P_, Pn = pP[us], 'ps:pP%d' % us
                q_t = mqT[:, hh, tsl]
                k_t = mkT[:, hh, tsl]
                kb.op('pe', lambda e: e.matmul(P_[:, 0:128], lhsT=k_t, rhs=q_t, start=True, stop=True), ['mkT', 'mqT'], [Pn])
                kb.op('dve', lambda e: e.scalar_tensor_tensor(out=SD[us][:], in0=P_[:, 0:128], scalar=KS, in1=DT[cs][:, hh, :], op0=ALU.mult, op1=ALU.mult),
                      [Pn, 'DT%d_%d' % (cs, hh)], ['SD%d' % us])
                kb.op('pe', lambda e: e.matmul(P_[:, 128:257], lhsT=SD[us][:], rhs=MV[:, c, hh, :], start=True, stop=True), ['SD%d' % us, 'MV', 'MVones'], [Pn])
                kb.op('pe', lambda e: e.matmul(P_[:, 257:386], lhsT=q_t, rhs=Cb[hh][:], start=True, stop=True), ['mqT', 'Cb%d' % hh], [Pn], attach='Cb%d' % hh)
                kb.op('act', lambda e: e.copy(out=intra_sb[us][:], in_=P_[:, 128:257]), [Pn], ['intra%d' % us])
                kb.op('dve', lambda e: e.scalar_tensor_tensor(out=num[cs][:, hh, :], in0=P_[:, 257:386], scalar=ea[:, c, hh:hh + 1], in1=intra_sb[us][:], op0=ALU.mult, op1=ALU.add),
                      [Pn, 'ea', 'intra%d' % us], ['num%d_%d' % (cs, hh)])
                kb.op('pe', lambda e: e.transpose(out=pTk[:, us, :], in_=k_t, identity=ident_b[:]), ['mkT', 'ident_b'], ['ps:pTk'])
                kb.op('dve', lambda e: e.tensor_scalar(out=KW[us][:], in0=pTk[:, us, :], scalar1=wcol[:, c, hh:hh + 1], scalar2=KS, op0=ALU.mult, op1=ALU.mult),
                      ['ps:pTk', 'wcol'], ['KW%d' % us])
                kb.op('pe', lambda e: e.matmul(pU[:, us, :], lhsT=KW[us][:], rhs=MV[:, c, hh, :], start=True, stop=True), ['KW%d' % us, 'MV', 'MVones'], ['ps:pU'])
                kb.op('pool', lambda e: e.tensor_scalar(out=Cf[hh][:], in0=Cf[hh][:], scalar1=eaL[:, c, hh:hh + 1], scalar2=None, op0=ALU.mult),
                      ['Cf%d' % hh, 'eaL'], ['Cf%d' % hh])
                kb.op('dve', lambda e: e.tensor_tensor(out=Cf[hh][:], in0=pU[:, us, :], in1=Cf[hh][:], op=ALU.add),
                      ['Cf%d' % hh, 'ps:pU'], ['Cf%d' % hh])
                kb.op('act', lambda e: e.copy(out=Cb[hh][:], in_=Cf[hh][:]), ['Cf%d' % hh], ['Cb%d' % hh])
                u_it += 1
            nn = ['num%d_%d' % (cs, hh) for hh in range(4)]
            kb.op('dve', lambda e: e.tensor_tensor(out=rdn[:], in0=num[cs][:, :, 128], in1=num[cs][:, :, 128], op=ALU.mult), nn, ['rdn'])
            kb.op('dve', lambda e: e.tensor_scalar(out=rdn[:], in0=rdn[:], scalar1=1.0, scalar2=None, op0=ALU.max), ['rdn'], ['rdn'])
            kb.op('act', lambda e: e.activation(out=rdn[:], in_=rdn[:], func=AF.Ln), ['rdn'], ['rdn'])
            kb.op('act', lambda e: e.activation(out=rdn[:], in_=rdn[:], func=AF.Exp, scale=-0.5), ['rdn'], ['rdn'])
            kb.op('dve', lambda e: e.tensor_tensor(out=hsc[cs][:], in0=num[cs][:, :, 0:128], in1=rdn[:].unsqueeze(2).to_broadcast([128, 4, 128]), op=ALU.mult),
                  nn + ['rdn'], ['hsc%d' % cs])
            for hh in range(4):
                kb.op('dve', lambda e: e.bn_stats(out=stats[:, hh, :], in_=hsc[cs][:, hh, :]), ['hsc%d' % cs], ['stats'])
                kb.op('dve', lambda e: e.bn_aggr(out=mvv[:, hh, :], in_=stats[:, hh, :]), ['stats'], ['mvv'])
            kb.op('act', lambda e: e.activation(out=rstd3[:], in_=mvv[:, :, 1], func=AF.Ln, bias=EPS), ['mvv'], ['rstd3'])
            kb.op('act', lambda e: e.activation(out=rstd3[:], in_=rstd3[:], func=AF.Exp, scale=-0.5), ['rstd3'], ['rstd3'])
            for hh in range(4):
                kb.op('dve', lambda e: e.tensor_scalar(out=hsc[cs][:, hh, :], in0=hsc[cs][:, hh, :], scalar1=mvv[:, hh, 0:1], scalar2=rstd3[:, hh:hh + 1], op0=ALU.subtract, op1=ALU.mult),
                      ['hsc%d' % cs, 'mvv', 'rstd3'], ['hsc%d' % cs])
            kb.op('dve', lambda e: e.tensor_tensor(out=yo3[cs][:], in0=hsc[cs][:].rearrange("p h d -> p (h d)"), in1=gg[cs][:], op=ALU.mult),
                  ['hsc%d' % cs, 'gg%d' % cs], ['yo3_%d' % cs])
            kb.dma('sp', y_s[tsl, 512:1024], yo3[cs][:], ['yo3_%d' % cs], [('yml', c)], 'p3_st%d' % cs)
    T.barrier()
    if stop_after == '3':
        return finish(kb, es, es_all, out_d)

    es4 = ExitStack()
    h2T = sbuf(es4, "h2T", [128, 8, S], BF16)
    with ExitStack() as es4a:
        wo = sbuf(es4a, "p4_wo", [128, 8, D], BF16)
        for kc in range(8):
            kb.dma('pool', wo[:, kc, :], wout_d[kc * 128:(kc + 1) * 128, :], [], ['p4_wo'], 'p4_wl')
        yt = [sbuf(es4a, "p4_yt%d" % i, [128, D], BF16) for i in range(2)]
        xt4 = [sbuf(es4a, "p4_xt%d" % i, [128, D], F32) for i in range(2)]
        yT = [sbuf(es4a, "p4_yT%d" % i, [128, 8, 128], BF16) for i in range(2)]
        x1t = [sbuf(es4a, "p4_x1_%d" % i, [128, D], F32) for i in range(2)]
        pTy = [psum(es4a, "p4_pT%d" % i, [128, 8, 128], BF16) for i in range(2)]
        po4 = [[psum(es4a, "p4_po%d_%d" % (i, j), [128, 512], F32) for j in range(2)] for i in range(2)]
        for t in range(NT):
            b = t % 2
            tsl = slice(t * 128, (t + 1) * 128)
            kb.dma('sp', yt[b][:], y_s[tsl, :], [], ['p4_yt%d' % b], 'p4_yl%d' % b)
            kb.dma('sp', xt4[b][:], x_d[tsl, :], [], ['p4_xt%d' % b], 'p4_xl%d' % b)
            for c in range(8):
                kb.op('pe', lambda e: e.transpose(out=pTy[b][:, c, :], in_=yt[b][:, c * 128:(c + 1) * 128], identity=ident_b[:]),
                      ['p4_yt%d' % b, 'ident_b'], ['ps:p4_pT%d' % b])
            kb.op('act', lambda e: e.copy(out=yT[b][:], in_=pTy[b][:]), ['ps:p4_pT%d' % b], ['p4_yT%d' % b])
            for hf in range(2):
                for kc in range(8):
                    kb.op('pe', lambda e: e.matmul(po4[b][hf][:], lhsT=yT[b][:, kc, :], rhs=wo[:, kc, hf * 512:(hf + 1) * 512], start=(kc == 0), stop=(kc == 7)),
                          ['p4_yT%d' % b, 'p4_wo'], ['ps:p4_po%d_%d' % (b, hf)])
                kb.op('dve', lambda e: e.tensor_tensor(out=x1t[b][:, hf * 512:(hf + 1) * 512], in0=po4[b][hf][:], in1=xt4[b][:, hf * 512:(hf + 1) * 512], op=ALU.add),
                      ['ps:p4_po%d_%d' % (b, hf), 'p4_xt%d' % b], ['p4_x1_%d_%d' % (b, hf)])
            kb.dma('sp', x1_s[tsl, :], x1t[b][:], ['p4_x1_%d_0' % b, 'p4_x1_%d_1' % b], [('x1', t)], 'p4_st%d' % b)
    T.barrier()
    with ExitStack() as es4b:
        pTt = [psum(es4b, "p4b_pT%d" % i, [128, 8, 128], BF16) for i in range(2)]
        norm_transpose(es4b, "p4b", lambda t: x1_s[t * 128:(t + 1) * 128, :], 1, h2T, NT, pTt, dst_tag='h2T_t')
    T.barrier()
    if stop_after == '4':
        return finish(kb, es, es_all, out_d)

    es5 = ExitStack()
    memT = sbuf(es5, "memT", [128, 8, MEM], BF16)
    with ExitStack() as es5a:
        pTt = [psum(es5a, "p5a_pT%d" % i, [128, 8, 128], BF16) for i in range(2)]
        norm_transpose(es5a, "p5a", lambda t: mem_d[t * 128:(t + 1) * 128, :], 2, memT, 2, pTt, dst_tag='memT_t')
    T.barrier()
    with ExitStack() as es5b:
        wx = {}
        for nm, wd in (('q', wxq_d), ('k', wxk_d), ('v', wxv_d), ('o', wxo_d)):
            wx[nm] = sbuf(es5b, "p5_w" + nm, [128, 8, D], BF16)
            for kc in range(8):
                kb.dma('pool', wx[nm][:, kc, :], wd[kc * 128:(kc + 1) * 128, :], [], ['p5_w' + nm], 'p5_wl' + nm)
        KT = sbuf(es5b, "p5_KT", [128, 8, MEM], BF16)
        Vx = sbuf(es5b, "p5_Vx", [128, 2, D], BF16)
        pA = [psum(es5b, "p5_pA%d" % i, [128, 512], F32) for i in range(2)]
        pS = [psum(es5b, "p5_pS%d" % i, [128, 512], F32) for i in range(2)]
        pZ = psum(es5b, "p5_pZ", [128, 512], F32)
        pO = [psum(es5b, "p5_pO%d" % i, [128, 512], F32) for i in range(2)]
        ia = 0
        for c in range(8):
            p_, pn = pA[ia % 2], 'ps:p5_pA%d' % (ia % 2)
            for kc in range(8):
                kb.op('pe', lambda e: e.matmul(p_[:, 0:MEM], lhsT=wx['k'][:, kc, c * 128:(c + 1) * 128], rhs=memT[:, kc, :], start=(kc == 0), stop=(kc == 7)),
                      ['p5_wk'], [pn])
            kb.op('act', lambda e: e.copy(out=KT[:, c, :], in_=p_[:, 0:MEM]), [pn], ['p5_KT'])
            ia += 1
        for kt in range(2):
            for hf in range(2):
                p_, pn = pA[ia % 2], 'ps:p5_pA%d' % (ia % 2)
                for kc in range(8):
                    kb.op('pe', lambda e: e.matmul(p_[:], lhsT=memT[:, kc, kt * 128:(kt + 1) * 128], rhs=wx['v'][:, kc, hf * 512:(hf + 1) * 512], start=(kc == 0), stop=(kc == 7)),
                          ['p5_wv'], [pn], attach='p5_wv')
                kb.op('act', lambda e: e.copy(out=Vx[:, kt, hf * 512:(hf + 1) * 512], in_=p_[:]), [pn], ['p5_Vx'])
                ia += 1
        qTx = [sbuf(es5b, "p5_qT%d" % i, [128, 8, 512], BF16) for i in range(2)]
        PTx = [sbuf(es5b, "p5_PT%d" % i, [128, 2, 512], BF16) for i in range(2)]
        rZ = [sbuf(es5b, "p5_rZ%d" % i, [128, 512], F32) for i in range(2)]
        oTx = [sbuf(es5b, "p5_oT%d" % i, [128, 8, 512], BF16) for i in range(2)]
        x1t5 = [sbuf(es5b, "p5_x1_%d" % i, [128, D], F32) for i in range(2)]
        x2t5 = [sbuf(es5b, "p5_x2_%d" % i, [128, D], F32) for i in range(2)]
        hi_ = 0
        ti_ = 0
        for blk in range(NB):
            bs = blk % 2
            bsl = slice(blk * 512, (blk + 1) * 512)
            for c in range(8):
                p_, pn = pA[ia % 2], 'ps:p5_pA%d' % (ia % 2)
                for kc in range(8):
                    kb.op('pe', lambda e: e.matmul(p_[:], lhsT=wx['q'][:, kc, c * 128:(c + 1) * 128], rhs=h2T[:, kc, bsl], start=(kc == 0), stop=(kc == 7)),
                          ['p5_wq'], [pn])
                kb.op('act', lambda e: e.copy(out=qTx[bs][:, c, :], in_=p_[:]), [pn], ['p5_qT%d_%d' % (bs, c)])
                ia += 1
            for h in range(4):
                hs = hi_ % 2
                for kt in range(2):
                    p_, pn = pS[kt], 'ps:p5_pS%d' % kt
                    for dc in range(2):
                        kb.op('pe', lambda e: e.matmul(p_[:], lhsT=KT[:, 2 * h + dc, kt * 128:(kt + 1) * 128], rhs=qTx[bs][:, 2 * h + dc, :], start=(dc == 0), stop=(dc == 1)),
                              ['p5_KT', 'p5_qT%d_%d' % (bs, 2 * h + dc)], [pn])
                    kb.op('act', lambda e: e.activation(out=PTx[hs][:, kt, :], in_=p_[:], func=AF.Exp, scale=1.0 / 16.0), [pn], ['p5_PT%d_%d' % (hs, kt)])
                for kt in range(2):
                    kb.op('pe', lambda e: e.matmul(pZ[:], lhsT=ones_bb[:], rhs=PTx[hs][:, kt, :], start=(kt == 0), stop=(kt == 1)),
                          ['ones_bb', 'p5_PT%d_%d' % (hs, kt)], ['ps:p5_pZ'])
                kb.op('dve', lambda e: e.reciprocal(out=rZ[hs][:], in_=pZ[:]), ['ps:p5_pZ'], ['p5_rZ%d' % hs])
                for dc in range(2):
                    p_, pn = pO[dc], 'ps:p5_pO%d' % dc
                    for kt in range(2):
                        kb.op('pe', lambda e: e.matmul(p_[:], lhsT=Vx[:, kt, h * 256 + dc * 128:h * 256 + (dc + 1) * 128], rhs=PTx[hs][:, kt, :], start=(kt == 0), stop=(kt == 1)),
                              ['p5_Vx', 'p5_PT%d_%d' % (hs, kt)], [pn])
                    kb.op('dve', lambda e: e.tensor_tensor(out=oTx[bs][:, 2 * h + dc, :], in0=p_[:], in1=rZ[hs][:], op=ALU.mult),
                          [pn, 'p5_rZ%d' % hs], ['p5_oT%d_%d' % (bs, 2 * h + dc)])
                hi_ += 1
            for sub in range(4):
                t = blk * 4 + sub
                ts_ = ti_ % 2
                tsl = slice(t * 128, (t + 1) * 128)
                kb.dma('sp', x1t5[ts_][:], x1_s[tsl, :], [], ['p5_x1_%d' % ts_], 'p5_xl%d' % ts_)
                for hf in range(2):
                    p_, pn = pA[ia % 2], 'ps:p5_pA%d' % (ia % 2)
                    for kc in range(8):
                        kb.op('pe', lambda e: e.matmul(p_[:], lhsT=oTx[bs][:, kc, sub * 128:(sub + 1) * 128], rhs=wx['o'][:, kc, hf * 512:(hf + 1) * 512], start=(kc == 0), stop=(kc == 7)),
                              ['p5_oT%d_%d' % (bs, kc), 'p5_wo'], [pn])
                    kb.op('dve', lambda e: e.tensor_tensor(out=x2t5[ts_][:, hf * 512:(hf + 1) * 512], in0=p_[:], in1=x1t5[ts_][:, hf * 512:(hf + 1) * 512], op=ALU.add),
                          [pn, 'p5_x1_%d' % ts_], ['p5_x2_%d_%d' % (ts_, hf)])
                    ia += 1
                kb.dma('sp', x2_s[tsl, :], x2t5[ts_][:], ['p5_x2_%d_0' % ts_, 'p5_x2_%d_1' % ts_], [('x2', t)], 'p5_st%d' % ts_)
                ti_ += 1
    es5.close()
    es4.close()
    T.barrier()
    if stop_after == '5':
        return finish(kb, es, es_all, out_d)

    with ExitStack() as es5c:
        gbc = sbuf(es5c, "p5c_gbc", [128, D], F32)
        bbc = sbuf(es5c, "p5c_bbc", [128, 36], F32)
        eoff = sbuf(es5c, "p5c_eoff", [128, NEXP], F32)
        trisf = sbuf(es5c, "p5c_trisf", [128, 128], F32)
        trisb = sbuf(es5c, "p5c_trisb", [128, 128], BF16)
        wr = sbuf(es5c, "p5c_wr", [128, 8, 36], BF16)
        tokid = sbuf(es5c, "p5c_tokid", [128, NT], I32)
        macc = sbuf(es5c, "p5c_macc", [128, NEXP], BF16)
        kb.dma('sp', gbc[:], gffn_d.partition_broadcast(128), [], ['gbc'], 'p5c_c0')
        kb.dma('sp', bbc[:], br_d.partition_broadcast(128), [], ['bbc'], 'p5c_c1')
        kb.dma('sp', eoff[:], eoff_d.partition_broadcast(128), [], ['eoff'], 'p5c_c2')
        kb.dma('sp', trisf[:], tris_d[:, :], [], ['trisf'], 'p5c_c3')
        kb.dma('sp', tokid[:], tokid_d[:, :], [], ['tokid'], 'p5c_c4')
        for kc in range(8):
            kb.dma('pool', wr[:, kc, :], wr_d[kc * 128:(kc + 1) * 128, :], [], ['wr'], 'p5c_c5')
        kb.op('dve', lambda e: e.tensor_copy(out=trisb[:], in_=trisf[:]), ['trisf'], ['trisb'])
        kb.op('pool', lambda e: e.memset(macc[:], 0.0), [], ['macc'])
        x2t = [sbuf(es5c, "p5c_x2_%d" % i, [128, D], F32) for i in range(2)]
        junkc = sbuf(es5c, "p5c_junk", [128, D], BF16)
        ssq = [sbuf(es5c, "p5c_ssq%d" % i, [128, 1], F32) for i in range(2)]
        h3 = [sbuf(es5c, "p5c_h3_%d" % i, [128, D], BF16) for i in range(2)]
        h3T = [sbuf(es5c, "p5c_h3T%d" % i, [128, 8, 128], BF16) for i in range(2)]
        pT5 = [psum(es5c, "p5c_pT%d" % i, [128, 8, 128], BF16) for i in range(2)]
        pL = [psum(es5c, "p5c_pL%d" % i, [128, 512], F32) for i in range(2)]
        pPos = [psum(es5c, "p5c_pPos%d" % i, [128, 512], F32) for i in range(2)]
        lg = sbuf(es5c, "p5c_lg", [128, 36], F32)
        sm = sbuf(es5c, "p5c_sm", [128, 16], F32)
        ge = sbuf(es5c, "p5c_ge", [128, 4], F32)
        oh = sbuf(es5c, "p5c_oh", [128, 4], F32)
        lm = sbuf(es5c, "p5c_lm", [128, 4, 8], F32)
        m8 = sbuf(es5c, "p5c_m8", [128, 8], F32)
        m1 = sbuf(es5c, "p5c_m1", [128, NEXP], F32)
        m2 = sbuf(es5c, "p5c_m2", [128, NEXP], F32)
        maskb5 = [sbuf(es5c, "p5c_mask%d" % i, [128, NEXP], BF16) for i in range(2)]
        sl = sbuf(es5c, "p5c_sl", [128, NEXP], F32)
        junk32 = sbuf(es5c, "p5c_junk32", [128, NEXP], F32)
        sf = sbuf(es5c, "p5c_sf", [128, 2], F32)
        lmf = lm[:].rearrange("p g e -> p (g e)")
        for t in range(NT):
            b = t % 2
            tsl = slice(t * 128, (t + 1) * 128)
            kb.dma('sp', x2t[b][:], x2_s[tsl, :], [], ['x2t%d' % b], 'p5c_xl%d' % b)
            kb.op('act', lambda e: e.activation(out=junkc[:], in_=x2t[b][:], func=AF.Square, accum_out=ssq[b][:]), ['x2t%d' % b], ['junkc', 'ssq%d' % b])
            kb.op('act', lambda e: e.activation(out=ssq[b][:], in_=ssq[b][:], func=AF.Ln, scale=1.0 / D, bias=EPS), ['ssq%d' % b], ['ssq%d' % b])
            kb.op('act', lambda e: e.activation(out=ssq[b][:], in_=ssq[b][:], func=AF.Exp, scale=-0.5), ['ssq%d' % b], ['ssq%d' % b])
            kb.op('dve', lambda e: e.scalar_tensor_tensor(out=h3[b][:], in0=x2t[b][:], scalar=ssq[b][:, 0:1], in1=gbc[:], op0=ALU.mult, op1=ALU.mult),
                  ['x2t%d' % b, 'ssq%d' % b, 'gbc'], ['h3_%d' % b])
            for c in range(8):
                kb.op('pe', lambda e: e.transpose(out=pT5[b][:, c, :], in_=h3[b][:, c * 128:(c + 1) * 128], identity=ident_b[:]), ['h3_%d' % b, 'ident_b'], ['ps:p5c_pT%d' % b])
            kb.op('act', lambda e: e.copy(out=h3T[b][:], in_=pT5[b][:]), ['ps:p5c_pT%d' % b], ['h3T%d' % b])
            for kc in range(8):
                kb.op('pe', lambda e: e.matmul(pL[b][:, 0:36], lhsT=h3T[b][:, kc, :], rhs=wr[:, kc, :], start=(kc == 0), stop=(kc == 7)), ['h3T%d' % b, 'wr'], ['ps:p5c_pL%d' % b])
            kb.op('dve', lambda e: e.tensor_tensor(out=lg[:], in0=pL[b][:, 0:36], in1=bbc[:], op=ALU.add), ['ps:p5c_pL%d' % b, 'bbc'], ['lg'])
            kb.op('dve', lambda e: e.reduce_max(out=sm[:, 0:1], in_=lg[:, 0:4], axis=AX.X), ['lg'], ['sm0'])
            kb.op('dve', lambda e: e.tensor_scalar(out=sm[:, 1:2], in0=sm[:, 0:1], scalar1=-1.0, scalar2=None, op0=ALU.mult), ['sm0'], ['sm1'])
            kb.op('act', lambda e: e.activation(out=ge[:], in_=lg[:, 0:4], func=AF.Exp, bias=sm[:, 1:2], accum_out=sm[:, 2:3]), ['lg', 'sm1'], ['ge', 'sm2'])
            kb.op('dve', lambda e: e.reciprocal(out=sm[:, 3:4], in_=sm[:, 2:3]), ['sm2'], ['sm3'])
            kb.op('dve', lambda e: e.tensor_scalar(out=oh[:], in0=lg[:, 0:4], scalar1=sm[:, 0:1], scalar2=None, op0=ALU.is_equal), ['lg', 'sm0'], ['oh'])
            kb.op('dve', lambda e: e.tensor_scalar(out=oh[:], in0=oh[:], scalar1=-1.0, scalar2=1.0e9, op0=ALU.add, op1=ALU.mult), ['oh'], ['oh'])
            kb.op('dve', lambda e: e.tensor_tensor(out=lm[:], in0=lg[:, 4:36].rearrange("p (g e) -> p g e", g=4), in1=oh[:].unsqueeze(2).to_broadcast([128, 4, 8]), op=ALU.add),
                  ['lg', 'oh'], ['lm'])
            kb.op('dve', lambda e: e.max(out=m8[:], in_=lmf), ['lm'], ['m8'])
            kb.op('dve', lambda e: e.tensor_scalar(out=sm[:, 4:5], in0=m8[:, 0:1], scalar1=-1.0, scalar2=None, op0=ALU.mult), ['m8'], ['sm4'])
            kb.op('act', lambda e: e.activation(out=sm[:, 5:6], in_=m8[:, 1:2], func=AF.Exp, bias=sm[:, 4:5]), ['m8', 'sm4'], ['sm5'])
            kb.op('dve', lambda e: e.tensor_scalar(out=sm[:, 6:7], in0=sm[:, 5:6], scalar1=1.0, scalar2=None, op0=ALU.add), ['sm5'], ['sm6'])
            kb.op('dve', lambda e: e.reciprocal(out=sm[:, 7:8], in_=sm[:, 6:7]), ['sm6'], ['sm7'])
            kb.op('dve', lambda e: e.tensor_tensor(out=comb_w[:, t, 0:1], in0=sm[:, 7:8], in1=sm[:, 3:4], op=ALU.mult), ['sm7', 'sm3'], [('cw', t)])
            kb.op('dve', lambda e: e.tensor_tensor(out=comb_w[:, t, 1:2], in0=comb_w[:, t, 0:1], in1=sm[:, 5:6], op=ALU.mult), [('cw', t), 'sm5'], [('cw2', t)])
            kb.op('dve', lambda e: e.tensor_scalar(out=m1[:], in0=lmf, scalar1=m8[:, 0:1], scalar2=None, op0=ALU.is_equal), ['lm', 'm8'], ['m1'])
            kb.op('dve', lambda e: e.tensor_scalar(out=m2[:], in0=lmf, scalar1=m8[:, 1:2], scalar2=None, op0=ALU.is_equal), ['lm', 'm8'], ['m2'])
            kb.op('dve', lambda e: e.tensor_tensor(out=maskb5[b][:], in0=m1[:], in1=m2[:], op=ALU.add), ['m1', 'm2'], ['mask%d' % b])
            kb.op('pe', lambda e: e.matmul(pPos[b][:, 0:NEXP], lhsT=trisb[:], rhs=maskb5[b][:], start=True, stop=False), ['trisb', 'mask%d' % b], ['ps:p5c_pPos%d' % b])
            kb.op('pe', lambda e: e.matmul(pPos[b][:, 0:NEXP], lhsT=ones_bb[:], rhs=macc[:], start=False, stop=True), ['ones_bb', 'macc'], ['ps:p5c_pPos%d' % b], attach='macc')
            kb.op('dve', lambda e: e.tensor_tensor(out=macc[:], in0=macc[:], in1=maskb5[b][:], op=ALU.add), ['macc', 'mask%d' % b], ['macc'])
            kb.op('dve', lambda e: e.scalar_tensor_tensor(out=sl[:], in0=pPos[b][:, 0:NEXP], scalar=float(CAP - 1), in1=eoff[:], op0=ALU.min, op1=ALU.add),
                  ['ps:p5c_pPos%d' % b, 'eoff'], ['sl'])
            kb.op('dve', lambda e: e.scalar_tensor_tensor(out=junk32[:], in0=sl[:], scalar=1.0, in1=m1[:], op0=ALU.mult, op1=ALU.mult, accum_out=sf[:, 0:1]), ['sl', 'm1'], ['junk32', 'sf0'])
            kb.op('dve', lambda e: e.scalar_tensor_tensor(out=junk32[:], in0=sl[:], scalar=1.0, in1=m2[:], op0=ALU.mult, op1=ALU.mult, accum_out=sf[:, 1:2]), ['sl', 'm2'], ['junk32', 'sf1'])
            kb.op('dve', lambda e: e.tensor_copy(out=slot_i[:, t, :], in_=sf[:]), ['sf0', 'sf1'], [('slot', t)])
            for k2 in range(2):
                T.op('pool', lambda e: e.indirect_dma_start(out=Xs_d[:, :], out_offset=bass.IndirectOffsetOnAxis(ap=slot_i[:, t, k2:k2 + 1], axis=0),
                                                            in_=h3[b][:], in_offset=None),
                     reads=['h3_%d' % b, ('slot', t)], writes=[('Xs', t, k2)], lane='p5c_sc%d_%d' % (b, k2))
    T.barrier()
    if stop_after == '5b':
        return finish(kb, es, es_all, out_d)

    with ExitStack() as es6:
        w1b = [sbuf(es6, "p6_w1_%d" % i, [128, 8, DEXP], BF16) for i in range(2)]
        w3b = [sbuf(es6, "p6_w3_%d" % i, [128, 8, DEXP], BF16) for i in range(2)]
        w2b = [sbuf(es6, "p6_w2_%d" % i, [128, 4, D], BF16) for i in range(2)]
        xb = [sbuf(es6, "p6_xb%d" % i, [128, D], BF16) for i in range(3)]
        XT = [sbuf(es6, "p6_XT%d" % i, [128, 8, 512], BF16) for i in range(2)]
        s1 = [sbuf(es6, "p6_s1_%d" % i, [128, 512], BF16) for i in range(2)]
        gT6 = [sbuf(es6, "p6_gT%d" % i, [128, 4, 512], BF16) for i in range(2)]
        ysb = [sbuf(es6, "p6_y%d" % i, [128, D], BF16) for i in range(2)]
        pT6 = [psum(es6, "p6_pT%d" % i, [128, 8, 128], BF16) for i in range(2)]
        p1 = [psum(es6, "p6_p1_%d" % i, [128, 512], F32) for i in range(2)]
        p3 = [psum(es6, "p6_p3_%d" % i, [128, 512], F32) for i in range(2)]
        py = [psum(es6, "p6_py%d" % i, [128, 512], F32) for i in range(2)]
        xi = 0
        ci = 0
        mi = 0
        yi = 0
        for ex in range(NEXP):
            ws = ex % 2
            kb.dma('pool', w1b[ws][:], w1_d[ex].rearrange("(c p) n -> p c n", p=128), [], ['w1_%d' % ws], 'p6_w1l%d' % ws)
            kb.dma('pool', w3b[ws][:], w3_d[ex].rearrange("(c p) n -> p c n", p=128), [], ['w3_%d' % ws], 'p6_w3l%d' % ws)
            kb.dma('pool', w2b[ws][:], w2_d[ex].rearrange("(c p) n -> p c n", p=128), [], ['w2_%d' % ws], 'p6_w2l%d' % ws)
            for hb in range(CAPB // 4):
                cs = ci % 2
                row0 = ex * CAP + hb * 512
                for j in range(4):
                    xs_ = xi % 3
                    kb.dma('sp', xb[xs_][:], Xs_d[row0 + j * 128:row0 + (j + 1) * 128, :], [], ['xb%d' % xs_], 'p6_xl%d' % xs_)
                    pt_ = xi % 2
                    for c in range(8):
                        kb.op('pe', lambda e: e.transpose(out=pT6[pt_][:, c, :], in_=xb[xs_][:, c * 128:(c + 1) * 128], identity=ident_b[:]), ['xb%d' % xs_, 'ident_b'], ['ps:p6_pT%d' % pt_])
                    if xi % 2 == 0:
                        kb.op('act', lambda e: e.copy(out=XT[cs][:, :, j * 128:(j + 1) * 128], in_=pT6[pt_][:]), ['ps:p6_pT%d' % pt_], ['XT%d_%d' % (cs, j)])
                    else:
                        kb.op('dve', lambda e: e.tensor_copy(out=XT[cs][:, :, j * 128:(j + 1) * 128], in_=pT6[pt_][:]), ['ps:p6_pT%d' % pt_], ['XT%d_%d' % (cs, j)])
                    xi += 1
                xtn = ['XT%d_%d' % (cs, j) for j in range(4)]
                for m in range(4):
                    ms = mi % 2
                    for kc in range(8):
                        kb.op('pe', lambda e: e.matmul(p1[ms][:], lhsT=w1b[ws][:, kc, m * 128:(m + 1) * 128], rhs=XT[cs][:, kc, :], start=(kc == 0), stop=(kc == 7)),
                              ['w1_%d' % ws] + xtn, ['ps:p6_p1_%d' % ms])
                    for kc in range(8):
                        kb.op('pe', lambda e: e.matmul(p3[ms][:], lhsT=w3b[ws][:, kc, m * 128:(m + 1) * 128], rhs=XT[cs][:, kc, :], start=(kc == 0), stop=(kc == 7)),
                              ['w3_%d' % ws] + xtn, ['ps:p6_p3_%d' % ms])
                    kb.op('act', lambda e: e.activation(out=s1[ms][:], in_=p1[ms][:], func=AF.Silu), ['ps:p6_p1_%d' % ms], ['s1_%d' % ms])
                    kb.op('dve', lambda e: e.tensor_tensor(out=gT6[cs][:, m, :], in0=p3[ms][:], in1=s1[ms][:], op=ALU.mult), ['ps:p6_p3_%d' % ms, 's1_%d' % ms], ['gT%d_%d' % (cs, m)])
                    mi += 1
                gtn = ['gT%d_%d' % (cs, m) for m in range(4)]
                for j in range(4):
                    ys_ = yi % 2
                    for hf in range(2):
                        for kc in range(4):
                            kb.op('pe', lambda e: e.matmul(py[hf][:], lhsT=gT6[cs][:, kc, j * 128:(j + 1) * 128], rhs=w2b[ws][:, kc, hf * 512:(hf + 1) * 512], start=(kc == 0), stop=(kc == 3)),
                                  gtn + ['w2_%d' % ws], ['ps:p6_py%d' % hf])
                        if hf == 0:
                            kb.op('act', lambda e: e.copy(out=ysb[ys_][:, 0:512], in_=py[0][:]), ['ps:p6_py0'], ['ysb%d_0' % ys_])
                        else:
                            kb.op('dve', lambda e: e.tensor_copy(out=ysb[ys_][:, 512:1024], in_=py[1][:]), ['ps:p6_py1'], ['ysb%d_1' % ys_])
                    kb.dma('sp', Y_d[row0 + j * 128:row0 + (j + 1) * 128, :], ysb[ys_][:], ['ysb%d_0' % ys_, 'ysb%d_1' % ys_], [('Y', ex, hb, j)], 'p6_st%d' % ys_)
                    yi += 1
                ci += 1
    T.barrier()
    if stop_after == '6':
        return finish(kb, es, es_all, out_d)

    with ExitStack() as es7:
        gfb = sbuf(es7, "p7_gfb", [128, D], F32)
        kb.dma('sp', gfb[:], gfin_d.partition_broadcast(128), [], ['gfb'], 'p7_c0')
        x2t7 = [sbuf(es7, "p7_x2_%d" % i, [128, D], F32) for i in range(2)]
        y1 = [sbuf(es7, "p7_y1_%d" % i, [128, D], BF16) for i in range(2)]
        y2 = [sbuf(es7, "p7_y2_%d" % i, [128, D], BF16) for i in range(2)]
        x3 = [sbuf(es7, "p7_x3_%d" % i, [128, D], F32) for i in range(2)]
        junk7 = sbuf(es7, "p7_junk", [128, D], BF16)
        ssq7 = [sbuf(es7, "p7_ssq%d" % i, [128, 1], F32) for i in range(2)]
        o7 = [sbuf(es7, "p7_o%d" % i, [128, D], F32) for i in range(2)]
        for t in range(NT):
            b = t % 2
            tsl = slice(t * 128, (t + 1) * 128)
            kb.dma('sp', x2t7[b][:], x2_s[tsl, :], [], ['x2t%d' % b], 'p7_xl%d' % b)
            for k2, yy in ((0, y1), (1, y2)):
                T.op('pool', lambda e: e.indirect_dma_start(out=yy[b][:], out_offset=None, in_=Y_d[:, :],
                                                            in_offset=bass.IndirectOffsetOnAxis(ap=slot_i[:, t, k2:k2 + 1], axis=0)),
                     reads=[], writes=['y%d_%d' % (k2, b)], lane='p7_g%d_%d' % (k2, b))
            kb.op('dve', lambda e: e.scalar_tensor_tensor(out=x3[b][:], in0=y1[b][:], scalar=comb_w[:, t, 0:1], in1=x2t7[b][:], op0=ALU.mult, op1=ALU.add),
                  ['y0_%d' % b, 'x2t%d' % b], ['x3_%d' % b])
            kb.op('dve', lambda e: e.scalar_tensor_tensor(out=x3[b][:], in0=y2[b][:], scalar=comb_w[:, t, 1:2], in1=x3[b][:], op0=ALU.mult, op1=ALU.add),
                  ['y1_%d' % b, 'x3_%d' % b], ['x3_%d' % b])
            kb.op('act', lambda e: e.activation(out=junk7[:], in_=x3[b][:], func=AF.Square, accum_out=ssq7[b][:]), ['x3_%d' % b], ['junk7', 'ssq7_%d' % b])
            kb.op('act', lambda e: e.activation(out=ssq7[b][:], in_=ssq7[b][:], func=AF.Ln, scale=1.0 / D, bias=EPS), ['ssq7_%d' % b], ['ssq7_%d' % b])
            kb.op('act', lambda e: e.activation(out=ssq7[b][:], in_=ssq7[b][:], func=AF.Exp, scale=-0.5), ['ssq7_%d' % b], ['ssq7_%d' % b])
            kb.op('dve', lambda e: e.scalar_tensor_tensor(out=o7[b][:], in0=x3[b][:], scalar=ssq7[b][:, 0:1], in1=gfb[:], op0=ALU.mult, op1=ALU.mult),
                  ['x3_%d' % b, 'ssq7_%d' % b, 'gfb'], ['o7_%d' % b])
            kb.dma('sp', out_d[tsl, :], o7[b][:], ['o7_%d' % b], [('out', t)], 'p7_st%d' % b)
    return finish(kb, es, es_all, out_d)


def finish(kb, es, es_all, out_d):
    kb.T.barrier()
    return kb


def prep_inputs(inputs, b):
    f = lambda k: np.ascontiguousarray(np.asarray(inputs[k], dtype=np.float32))
    c = host_consts()
    m = {}
    m['x'] = f('x')[b]
    m['mem'] = f('mem')[b]
    perm = win_perm()
    m['w_in_ext'] = np.ascontiguousarray(f('w_in')[0][:, perm])
    m['w_out'] = f('w_out')[0]

    def g128(v):
        return v.reshape(8, 128).T
    m['gT'] = np.ascontiguousarray(np.concatenate([g128(f('norm_mix_g')[0]), g128(f('norm_x_g')[0]),
                                                   g128(f('norm_mem_g')[0]), g128(f('norm_ffn_g')[0])], axis=1))
    m['g_final'] = f('norm_final_g')
    m['g_ffn_row'] = f('norm_ffn_g')[0]
    kk = np.arange(128)[:, None]
    qq = np.arange(128)[None, :]
    m['tri_strict'] = (kk < qq).astype(np.float32)
    m['eoff'] = (np.arange(NEXP) * CAP).astype(np.float32)
    m['tokid'] = (np.arange(NT)[None, :] * 128 + np.arange(128)[:, None]).astype(np.int32)
    m['cosT'] = c['cosT']; m['sinT'] = c['sinT']; m['ident_f'] = c['ident_f']; m['maskb'] = c['maskb']; m['tri_incl'] = c['tri_incl']
    m['da_lambda'] = f('da_lambda')[0].reshape(256)
    m['da_subln_g'] = f('da_subln_g')[0]
    cw = f('ml_conv_w')[0][:, 0, :]
    cb = f('ml_conv_b')[0]
    convT = np.zeros((128, 40), np.float32)
    for gidx in range(8):
        cols = slice(gidx * 128, (gidx + 1) * 128)
        convT[:, gidx * 5:gidx * 5 + 4] = cw[:, cols].T
        convT[:, gidx * 5 + 4] = cb[cols]
    m['convT'] = convT
    m['ml_gate_b'] = f('ml_gate_b')[0].reshape(8)
    m['ml_norm_g'] = f('ml_norm_g')[0]
    for k in ('w_xq', 'w_xk', 'w_xv', 'w_xo'):
        m[k] = f(k)[0]
    m['w_router'] = np.ascontiguousarray(np.concatenate([f('w_router_group')[0], f('w_router_expert')[0]], axis=1))
    m['b_router'] = np.concatenate([f('b_router_group')[0], f('b_router_expert')[0]])
    m['w1'] = f('w1')[0]; m['w3'] = f('w3')[0]; m['w2'] = f('w2')[0]
    return m


_CACHE = {}


def kernel(**inputs):
    if 'kb' not in _CACHE:
        _CACHE['kb'] = build()
    kb = _CACHE['kb']
    n = 8
    maps = []
    for b in range(n):
        m = prep_inputs(inputs, b)
        maps.append({k: v for k, v in m.items() if k in kb.inp})
    res = run_bass_kernel_spmd(kb.nc, maps, core_ids=list(range(n)))
    return np.stack([np.asarray(r["out"], dtype=np.float32) for r in res.results], axis=0)
```

```python
import numpy as np
from contextlib import ExitStack
import concourse.bass as bass
import concourse.mybir as mybir
from concourse.bass_utils import run_bass_kernel_spmd

F32 = mybir.dt.float32
BF16 = mybir.dt.bfloat16
I32 = mybir.dt.int32
AF = mybir.ActivationFunctionType
ALU = mybir.AluOpType
AX = mybir.AxisListType

S = 4096
D = 1024
NT = S // 128
NB = S // 512
EPS = 1e-6
MEM = 256
NEXP = 32
DEXP = 512
CAPB = 6
CHB = 3
CAP = CAPB * 128
LAM_INIT = 0.2
NEG = -30000.0
STRICT = False

FM_COLS = 24 * 128
TM_COLS = 512 * 3 + 8
WIN_COLS = FM_COLS + TM_COLS


class Trk:
    def __init__(self, nc, needed=None):
        self.nc = nc
        self.needed_in = needed
        self.needed = {}
        self.phys = {}
        self.pcnt = {}
        self.eng = {'pe': nc.tensor, 'act': nc.scalar, 'dve': nc.vector, 'pool': nc.gpsimd, 'sp': nc.sync}
        self.sem = {}
        self.cnt = {}
        self.seen = {e: {} for e in self.eng}
        self.lastw = {}
        self.reads = {}
        self.stack = ExitStack()
        self.phase = 0
        self.nsem = 0
        self.pool = {'sw': [], 'hw': []}
        self.kind = {}

    def lane(self, name, eng='sp'):
        if name not in self.sem:
            kind = 'sw' if eng == 'pool' else 'hw'
            self.kind[name] = kind
            if self.pool[kind] and not name.startswith('eng_'):
                sh, c = self.pool[kind].pop()
                self.sem[name] = sh
                self.cnt[name] = c
            else:
                self.nsem += 1
                s = self.stack.enter_context(self.nc.semaphore("s%d" % self.nsem))
                self.sem[name] = s
                self.cnt[name] = 0
        return name

    def elane(self, eng):
        return "eng_%s" % eng

    def pval(self, ln, v):
        if not ln.startswith("eng_"):
            return v
        self.needed.setdefault(ln, set()).add(v)
        if self.needed_in is None:
            return v
        return self.phys[ln][v]

    def wait(self, eng, ticket):
        ln, v = ticket
        if self.seen[eng].get(ln, 0) < v:
            self.eng[eng].wait_ge(self.sem[ln], self.pval(ln, v))
            self.seen[eng][ln] = v

    def op(self, eng, fn, reads=(), writes=(), lane=None, inc=None, attach=None):
        deps = {}
        own = self.elane(eng)
        psr = [r for r in reads if isinstance(r, str) and r.startswith('ps:')]
        if psr:
            reads = [r for r in reads if r not in psr]
            writes = list(writes) + [r for r in psr if r not in writes]
        for r in reads:
            t = self.lastw.get(r)
            if t is not None:
                deps[t[0]] = max(deps.get(t[0], 0), t[1])
        for w in writes:
            t = self.lastw.get(w)
            if t is not None and (STRICT or t[0] != own):
                deps[t[0]] = max(deps.get(t[0], 0), t[1])
            for t in self.reads.get(w, ()):
                if STRICT or t[0] != own:
                    deps[t[0]] = max(deps.get(t[0], 0), t[1])
        for ln, v in deps.items():
            self.wait(eng, (ln, v))
        ins = fn(self.eng[eng])
        if attach is not None:
            t = self.lastw.get(attach)
            if t is not None:
                ins._wait_ge(self.sem[t[0]], self.pval(t[0], t[1]))
        if lane is None:
            lane = own
            inc = 1
        elif inc is None:
            inc = 16
        self.lane(lane, eng)
        self.cnt[lane] += inc
        t = (lane, self.cnt[lane])
        if lane.startswith("eng_"):
            if self.needed_in is None or t[1] in self.needed_in.get(lane, ()):
                ins.then_inc(self.sem[lane], 1)
                self.pcnt[lane] = self.pcnt.get(lane, 0) + 1
                self.phys.setdefault(lane, {})[t[1]] = self.pcnt[lane]
        else:
            ins.then_inc(self.sem[lane], inc)
        for r in reads:
            self.reads.setdefault(r, []).append(t)
        for w in writes:
            self.lastw[w] = t
            self.reads[w] = []
        return t

    def barrier(self):
        for e in self.eng:
            for ln, v in self.cnt.items():
                if v > 0:
                    self.wait(e, (ln, v))
        self.lastw = {}
        self.reads = {}
        self.phase += 1
        for ln in list(self.sem.keys()):
            if not ln.startswith("eng_"):
                self.pool[self.kind.pop(ln)].append((self.sem.pop(ln), self.cnt.pop(ln)))
                for e in self.eng:
                    self.seen[e].pop(ln, None)


def host_consts():
    c = {}
    c['ident_f'] = np.eye(128, dtype=np.float32)
    k = np.arange(128)[:, None]
    q = np.arange(128)[None, :]
    c['maskb'] = np.where(k <= q, 0.0, NEG).astype(np.float32)
    c['tri_incl'] = (k <= q).astype(np.float32)
    inv_freq = (10000.0 ** (-np.arange(0, 64, 2, dtype=np.float32) / np.float32(64))).astype(np.float32)
    pos = np.arange(S, dtype=np.float32)
    ang = (pos[:, None] * inv_freq[None, :]).astype(np.float32)
    cs = np.cos(ang).astype(np.float32).T
    sn = np.sin(ang).astype(np.float32).T
    cosT = np.zeros((128, S), np.float32)
    sinT = np.zeros((128, S), np.float32)
    for p in range(128):
        d = p % 64
        cosT[p] = cs[d % 32]
        sinT[p] = -sn[d % 32] if d < 32 else sn[d % 32]
    c['cosT'] = cosT
    c['sinT'] = sinT
    return c


def win_perm():
    idx = []
    off_q, off_k, off_v = 0, 512, 1024
    off_mq, off_mk, off_mv, off_mo, off_mi, off_mf = 1536, 2048, 2560, 3072, 3584, 3588

    def sw(base):
        out = []
        for m in range(2):
            b = base + m * 64
            out += list(range(b + 32, b + 64)) + list(range(b, b + 32))
        return out
    for h in range(4):
        idx += list(range(off_q + h * 128, off_q + (h + 1) * 128))
        idx += sw(off_q + h * 128)
        idx += list(range(off_k + h * 128, off_k + (h + 1) * 128))
        idx += sw(off_k + h * 128)
    idx += list(range(off_mq, off_mq + 512))
    idx += list(range(off_mk, off_mk + 512))
    idx += list(range(off_v, off_v + 512))
    idx += list(range(off_mv, off_mv + 512))
    idx += list(range(off_mo, off_mo + 512))
    idx += list(range(off_mi, off_mi + 4)) + list(range(off_mf, off_mf + 4))
    assert len(idx) == WIN_COLS
    return np.array(idx)


class K:
    def __init__(self, debug=None, needed=None):
        self.debug = debug or ()
        nc = bass.Bass("TRN2", target_bir_lowering=False)
        self.nc = nc
        self.T = Trk(nc, needed)
        self.inp = {}
        self.scr = {}
        self.dmaq = 0

    def din(self, name, shape, dt=F32):
        kb = self

        class Lazy:
            def _get(s_):
                if name not in kb.inp:
                    kb.inp[name] = kb.nc.dram_tensor(name, list(shape), dt, kind="ExternalInput").ap()
                return kb.inp[name]

            def __getitem__(s_, k):
                return s_._get()[k]

            def __getattr__(s_, a):
                return getattr(s_._get(), a)
        return Lazy()

    def dscr(self, name, shape, dt):
        kind = "ExternalOutput" if name in self.debug else "Internal"
        self.scr[name] = self.nc.dram_tensor(name, list(shape), dt, kind=kind).ap()
        return self.scr[name]

    def dma(self, eng, out, in_, reads, writes, lane, **kw):
        return self.T.op(eng, lambda e: e.dma_start(out=out, in_=in_, **kw), reads=reads, writes=writes, lane=lane)

    def op(self, eng, fn, reads=(), writes=(), attach=None):
        if eng == 'pe' and attach is None and reads:
            attach = reads[0]
        return self.T.op(eng, fn, reads=reads, writes=writes, attach=attach)


def build(debug=None, stop_after=None, skip12=False, p3_tiles=NT):
    rec = _build(debug, stop_after, skip12, p3_tiles, None)
    return _build(debug, stop_after, skip12, p3_tiles, rec.T.needed)


def _build(debug, stop_after, skip12, p3_tiles, needed):
    kb = K(debug, needed)
    nc, T = kb.nc, kb.T
    din, dscr = kb.din, kb.dscr
    x_d = din("x", [S, D])
    mem_d = din("mem", [MEM, D])
    win_d = din("w_in_ext", [D, WIN_COLS])
    wout_d = din("w_out", [D, D])
    gT_d = din("gT", [128, 4 * 8])
    gfin_d = din("g_final", [D])
    gffn_d = din("g_ffn_row", [D])
    tris_d = din("tri_strict", [128, 128])
    eoff_d = din("eoff", [NEXP])
    tokid_d = din("tokid", [128, NT], I32)
    cos_d = din("cosT", [128, S])
    sin_d = din("sinT", [128, S])
    ident_d = din("ident_f", [128, 128])
    maskb_d = din("maskb", [128, 128])
    tri_d = din("tri_incl", [128, 128])
    lam_d = din("da_lambda", [256])
    subln_d = din("da_subln_g", [128])
    convw_d = din("convT", [128, 8 * 5])
    gateb_d = din("ml_gate_b", [8])
    mlg_d = din("ml_norm_g", [512])
    wxq_d = din("w_xq", [D, D]); wxk_d = din("w_xk", [D, D]); wxv_d = din("w_xv", [D, D]); wxo_d = din("w_xo", [D, D])
    wr_d = din("w_router", [D, 36])
    br_d = din("b_router", [36])
    w1_d = din("w1", [NEXP, D, DEXP]); w3_d = din("w3", [NEXP, D, DEXP]); w2_d = din("w2", [NEXP, DEXP, D])
    out_d = nc.dram_tensor("out", [S, D], F32, kind="ExternalOutput").ap()

    qT_s = dscr("qT_s", [4, 128, S], BF16)
    kT_s = dscr("kT_s", [4, 128, S], BF16)
    mqT_s = dscr("mqT_s", [4, 128, S], BF16)
    mkT_s = dscr("mkT_s", [4, 128, S], BF16)
    tmb_s = dscr("tmb_s", [S, 1536], BF16)
    gate_s = dscr("gate_s", [128, NT, 8], F32)
    y_s = dscr("y_s", [S, D], BF16)
    x1_s = dscr("x1_s", [S, D], F32)
    x2_s = dscr("x2_s", [S, D], F32)
    Xs_d = dscr("Xs_d", [NEXP * CAP, D], BF16)
    Y_d = dscr("Y_d", [NEXP * CAP, D], BF16)

    es_all = ExitStack()

    def sbuf(es, name, shape, dt):
        return es.enter_context(nc.sbuf_tensor("sb_" + name, list(shape), dt))

    def psum(es, name, shape, dt):
        return es.enter_context(nc.psum_tensor("ps_" + name, list(shape), dt))

    ident_f = sbuf(es_all, "ident_f", [128, 128], F32)
    ident_b = sbuf(es_all, "ident_b", [128, 128], BF16)
    gT = sbuf(es_all, "gT", [128, 32], F32)
    kb.dma('sp', ident_f[:], ident_d[:, :], [], ['ident_f'], 'c0')
    kb.dma('sp', gT[:], gT_d[:, :], [], ['gT'], 'c1')
    kb.dma('pool', ident_b[:], ident_d[:, :], [], ['ident_b'], 'c2')
    slot_i = sbuf(es_all, "slot_i", [128, NT, 2], I32)
    comb_w = sbuf(es_all, "comb_w", [128, NT, 2], F32)
    ones_bb = sbuf(es_all, "ones_bb", [128, 128], BF16)
    kb.op('pool', lambda e: e.memset(ones_bb[:], 1.0), [], ['ones_bb'])
    es = ExitStack()
    if True:
        zt = sbuf(es, "zt", [128, 8192], BF16)
        kb.op('pool', lambda e: e.memset(zt[:], 0.0), [], ['zt'])
        for e_ in range(NEXP):
            kb.dma('pool', Xs_d[e_ * CAP:(e_ + 1) * CAP, :].rearrange("(p r) c -> p (r c)", p=128), zt[:, 0:CAP * D // 128], ['zt'], [('Xs0', e_)], 'zX%d' % (e_ % 4))

    hT = sbuf(es, "hT", [128, 8, S], BF16)
    cosT = sbuf(es, "cosT", [128, S], F32)
    sinT = sbuf(es, "sinT", [128, S], F32)
    convT = sbuf(es, "convT", [128, 40], F32)
    kb.dma('sp', cosT[:], cos_d[:, :], [], ['cosT'], 'c3')
    kb.dma('sp', sinT[:], sin_d[:, :], [], ['sinT'], 'c4')
    kb.dma('sp', convT[:], convw_d[:, :], [], ['convT'], 'c5')

    def norm_transpose(es_, tagp, x_src_tiles, gcol, dstT, ntiles, pT_tiles, dst_tag='hT_t'):
        xt = [sbuf(es_, "%s_xt%d" % (tagp, i), [128, D], F32) for i in range(3)]
        junk = sbuf(es_, tagp + "_junk", [128, D], BF16)
        ssq = [sbuf(es_, "%s_ssq%d" % (tagp, i), [128, 1], F32) for i in range(2)]
        rstd = [sbuf(es_, "%s_rstd%d" % (tagp, i), [128, 1], F32) for i in range(2)]
        xs = [sbuf(es_, "%s_xs%d" % (tagp, i), [128, D], BF16) for i in range(2)]
        for t in range(ntiles):
            a, b = t % 3, t % 2
            kb.dma('sp', xt[a][:], x_src_tiles(t), [], [tagp + 'xt%d' % a], tagp + 'ld%d' % a)
            kb.op('act', lambda e: e.activation(out=junk[:], in_=xt[a][:], func=AF.Square, accum_out=ssq[b][:]),
                  [tagp + 'xt%d' % a], [tagp + 'junk', tagp + 'ssq%d' % b])
            kb.op('act', lambda e: e.activation(out=rstd[b][:], in_=ssq[b][:], func=AF.Sqrt, scale=1.0 / D, bias=EPS),
                  [tagp + 'ssq%d' % b], [tagp + 'rstd%d' % b])
            kb.op('dve', lambda e: e.reciprocal(out=rstd[b][:], in_=rstd[b][:]),
                  [tagp + 'rstd%d' % b], [tagp + 'rstd%d' % b])
            kb.op('dve', lambda e: e.tensor_scalar(out=xs[b][:], in0=xt[a][:], scalar1=rstd[b][:], scalar2=None, op0=ALU.mult),
                  [tagp + 'xt%d' % a, tagp + 'rstd%d' % b], [tagp + 'xs%d' % b])
            pT = pT_tiles[b]
            for c in range(8):
                kb.op('pe', lambda e: e.transpose(out=pT[:, c, :], in_=xs[b][:, c * 128:(c + 1) * 128], identity=ident_b[:]),
                      [tagp + 'xs%d' % b, 'ident_b'], ['ps:' + tagp + 'pT%d' % b])
            kb.op('dve', lambda e: e.tensor_tensor(out=dstT[:, :, t * 128:(t + 1) * 128], in0=pT[:],
                                                    in1=gT[:, gcol * 8:(gcol + 1) * 8].unsqueeze(2).to_broadcast([128, 8, 128]),
                                                    op=ALU.mult),
                  ['ps:' + tagp + 'pT%d' % b, 'gT'], [dst_tag + '%d' % t])

    NT1 = 0 if skip12 else NT
    NB1 = 0 if skip12 else NB
    with ExitStack() as es1a:
        pTt = [psum(es1a, "p1a_pT%d" % i, [128, 8, 128], BF16) for i in range(2)]
        norm_transpose(es1a, "p1a", lambda t: x_d[t * 128:(t + 1) * 128, :], 0, hT, NT1, pTt)
    T.barrier()

    with ExitStack() as es1b:
        wq = [sbuf(es1b, "p1b_w%d" % i, [128, 8, 256], BF16) for i in range(2)]
        pq = [psum(es1b, "p1b_pq%d" % i, [128, 512], F32) for i in range(4)]
        r1 = [sbuf(es1b, "p1b_r1_%d" % i, [128, 512], F32) for i in range(2)]
        r2 = [sbuf(es1b, "p1b_r2_%d" % i, [128, 512], F32) for i in range(2)]
        ro = [sbuf(es1b, "p1b_ro%d" % i, [128, 512], BF16) for i in range(2)]
        it = 0
        for pair in range(0 if skip12 else 8):
            h, isk = pair // 2, pair % 2
            wbuf = wq[pair % 2]
            c0 = pair * 256
            kb.dma('pool', wbuf[:], win_d[:, c0:c0 + 256].rearrange("(c p) n -> p c n", p=128), [], ['p1b_w%d' % (pair % 2)], 'p1b_wl%d' % (pair % 2))
            dst = (kT_s if isk else qT_s)
            for blk in range(NB):
                pa, pb = pq[(it % 2) * 2], pq[(it % 2) * 2 + 1]
                na, nb_ = 'ps:p1b_pq%d' % ((it % 2) * 2), 'ps:p1b_pq%d' % ((it % 2) * 2 + 1)
                for kc in range(8):
                    kb.op('pe', lambda e: e.matmul(pa[:], lhsT=wbuf[:, kc, 0:128], rhs=hT[:, kc, blk * 512:(blk + 1) * 512], start=(kc == 0), stop=(kc == 7)),
                          ['p1b_w%d' % (pair % 2)] + ['hT_t%d' % (blk * 4 + j) for j in range(4)], [na])
                for kc in range(8):
                    kb.op('pe', lambda e: e.matmul(pb[:], lhsT=wbuf[:, kc, 128:256], rhs=hT[:, kc, blk * 512:(blk + 1) * 512], start=(kc == 0), stop=(kc == 7)),
                          ['p1b_w%d' % (pair % 2)] + ['hT_t%d' % (blk * 4 + j) for j in range(4)], [nb_])
                s_ = it % 2
                kb.op('dve', lambda e: e.tensor_tensor(out=r1[s_][:], in0=pa[:], in1=cosT[:, blk * 512:(blk + 1) * 512], op=ALU.mult),
                      [na, 'cosT'], ['p1b_r1_%d' % s_])
                kb.op('dve', lambda e: e.tensor_tensor(out=r2[s_][:], in0=pb[:], in1=sinT[:, blk * 512:(blk + 1) * 512], op=ALU.mult),
                      [nb_, 'sinT'], ['p1b_r2_%d' % s_])
                kb.op('pool', lambda e: e.tensor_tensor(out=ro[s_][:], in0=r1[s_][:], in1=r2[s_][:], op=ALU.add),
                      ['p1b_r1_%d' % s_, 'p1b_r2_%d' % s_], ['p1b_ro%d' % s_])
                kb.dma('sp', dst[h, :, blk * 512:(blk + 1) * 512], ro[s_][:], ['p1b_ro%d' % s_], [('qk', pair, blk)], 'p1b_st%d' % s_)
                it += 1

    if stop_after == '1b':
        return finish(kb, es, es_all, out_d)
    T.barrier()
    with ExitStack() as es1c:
        wm = [sbuf(es1c, "p1c_w%d" % i, [128, 8, 128], BF16) for i in range(2)]
        pm = [psum(es1c, "p1c_pm%d" % i, [128, 512], F32) for i in range(2)]
        ub = [sbuf(es1c, "p1c_ub%d" % i, [128, 515], F32) for i in range(2)]
        ca = [sbuf(es1c, "p1c_ca%d" % i, [128, 512], F32) for i in range(2)]
        co = [sbuf(es1c, "p1c_co%d" % i, [128, 512], BF16) for i in range(2)]
        it = 0
        for g in range(0 if skip12 else 8):
            wbuf = wm[g % 2]
            wn = 'p1c_w%d' % (g % 2)
            c0 = 16 * 128 + g * 128
            kb.dma('pool', wbuf[:], win_d[:, c0:c0 + 128].rearrange("(c p) n -> p c n", p=128), [], [wn], 'p1c_wl%d' % (g % 2))
            dst = (mqT_s if g < 4 else mkT_s)
            h = g % 4
            cw = lambda i: convT[:, g * 5 + i:g * 5 + i + 1]
            for blk in range(NB):
                s_ = it % 2
                p_, pn = pm[s_], 'ps:p1c_pm%d' % s_
                u_, un = ub[s_], 'p1c_ub%d' % s_
                for kc in range(8):
                    kb.op('pe', lambda e: e.matmul(p_[:], lhsT=wbuf[:, kc, :], rhs=hT[:, kc, blk * 512:(blk + 1) * 512], start=(kc == 0), stop=(kc == 7)),
                          [wn], [pn])
                if blk == 0:
                    kb.op('pool', lambda e: e.memset(u_[:, 0:3], 0.0), [], [un + 'h'])
                else:
                    up = ub[1 - s_]
                    kb.op('act', lambda e: e.copy(out=u_[:, 0:3], in_=up[:, 512:515]), ['p1c_ub%d' % (1 - s_)], [un + 'h'])
                kb.op('act', lambda e: e.copy(out=u_[:, 3:515], in_=p_[:]), [pn], [un])
                a_, an = ca[s_], 'p1c_ca%d' % s_
                kb.op('dve', lambda e: e.tensor_scalar(out=a_[:], in0=u_[:, 0:512], scalar1=cw(0), scalar2=None, op0=ALU.mult),
                      [un, un + 'h', 'convT'], [an])
                for i in (1, 2, 3):
                    kb.op('dve', lambda e: e.scalar_tensor_tensor(out=a_[:], in0=u_[:, i:i + 512], scalar=cw(i), in1=a_[:], op0=ALU.mult, op1=ALU.add),
                          [un, un + 'h', an, 'convT'], [an])
                o_, on = co[s_], 'p1c_co%d' % s_
                kb.op('act', lambda e: e.activation(out=o_[:], in_=a_[:], func=AF.Silu, bias=cw(4)), [an, 'convT'], [on])
                kb.dma('sp', dst[h, :, blk * 512:(blk + 1) * 512], o_[:], [on], [('mqk', g, blk)], 'p1c_st%d' % s_)
                it += 1
    T.barrier()
    with ExitStack() as es1d:
        wt = sbuf(es1d, "p1d_w", [128, 8, TM_COLS], BF16)
        for kc in range(8):
            kb.dma('pool', wt[:, kc, :], win_d[kc * 128:(kc + 1) * 128, FM_COLS:WIN_COLS], [], ['p1d_w'], 'p1d_wl')
        pt = [[psum(es1d, "p1d_p%d_%d" % (i, j), [128, 512], F32) for j in range(4)] for i in range(2)]
        ot = [sbuf(es1d, "p1d_o%d" % i, [128, 1536], BF16) for i in range(2)]
        og = [sbuf(es1d, "p1d_g%d" % i, [128, 8], F32) for i in range(2)]
        for t in range(NT1):
            s_ = t % 2
            for j in range(4):
                n0, n1 = (j * 512, (j + 1) * 512) if j < 3 else (1536, 1544)
                for kc in range(8):
                    kb.op('pe', lambda e: e.matmul(pt[s_][j][:, 0:n1 - n0], lhsT=hT[:, kc, t * 128:(t + 1) * 128], rhs=wt[:, kc, n0:n1], start=(kc == 0), stop=(kc == 7)),
                          ['p1d_w'], ['ps:p1d_p%d_%d' % (s_, j)], attach='p1d_w')
            for j in range(3):
                eng = 'act' if j != 1 else 'dve'
                if eng == 'act':
                    kb.op('act', lambda e: e.copy(out=ot[s_][:, j * 512:(j + 1) * 512], in_=pt[s_][j][:]), ['ps:p1d_p%d_%d' % (s_, j)], ['p1d_o%d_%d' % (s_, j)])
                else:
                    kb.op('dve', lambda e: e.tensor_copy(out=ot[s_][:, j * 512:(j + 1) * 512], in_=pt[s_][j][:]), ['ps:p1d_p%d_%d' % (s_, j)], ['p1d_o%d_%d' % (s_, j)])
            kb.op('dve', lambda e: e.tensor_copy(out=og[s_][:], in_=pt[s_][3][:, 0:8]), ['ps:p1d_p%d_3' % s_], ['p1d_g%d' % s_])
            kb.dma('sp', tmb_s[t * 128:(t + 1) * 128, :], ot[s_][:], ['p1d_o%d_%d' % (s_, j) for j in range(3)], [('tmb', t)], 'p1d_st%d' % s_)
            kb.dma('sp', gate_s[:, t, :], og[s_][:], ['p1d_g%d' % s_], [('gate', t)], 'p1d_sg%d' % s_)
    es.close()
    T.barrier()
    if stop_after == '1':
        return finish(kb, es, es_all, out_d)

    with ExitStack() as es2:
        lamb = sbuf(es2, "p2_lamb", [128, 256], F32)
        lj = sbuf(es2, "p2_lj", [128, 64], F32)
        ls = sbuf(es2, "p2_ls", [128, 2], F32)
        nlam = sbuf(es2, "p2_nlam", [128, 1], F32)
        sg = sbuf(es2, "p2_sg", [128, 128], F32)
        maskf = sbuf(es2, "p2_maskf", [128, 128], F32)
        maskb = sbuf(es2, "p2_maskb", [128, 128], BF16)
        kb.dma('sp', lamb[:], lam_d.partition_broadcast(128), [], ['lamb'], 'p2_c0')
        kb.dma('sp', sg[:], subln_d.partition_broadcast(128), [], ['sg'], 'p2_c1')
        kb.dma('sp', maskf[:], maskb_d[:, :], [], ['maskf'], 'p2_c2')
        kb.op('dve', lambda e: e.tensor_copy(out=maskb[:], in_=maskf[:]), ['maskf'], ['maskb'])
        for i in range(2):
            kb.op('dve', lambda e: e.scalar_tensor_tensor(out=lj[:], in0=lamb[:, i * 128:i * 128 + 64], scalar=1.0, in1=lamb[:, i * 128 + 64:i * 128 + 128],
                                                          op0=ALU.mult, op1=ALU.mult, accum_out=ls[:, i:i + 1]), ['lamb'], ['lj', 'ls'])
        kb.op('act', lambda e: e.activation(out=ls[:], in_=ls[:], func=AF.Exp), ['ls'], ['ls'])
        kb.op('dve', lambda e: e.tensor_tensor(out=nlam[:], in0=ls[:, 1:2], in1=ls[:, 0:1], op=ALU.subtract), ['ls'], ['nlam'])
        kb.op('dve', lambda e: e.tensor_scalar(out=nlam[:], in0=nlam[:], scalar1=-LAM_INIT, scalar2=None, op0=ALU.add), ['nlam'], ['nlam'])
        kb.op('dve', lambda e: e.tensor_scalar(out=sg[:], in0=sg[:], scalar1=1.0 - LAM_INIT, scalar2=None, op0=ALU.mult), ['sg'], ['sg'])

        kTh = [sbuf(es2, "p2_kT%d" % i, [128, S], BF16) for i in range(2)]
        Vh = [sbuf(es2, "p2_V%d" % i, [128, NT, 129], BF16) for i in range(2)]
        qTb = [sbuf(es2, "p2_q%d" % i, [128, 512], BF16) for i in range(2)]
        PT = [sbuf(es2, "p2_PT%d" % i, [128, 512], BF16) for i in range(3)]
        ps_s = [psum(es2, "p2_ps%d" % i, [128, 512], F32) for i in range(2)]
        accA_b = [psum(es2, "p2_accA%d" % i, [128, 512], F32) for i in range(2)]
        accB_b = [psum(es2, "p2_accB%d" % i, [128, 512], F32) for i in range(2)]
        accA = [b_[:, 0:387].rearrange("p (q c) -> p q c", c=129) for b_ in accA_b]
        accB = [b_[:, 0:129].rearrange("p (q c) -> p q c", c=129) for b_ in accB_b]
        rz = sbuf(es2, "p2_rz", [128, 2, 4], F32)
        o0 = [sbuf(es2, "p2_o0_%d" % i, [128, 4, 128], F32) for i in range(2)]
        oo = [sbuf(es2, "p2_oo_%d" % i, [128, 4, 128], F32) for i in range(2)]
        junk = sbuf(es2, "p2_junk", [128, 128], F32)
        ss = sbuf(es2, "p2_ss", [128, 4], F32)
        yo = [sbuf(es2, "p2_yo%d" % i, [128, 4, 128], BF16) for i in range(2)]
        for i in range(2):
            kb.op('pool', lambda e: e.memset(Vh[i][:, :, 128:129], 1.0), [], ['V%dones' % i])
        sc_it = 0
        hq = 0
        for h in range(0 if skip12 else 4):
            hs = h % 2
            kb.dma('sp', kTh[hs][:], kT_s[h, :, :], [], ['kT%d' % hs], 'p2_kl%d' % hs)
            kb.dma('sp', Vh[hs][:, :, 0:128], tmb_s[:, h * 128:(h + 1) * 128].rearrange("(t p) c -> p t c", p=128), [], ['V%d' % hs], 'p2_vl%d' % hs)
            for qb in range(NB):
                qs_ = hq % 2
                kb.dma('sp', qTb[qs_][:], qT_s[h, :, qb * 512:(qb + 1) * 512], [], ['q%d' % qs_], 'p2_ql%d' % qs_)
                nkt = 4 * qb + 4
                for m in range(2):
                    a_i = (hq * 2 + m) % 2
                    aA, aB = accA[a_i], accB[a_i]
                    an = 'ps:acc%d' % a_i
                    rows = slice(m * 64, (m + 1) * 64)
                    state = {'startedA': False}

                    def emit_scores(kt, sc):
                        j = kt - 4 * qb
                        c0 = max(j, 0) * 128
                        p_s, pn = ps_s[sc % 2], 'ps:s%d' % (sc % 2)
                        P_, Pn = PT[sc % 3], 'PT%d' % (sc % 3)
                        kb.op('pe', lambda e: e.matmul(p_s[:, c0:512], lhsT=kTh[hs][rows, kt * 128:(kt + 1) * 128], rhs=qTb[qs_][rows, c0:512],
                                                        start=True, stop=(j < 0)), ['kT%d' % hs, 'q%d' % qs_], [pn])
                        if j >= 0:
                            kb.op('pe', lambda e: e.matmul(p_s[:, c0:c0 + 128], lhsT=ident_b[:], rhs=maskb[:], start=False, stop=True),
                                  ['ident_b', 'maskb'], [pn])
                        kb.op('act', lambda e: e.activation(out=P_[:, c0:512], in_=p_s[:, c0:512], func=AF.Exp, scale=0.125), [pn], [Pn])

                    def emit_pv(kt, sc):
                        j = kt - 4 * qb
                        P_, Pn = PT[sc % 3], 'PT%d' % (sc % 3)
                        for qs in range(max(j, 0), 4):
                            last = (kt == 4 * qb + qs)
                            if qs < 3:
                                o_ap = aA[:, qs, :]
                                st = not state['startedA']
                                state['startedA'] = True
                            else:
                                o_ap = aB[:, 0, :]
                                st = (kt == 0)
                            kb.op('pe', lambda e: e.matmul(o_ap, lhsT=P_[:, qs * 128:(qs + 1) * 128], rhs=Vh[hs][:, kt, :], start=st, stop=last, skip_group_check=True),
                                  [Pn, 'V%d' % hs, 'V%dones' % hs], [an])

                    prev = None
                    for kt in range(nkt):
                        emit_scores(kt, sc_it)
                        if prev is not None:
                            emit_pv(*prev)
                        prev = (kt, sc_it)
                        sc_it += 1
                    emit_pv(*prev)
                    pq_ = hq % 2
                    kb.op('dve', lambda e: e.reciprocal(out=rz[:, m, 0:3], in_=aA[:, :, 128]), [an], ['rz%d' % m])
                    kb.op('dve', lambda e: e.reciprocal(out=rz[:, m, 3:4], in_=aB[:, :, 128]), [an], ['rz%d' % m])
                    if m == 0:
                        for qs in range(4):
                            src = aA[:, qs, 0:128] if qs < 3 else aB[:, 0, 0:128]
                            kb.op('dve', lambda e: e.tensor_scalar(out=o0[pq_][:, qs, :], in0=src, scalar1=rz[:, 0, qs:qs + 1], scalar2=None, op0=ALU.mult),
                                  [an, 'rz0'], ['o0_%d' % pq_])
                    else:
                        kb.op('dve', lambda e: e.tensor_scalar(out=rz[:, 1, :], in0=rz[:, 1, :], scalar1=nlam[:, 0:1], scalar2=None, op0=ALU.mult),
                              ['rz1', 'nlam'], ['rz1'])
                        for qs in range(4):
                            src = aA[:, qs, 0:128] if qs < 3 else aB[:, 0, 0:128]
                            kb.op('dve', lambda e: e.scalar_tensor_tensor(out=oo[pq_][:, qs, :], in0=src, scalar=rz[:, 1, qs:qs + 1], in1=o0[pq_][:, qs, :],
                                                                          op0=ALU.mult, op1=ALU.add), [an, 'rz1', 'o0_%d' % pq_], ['oo_%d' % pq_])
                pq_ = hq % 2
                for qs in range(4):
                    kb.op('dve', lambda e: e.scalar_tensor_tensor(out=junk[:], in0=oo[pq_][:, qs, :], scalar=1.0, in1=oo[pq_][:, qs, :], op0=ALU.mult, op1=ALU.mult,
                                                                  accum_out=ss[:, qs:qs + 1]), ['oo_%d' % pq_], ['junk', 'ss'])
                kb.op('act', lambda e: e.activation(out=ss[:], in_=ss[:], func=AF.Ln, scale=1.0 / 128, bias=EPS), ['ss'], ['ss'])
                kb.op('act', lambda e: e.activation(out=ss[:], in_=ss[:], func=AF.Exp, scale=-0.5), ['ss'], ['ss'])
                for qs in range(4):
                    kb.op('dve', lambda e: e.scalar_tensor_tensor(out=yo[pq_][:, qs, :], in0=oo[pq_][:, qs, :], scalar=ss[:, qs:qs + 1], in1=sg[:], op0=ALU.mult, op1=ALU.mult),
                          ['oo_%d' % pq_, 'ss', 'sg'], ['yo%d' % pq_])
                kb.dma('sp', y_s[qb * 512:(qb + 1) * 512, h * 128:(h + 1) * 128].rearrange("(qs p) c -> p qs c", p=128), yo[pq_][:],
                       ['yo%d' % pq_], [('yda', h, qb)], 'p2_st%d' % pq_)
                hq += 1
    T.barrier()
    if stop_after == '2':
        return finish(kb, es, es_all, out_d)

    with ExitStack() as es3:
        DH = 128
        KS = float(DH) ** -0.5
        tri = sbuf(es3, "p3_tri", [128, 128], F32)
        maskf4 = sbuf(es3, "p3_maskf4", [128, 4, 128], F32)
        ones_f = sbuf(es3, "p3_ones", [128, 128], F32)
        gb = sbuf(es3, "p3_gb", [128, 8], F32)
        mlg = sbuf(es3, "p3_mlg", [128, 512], F32)
        G = sbuf(es3, "p3_G", [128, NT, 8], F32)
        ig = sbuf(es3, "p3_ig", [128, NT, 4], F32)
        lf = sbuf(es3, "p3_lf", [128, NT, 4], F32)
        bcol = sbuf(es3, "p3_bcol", [128, NT, 4], F32)
        ea = sbuf(es3, "p3_ea", [128, NT, 4], F32)
        wcol = sbuf(es3, "p3_wcol", [128, NT, 4], F32)
        eaL = sbuf(es3, "p3_eaL", [128, NT, 4], F32)
        mqT = sbuf(es3, "p3_mqT", [128, 4, S], BF16)
        mkT = sbuf(es3, "p3_mkT", [128, 4, S], BF16)
        MV = sbuf(es3, "p3_MV", [128, NT, 4, 129], BF16)
        kb.dma('sp', tri[:], tri_d[:, :], [], ['tri'], 'p3_c0')
        for hh in range(4):
            kb.dma('sp', maskf4[:, hh, :], maskb_d[:, :], [], ['maskf4'], 'p3_c1')
        kb.dma('sp', gb[:], gateb_d.partition_broadcast(128), [], ['gb'], 'p3_c2')
        kb.dma('sp', mlg[:], mlg_d.partition_broadcast(128), [], ['mlg'], 'p3_c3')
        kb.dma('sp', G[:], gate_s, [], ['G'], 'p3_c4')
        for hh in range(4):
            kb.dma('sp', mqT[:, hh, :], mqT_s[hh, :, :], [], ['mqT'], 'p3_c5')
            kb.dma('sp', mkT[:, hh, :], mkT_s[hh, :, :], [], ['mkT'], 'p3_c6')
        kb.op('pool', lambda e: e.memset(MV[:, :, :, 128:129], 1.0), [], ['MVones'])
        for hh in range(4):
            kb.dma('sp', MV[:, :, hh, 0:128], tmb_s[:, 512 + hh * 128:512 + (hh + 1) * 128].rearrange("(t p) c -> p t c", p=128), [], ['MV'], 'p3_c7')
        kb.op('pool', lambda e: e.memset(ones_f[:], 1.0), [], ['ones_f'])
        if stop_after == '3a':
            T.barrier()
            return finish(kb, es, es_all, out_d)
        kb.op('dve', lambda e: e.tensor_tensor(out=ig[:], in0=G[:, :, 0:4], in1=gb[:, 0:4].unsqueeze(1).to_broadcast([128, NT, 4]), op=ALU.add), ['G', 'gb'], ['ig'])
        kb.op('dve', lambda e: e.tensor_tensor(out=lf[:], in0=G[:, :, 4:8], in1=gb[:, 4:8].unsqueeze(1).to_broadcast([128, NT, 4]), op=ALU.add), ['G', 'gb'], ['lf'])
        kb.op('act', lambda e: e.activation(out=lf[:], in_=lf[:], func=AF.Exp, scale=-1.0), ['lf'], ['lf'])
        kb.op('act', lambda e: e.activation(out=lf[:], in_=lf[:], func=AF.Ln, bias=1.0), ['lf'], ['lf'])
        kb.op('dve', lambda e: e.tensor_scalar(out=lf[:], in0=lf[:], scalar1=-1.0, scalar2=None, op0=ALU.mult), ['lf'], ['lf'])
        if stop_after == '3b':
            T.barrier()
            return finish(kb, es, es_all, out_d)
        pg_b = psum(es3, "p3_pg", [128, 512], F32)
        pg = pg_b[:, 0:256].rearrange("p (a b) -> p a b", a=2)
        tri_b = sbuf(es3, "p3_tri_b", [128, 128], BF16)
        ones_b = sbuf(es3, "p3_ones_b", [128, 128], BF16)
        maskb4 = sbuf(es3, "p3_maskb4", [128, 4, 128], BF16)
        lf_hi = sbuf(es3, "p3_lf_hi", [128, NT, 4], BF16)
        lf_lo = sbuf(es3, "p3_lf_lo", [128, NT, 4], BF16)
        kb.op('dve', lambda e: e.tensor_copy(out=tri_b[:], in_=tri[:]), ['tri'], ['tri_b'])
        kb.op('dve', lambda e: e.tensor_copy(out=ones_b[:], in_=ones_f[:]), ['ones_f'], ['ones_b'])
        kb.op('dve', lambda e: e.tensor_copy(out=maskb4[:], in_=maskf4[:]), ['maskf4'], ['maskb4'])
        kb.op('dve', lambda e: e.tensor_copy(out=lf_hi[:], in_=lf[:]), ['lf'], ['lf_hi'])
        kb.op('dve', lambda e: e.tensor_tensor(out=lf_lo[:], in0=lf[:], in1=lf_hi[:], op=ALU.subtract), ['lf', 'lf_hi'], ['lf_lo'])
        if stop_after == '3b1':
            T.barrier()
            return finish(kb, es, es_all, out_d)
        lfh2 = lf_hi[:].rearrange("p t h -> p (t h)")
        lfl2 = lf_lo[:].rearrange("p t h -> p (t h)")
        kb.op('pe', lambda e: e.matmul(pg[:, 0, :], lhsT=tri_b[:], rhs=lfh2, start=True, stop=False), ['tri_b', 'lf_hi'], ['ps:pg'])
        kb.op('pe', lambda e: e.matmul(pg[:, 0, :], lhsT=tri_b[:], rhs=lfl2, start=False, stop=True), ['tri_b', 'lf_lo'], ['ps:pg'])
        kb.op('pe', lambda e: e.matmul(pg[:, 1, :], lhsT=ones_b[:], rhs=lfh2, start=True, stop=False), ['ones_b', 'lf_hi'], ['ps:pg'])
        kb.op('pe', lambda e: e.matmul(pg[:, 1, :], lhsT=ones_b[:], rhs=lfl2, start=False, stop=True), ['ones_b', 'lf_lo'], ['ps:pg'])
        if stop_after == '3b2':
            T.barrier()
            return finish(kb, es, es_all, out_d)
        f2 = lambda t_: t_[:].rearrange("p t h -> p (t h)")
        kb.op('dve', lambda e: e.scalar_tensor_tensor(out=f2(bcol), in0=pg[:, 0, :], scalar=-1.0, in1=f2(ig), op0=ALU.mult, op1=ALU.add), ['ig', 'ps:pg'], ['bcol'])
        kb.op('act', lambda e: e.activation(out=f2(ea), in_=pg[:, 0, :], func=AF.Exp), ['ps:pg'], ['ea'])
        if stop_after == '3b3':
            T.barrier()
            return finish(kb, es, es_all, out_d)
        kb.op('dve', lambda e: e.tensor_tensor(out=f2(wcol), in0=pg[:, 1, :], in1=f2(bcol), op=ALU.add), ['bcol', 'ps:pg'], ['wcol'])
        kb.op('act', lambda e: e.activation(out=f2(wcol), in_=f2(wcol), func=AF.Exp), ['wcol'], ['wcol'])
        kb.op('act', lambda e: e.activation(out=f2(eaL), in_=pg[:, 1, :], func=AF.Exp), ['ps:pg'], ['eaL'])

        if stop_after == '3c':
            T.barrier()
            return finish(kb, es, es_all, out_d)
        pX = [psum(es3, "p3_pX%d" % i, [128, 4, 128], F32) for i in range(2)]
        pP = [psum(es3, "p3_pP%d" % i, [128, 512], F32) for i in range(2)]
        pU_b = psum(es3, "p3_pU", [128, 512], F32)
        pU = pU_b[:, 0:258].rearrange("p (a b) -> p a b", a=2)
        pTk_b = psum(es3, "p3_pTk", [128, 1024], BF16)
        pTk = pTk_b[:, 0:256].rearrange("p (a b) -> p a b", a=2)
        Rc = [sbuf(es3, "p3_Rc%d" % i, [128, 4, 128], BF16) for i in range(2)]
        Rl = [sbuf(es3, "p3_Rl%d" % i, [128, 4, 128], BF16) for i in range(2)]
        DT = [sbuf(es3, "p3_DT%d" % i, [128, 4, 128], F32) for i in range(2)]
        SD = [sbuf(es3, "p3_SD%d" % i, [128, 128], BF16) for i in range(2)]
        KW = [sbuf(es3, "p3_KW%d" % i, [128, 128], BF16) for i in range(2)]
        Cf = [sbuf(es3, "p3_Cf%d" % i, [128, 129], F32) for i in range(4)]
        Cb = [sbuf(es3, "p3_Cb%d" % i, [128, 129], BF16) for i in range(4)]
        intra_sb = [sbuf(es3, "p3_intra%d" % i, [128, 129], F32) for i in range(2)]
        num = [sbuf(es3, "p3_num%d" % i, [128, 4, 129], F32) for i in range(2)]
        rdn = sbuf(es3, "p3_rdn", [128, 4], F32)
        hsc = [sbuf(es3, "p3_hsc%d" % i, [128, 4, 128], F32) for i in range(2)]
        stats = sbuf(es3, "p3_stats", [128, 4, 6], F32)
        mvv = sbuf(es3, "p3_mvv", [128, 4, 2], F32)
        rstd3 = sbuf(es3, "p3_rstd", [128, 4], F32)
        mo_t = [sbuf(es3, "p3_mo%d" % i, [128, 512], BF16) for i in range(2)]
        gg = [sbuf(es3, "p3_gg%d" % i, [128, 512], F32) for i in range(2)]
        yo3 = [sbuf(es3, "p3_yo%d" % i, [128, 512], BF16) for i in range(2)]
        for hh in range(4):
            kb.op('pool', lambda e: e.memset(Cf[hh][:], 0.0), [], ['Cf%d' % hh])
            kb.op('pool', lambda e: e.memset(Cb[hh][:], 0.0), [], ['Cb%d' % hh])
        u_it = 0
        for c in range(p3_tiles):
            cs = c % 2
            tsl = slice(c * 128, (c + 1) * 128)
            kb.dma('sp', mo_t[cs][:], tmb_s[tsl, 1024:1536], [], ['mo%d' % cs], 'p3_mol%d' % cs)
            kb.op('act', lambda e: e.activation(out=gg[cs][:], in_=mo_t[cs][:], func=AF.Exp, scale=-1.0), ['mo%d' % cs], ['gg%d' % cs])
            kb.op('pool', lambda e: e.tensor_scalar(out=gg[cs][:], in0=gg[cs][:], scalar1=1.0, scalar2=None, op0=ALU.add), ['gg%d' % cs], ['gg%d' % cs])
            kb.op('dve', lambda e: e.reciprocal(out=gg[cs][:], in_=gg[cs][:]), ['gg%d' % cs], ['gg%d' % cs])
            kb.op('pool', lambda e: e.tensor_tensor(out=gg[cs][:], in0=gg[cs][:], in1=mlg[:], op=ALU.mult), ['gg%d' % cs, 'mlg'], ['gg%d' % cs])
            kb.op('dve', lambda e: e.tensor_tensor(out=Rc[cs][:], in0=tri_b[:].unsqueeze(1).to_broadcast([128, 4, 128]),
                                                    in1=lf_hi[:, c, :].unsqueeze(2).to_broadcast([128, 4, 128]), op=ALU.mult), ['tri_b', 'lf_hi'], ['Rc%d' % cs])
            kb.op('dve', lambda e: e.tensor_tensor(out=Rl[cs][:], in0=tri_b[:].unsqueeze(1).to_broadcast([128, 4, 128]),
                                                    in1=lf_lo[:, c, :].unsqueeze(2).to_broadcast([128, 4, 128]), op=ALU.mult), ['tri_b', 'lf_lo'], ['Rl%d' % cs])
            pXf = pX[cs][:].rearrange("p h j -> p (h j)")
            kb.op('pe', lambda e: e.matmul(pXf, lhsT=ones_b[:], rhs=Rc[cs][:].rearrange("p h j -> p (h j)"), start=True, stop=False),
                  ['ones_b', 'Rc%d' % cs], ['ps:pX%d' % cs])
            kb.op('pe', lambda e: e.matmul(pXf, lhsT=ones_b[:], rhs=Rl[cs][:].rearrange("p h j -> p (h j)"), start=False, stop=False),
                  ['ones_b', 'Rl%d' % cs], ['ps:pX%d' % cs])
            kb.op('pe', lambda e: e.matmul(pXf, lhsT=ident_b[:], rhs=maskb4[:].rearrange("p h j -> p (h j)"), start=False, stop=True),
                  ['ident_b', 'maskb4'], ['ps:pX%d' % cs])
            for hh in range(4):
                kb.op('act', lambda e: e.activation(out=DT[cs][:, hh, :], in_=pX[cs][:, hh, :], func=AF.Exp, bias=bcol[:, c, hh:hh + 1]),
                      ['ps:pX%d' % cs, 'bcol'], ['DT%d_%d' % (cs, hh)])
            for hh in range(4):
                us = u_it % 2
                P_, Pn = pP[us], 'ps:pP%d' % us
                q_t = mqT[:, hh, tsl]
                k_t = mkT[:, hh, tsl]
                kb.op('pe', lambda e: e.matmul(P_[:, 0:128], lhsT=k_t, rhs=q_t, start=True, stop=True), ['mkT', 'mqT'], [Pn])
                kb.op('dve', lambda e: e.scalar_tensor_tensor(out=SD[us][:], in0=P_[:, 0:128], scalar=KS, in1=DT[cs][:, hh, :], op0=ALU.mult, op1=ALU.mult),
                      [Pn, 'DT%d_%d' % (cs, hh)], ['SD%d' % us])
                kb.op('pe', lambda e: e.matmul(P_[:, 128:257], lhsT=SD[us][:], rhs=MV[:, c, hh, :], start=True, stop=True), ['SD%d' % us, 'MV', 'MVones'], [Pn])
                kb.op('pe', lambda e: e.matmul(P_[:, 257:386], lhsT=q_t, rhs=Cb[hh][:], start=True, stop=True), ['mqT', 'Cb%d' % hh], [Pn], attach='Cb%d' % hh)
                kb.op('act', lambda e: e.copy(out=intra_sb[us][:], in_=P_[:, 128:257]), [Pn], ['intra%d' % us])
                kb.op('dve', lambda e: e.scalar_tensor_tensor(out=num[cs][:, hh, :], in0=P_[:, 257:386], scalar=ea[:, c, hh:hh + 1], in1=intra_sb[us][:], op0=ALU.mult, op1=ALU.add),
                      [Pn, 'ea', 'intra%d' % us], ['num%d_%d' % (cs, hh)])
                kb.op('pe', lambda e: e.transpose(out=pTk[:, us, :], in_=k_t, identity=ident_b[:]), ['mkT', 'ident_b'], ['ps:pTk'])
                kb.op('dve', lambda e: e.tensor_scalar(out=KW[us][:], in0=pTk[:, us, :], scalar1=wcol[:, c, hh:hh + 1], scalar2=KS, op0=ALU.mult, op1=ALU.mult),
                      ['ps:pTk', 'wcol'], ['KW%d' % us])
                kb.op('pe', lambda e: e.matmul(pU[:, us, :], lhsT=KW[us][:], rhs=MV[:, c, hh, :], start=True, stop=True), ['KW%d' % us, 'MV', 'MVones'], ['ps:pU'])
                kb.op('pool', lambda e: e.tensor_scalar(out=Cf[hh][:], in0=Cf[hh][:], scalar1=eaL[:, c, hh:hh + 1], scalar2=None, op0=ALU.mult),
                      ['Cf%d' % hh, 'eaL'], ['Cf%d' % hh])
                kb.op('dve', lambda e: e.tensor_tensor(out=Cf[hh][:], in0=pU[:, us, :], in1=Cf[hh][:], op=ALU.add),
                      ['Cf%d' % hh, 'ps:pU'], ['Cf%d' % hh])
                kb.op('act', lambda e: e.copy(out=Cb[hh][:], in_=Cf[hh][:]), ['Cf%d' % hh], ['Cb%d' % hh])
                u_it += 1
            nn = ['num%d_%d' % (cs, hh) for hh in range(4)]
            kb.op('dve', lambda e: e.tensor_tensor(out=rdn[:], in0=num[cs][:, :, 128], in1=num[cs][:, :, 128], op=ALU.mult), nn, ['rdn'])
            kb.op('dve', lambda e: e.tensor_scalar(out=rdn[:], in0=rdn[:], scalar1=1.0, scalar2=None, op0=ALU.max), ['rdn'], ['rdn'])
            kb.op('act', lambda e: e.activation(out=rdn[:], in_=rdn[:], func=AF.Ln), ['rdn'], ['rdn'])
            kb.op('act', lambda e: e.activation(out=rdn[:], in_=rdn[:], func=AF.Exp, scale=-0.5), ['rdn'], ['rdn'])
            kb.op('dve', lambda e: e.tensor_tensor(out=hsc[cs][:], in0=num[cs][:, :, 0:128], in1=rdn[:].unsqueeze(2).to_broadcast([128, 4, 128]), op=ALU.mult),
                  nn + ['rdn'], ['hsc%d' % cs])
            for hh in range(4):
                kb.op('dve', lambda e: e.bn_stats(out=stats[:, hh, :], in_=hsc[cs][:, hh, :]), ['hsc%d' % cs], ['stats'])
                kb.op('dve', lambda e: e.bn_aggr(out=mvv[:, hh, :], in_=stats[:, hh, :]), ['stats'], ['mvv'])
            kb.op('act', lambda e: e.activation(out=rstd3[:], in_=mvv[:, :, 1], func=AF.Ln, bias=EPS), ['mvv'], ['rstd3'])
            kb.op('act', lambda e: e.activation(out=rstd3[:], in_=rstd3[:], func=AF.Exp, scale=-0.5), ['rstd3'], ['rstd3'])
            for hh in range(4):
                kb.op('dve', lambda e: e.tensor_scalar(out=hsc[cs][:, hh, :], in0=hsc[cs][:, hh, :], scalar1=mvv[:, hh, 0:1], scalar2=rstd3[:, hh:hh + 1], op0=ALU.subtract, op1=ALU.mult),
                      ['hsc%d' % cs, 'mvv', 'rstd3'], ['hsc%d' % cs])
            kb.op('dve', lambda e: e.tensor_tensor(out=yo3[cs][:], in0=hsc[cs][:].rearrange("p h d -> p (h d)"), in1=gg[cs][:], op=ALU.mult),
                  ['hsc%d' % cs, 'gg%d' % cs], ['yo3_%d' % cs])
            kb.dma('sp', y_s[tsl, 512:1024], yo3[cs][:], ['yo3_%d' % cs], [('yml', c)], 'p3_st%d' % cs)
    T.barrier()
    if stop_after == '3':
        return finish(kb, es, es_all, out_d)

    es4 = ExitStack()
    h2T = sbuf(es4, "h2T", [128, 8, S], BF16)
    with ExitStack() as es4a:
        wo = sbuf(es4a, "p4_wo", [128, 8, D], BF16)
        for kc in range(8):
            kb.dma('pool', wo[:, kc, :], wout_d[kc * 128:(kc + 1) * 128, :], [], ['p4_wo'], 'p4_wl')
        yt = [sbuf(es4a, "p4_yt%d" % i, [128, D], BF16) for i in range(2)]
        xt4 = [sbuf(es4a, "p4_xt%d" % i, [128, D], F32) for i in range(2)]
        yT = [sbuf(es4a, "p4_yT%d" % i, [128, 8, 128], BF16) for i in range(2)]
        x1t = [sbuf(es4a, "p4_x1_%d" % i, [128, D], F32) for i in range(2)]
        pTy = [psum(es4a, "p4_pT%d" % i, [128, 8, 128], BF16) for i in range(2)]
        po4 = [[psum(es4a, "p4_po%d_%d" % (i, j), [128, 512], F32) for j in range(2)] for i in range(2)]
        for t in range(NT):
            b = t % 2
            tsl = slice(t * 128, (t + 1) * 128)
            kb.dma('sp', yt[b][:], y_s[tsl, :], [], ['p4_yt%d' % b], 'p4_yl%d' % b)
            kb.dma('sp', xt4[b][:], x_d[tsl, :], [], ['p4_xt%d' % b], 'p4_xl%d' % b)
            for c in range(8):
                kb.op('pe', lambda e: e.transpose(out=pTy[b][:, c, :], in_=yt[b][:, c * 128:(c + 1) * 128], identity=ident_b[:]),
                      ['p4_yt%d' % b, 'ident_b'], ['ps:p4_pT%d' % b])
            kb.op('act', lambda e: e.copy(out=yT[b][:], in_=pTy[b][:]), ['ps:p4_pT%d' % b], ['p4_yT%d' % b])
            for hf in range(2):
                for kc in range(8):
                    kb.op('pe', lambda e: e.matmul(po4[b][hf][:], lhsT=yT[b][:, kc, :], rhs=wo[:, kc, hf * 512:(hf + 1) * 512], start=(kc == 0), stop=(kc == 7)),
                          ['p4_yT%d' % b, 'p4_wo'], ['ps:p4_po%d_%d' % (b, hf)])
                kb.op('dve', lambda e: e.tensor_tensor(out=x1t[b][:, hf * 512:(hf + 1) * 512], in0=po4[b][hf][:], in1=xt4[b][:, hf * 512:(hf + 1) * 512], op=ALU.add),
                      ['ps:p4_po%d_%d' % (b, hf), 'p4_xt%d' % b], ['p4_x1_%d_%d' % (b, hf)])
            kb.dma('sp', x1_s[tsl, :], x1t[b][:], ['p4_x1_%d_0' % b, 'p4_x1_%d_1' % b], [('x1', t)], 'p4_st%d' % b)
    T.barrier()
    with ExitStack() as es4b:
        pTt = [psum(es4b, "p4b_pT%d" % i, [128, 8, 128], BF16) for i in range(2)]
        norm_transpose(es4b, "p4b", lambda t: x1_s[t * 128:(t + 1) * 128, :], 1, h2T, NT, pTt, dst_tag='h2T_t')
    T.barrier()
    if stop_after == '4':
        return finish(kb, es, es_all, out_d)

    es5 = ExitStack()
    memT = sbuf(es5, "memT", [128, 8, MEM], BF16)
    with ExitStack() as es5a:
        pTt = [psum(es5a, "p5a_pT%d" % i, [128, 8, 128], BF16) for i in range(2)]
        norm_transpose(es5a, "p5a", lambda t: mem_d[t * 128:(t + 1) * 128, :], 2, memT, 2, pTt, dst_tag='memT_t')
    T.barrier()
    with ExitStack() as es5b:
        wx = {}
        for nm, wd in (('q', wxq_d), ('k', wxk_d), ('v', wxv_d), ('o', wxo_d)):
            wx[nm] = sbuf(es5b, "p5_w" + nm, [128, 8, D], BF16)
            for kc in range(8):
                kb.dma('pool', wx[nm][:, kc, :], wd[kc * 128:(kc + 1) * 128, :], [], ['p5_w' + nm], 'p5_wl' + nm)
        KT = sbuf(es5b, "p5_KT", [128, 8, MEM], BF16)
        Vx = sbuf(es5b, "p5_Vx", [128, 2, D], BF16)
        pA = [psum(es5b, "p5_pA%d" % i, [128, 512], F32) for i in range(2)]
        pS = [psum(es5b, "p5_pS%d" % i, [128, 512], F32) for i in range(2)]
        pZ = psum(es5b, "p5_pZ", [128, 512], F32)
        pO = [psum(es5b, "p5_pO%d" % i, [128, 512], F32) for i in range(2)]
        ia = 0
        for c in range(8):
            p_, pn = pA[ia % 2], 'ps:p5_pA%d' % (ia % 2)
            for kc in range(8):
                kb.op('pe', lambda e: e.matmul(p_[:, 0:MEM], lhsT=wx['k'][:, kc, c * 128:(c + 1) * 128], rhs=memT[:, kc, :], start=(kc == 0), stop=(kc == 7)),
                      ['p5_wk'], [pn])
            kb.op('act', lambda e: e.copy(out=KT[:, c, :], in_=p_[:, 0:MEM]), [pn], ['p5_KT'])
            ia += 1
        for kt in range(2):
            for hf in range(2):
                p_, pn = pA[ia % 2], 'ps:p5_pA%d' % (ia % 2)
                for kc in range(8):
                    kb.op('pe', lambda e: e.matmul(p_[:], lhsT=memT[:, kc, kt * 128:(kt + 1) * 128], rhs=wx['v'][:, kc, hf * 512:(hf + 1) * 512], start=(kc == 0), stop=(kc == 7)),
                          ['p5_wv'], [pn], attach='p5_wv')
                kb.op('act', lambda e: e.copy(out=Vx[:, kt, hf * 512:(hf + 1) * 512], in_=p_[:]), [pn], ['p5_Vx'])
                ia += 1
        qTx = [sbuf(es5b, "p5_qT%d" % i, [128, 8, 512], BF16) for i in range(2)]
        PTx = [sbuf(es5b, "p5_PT%d" % i, [128, 2, 512], BF16) for i in range(2)]
        rZ = [sbuf(es5b, "p5_rZ%d" % i, [128, 512], F32) for i in range(2)]
        oTx = [sbuf(es5b, "p5_oT%d" % i, [128, 8, 512], BF16) for i in range(2)]
        x1t5 = [sbuf(es5b, "p5_x1_%d" % i, [128, D], F32) for i in range(2)]
        x2t5 = [sbuf(es5b, "p5_x2_%d" % i, [128, D], F32) for i in range(2)]
        hi_ = 0
        ti_ = 0
        for blk in range(NB):
            bs = blk % 2
            bsl = slice(blk * 512, (blk + 1) * 512)
            for c in range(8):
                p_, pn = pA[ia % 2], 'ps:p5_pA%d' % (ia % 2)
                for kc in range(8):
                    kb.op('pe', lambda e: e.matmul(p_[:], lhsT=wx['q'][:, kc, c * 128:(c + 1) * 128], rhs=h2T[:, kc, bsl], start=(kc == 0), stop=(kc == 7)),
                          ['p5_wq'], [pn])
                kb.op('act', lambda e: e.copy(out=qTx[bs][:, c, :], in_=p_[:]), [pn], ['p5_qT%d_%d' % (bs, c)])
                ia += 1
            for h in range(4):
                hs = hi_ % 2
                for kt in range(2):
                    p_, pn = pS[kt], 'ps:p5_pS%d' % kt
                    for dc in range(2):
                        kb.op('pe', lambda e: e.matmul(p_[:], lhsT=KT[:, 2 * h + dc, kt * 128:(kt + 1) * 128], rhs=qTx[bs][:, 2 * h + dc, :], start=(dc == 0), stop=(dc == 1)),
                              ['p5_KT', 'p5_qT%d_%d' % (bs, 2 * h + dc)], [pn])
                    kb.op('act', lambda e: e.activation(out=PTx[hs][:, kt, :], in_=p_[:], func=AF.Exp, scale=1.0 / 16.0), [pn], ['p5_PT%d_%d' % (hs, kt)])
                for kt in range(2):
                    kb.op('pe', lambda e: e.matmul(pZ[:], lhsT=ones_bb[:], rhs=PTx[hs][:, kt, :], start=(kt == 0), stop=(kt == 1)),
                          ['ones_bb', 'p5_PT%d_%d' % (hs, kt)], ['ps:p5_pZ'])
                kb.op('dve', lambda e: e.reciprocal(out=rZ[hs][:], in_=pZ[:]), ['ps:p5_pZ'], ['p5_rZ%d' % hs])
                for dc in range(2):
                    p_, pn = pO[dc], 'ps:p5_pO%d' % dc
                    for kt in range(2):
                        kb.op('pe', lambda e: e.matmul(p_[:], lhsT=Vx[:, kt, h * 256 + dc * 128:h * 256 + (dc + 1) * 128], rhs=PTx[hs][:, kt, :], start=(kt == 0), stop=(kt == 1)),
                              ['p5_Vx', 'p5_PT%d_%d' % (hs, kt)], [pn])
                    kb.op('dve', lambda e: e.tensor_tensor(out=oTx[bs][:, 2 * h + dc, :], in0=p_[:], in1=rZ[hs][:], op=ALU.mult),
                          [pn, 'p5_rZ%d' % hs], ['p5_oT%d_%d' % (bs, 2 * h + dc)])
                hi_ += 1
            for sub in range(4):
                t = blk * 4 + sub
                ts_ = ti_ % 2
                tsl = slice(t * 128, (t + 1) * 128)
                kb.dma('sp', x1t5[ts_][:], x1_s[tsl, :], [], ['p5_x1_%d' % ts_], 'p5_xl%d' % ts_)
                for hf in range(2):
                    p_, pn = pA[ia % 2], 'ps:p5_pA%d' % (ia % 2)
                    for kc in range(8):
                        kb.op('pe', lambda e: e.matmul(p_[:], lhsT=oTx[bs][:, kc, sub * 128:(sub + 1) * 128], rhs=wx['o'][:, kc, hf * 512:(hf + 1) * 512], start=(kc == 0), stop=(kc == 7)),
                              ['p5_oT%d_%d' % (bs, kc), 'p5_wo'], [pn])
                    kb.op('dve', lambda e: e.tensor_tensor(out=x2t5[ts_][:, hf * 512:(hf + 1) * 512], in0=p_[:], in1=x1t5[ts_][:, hf * 512:(hf + 1) * 512], op=ALU.add),
                          [pn, 'p5_x1_%d' % ts_], ['p5_x2_%d_%d' % (ts_, hf)])
                    ia += 1
                kb.dma('sp', x2_s[tsl, :], x2t5[ts_][:], ['p5_x2_%d_0' % ts_, 'p5_x2_%d_1' % ts_], [('x2', t)], 'p5_st%d' % ts_)
                ti_ += 1
    es5.close()
    es4.close()
    T.barrier()
    if stop_after == '5':
        return finish(kb, es, es_all, out_d)

    with ExitStack() as es5c:
        gbc = sbuf(es5c, "p5c_gbc", [128, D], F32)
        bbc = sbuf(es5c, "p5c_bbc", [128, 36], F32)
        eoff = sbuf(es5c, "p5c_eoff", [128, NEXP], F32)
        trisf = sbuf(es5c, "p5c_trisf", [128, 128], F32)
        trisb = sbuf(es5c, "p5c_trisb", [128, 128], BF16)
        wr = sbuf(es5c, "p5c_wr", [128, 8, 36], BF16)
        tokid = sbuf(es5c, "p5c_tokid", [128, NT], I32)
        macc = sbuf(es5c, "p5c_macc", [128, NEXP], BF16)
        kb.dma('sp', gbc[:], gffn_d.partition_broadcast(128), [], ['gbc'], 'p5c_c0')
        kb.dma('sp', bbc[:], br_d.partition_broadcast(128), [], ['bbc'], 'p5c_c1')
        kb.dma('sp', eoff[:], eoff_d.partition_broadcast(128), [], ['eoff'], 'p5c_c2')
        kb.dma('sp', trisf[:], tris_d[:, :], [], ['trisf'], 'p5c_c3')
        kb.dma('sp', tokid[:], tokid_d[:, :], [], ['tokid'], 'p5c_c4')
        for kc in range(8):
            kb.dma('pool', wr[:, kc, :], wr_d[kc * 128:(kc + 1) * 128, :], [], ['wr'], 'p5c_c5')
        kb.op('dve', lambda e: e.tensor_copy(out=trisb[:], in_=trisf[:]), ['trisf'], ['trisb'])
        kb.op('pool', lambda e: e.memset(macc[:], 0.0), [], ['macc'])
        x2t = [sbuf(es5c, "p5c_x2_%d" % i, [128, D], F32) for i in range(2)]
        junkc = sbuf(es5c, "p5c_junk", [128, D], BF16)
        ssq = [sbuf(es5c, "p5c_ssq%d" % i, [128, 1], F32) for i in range(2)]
        h3 = [sbuf(es5c, "p5c_h3_%d" % i, [128, D], BF16) for i in range(2)]
        h3T = [sbuf(es5c, "p5c_h3T%d" % i, [128, 8, 128], BF16) for i in range(2)]
        pT5 = [psum(es5c, "p5c_pT%d" % i, [128, 8, 128], BF16) for i in range(2)]
        pL = [psum(es5c, "p5c_pL%d" % i, [128, 512], F32) for i in range(2)]
        pPos = [psum(es5c, "p5c_pPos%d" % i, [128, 512], F32) for i in range(2)]
        lg = sbuf(es5c, "p5c_lg", [128, 36], F32)
        sm = sbuf(es5c, "p5c_sm", [128, 16], F32)
        ge = sbuf(es5c, "p5c_ge", [128, 4], F32)
        oh = sbuf(es5c, "p5c_oh", [128, 4], F32)
        lm = sbuf(es5c, "p5c_lm", [128, 4, 8], F32)
        m8 = sbuf(es5c, "p5c_m8", [128, 8], F32)
        m1 = sbuf(es5c, "p5c_m1", [128, NEXP], F32)
        m2 = sbuf(es5c, "p5c_m2", [128, NEXP], F32)
        maskb5 = [sbuf(es5c, "p5c_mask%d" % i, [128, NEXP], BF16) for i in range(2)]
        sl = sbuf(es5c, "p5c_sl", [128, NEXP], F32)
        junk32 = sbuf(es5c, "p5c_junk32", [128, NEXP], F32)
        sf = sbuf(es5c, "p5c_sf", [128, 2], F32)
        lmf = lm[:].rearrange("p g e -> p (g e)")
        for t in range(NT):
            b = t % 2
            tsl = slice(t * 128, (t + 1) * 128)
            kb.dma('sp', x2t[b][:], x2_s[tsl, :], [], ['x2t%d' % b], 'p5c_xl%d' % b)
            kb.op('act', lambda e: e.activation(out=junkc[:], in_=x2t[b][:], func=AF.Square, accum_out=ssq[b][:]), ['x2t%d' % b], ['junkc', 'ssq%d' % b])
            kb.op('act', lambda e: e.activation(out=ssq[b][:], in_=ssq[b][:], func=AF.Ln, scale=1.0 / D, bias=EPS), ['ssq%d' % b], ['ssq%d' % b])
            kb.op('act', lambda e: e.activation(out=ssq[b][:], in_=ssq[b][:], func=AF.Exp, scale=-0.5), ['ssq%d' % b], ['ssq%d' % b])
            kb.op('dve', lambda e: e.scalar_tensor_tensor(out=h3[b][:], in0=x2t[b][:], scalar=ssq[b][:, 0:1], in1=gbc[:], op0=ALU.mult, op1=ALU.mult),
                  ['x2t%d' % b, 'ssq%d' % b, 'gbc'], ['h3_%d' % b])
            for c in range(8):
                kb.op('pe', lambda e: e.transpose(out=pT5[b][:, c, :], in_=h3[b][:, c * 128:(c + 1) * 128], identity=ident_b[:]), ['h3_%d' % b, 'ident_b'], ['ps:p5c_pT%d' % b])
            kb.op('act', lambda e: e.copy(out=h3T[b][:], in_=pT5[b][:]), ['ps:p5c_pT%d' % b], ['h3T%d' % b])
            for kc in range(8):
                kb.op('pe', lambda e: e.matmul(pL[b][:, 0:36], lhsT=h3T[b][:, kc, :], rhs=wr[:, kc, :], start=(kc == 0), stop=(kc == 7)), ['h3T%d' % b, 'wr'], ['ps:p5c_pL%d' % b])
            kb.op('dve', lambda e: e.tensor_tensor(out=lg[:], in0=pL[b][:, 0:36], in1=bbc[:], op=ALU.add), ['ps:p5c_pL%d' % b, 'bbc'], ['lg'])
            kb.op('dve', lambda e: e.reduce_max(out=sm[:, 0:1], in_=lg[:, 0:4], axis=AX.X), ['lg'], ['sm0'])
            kb.op('dve', lambda e: e.tensor_scalar(out=sm[:, 1:2], in0=sm[:, 0:1], scalar1=-1.0, scalar2=None, op0=ALU.mult), ['sm0'], ['sm1'])
            kb.op('act', lambda e: e.activation(out=ge[:], in_=lg[:, 0:4], func=AF.Exp, bias=sm[:, 1:2], accum_out=sm[:, 2:3]), ['lg', 'sm1'], ['ge', 'sm2'])
            kb.op('dve', lambda e: e.reciprocal(out=sm[:, 3:4], in_=sm[:, 2:3]), ['sm2'], ['sm3'])
            kb.op('dve', lambda e: e.tensor_scalar(out=oh[:], in0=lg[:, 0:4], scalar1=sm[:, 0:1], scalar2=None, op0=ALU.is_equal), ['lg', 'sm0'], ['oh'])
            kb.op('dve', lambda e: e.tensor_scalar(out=oh[:], in0=oh[:], scalar1=-1.0, scalar2=1.0e9, op0=ALU.add, op1=ALU.mult), ['oh'], ['oh'])
            kb.op('dve', lambda e: e.tensor_tensor(out=lm[:], in0=lg[:, 4:36].rearrange("p (g e) -> p g e", g=4), in1=oh[:].unsqueeze(2).to_broadcast([128, 4, 8]), op=ALU.add),
                  ['lg', 'oh'], ['lm'])
            kb.op('dve', lambda e: e.max(out=m8[:], in_=lmf), ['lm'], ['m8'])
            kb.op('dve', lambda e: e.tensor_scalar(out=sm[:, 4:5], in0=m8[:, 0:1], scalar1=-1.0, scalar2=None, op0=ALU.mult), ['m8'], ['sm4'])
            kb.op('act', lambda e: e.activation(out=sm[:, 5:6], in_=m8[:, 1:2], func=AF.Exp, bias=sm[:, 4:5]), ['m8', 'sm4'], ['sm5'])
            kb.op('dve', lambda e: e.tensor_scalar(out=sm[:, 6:7], in0=sm[:, 5:6], scalar1=1.0, scalar2=None, op0=ALU.add), ['sm5'], ['sm6'])
            kb.op('dve', lambda e: e.reciprocal(out=sm[:, 7:8], in_=sm[:, 6:7]), ['sm6'], ['sm7'])
            kb.op('dve', lambda e: e.tensor_tensor(out=comb_w[:, t, 0:1], in0=sm[:, 7:8], in1=sm[:, 3:4], op=ALU.mult), ['sm7', 'sm3'], [('cw', t)])
            kb.op('dve', lambda e: e.tensor_tensor(out=comb_w[:, t, 1:2], in0=comb_w[:, t, 0:1], in1=sm[:, 5:6], op=ALU.mult), [('cw', t), 'sm5'], [('cw2', t)])
            kb.op('dve', lambda e: e.tensor_scalar(out=m1[:], in0=lmf, scalar1=m8[:, 0:1], scalar2=None, op0=ALU.is_equal), ['lm', 'm8'], ['m1'])
            kb.op('dve', lambda e: e.tensor_scalar(out=m2[:], in0=lmf, scalar1=m8[:, 1:2], scalar2=None, op0=ALU.is_equal), ['lm', 'm8'], ['m2'])
            kb.op('dve', lambda e: e.tensor_tensor(out=maskb5[b][:], in0=m1[:], in1=m2[:], op=ALU.add), ['m1', 'm2'], ['mask%d' % b])
            kb.op('pe', lambda e: e.matmul(pPos[b][:, 0:NEXP], lhsT=trisb[:], rhs=maskb5[b][:], start=True, stop=False), ['trisb', 'mask%d' % b], ['ps:p5c_pPos%d' % b])
            kb.op('pe', lambda e: e.matmul(pPos[b][:, 0:NEXP], lhsT=ones_bb[:], rhs=macc[:], start=False, stop=True), ['ones_bb', 'macc'], ['ps:p5c_pPos%d' % b], attach='macc')
            kb.op('dve', lambda e: e.tensor_tensor(out=macc[:], in0=macc[:], in1=maskb5[b][:], op=ALU.add), ['macc', 'mask%d' % b], ['macc'])
            kb.op('dve', lambda e: e.scalar_tensor_tensor(out=sl[:], in0=pPos[b][:, 0:NEXP], scalar=float(CAP - 1), in1=eoff[:], op0=ALU.min, op1=ALU.add),
                  ['ps:p5c_pPos%d' % b, 'eoff'], ['sl'])
            kb.op('dve', lambda e: e.scalar_tensor_tensor(out=junk32[:], in0=sl[:], scalar=1.0, in1=m1[:], op0=ALU.mult, op1=ALU.mult, accum_out=sf[:, 0:1]), ['sl', 'm1'], ['junk32', 'sf0'])
            kb.op('dve', lambda e: e.scalar_tensor_tensor(out=junk32[:], in0=sl[:], scalar=1.0, in1=m2[:], op0=ALU.mult, op1=ALU.mult, accum_out=sf[:, 1:2]), ['sl', 'm2'], ['junk32', 'sf1'])
            kb.op('dve', lambda e: e.tensor_copy(out=slot_i[:, t, :], in_=sf[:]), ['sf0', 'sf1'], [('slot', t)])
            for k2 in range(2):
                T.op('pool', lambda e: e.indirect_dma_start(out=Xs_d[:, :], out_offset=bass.IndirectOffsetOnAxis(ap=slot_i[:, t, k2:k2 + 1], axis=0),
                                                            in_=h3[b][:], in_offset=None),
                     reads=['h3_%d' % b, ('slot', t)], writes=[('Xs', t, k2)], lane='p5c_sc%d_%d' % (b, k2))
    T.barrier()
    if stop_after == '5b':
        return finish(kb, es, es_all, out_d)

    with ExitStack() as es6:
        w1b = [sbuf(es6, "p6_w1_%d" % i, [128, 8, DEXP], BF16) for i in range(2)]
        w3b = [sbuf(es6, "p6_w3_%d" % i, [128, 8, DEXP], BF16) for i in range(2)]
        w2b = [sbuf(es6, "p6_w2_%d" % i, [128, 4, D], BF16) for i in range(2)]
        xb = [sbuf(es6, "p6_xb%d" % i, [128, D], BF16) for i in range(3)]
        CHN = CHB * 128
        XT = [sbuf(es6, "p6_XT%d" % i, [128, 8, CHN], BF16) for i in range(2)]
        s1 = [sbuf(es6, "p6_s1_%d" % i, [128, CHN], BF16) for i in range(2)]
        gT6 = [sbuf(es6, "p6_gT%d" % i, [128, 4, CHN], BF16) for i in range(2)]
        ysb = [sbuf(es6, "p6_y%d" % i, [128, D], BF16) for i in range(2)]
        pT6 = [psum(es6, "p6_pT%d" % i, [128, 8, 128], BF16) for i in range(2)]
        p1 = [psum(es6, "p6_p1_%d" % i, [128, 512], F32) for i in range(2)]
        p3 = [psum(es6, "p6_p3_%d" % i, [128, 512], F32) for i in range(2)]
        py = [psum(es6, "p6_py%d" % i, [128, 512], F32) for i in range(2)]
        xi = 0
        ci = 0
        mi = 0
        yi = 0
        for ex in range(NEXP):
            ws = ex % 2
            kb.dma('pool', w1b[ws][:], w1_d[ex].rearrange("(c p) n -> p c n", p=128), [], ['w1_%d' % ws], 'p6_w1l%d' % ws)
            kb.dma('pool', w3b[ws][:], w3_d[ex].rearrange("(c p) n -> p c n", p=128), [], ['w3_%d' % ws], 'p6_w3l%d' % ws)
            kb.dma('pool', w2b[ws][:], w2_d[ex].rearrange("(c p) n -> p c n", p=128), [], ['w2_%d' % ws], 'p6_w2l%d' % ws)
            for hb in range(CAPB // CHB):
                cs = ci % 2
                row0 = ex * CAP + hb * CHN
                for j in range(CHB):
                    xs_ = xi % 3
                    kb.dma('sp', xb[xs_][:], Xs_d[row0 + j * 128:row0 + (j + 1) * 128, :], [], ['xb%d' % xs_], 'p6_xl%d' % xs_)
                    pt_ = xi % 2
                    for c in range(8):
                        kb.op('pe', lambda e: e.transpose(out=pT6[pt_][:, c, :], in_=xb[xs_][:, c * 128:(c + 1) * 128], identity=ident_b[:]), ['xb%d' % xs_, 'ident_b'], ['ps:p6_pT%d' % pt_])
                    if xi % 2 == 0:
                        kb.op('act', lambda e: e.copy(out=XT[cs][:, :, j * 128:(j + 1) * 128], in_=pT6[pt_][:]), ['ps:p6_pT%d' % pt_], ['XT%d_%d' % (cs, j)])
                    else:
                        kb.op('dve', lambda e: e.tensor_copy(out=XT[cs][:, :, j * 128:(j + 1) * 128], in_=pT6[pt_][:]), ['ps:p6_pT%d' % pt_], ['XT%d_%d' % (cs, j)])
                    xi += 1
                xtn = ['XT%d_%d' % (cs, j) for j in range(CHB)]
                for m in range(4):
                    ms = mi % 2
                    for kc in range(8):
                        kb.op('pe', lambda e: e.matmul(p1[ms][:, 0:CHN], lhsT=w1b[ws][:, kc, m * 128:(m + 1) * 128], rhs=XT[cs][:, kc, :], start=(kc == 0), stop=(kc == 7)),
                              ['w1_%d' % ws] + xtn, ['ps:p6_p1_%d' % ms])
                    for kc in range(8):
                        kb.op('pe', lambda e: e.matmul(p3[ms][:, 0:CHN], lhsT=w3b[ws][:, kc, m * 128:(m + 1) * 128], rhs=XT[cs][:, kc, :], start=(kc == 0), stop=(kc == 7)),
                              ['w3_%d' % ws] + xtn, ['ps:p6_p3_%d' % ms])
                    kb.op('act', lambda e: e.activation(out=s1[ms][:], in_=p1[ms][:, 0:CHN], func=AF.Silu), ['ps:p6_p1_%d' % ms], ['s1_%d' % ms])
                    kb.op('dve', lambda e: e.tensor_tensor(out=gT6[cs][:, m, :], in0=p3[ms][:, 0:CHN], in1=s1[ms][:], op=ALU.mult), ['ps:p6_p3_%d' % ms, 's1_%d' % ms], ['gT%d_%d' % (cs, m)])
                    mi += 1
                gtn = ['gT%d_%d' % (cs, m) for m in range(4)]
                for j in range(CHB):
                    ys_ = yi % 2
                    for hf in range(2):
                        for kc in range(4):
                            kb.op('pe', lambda e: e.matmul(py[hf][:], lhsT=gT6[cs][:, kc, j * 128:(j + 1) * 128], rhs=w2b[ws][:, kc, hf * 512:(hf + 1) * 512], start=(kc == 0), stop=(kc == 3)),
                                  gtn + ['w2_%d' % ws], ['ps:p6_py%d' % hf])
                        if hf == 0:
                            kb.op('act', lambda e: e.copy(out=ysb[ys_][:, 0:512], in_=py[0][:]), ['ps:p6_py0'], ['ysb%d_0' % ys_])
                        else:
                            kb.op('dve', lambda e: e.tensor_copy(out=ysb[ys_][:, 512:1024], in_=py[1][:]), ['ps:p6_py1'], ['ysb%d_1' % ys_])
                    kb.dma('sp', Y_d[row0 + j * 128:row0 + (j + 1) * 128, :], ysb[ys_][:], ['ysb%d_0' % ys_, 'ysb%d_1' % ys_], [('Y', ex, hb, j)], 'p6_st%d' % ys_)
                    yi += 1
                ci += 1
    T.barrier()
    if stop_after == '6':
        return finish(kb, es, es_all, out_d)

    with ExitStack() as es7:
        gfb = sbuf(es7, "p7_gfb", [128, D], F32)
        kb.dma('sp', gfb[:], gfin_d.partition_broadcast(128), [], ['gfb'], 'p7_c0')
        x2t7 = [sbuf(es7, "p7_x2_%d" % i, [128, D], F32) for i in range(2)]
        y1 = [sbuf(es7, "p7_y1_%d" % i, [128, D], BF16) for i in range(2)]
        y2 = [sbuf(es7, "p7_y2_%d" % i, [128, D], BF16) for i in range(2)]
        x3 = [sbuf(es7, "p7_x3_%d" % i, [128, D], F32) for i in range(2)]
        junk7 = sbuf(es7, "p7_junk", [128, D], BF16)
        ssq7 = [sbuf(es7, "p7_ssq%d" % i, [128, 1], F32) for i in range(2)]
        o7 = [sbuf(es7, "p7_o%d" % i, [128, D], F32) for i in range(2)]
        for t in range(NT):
            b = t % 2
            tsl = slice(t * 128, (t + 1) * 128)
            kb.dma('sp', x2t7[b][:], x2_s[tsl, :], [], ['x2t%d' % b], 'p7_xl%d' % b)
            for k2, yy in ((0, y1), (1, y2)):
                T.op('pool', lambda e: e.indirect_dma_start(out=yy[b][:], out_offset=None, in_=Y_d[:, :],
                                                            in_offset=bass.IndirectOffsetOnAxis(ap=slot_i[:, t, k2:k2 + 1], axis=0)),
                     reads=[], writes=['y%d_%d' % (k2, b)], lane='p7_g%d_%d' % (k2, b))
            kb.op('dve', lambda e: e.scalar_tensor_tensor(out=x3[b][:], in0=y1[b][:], scalar=comb_w[:, t, 0:1], in1=x2t7[b][:], op0=ALU.mult, op1=ALU.add),
                  ['y0_%d' % b, 'x2t%d' % b], ['x3_%d' % b])
            kb.op('dve', lambda e: e.scalar_tensor_tensor(out=x3[b][:], in0=y2[b][:], scalar=comb_w[:, t, 1:2], in1=x3[b][:], op0=ALU.mult, op1=ALU.add),
                  ['y1_%d' % b, 'x3_%d' % b], ['x3_%d' % b])
            kb.op('act', lambda e: e.activation(out=junk7[:], in_=x3[b][:], func=AF.Square, accum_out=ssq7[b][:]), ['x3_%d' % b], ['junk7', 'ssq7_%d' % b])
            kb.op('act', lambda e: e.activation(out=ssq7[b][:], in_=ssq7[b][:], func=AF.Ln, scale=1.0 / D, bias=EPS), ['ssq7_%d' % b], ['ssq7_%d' % b])
            kb.op('act', lambda e: e.activation(out=ssq7[b][:], in_=ssq7[b][:], func=AF.Exp, scale=-0.5), ['ssq7_%d' % b], ['ssq7_%d' % b])
            kb.op('dve', lambda e: e.scalar_tensor_tensor(out=o7[b][:], in0=x3[b][:], scalar=ssq7[b][:, 0:1], in1=gfb[:], op0=ALU.mult, op1=ALU.mult),
                  ['x3_%d' % b, 'ssq7_%d' % b, 'gfb'], ['o7_%d' % b])
            kb.dma('sp', out_d[tsl, :], o7[b][:], ['o7_%d' % b], [('out', t)], 'p7_st%d' % b)
    return finish(kb, es, es_all, out_d)


def finish(kb, es, es_all, out_d):
    kb.T.barrier()
    return kb


def prep_inputs(inputs, b):
    f = lambda k: np.ascontiguousarray(np.asarray(inputs[k], dtype=np.float32))
    c = host_consts()
    m = {}
    m['x'] = f('x')[b]
    m['mem'] = f('mem')[b]
    perm = win_perm()
    m['w_in_ext'] = np.ascontiguousarray(f('w_in')[0][:, perm])
    m['w_out'] = f('w_out')[0]

    def g128(v):
        return v.reshape(8, 128).T
    m['gT'] = np.ascontiguousarray(np.concatenate([g128(f('norm_mix_g')[0]), g128(f('norm_x_g')[0]),
                                                   g128(f('norm_mem_g')[0]), g128(f('norm_ffn_g')[0])], axis=1))
    m['g_final'] = f('norm_final_g')
    m['g_ffn_row'] = f('norm_ffn_g')[0]
    kk = np.arange(128)[:, None]
    qq = np.arange(128)[None, :]
    m['tri_strict'] = (kk < qq).astype(np.float32)
    m['eoff'] = (np.arange(NEXP) * CAP).astype(np.float32)
    m['tokid'] = (np.arange(NT)[None, :] * 128 + np.arange(128)[:, None]).astype(np.int32)
    m['cosT'] = c['cosT']; m['sinT'] = c['sinT']; m['ident_f'] = c['ident_f']; m['maskb'] = c['maskb']; m['tri_incl'] = c['tri_incl']
    m['da_lambda'] = f('da_lambda')[0].reshape(256)
    m['da_subln_g'] = f('da_subln_g')[0]
    cw = f('ml_conv_w')[0][:, 0, :]
    cb = f('ml_conv_b')[0]
    convT = np.zeros((128, 40), np.float32)
    for gidx in range(8):
        cols = slice(gidx * 128, (gidx + 1) * 128)
        convT[:, gidx * 5:gidx * 5 + 4] = cw[:, cols].T
        convT[:, gidx * 5 + 4] = cb[cols]
    m['convT'] = convT
    m['ml_gate_b'] = f('ml_gate_b')[0].reshape(8)
    m['ml_norm_g'] = f('ml_norm_g')[0]
    for k in ('w_xq', 'w_xk', 'w_xv', 'w_xo'):
        m[k] = f(k)[0]
    m['w_router'] = np.ascontiguousarray(np.concatenate([f('w_router_group')[0], f('w_router_expert')[0]], axis=1))
    m['b_router'] = np.concatenate([f('b_router_group')[0], f('b_router_expert')[0]])
    m['w1'] = f('w1')[0]; m['w3'] = f('w3')[0]; m['w2'] = f('w2')[0]
    return m


_CACHE = {}


def kernel(**inputs):
    if 'kb' not in _CACHE:
        _CACHE['kb'] = build()
    kb = _CACHE['kb']
    n = 8
    maps = []
    for b in range(n):
        m = prep_inputs(inputs, b)
        maps.append({k: v for k, v in m.items() if k in kb.inp})
    res = run_bass_kernel_spmd(kb.nc, maps, core_ids=list(range(n)))
    return np.stack([np.asarray(r["out"], dtype=np.float32) for r in res.results], axis=0)
```

```python
import numpy as np
from contextlib import ExitStack
import concourse.bass as bass
import concourse.mybir as mybir
from concourse.bass_utils import run_bass_kernel_spmd

F32 = mybir.dt.float32
BF16 = mybir.dt.bfloat16
I32 = mybir.dt.int32
AF = mybir.ActivationFunctionType
ALU = mybir.AluOpType
AX = mybir.AxisListType

S = 4096
D = 1024
NT = S // 128
NB = S // 512
EPS = 1e-6
MEM = 256
NEXP = 32
DEXP = 512
CAPB = 6
CHB = 3
CAP = CAPB * 128
LAM_INIT = 0.2
NEG = -30000.0
STRICT = False

FM_COLS = 24 * 128
TM_COLS = 512 * 3 + 8
WIN_COLS = FM_COLS + TM_COLS


class Trk:
    def __init__(self, nc, needed=None):
        self.nc = nc
        self.needed_in = needed
        self.needed = {}
        self.phys = {}
        self.pcnt = {}
        self.eng = {'pe': nc.tensor, 'act': nc.scalar, 'dve': nc.vector, 'pool': nc.gpsimd, 'sp': nc.sync}
        self.sem = {}
        self.cnt = {}
        self.seen = {e: {} for e in self.eng}
        self.lastw = {}
        self.reads = {}
        self.stack = ExitStack()
        self.phase = 0
        self.nsem = 0
        self.pool = {'sw': [], 'hw': []}
        self.kind = {}

    def lane(self, name, eng='sp'):
        if name not in self.sem:
            kind = 'sw' if eng == 'pool' else 'hw'
            self.kind[name] = kind
            if self.pool[kind] and not name.startswith('eng_'):
                sh, c = self.pool[kind].pop()
                self.sem[name] = sh
                self.cnt[name] = c
            else:
                self.nsem += 1
                s = self.stack.enter_context(self.nc.semaphore("s%d" % self.nsem))
                self.sem[name] = s
                self.cnt[name] = 0
        return name

    def elane(self, eng):
        return "eng_%s" % eng

    def pval(self, ln, v):
        if not ln.startswith("eng_"):
            return v
        self.needed.setdefault(ln, set()).add(v)
        if self.needed_in is None:
            return v
        return self.phys[ln][v]

    def wait(self, eng, ticket):
        ln, v = ticket
        if self.seen[eng].get(ln, 0) < v:
            self.eng[eng].wait_ge(self.sem[ln], self.pval(ln, v))
            self.seen[eng][ln] = v

    def op(self, eng, fn, reads=(), writes=(), lane=None, inc=None, attach=None):
        deps = {}
        own = self.elane(eng)
        psr = [r for r in reads if isinstance(r, str) and r.startswith('ps:')]
        if psr:
            reads = [r for r in reads if r not in psr]
            writes = list(writes) + [r for r in psr if r not in writes]
        for r in reads:
            t = self.lastw.get(r)
            if t is not None:
                deps[t[0]] = max(deps.get(t[0], 0), t[1])
        for w in writes:
            t = self.lastw.get(w)
            if t is not None and (STRICT or t[0] != own):
                deps[t[0]] = max(deps.get(t[0], 0), t[1])
            for t in self.reads.get(w, ()):
                if STRICT or t[0] != own:
                    deps[t[0]] = max(deps.get(t[0], 0), t[1])
        for ln, v in deps.items():
            self.wait(eng, (ln, v))
        ins = fn(self.eng[eng])
        if attach is not None:
            t = self.lastw.get(attach)
            if t is not None:
                ins._wait_ge(self.sem[t[0]], self.pval(t[0], t[1]))
        if lane is None:
            lane = own
            inc = 1
        elif inc is None:
            inc = 16
        self.lane(lane, eng)
        self.cnt[lane] += inc
        t = (lane, self.cnt[lane])
        if lane.startswith("eng_"):
            if self.needed_in is None or t[1] in self.needed_in.get(lane, ()):
                ins.then_inc(self.sem[lane], 1)
                self.pcnt[lane] = self.pcnt.get(lane, 0) + 1
                self.phys.setdefault(lane, {})[t[1]] = self.pcnt[lane]
        else:
            ins.then_inc(self.sem[lane], inc)
        for r in reads:
            self.reads.setdefault(r, []).append(t)
        for w in writes:
            self.lastw[w] = t
            self.reads[w] = []
        return t

    def barrier(self):
        for e in self.eng:
            for ln, v in self.cnt.items():
                if v > 0:
                    self.wait(e, (ln, v))
        self.lastw = {}
        self.reads = {}
        self.phase += 1
        for ln in list(self.sem.keys()):
            if not ln.startswith("eng_"):
                self.pool[self.kind.pop(ln)].append((self.sem.pop(ln), self.cnt.pop(ln)))
                for e in self.eng:
                    self.seen[e].pop(ln, None)


def host_consts():
    c = {}
    c['ident_f'] = np.eye(128, dtype=np.float32)
    k = np.arange(128)[:, None]
    q = np.arange(128)[None, :]
    c['maskb'] = np.where(k <= q, 0.0, NEG).astype(np.float32)
    c['tri_incl'] = (k <= q).astype(np.float32)
    inv_freq = (10000.0 ** (-np.arange(0, 64, 2, dtype=np.float32) / np.float32(64))).astype(np.float32)
    pos = np.arange(S, dtype=np.float32)
    ang = (pos[:, None] * inv_freq[None, :]).astype(np.float32)
    cs = np.cos(ang).astype(np.float32).T
    sn = np.sin(ang).astype(np.float32).T
    cosT = np.zeros((128, S), np.float32)
    sinT = np.zeros((128, S), np.float32)
    for p in range(128):
        d = p % 64
        cosT[p] = cs[d % 32]
        sinT[p] = -sn[d % 32] if d < 32 else sn[d % 32]
    c['cosT'] = cosT
    c['sinT'] = sinT
    return c


def win_perm():
    idx = []
    off_q, off_k, off_v = 0, 512, 1024
    off_mq, off_mk, off_mv, off_mo, off_mi, off_mf = 1536, 2048, 2560, 3072, 3584, 3588

    def sw(base):
        out = []
        for m in range(2):
            b = base + m * 64
            out += list(range(b + 32, b + 64)) + list(range(b, b + 32))
        return out
    for h in range(4):
        idx += list(range(off_q + h * 128, off_q + (h + 1) * 128))
        idx += sw(off_q + h * 128)
        idx += list(range(off_k + h * 128, off_k + (h + 1) * 128))
        idx += sw(off_k + h * 128)
    idx += list(range(off_mq, off_mq + 512))
    idx += list(range(off_mk, off_mk + 512))
    idx += list(range(off_v, off_v + 512))
    idx += list(range(off_mv, off_mv + 512))
    idx += list(range(off_mo, off_mo + 512))
    idx += list(range(off_mi, off_mi + 4)) + list(range(off_mf, off_mf + 4))
    assert len(idx) == WIN_COLS
    return np.array(idx)


class K:
    def __init__(self, debug=None, needed=None):
        self.debug = debug or ()
        nc = bass.Bass("TRN2", target_bir_lowering=False)
        self.nc = nc
        self.T = Trk(nc, needed)
        self.inp = {}
        self.scr = {}
        self.dmaq = 0

    def din(self, name, shape, dt=F32):
        kb = self

        class Lazy:
            def _get(s_):
                if name not in kb.inp:
                    kb.inp[name] = kb.nc.dram_tensor(name, list(shape), dt, kind="ExternalInput").ap()
                return kb.inp[name]

            def __getitem__(s_, k):
                return s_._get()[k]

            def __getattr__(s_, a):
                return getattr(s_._get(), a)
        return Lazy()

    def dscr(self, name, shape, dt):
        kind = "ExternalOutput" if name in self.debug else "Internal"
        self.scr[name] = self.nc.dram_tensor(name, list(shape), dt, kind=kind).ap()
        return self.scr[name]

    def dma(self, eng, out, in_, reads, writes, lane, **kw):
        return self.T.op(eng, lambda e: e.dma_start(out=out, in_=in_, **kw), reads=reads, writes=writes, lane=lane)

    def op(self, eng, fn, reads=(), writes=(), attach=None):
        if eng == 'pe' and attach is None and reads:
            attach = reads[0]
        return self.T.op(eng, fn, reads=reads, writes=writes, attach=attach)


def build(debug=None, stop_after=None, skip12=False, p3_tiles=NT):
    rec = _build(debug, stop_after, skip12, p3_tiles, None)
    return _build(debug, stop_after, skip12, p3_tiles, rec.T.needed)


def _build(debug, stop_after, skip12, p3_tiles, needed):
    kb = K(debug, needed)
    nc, T = kb.nc, kb.T
    din, dscr = kb.din, kb.dscr
    x_d = din("x", [S, D])
    mem_d = din("mem", [MEM, D])
    win_d = din("w_in_ext", [D, WIN_COLS])
    wout_d = din("w_out", [D, D])
    gT_d = din("gT", [128, 4 * 8])
    gfin_d = din("g_final", [D])
    gffn_d = din("g_ffn_row", [D])
    tris_d = din("tri_strict", [128, 128])
    eoff_d = din("eoff", [NEXP])
    tokid_d = din("tokid", [128, NT], I32)
    cos_d = din("cosT", [128, S])
    sin_d = din("sinT", [128, S])
    ident_d = din("ident_f", [128, 128])
    maskb_d = din("maskb", [128, 128])
    tri_d = din("tri_incl", [128, 128])
    lam_d = din("da_lambda", [256])
    subln_d = din("da_subln_g", [128])
    convw_d = din("convT", [128, 8 * 5])
    gateb_d = din("ml_gate_b", [8])
    mlg_d = din("ml_norm_g", [512])
    wxq_d = din("w_xq", [D, D]); wxk_d = din("w_xk", [D, D]); wxv_d = din("w_xv", [D, D]); wxo_d = din("w_xo", [D, D])
    wr_d = din("w_router", [D, 36])
    br_d = din("b_router", [36])
    w1_d = din("w1", [NEXP, D, DEXP]); w3_d = din("w3", [NEXP, D, DEXP]); w2_d = din("w2", [NEXP, DEXP, D])
    out_d = nc.dram_tensor("out", [S, D], F32, kind="ExternalOutput").ap()

    qT_s = dscr("qT_s", [4, 128, S], BF16)
    kT_s = dscr("kT_s", [4, 128, S], BF16)
    mqT_s = dscr("mqT_s", [4, 128, S], BF16)
    mkT_s = dscr("mkT_s", [4, 128, S], BF16)
    tmb_s = dscr("tmb_s", [S, 1536], BF16)
    gate_s = dscr("gate_s", [128, NT, 8], F32)
    y_s = dscr("y_s", [S, D], BF16)
    x1_s = dscr("x1_s", [S, D], F32)
    x2_s = dscr("x2_s", [S, D], F32)
    Xs_d = dscr("Xs_d", [NEXP * CAP, D], BF16)
    Y_d = dscr("Y_d", [NEXP * CAP, D], BF16)

    es_all = ExitStack()

    def sbuf(es, name, shape, dt):
        return es.enter_context(nc.sbuf_tensor("sb_" + name, list(shape), dt))

    def psum(es, name, shape, dt):
        return es.enter_context(nc.psum_tensor("ps_" + name, list(shape), dt))

    ident_f = sbuf(es_all, "ident_f", [128, 128], F32)
    ident_b = sbuf(es_all, "ident_b", [128, 128], BF16)
    gT = sbuf(es_all, "gT", [128, 32], F32)
    kb.dma('sp', ident_f[:], ident_d[:, :], [], ['ident_f'], 'c0')
    kb.dma('sp', gT[:], gT_d[:, :], [], ['gT'], 'c1')
    kb.dma('pool', ident_b[:], ident_d[:, :], [], ['ident_b'], 'c2')
    slot_i = sbuf(es_all, "slot_i", [128, NT, 2], I32)
    comb_w = sbuf(es_all, "comb_w", [128, NT, 2], F32)
    ones_bb = sbuf(es_all, "ones_bb", [128, 128], BF16)
    kb.op('pool', lambda e: e.memset(ones_bb[:], 1.0), [], ['ones_bb'])
    es = ExitStack()
    if True:
        zt = sbuf(es, "zt", [128, 8192], BF16)
        kb.op('pool', lambda e: e.memset(zt[:], 0.0), [], ['zt'])
        for e_ in range(NEXP):
            kb.dma('pool', Xs_d[e_ * CAP:(e_ + 1) * CAP, :].rearrange("(p r) c -> p (r c)", p=128), zt[:, 0:CAP * D // 128], ['zt'], [('Xs0', e_)], 'zX%d' % (e_ % 4))

    hT = sbuf(es, "hT", [128, 8, S], BF16)
    cosT = sbuf(es, "cosT", [128, S], F32)
    sinT = sbuf(es, "sinT", [128, S], F32)
    convT = sbuf(es, "convT", [128, 40], F32)
    kb.dma('sp', cosT[:], cos_d[:, :], [], ['cosT'], 'c3')
    kb.dma('sp', sinT[:], sin_d[:, :], [], ['sinT'], 'c4')
    kb.dma('sp', convT[:], convw_d[:, :], [], ['convT'], 'c5')

    def norm_transpose(es_, tagp, x_src_tiles, gcol, dstT, ntiles, pT_tiles, dst_tag='hT_t'):
        xt = [sbuf(es_, "%s_xt%d" % (tagp, i), [128, D], F32) for i in range(3)]
        junk = sbuf(es_, tagp + "_junk", [128, D], BF16)
        ssq = [sbuf(es_, "%s_ssq%d" % (tagp, i), [128, 1], F32) for i in range(2)]
        rstd = [sbuf(es_, "%s_rstd%d" % (tagp, i), [128, 1], F32) for i in range(2)]
        xs = [sbuf(es_, "%s_xs%d" % (tagp, i), [128, D], BF16) for i in range(2)]
        for t in range(ntiles):
            a, b = t % 3, t % 2
            kb.dma('sp', xt[a][:], x_src_tiles(t), [], [tagp + 'xt%d' % a], tagp + 'ld%d' % a)
            kb.op('act', lambda e: e.activation(out=junk[:], in_=xt[a][:], func=AF.Square, accum_out=ssq[b][:]),
                  [tagp + 'xt%d' % a], [tagp + 'junk', tagp + 'ssq%d' % b])
            kb.op('act', lambda e: e.activation(out=rstd[b][:], in_=ssq[b][:], func=AF.Sqrt, scale=1.0 / D, bias=EPS),
                  [tagp + 'ssq%d' % b], [tagp + 'rstd%d' % b])
            kb.op('dve', lambda e: e.reciprocal(out=rstd[b][:], in_=rstd[b][:]),
                  [tagp + 'rstd%d' % b], [tagp + 'rstd%d' % b])
            kb.op('dve', lambda e: e.tensor_scalar(out=xs[b][:], in0=xt[a][:], scalar1=rstd[b][:], scalar2=None, op0=ALU.mult),
                  [tagp + 'xt%d' % a, tagp + 'rstd%d' % b], [tagp + 'xs%d' % b])
            pT = pT_tiles[b]
            for c in range(8):
                kb.op('pe', lambda e: e.transpose(out=pT[:, c, :], in_=xs[b][:, c * 128:(c + 1) * 128], identity=ident_b[:]),
                      [tagp + 'xs%d' % b, 'ident_b'], ['ps:' + tagp + 'pT%d' % b])
            kb.op('dve', lambda e: e.tensor_tensor(out=dstT[:, :, t * 128:(t + 1) * 128], in0=pT[:],
                                                    in1=gT[:, gcol * 8:(gcol + 1) * 8].unsqueeze(2).to_broadcast([128, 8, 128]),
                                                    op=ALU.mult),
                  ['ps:' + tagp + 'pT%d' % b, 'gT'], [dst_tag + '%d' % t])

    NT1 = 0 if skip12 else NT
    NB1 = 0 if skip12 else NB
    with ExitStack() as es1a:
        pTt = [psum(es1a, "p1a_pT%d" % i, [128, 8, 128], BF16) for i in range(2)]
        norm_transpose(es1a, "p1a", lambda t: x_d[t * 128:(t + 1) * 128, :], 0, hT, NT1, pTt)
    T.barrier()

    with ExitStack() as es1b:
        wq = [sbuf(es1b, "p1b_w%d" % i, [128, 8, 256], BF16) for i in range(2)]
        pq = [psum(es1b, "p1b_pq%d" % i, [128, 512], F32) for i in range(4)]
        r1 = [sbuf(es1b, "p1b_r1_%d" % i, [128, 512], F32) for i in range(2)]
        r2 = [sbuf(es1b, "p1b_r2_%d" % i, [128, 512], F32) for i in range(2)]
        ro = [sbuf(es1b, "p1b_ro%d" % i, [128, 512], BF16) for i in range(2)]
        it = 0
        for pair in range(0 if skip12 else 8):
            h, isk = pair // 2, pair % 2
            wbuf = wq[pair % 2]
            c0 = pair * 256
            kb.dma('pool', wbuf[:], win_d[:, c0:c0 + 256].rearrange("(c p) n -> p c n", p=128), [], ['p1b_w%d' % (pair % 2)], 'p1b_wl%d' % (pair % 2))
            dst = (kT_s if isk else qT_s)
            for blk in range(NB):
                pa, pb = pq[(it % 2) * 2], pq[(it % 2) * 2 + 1]
                na, nb_ = 'ps:p1b_pq%d' % ((it % 2) * 2), 'ps:p1b_pq%d' % ((it % 2) * 2 + 1)
                for kc in range(8):
                    kb.op('pe', lambda e: e.matmul(pa[:], lhsT=wbuf[:, kc, 0:128], rhs=hT[:, kc, blk * 512:(blk + 1) * 512], start=(kc == 0), stop=(kc == 7)),
                          ['p1b_w%d' % (pair % 2)] + ['hT_t%d' % (blk * 4 + j) for j in range(4)], [na])
                for kc in range(8):
                    kb.op('pe', lambda e: e.matmul(pb[:], lhsT=wbuf[:, kc, 128:256], rhs=hT[:, kc, blk * 512:(blk + 1) * 512], start=(kc == 0), stop=(kc == 7)),
                          ['p1b_w%d' % (pair % 2)] + ['hT_t%d' % (blk * 4 + j) for j in range(4)], [nb_])
                s_ = it % 2
                kb.op('dve', lambda e: e.tensor_tensor(out=r1[s_][:], in0=pa[:], in1=cosT[:, blk * 512:(blk + 1) * 512], op=ALU.mult),
                      [na, 'cosT'], ['p1b_r1_%d' % s_])
                kb.op('dve', lambda e: e.tensor_tensor(out=r2[s_][:], in0=pb[:], in1=sinT[:, blk * 512:(blk + 1) * 512], op=ALU.mult),
                      [nb_, 'sinT'], ['p1b_r2_%d' % s_])
                kb.op('pool', lambda e: e.tensor_tensor(out=ro[s_][:], in0=r1[s_][:], in1=r2[s_][:], op=ALU.add),
                      ['p1b_r1_%d' % s_, 'p1b_r2_%d' % s_], ['p1b_ro%d' % s_])
                kb.dma('sp', dst[h, :, blk * 512:(blk + 1) * 512], ro[s_][:], ['p1b_ro%d' % s_], [('qk', pair, blk)], 'p1b_st%d' % s_)
                it += 1

    if stop_after == '1b':
        return finish(kb, es, es_all, out_d)
    T.barrier()
    with ExitStack() as es1c:
        wm = [sbuf(es1c, "p1c_w%d" % i, [128, 8, 128], BF16) for i in range(2)]
        pm = [psum(es1c, "p1c_pm%d" % i, [128, 512], F32) for i in range(2)]
        ub = [sbuf(es1c, "p1c_ub%d" % i, [128, 515], F32) for i in range(2)]
        ca = [sbuf(es1c, "p1c_ca%d" % i, [128, 512], F32) for i in range(2)]
        co = [sbuf(es1c, "p1c_co%d" % i, [128, 512], BF16) for i in range(2)]
        it = 0
        for g in range(0 if skip12 else 8):
            wbuf = wm[g % 2]
            wn = 'p1c_w%d' % (g % 2)
            c0 = 16 * 128 + g * 128
            kb.dma('pool', wbuf[:], win_d[:, c0:c0 + 128].rearrange("(c p) n -> p c n", p=128), [], [wn], 'p1c_wl%d' % (g % 2))
            dst = (mqT_s if g < 4 else mkT_s)
            h = g % 4
            cw = lambda i: convT[:, g * 5 + i:g * 5 + i + 1]
            for blk in range(NB):
                s_ = it % 2
                p_, pn = pm[s_], 'ps:p1c_pm%d' % s_
                u_, un = ub[s_], 'p1c_ub%d' % s_
                for kc in range(8):
                    kb.op('pe', lambda e: e.matmul(p_[:], lhsT=wbuf[:, kc, :], rhs=hT[:, kc, blk * 512:(blk + 1) * 512], start=(kc == 0), stop=(kc == 7)),
                          [wn], [pn])
                if blk == 0:
                    kb.op('pool', lambda e: e.memset(u_[:, 0:3], 0.0), [], [un + 'h'])
                else:
                    up = ub[1 - s_]
                    kb.op('act', lambda e: e.copy(out=u_[:, 0:3], in_=up[:, 512:515]), ['p1c_ub%d' % (1 - s_)], [un + 'h'])
                kb.op('act', lambda e: e.copy(out=u_[:, 3:515], in_=p_[:]), [pn], [un])
                a_, an = ca[s_], 'p1c_ca%d' % s_
                kb.op('dve', lambda e: e.tensor_scalar(out=a_[:], in0=u_[:, 0:512], scalar1=cw(0), scalar2=None, op0=ALU.mult),
                      [un, un + 'h', 'convT'], [an])
                for i in (1, 2, 3):
                    kb.op('dve', lambda e: e.scalar_tensor_tensor(out=a_[:], in0=u_[:, i:i + 512], scalar=cw(i), in1=a_[:], op0=ALU.mult, op1=ALU.add),
                          [un, un + 'h', an, 'convT'], [an])
                o_, on = co[s_], 'p1c_co%d' % s_
                kb.op('act', lambda e: e.activation(out=o_[:], in_=a_[:], func=AF.Silu, bias=cw(4)), [an, 'convT'], [on])
                kb.dma('sp', dst[h, :, blk * 512:(blk + 1) * 512], o_[:], [on], [('mqk', g, blk)], 'p1c_st%d' % s_)
                it += 1
    T.barrier()
    with ExitStack() as es1d:
        wt = sbuf(es1d, "p1d_w", [128, 8, TM_COLS], BF16)
        for kc in range(8):
            kb.dma('pool', wt[:, kc, :], win_d[kc * 128:(kc + 1) * 128, FM_COLS:WIN_COLS], [], ['p1d_w'], 'p1d_wl')
        pt = [[psum(es1d, "p1d_p%d_%d" % (i, j), [128, 512], F32) for j in range(4)] for i in range(2)]
        ot = [sbuf(es1d, "p1d_o%d" % i, [128, 1536], BF16) for i in range(2)]
        og = [sbuf(es1d, "p1d_g%d" % i, [128, 8], F32) for i in range(2)]
        for t in range(NT1):
            s_ = t % 2
            for j in range(4):
                n0, n1 = (j * 512, (j + 1) * 512) if j < 3 else (1536, 1544)
                for kc in range(8):
                    kb.op('pe', lambda e: e.matmul(pt[s_][j][:, 0:n1 - n0], lhsT=hT[:, kc, t * 128:(t + 1) * 128], rhs=wt[:, kc, n0:n1], start=(kc == 0), stop=(kc == 7)),
                          ['p1d_w'], ['ps:p1d_p%d_%d' % (s_, j)], attach='p1d_w')
            for j in range(3):
                eng = 'act' if j != 1 else 'dve'
                if eng == 'act':
                    kb.op('act', lambda e: e.copy(out=ot[s_][:, j * 512:(j + 1) * 512], in_=pt[s_][j][:]), ['ps:p1d_p%d_%d' % (s_, j)], ['p1d_o%d_%d' % (s_, j)])
                else:
                    kb.op('dve', lambda e: e.tensor_copy(out=ot[s_][:, j * 512:(j + 1) * 512], in_=pt[s_][j][:]), ['ps:p1d_p%d_%d' % (s_, j)], ['p1d_o%d_%d' % (s_, j)])
            kb.op('dve', lambda e: e.tensor_copy(out=og[s_][:], in_=pt[s_][3][:, 0:8]), ['ps:p1d_p%d_3' % s_], ['p1d_g%d' % s_])
            kb.dma('sp', tmb_s[t * 128:(t + 1) * 128, :], ot[s_][:], ['p1d_o%d_%d' % (s_, j) for j in range(3)], [('tmb', t)], 'p1d_st%d' % s_)
            kb.dma('sp', gate_s[:, t, :], og[s_][:], ['p1d_g%d' % s_], [('gate', t)], 'p1d_sg%d' % s_)
    es.close()
    T.barrier()
    if stop_after == '1':
        return finish(kb, es, es_all, out_d)

    with ExitStack() as es2:
        lamb = sbuf(es2, "p2_lamb", [128, 256], F32)
        lj = sbuf(es2, "p2_lj", [128, 64], F32)
        ls = sbuf(es2, "p2_ls", [128, 2], F32)
        nlam = sbuf(es2, "p2_nlam", [128, 1], F32)
        sg = sbuf(es2, "p2_sg", [128, 128], F32)
        maskf = sbuf(es2, "p2_maskf", [128, 128], F32)
        maskb = sbuf(es2, "p2_maskb", [128, 128], BF16)
        kb.dma('sp', lamb[:], lam_d.partition_broadcast(128), [], ['lamb'], 'p2_c0')
        kb.dma('sp', sg[:], subln_d.partition_broadcast(128), [], ['sg'], 'p2_c1')
        kb.dma('sp', maskf[:], maskb_d[:, :], [], ['maskf'], 'p2_c2')
        kb.op('dve', lambda e: e.tensor_copy(out=maskb[:], in_=maskf[:]), ['maskf'], ['maskb'])
        for i in range(2):
            kb.op('dve', lambda e: e.scalar_tensor_tensor(out=lj[:], in0=lamb[:, i * 128:i * 128 + 64], scalar=1.0, in1=lamb[:, i * 128 + 64:i * 128 + 128],
                                                          op0=ALU.mult, op1=ALU.mult, accum_out=ls[:, i:i + 1]), ['lamb'], ['lj', 'ls'])
        kb.op('act', lambda e: e.activation(out=ls[:], in_=ls[:], func=AF.Exp), ['ls'], ['ls'])
        kb.op('dve', lambda e: e.tensor_tensor(out=nlam[:], in0=ls[:, 1:2], in1=ls[:, 0:1], op=ALU.subtract), ['ls'], ['nlam'])
        kb.op('dve', lambda e: e.tensor_scalar(out=nlam[:], in0=nlam[:], scalar1=-LAM_INIT, scalar2=None, op0=ALU.add), ['nlam'], ['nlam'])
        kb.op('dve', lambda e: e.tensor_scalar(out=sg[:], in0=sg[:], scalar1=1.0 - LAM_INIT, scalar2=None, op0=ALU.mult), ['sg'], ['sg'])

        kTh = [sbuf(es2, "p2_kT%d" % i, [128, S], BF16) for i in range(2)]
        Vh = [sbuf(es2, "p2_V%d" % i, [128, NT, 129], BF16) for i in range(2)]
        qTb = [sbuf(es2, "p2_q%d" % i, [128, 512], BF16) for i in range(2)]
        PT = [sbuf(es2, "p2_PT%d" % i, [128, 512], BF16) for i in range(3)]
        ps_s = [psum(es2, "p2_ps%d" % i, [128, 512], F32) for i in range(2)]
        accA_b = [psum(es2, "p2_accA%d" % i, [128, 512], F32) for i in range(2)]
        accB_b = [psum(es2, "p2_accB%d" % i, [128, 512], F32) for i in range(2)]
        accA = [b_[:, 0:387].rearrange("p (q c) -> p q c", c=129) for b_ in accA_b]
        accB = [b_[:, 0:129].rearrange("p (q c) -> p q c", c=129) for b_ in accB_b]
        rz = sbuf(es2, "p2_rz", [128, 2, 4], F32)
        o0 = [sbuf(es2, "p2_o0_%d" % i, [128, 4, 128], F32) for i in range(2)]
        oo = [sbuf(es2, "p2_oo_%d" % i, [128, 4, 128], F32) for i in range(2)]
        junk = sbuf(es2, "p2_junk", [128, 128], F32)
        ss = sbuf(es2, "p2_ss", [128, 4], F32)
        yo = [sbuf(es2, "p2_yo%d" % i, [128, 4, 128], BF16) for i in range(2)]
        for i in range(2):
            kb.op('pool', lambda e: e.memset(Vh[i][:, :, 128:129], 1.0), [], ['V%dones' % i])
        sc_it = 0
        hq = 0
        for h in range(0 if skip12 else 4):
            hs = h % 2
            kb.dma('sp', kTh[hs][:], kT_s[h, :, :], [], ['kT%d' % hs], 'p2_kl%d' % hs)
            kb.dma('sp', Vh[hs][:, :, 0:128], tmb_s[:, h * 128:(h + 1) * 128].rearrange("(t p) c -> p t c", p=128), [], ['V%d' % hs], 'p2_vl%d' % hs)
            for qb in range(NB):
                qs_ = hq % 2
                kb.dma('sp', qTb[qs_][:], qT_s[h, :, qb * 512:(qb + 1) * 512], [], ['q%d' % qs_], 'p2_ql%d' % qs_)
                nkt = 4 * qb + 4
                for m in range(2):
                    a_i = (hq * 2 + m) % 2
                    aA, aB = accA[a_i], accB[a_i]
                    an = 'ps:acc%d' % a_i
                    rows = slice(m * 64, (m + 1) * 64)
                    state = {'startedA': False}

                    def emit_scores(kt, sc):
                        j = kt - 4 * qb
                        c0 = max(j, 0) * 128
                        p_s, pn = ps_s[sc % 2], 'ps:s%d' % (sc % 2)
                        P_, Pn = PT[sc % 3], 'PT%d' % (sc % 3)
                        kb.op('pe', lambda e: e.matmul(p_s[:, c0:512], lhsT=kTh[hs][rows, kt * 128:(kt + 1) * 128], rhs=qTb[qs_][rows, c0:512],
                                                        start=True, stop=(j < 0)), ['kT%d' % hs, 'q%d' % qs_], [pn])
                        if j >= 0:
                            kb.op('pe', lambda e: e.matmul(p_s[:, c0:c0 + 128], lhsT=ident_b[:], rhs=maskb[:], start=False, stop=True),
                                  ['ident_b', 'maskb'], [pn])
                        kb.op('act', lambda e: e.activation(out=P_[:, c0:512], in_=p_s[:, c0:512], func=AF.Exp, scale=0.125), [pn], [Pn])

                    def emit_pv(kt, sc):
                        j = kt - 4 * qb
                        P_, Pn = PT[sc % 3], 'PT%d' % (sc % 3)
                        for qs in range(max(j, 0), 4):
                            last = (kt == 4 * qb + qs)
                            if qs < 3:
                                o_ap = aA[:, qs, :]
                                st = not state['startedA']
                                state['startedA'] = True
                            else:
                                o_ap = aB[:, 0, :]
                                st = (kt == 0)
                            kb.op('pe', lambda e: e.matmul(o_ap, lhsT=P_[:, qs * 128:(qs + 1) * 128], rhs=Vh[hs][:, kt, :], start=st, stop=last, skip_group_check=True),
                                  [Pn, 'V%d' % hs, 'V%dones' % hs], [an])

                    prev = None
                    for kt in range(nkt):
                        emit_scores(kt, sc_it)
                        if prev is not None:
                            emit_pv(*prev)
                        prev = (kt, sc_it)
                        sc_it += 1
                    emit_pv(*prev)
                    pq_ = hq % 2
                    kb.op('dve', lambda e: e.reciprocal(out=rz[:, m, 0:3], in_=aA[:, :, 128]), [an], ['rz%d' % m])
                    kb.op('dve', lambda e: e.reciprocal(out=rz[:, m, 3:4], in_=aB[:, :, 128]), [an], ['rz%d' % m])
                    if m == 0:
                        for qs in range(4):
                            src = aA[:, qs, 0:128] if qs < 3 else aB[:, 0, 0:128]
                            kb.op('dve', lambda e: e.tensor_scalar(out=o0[pq_][:, qs, :], in0=src, scalar1=rz[:, 0, qs:qs + 1], scalar2=None, op0=ALU.mult),
                                  [an, 'rz0'], ['o0_%d' % pq_])
                    else:
                        kb.op('dve', lambda e: e.tensor_scalar(out=rz[:, 1, :], in0=rz[:, 1, :], scalar1=nlam[:, 0:1], scalar2=None, op0=ALU.mult),
                              ['rz1', 'nlam'], ['rz1'])
                        for qs in range(4):
                            src = aA[:, qs, 0:128] if qs < 3 else aB[:, 0, 0:128]
                            kb.op('dve', lambda e: e.scalar_tensor_tensor(out=oo[pq_][:, qs, :], in0=src, scalar=rz[:, 1, qs:qs + 1], in1=o0[pq_][:, qs, :],
                                                                          op0=ALU.mult, op1=ALU.add), [an, 'rz1', 'o0_%d' % pq_], ['oo_%d' % pq_])
                pq_ = hq % 2
                for qs in range(4):
                    kb.op('dve', lambda e: e.scalar_tensor_tensor(out=junk[:], in0=oo[pq_][:, qs, :], scalar=1.0, in1=oo[pq_][:, qs, :], op0=ALU.mult, op1=ALU.mult,
                                                                  accum_out=ss[:, qs:qs + 1]), ['oo_%d' % pq_], ['junk', 'ss'])
                kb.op('act', lambda e: e.activation(out=ss[:], in_=ss[:], func=AF.Ln, scale=1.0 / 128, bias=EPS), ['ss'], ['ss'])
                kb.op('act', lambda e: e.activation(out=ss[:], in_=ss[:], func=AF.Exp, scale=-0.5), ['ss'], ['ss'])
                for qs in range(4):
                    kb.op('dve', lambda e: e.scalar_tensor_tensor(out=yo[pq_][:, qs, :], in0=oo[pq_][:, qs, :], scalar=ss[:, qs:qs + 1], in1=sg[:], op0=ALU.mult, op1=ALU.mult),
                          ['oo_%d' % pq_, 'ss', 'sg'], ['yo%d' % pq_])
                kb.dma('sp', y_s[qb * 512:(qb + 1) * 512, h * 128:(h + 1) * 128].rearrange("(qs p) c -> p qs c", p=128), yo[pq_][:],
                       ['yo%d' % pq_], [('yda', h, qb)], 'p2_st%d' % pq_)
                hq += 1
    T.barrier()
    if stop_after == '2':
        return finish(kb, es, es_all, out_d)

    with ExitStack() as es3:
        DH = 128
        KS = float(DH) ** -0.5
        tri = sbuf(es3, "p3_tri", [128, 128], F32)
        maskf4 = sbuf(es3, "p3_maskf4", [128, 4, 128], F32)
        ones_f = sbuf(es3, "p3_ones", [128, 128], F32)
        gb = sbuf(es3, "p3_gb", [128, 8], F32)
        mlg = sbuf(es3, "p3_mlg", [128, 512], F32)
        G = sbuf(es3, "p3_G", [128, NT, 8], F32)
        ig = sbuf(es3, "p3_ig", [128, NT, 4], F32)
        lf = sbuf(es3, "p3_lf", [128, NT, 4], F32)
        bcol = sbuf(es3, "p3_bcol", [128, NT, 4], F32)
        ea = sbuf(es3, "p3_ea", [128, NT, 4], F32)
        wcol = sbuf(es3, "p3_wcol", [128, NT, 4], F32)
        eaL = sbuf(es3, "p3_eaL", [128, NT, 4], F32)
        mqT = sbuf(es3, "p3_mqT", [128, 4, S], BF16)
        mkT = sbuf(es3, "p3_mkT", [128, 4, S], BF16)
        MV = sbuf(es3, "p3_MV", [128, NT, 4, 129], BF16)
        kb.dma('sp', tri[:], tri_d[:, :], [], ['tri'], 'p3_c0')
        for hh in range(4):
            kb.dma('sp', maskf4[:, hh, :], maskb_d[:, :], [], ['maskf4'], 'p3_c1')
        kb.dma('sp', gb[:], gateb_d.partition_broadcast(128), [], ['gb'], 'p3_c2')
        kb.dma('sp', mlg[:], mlg_d.partition_broadcast(128), [], ['mlg'], 'p3_c3')
        kb.dma('sp', G[:], gate_s, [], ['G'], 'p3_c4')
        for hh in range(4):
            kb.dma('sp', mqT[:, hh, :], mqT_s[hh, :, :], [], ['mqT'], 'p3_c5')
            kb.dma('sp', mkT[:, hh, :], mkT_s[hh, :, :], [], ['mkT'], 'p3_c6')
        kb.op('pool', lambda e: e.memset(MV[:, :, :, 128:129], 1.0), [], ['MVones'])
        for hh in range(4):
            kb.dma('sp', MV[:, :, hh, 0:128], tmb_s[:, 512 + hh * 128:512 + (hh + 1) * 128].rearrange("(t p) c -> p t c", p=128), [], ['MV'], 'p3_c7')
        kb.op('pool', lambda e: e.memset(ones_f[:], 1.0), [], ['ones_f'])
        if stop_after == '3a':
            T.barrier()
            return finish(kb, es, es_all, out_d)
        kb.op('dve', lambda e: e.tensor_tensor(out=ig[:], in0=G[:, :, 0:4], in1=gb[:, 0:4].unsqueeze(1).to_broadcast([128, NT, 4]), op=ALU.add), ['G', 'gb'], ['ig'])
        kb.op('dve', lambda e: e.tensor_tensor(out=lf[:], in0=G[:, :, 4:8], in1=gb[:, 4:8].unsqueeze(1).to_broadcast([128, NT, 4]), op=ALU.add), ['G', 'gb'], ['lf'])
        kb.op('act', lambda e: e.activation(out=lf[:], in_=lf[:], func=AF.Exp, scale=-1.0), ['lf'], ['lf'])
        kb.op('act', lambda e: e.activation(out=lf[:], in_=lf[:], func=AF.Ln, bias=1.0), ['lf'], ['lf'])
        kb.op('dve', lambda e: e.tensor_scalar(out=lf[:], in0=lf[:], scalar1=-1.0, scalar2=None, op0=ALU.mult), ['lf'], ['lf'])
        if stop_after == '3b':
            T.barrier()
            return finish(kb, es, es_all, out_d)
        pg_b = psum(es3, "p3_pg", [128, 512], F32)
        pg = pg_b[:, 0:256].rearrange("p (a b) -> p a b", a=2)
        tri_b = sbuf(es3, "p3_tri_b", [128, 128], BF16)
        ones_b = sbuf(es3, "p3_ones_b", [128, 128], BF16)
        maskb4 = sbuf(es3, "p3_maskb4", [128, 4, 128], BF16)
        lf_hi = sbuf(es3, "p3_lf_hi", [128, NT, 4], BF16)
        lf_lo = sbuf(es3, "p3_lf_lo", [128, NT, 4], BF16)
        kb.op('dve', lambda e: e.tensor_copy(out=tri_b[:], in_=tri[:]), ['tri'], ['tri_b'])
        kb.op('dve', lambda e: e.tensor_copy(out=ones_b[:], in_=ones_f[:]), ['ones_f'], ['ones_b'])
        kb.op('dve', lambda e: e.tensor_copy(out=maskb4[:], in_=maskf4[:]), ['maskf4'], ['maskb4'])
        kb.op('dve', lambda e: e.tensor_copy(out=lf_hi[:], in_=lf[:]), ['lf'], ['lf_hi'])
        kb.op('dve', lambda e: e.tensor_tensor(out=lf_lo[:], in0=lf[:], in1=lf_hi[:], op=ALU.subtract), ['lf', 'lf_hi'], ['lf_lo'])
        if stop_after == '3b1':
            T.barrier()
            return finish(kb, es, es_all, out_d)
        lfh2 = lf_hi[:].rearrange("p t h -> p (t h)")
        lfl2 = lf_lo[:].rearrange("p t h -> p (t h)")
        kb.op('pe', lambda e: e.matmul(pg[:, 0, :], lhsT=tri_b[:], rhs=lfh2, start=True, stop=False), ['tri_b', 'lf_hi'], ['ps:pg'])
        kb.op('pe', lambda e: e.matmul(pg[:, 0, :], lhsT=tri_b[:], rhs=lfl2, start=False, stop=True), ['tri_b', 'lf_lo'], ['ps:pg'])
        kb.op('pe', lambda e: e.matmul(pg[:, 1, :], lhsT=ones_b[:], rhs=lfh2, start=True, stop=False), ['ones_b', 'lf_hi'], ['ps:pg'])
        kb.op('pe', lambda e: e.matmul(pg[:, 1, :], lhsT=ones_b[:], rhs=lfl2, start=False, stop=True), ['ones_b', 'lf_lo'], ['ps:pg'])
        if stop_after == '3b2':
            T.barrier()
            return finish(kb, es, es_all, out_d)
        f2 = lambda t_: t_[:].rearrange("p t h -> p (t h)")
        kb.op('dve', lambda e: e.scalar_tensor_tensor(out=f2(bcol), in0=pg[:, 0, :], scalar=-1.0, in1=f2(ig), op0=ALU.mult, op1=ALU.add), ['ig', 'ps:pg'], ['bcol'])
        kb.op('act', lambda e: e.activation(out=f2(ea), in_=pg[:, 0, :], func=AF.Exp), ['ps:pg'], ['ea'])
        if stop_after == '3b3':
            T.barrier()
            return finish(kb, es, es_all, out_d)
        kb.op('dve', lambda e: e.tensor_tensor(out=f2(wcol), in0=pg[:, 1, :], in1=f2(bcol), op=ALU.add), ['bcol', 'ps:pg'], ['wcol'])
        kb.op('act', lambda e: e.activation(out=f2(wcol), in_=f2(wcol), func=AF.Exp), ['wcol'], ['wcol'])
        kb.op('act', lambda e: e.activation(out=f2(eaL), in_=pg[:, 1, :], func=AF.Exp), ['ps:pg'], ['eaL'])

        if stop_after == '3c':
            T.barrier()
            return finish(kb, es, es_all, out_d)
        pX = [psum(es3, "p3_pX%d" % i, [128, 4, 128], F32) for i in range(2)]
        pP = [psum(es3, "p3_pP%d" % i, [128, 512], F32) for i in range(4)]
        pTk_b = psum(es3, "p3_pTk", [128, 1024], BF16)
        pTk = pTk_b[:, 0:512].rearrange("p (a b) -> p a b", a=4)
        Rc = [sbuf(es3, "p3_Rc%d" % i, [128, 4, 128], BF16) for i in range(2)]
        Rl = [sbuf(es3, "p3_Rl%d" % i, [128, 4, 128], BF16) for i in range(2)]
        DT = [sbuf(es3, "p3_DT%d" % i, [128, 4, 128], F32) for i in range(2)]
        SD = [sbuf(es3, "p3_SD%d" % i, [128, 128], BF16) for i in range(4)]
        KW = [sbuf(es3, "p3_KW%d" % i, [128, 128], BF16) for i in range(4)]
        Cf = [sbuf(es3, "p3_Cf%d" % i, [128, 129], F32) for i in range(4)]
        Cb = [sbuf(es3, "p3_Cb%d" % i, [128, 129], BF16) for i in range(4)]
        intra_sb = [sbuf(es3, "p3_intra%d" % i, [128, 129], F32) for i in range(4)]
        num = [sbuf(es3, "p3_num%d" % i, [128, 4, 129], F32) for i in range(2)]
        rdn = sbuf(es3, "p3_rdn", [128, 4], F32)
        hsc = [sbuf(es3, "p3_hsc%d" % i, [128, 4, 128], F32) for i in range(2)]
        stats = sbuf(es3, "p3_stats", [128, 4, 6], F32)
        mvv = sbuf(es3, "p3_mvv", [128, 4, 2], F32)
        rstd3 = sbuf(es3, "p3_rstd", [128, 4], F32)
        mo_t = [sbuf(es3, "p3_mo%d" % i, [128, 512], BF16) for i in range(2)]
        gg = [sbuf(es3, "p3_gg%d" % i, [128, 512], F32) for i in range(2)]
        yo3 = [sbuf(es3, "p3_yo%d" % i, [128, 512], BF16) for i in range(2)]
        for hh in range(4):
            kb.op('pool', lambda e: e.memset(Cf[hh][:], 0.0), [], ['Cf%d' % hh])
            kb.op('pool', lambda e: e.memset(Cb[hh][:], 0.0), [], ['Cb%d' % hh])
        u_it = 0
        for c in range(p3_tiles):
            cs = c % 2
            tsl = slice(c * 128, (c + 1) * 128)
            kb.dma('sp', mo_t[cs][:], tmb_s[tsl, 1024:1536], [], ['mo%d' % cs], 'p3_mol%d' % cs)
            kb.op('act', lambda e: e.activation(out=gg[cs][:], in_=mo_t[cs][:], func=AF.Exp, scale=-1.0), ['mo%d' % cs], ['gg%d' % cs])
            kb.op('pool', lambda e: e.tensor_scalar(out=gg[cs][:], in0=gg[cs][:], scalar1=1.0, scalar2=None, op0=ALU.add), ['gg%d' % cs], ['gg%d' % cs])
            kb.op('dve', lambda e: e.reciprocal(out=gg[cs][:], in_=gg[cs][:]), ['gg%d' % cs], ['gg%d' % cs])
            kb.op('pool', lambda e: e.tensor_tensor(out=gg[cs][:], in0=gg[cs][:], in1=mlg[:], op=ALU.mult), ['gg%d' % cs, 'mlg'], ['gg%d' % cs])
            kb.op('dve', lambda e: e.tensor_tensor(out=Rc[cs][:], in0=tri_b[:].unsqueeze(1).to_broadcast([128, 4, 128]),
                                                    in1=lf_hi[:, c, :].unsqueeze(2).to_broadcast([128, 4, 128]), op=ALU.mult), ['tri_b', 'lf_hi'], ['Rc%d' % cs])
            kb.op('dve', lambda e: e.tensor_tensor(out=Rl[cs][:], in0=tri_b[:].unsqueeze(1).to_broadcast([128, 4, 128]),
                                                    in1=lf_lo[:, c, :].unsqueeze(2).to_broadcast([128, 4, 128]), op=ALU.mult), ['tri_b', 'lf_lo'], ['Rl%d' % cs])
            pXf = pX[cs][:].rearrange("p h j -> p (h j)")
            kb.op('pe', lambda e: e.matmul(pXf, lhsT=ones_b[:], rhs=Rc[cs][:].rearrange("p h j -> p (h j)"), start=True, stop=False),
                  ['ones_b', 'Rc%d' % cs], ['ps:pX%d' % cs])
            kb.op('pe', lambda e: e.matmul(pXf, lhsT=ones_b[:], rhs=Rl[cs][:].rearrange("p h j -> p (h j)"), start=False, stop=False),
                  ['ones_b', 'Rl%d' % cs], ['ps:pX%d' % cs])
            kb.op('pe', lambda e: e.matmul(pXf, lhsT=ident_b[:], rhs=maskb4[:].rearrange("p h j -> p (h j)"), start=False, stop=True),
                  ['ident_b', 'maskb4'], ['ps:pX%d' % cs])
            for hh in range(4):
                kb.op('act', lambda e: e.activation(out=DT[cs][:, hh, :], in_=pX[cs][:, hh, :], func=AF.Exp, bias=bcol[:, c, hh:hh + 1]),
                      ['ps:pX%d' % cs, 'bcol'], ['DT%d_%d' % (cs, hh)])
            q_t = lambda hh: mqT[:, hh, tsl]
            k_t = lambda hh: mkT[:, hh, tsl]
            Pn = lambda hh: 'ps:pP%d' % hh
            for hh in range(4):
                kb.op('pe', lambda e: e.matmul(pP[hh][:, 0:128], lhsT=k_t(hh), rhs=q_t(hh), start=True, stop=True), ['mkT', 'mqT'], [Pn(hh)])
            for hh in range(4):
                kb.op('pe', lambda e: e.transpose(out=pTk[:, hh, :], in_=k_t(hh), identity=ident_b[:]), ['mkT', 'ident_b'], ['ps:pTk'])
            for hh in range(4):
                kb.op('dve', lambda e: e.scalar_tensor_tensor(out=SD[hh][:], in0=pP[hh][:, 0:128], scalar=KS, in1=DT[cs][:, hh, :], op0=ALU.mult, op1=ALU.mult),
                      [Pn(hh), 'DT%d_%d' % (cs, hh)], ['SD%d' % hh])
            for hh in range(4):
                kb.op('dve', lambda e: e.tensor_scalar(out=KW[hh][:], in0=pTk[:, hh, :], scalar1=wcol[:, c, hh:hh + 1], scalar2=KS, op0=ALU.mult, op1=ALU.mult),
                      ['ps:pTk', 'wcol'], ['KW%d' % hh])
            for hh in range(4):
                kb.op('pe', lambda e: e.matmul(pP[hh][:, 129:258], lhsT=SD[hh][:], rhs=MV[:, c, hh, :], start=True, stop=True), ['SD%d' % hh, 'MV', 'MVones'], [Pn(hh)])
                kb.op('pe', lambda e: e.matmul(pP[hh][:, 258:387], lhsT=q_t(hh), rhs=Cb[hh][:], start=True, stop=True), ['mqT', 'Cb%d' % hh], [Pn(hh)], attach='Cb%d' % hh)
            for hh in range(4):
                kb.op('act', lambda e: e.copy(out=intra_sb[hh][:], in_=pP[hh][:, 129:258]), [Pn(hh)], ['intra%d' % hh])
                kb.op('dve', lambda e: e.scalar_tensor_tensor(out=num[cs][:, hh, :], in0=pP[hh][:, 258:387], scalar=ea[:, c, hh:hh + 1], in1=intra_sb[hh][:], op0=ALU.mult, op1=ALU.add),
                      [Pn(hh), 'ea', 'intra%d' % hh], ['num%d_%d' % (cs, hh)])
            for hh in range(4):
                kb.op('pe', lambda e: e.matmul(pP[hh][:, 0:129], lhsT=KW[hh][:], rhs=MV[:, c, hh, :], start=True, stop=True), ['KW%d' % hh, 'MV', 'MVones'], [Pn(hh)])
            for hh in range(4):
                kb.op('pool', lambda e: e.tensor_scalar(out=Cf[hh][:], in0=Cf[hh][:], scalar1=eaL[:, c, hh:hh + 1], scalar2=None, op0=ALU.mult),
                      ['Cf%d' % hh, 'eaL'], ['Cf%d' % hh])
            for hh in range(4):
                kb.op('dve', lambda e: e.tensor_tensor(out=Cf[hh][:], in0=pP[hh][:, 0:129], in1=Cf[hh][:], op=ALU.add),
                      ['Cf%d' % hh, Pn(hh)], ['Cf%d' % hh])
                kb.op('act', lambda e: e.copy(out=Cb[hh][:], in_=Cf[hh][:]), ['Cf%d' % hh], ['Cb%d' % hh])
            nn = ['num%d_%d' % (cs, hh) for hh in range(4)]
            kb.op('dve', lambda e: e.tensor_tensor(out=rdn[:], in0=num[cs][:, :, 128], in1=num[cs][:, :, 128], op=ALU.mult), nn, ['rdn'])
            kb.op('dve', lambda e: e.tensor_scalar(out=rdn[:], in0=rdn[:], scalar1=1.0, scalar2=None, op0=ALU.max), ['rdn'], ['rdn'])
            kb.op('act', lambda e: e.activation(out=rdn[:], in_=rdn[:], func=AF.Ln), ['rdn'], ['rdn'])
            kb.op('act', lambda e: e.activation(out=rdn[:], in_=rdn[:], func=AF.Exp, scale=-0.5), ['rdn'], ['rdn'])
            kb.op('dve', lambda e: e.tensor_tensor(out=hsc[cs][:], in0=num[cs][:, :, 0:128], in1=rdn[:].unsqueeze(2).to_broadcast([128, 4, 128]), op=ALU.mult),
                  nn + ['rdn'], ['hsc%d' % cs])
            for hh in range(4):
                kb.op('dve', lambda e: e.bn_stats(out=stats[:, hh, :], in_=hsc[cs][:, hh, :]), ['hsc%d' % cs], ['stats'])
                kb.op('dve', lambda e: e.bn_aggr(out=mvv[:, hh, :], in_=stats[:, hh, :]), ['stats'], ['mvv'])
            kb.op('act', lambda e: e.activation(out=rstd3[:], in_=mvv[:, :, 1], func=AF.Ln, bias=EPS), ['mvv'], ['rstd3'])
            kb.op('act', lambda e: e.activation(out=rstd3[:], in_=rstd3[:], func=AF.Exp, scale=-0.5), ['rstd3'], ['rstd3'])
            for hh in range(4):
                kb.op('dve', lambda e: e.tensor_scalar(out=hsc[cs][:, hh, :], in0=hsc[cs][:, hh, :], scalar1=mvv[:, hh, 0:1], scalar2=rstd3[:, hh:hh + 1], op0=ALU.subtract, op1=ALU.mult),
                      ['hsc%d' % cs, 'mvv', 'rstd3'], ['hsc%d' % cs])
            kb.op('dve', lambda e: e.tensor_tensor(out=yo3[cs][:], in0=hsc[cs][:].rearrange("p h d -> p (h d)"), in1=gg[cs][:], op=ALU.mult),
                  ['hsc%d' % cs, 'gg%d' % cs], ['yo3_%d' % cs])
            kb.dma('sp', y_s[tsl, 512:1024], yo3[cs][:], ['yo3_%d' % cs], [('yml', c)], 'p3_st%d' % cs)
    T.barrier()
    if stop_after == '3':
        return finish(kb, es, es_all, out_d)

    es4 = ExitStack()
    h2T = sbuf(es4, "h2T", [128, 8, S], BF16)
    with ExitStack() as es4a:
        wo = sbuf(es4a, "p4_wo", [128, 8, D], BF16)
        for kc in range(8):
            kb.dma('pool', wo[:, kc, :], wout_d[kc * 128:(kc + 1) * 128, :], [], ['p4_wo'], 'p4_wl')
        yt = [sbuf(es4a, "p4_yt%d" % i, [128, D], BF16) for i in range(2)]
        xt4 = [sbuf(es4a, "p4_xt%d" % i, [128, D], F32) for i in range(2)]
        yT = [sbuf(es4a, "p4_yT%d" % i, [128, 8, 128], BF16) for i in range(2)]
        x1t = [sbuf(es4a, "p4_x1_%d" % i, [128, D], F32) for i in range(2)]
        pTy = [psum(es4a, "p4_pT%d" % i, [128, 8, 128], BF16) for i in range(2)]
        po4 = [[psum(es4a, "p4_po%d_%d" % (i, j), [128, 512], F32) for j in range(2)] for i in range(2)]
        def p4_front(t):
            b = t % 2
            tsl = slice(t * 128, (t + 1) * 128)
            kb.dma('sp', yt[b][:], y_s[tsl, :], [], ['p4_yt%d' % b], 'p4_yl%d' % b)
            kb.dma('sp', xt4[b][:], x_d[tsl, :], [], ['p4_xt%d' % b], 'p4_xl%d' % b)
            for c in range(8):
                kb.op('pe', lambda e: e.transpose(out=pTy[b][:, c, :], in_=yt[b][:, c * 128:(c + 1) * 128], identity=ident_b[:]),
                      ['p4_yt%d' % b, 'ident_b'], ['ps:p4_pT%d' % b])
            kb.op('act', lambda e: e.copy(out=yT[b][:], in_=pTy[b][:]), ['ps:p4_pT%d' % b], ['p4_yT%d' % b])

        def p4_back(t):
            b = t % 2
            tsl = slice(t * 128, (t + 1) * 128)
            for hf in range(2):
                for kc in range(8):
                    kb.op('pe', lambda e: e.matmul(po4[b][hf][:], lhsT=yT[b][:, kc, :], rhs=wo[:, kc, hf * 512:(hf + 1) * 512], start=(kc == 0), stop=(kc == 7)),
                          ['p4_yT%d' % b, 'p4_wo'], ['ps:p4_po%d_%d' % (b, hf)])
                kb.op('dve', lambda e: e.tensor_tensor(out=x1t[b][:, hf * 512:(hf + 1) * 512], in0=po4[b][hf][:], in1=xt4[b][:, hf * 512:(hf + 1) * 512], op=ALU.add),
                      ['ps:p4_po%d_%d' % (b, hf), 'p4_xt%d' % b], ['p4_x1_%d_%d' % (b, hf)])
            kb.dma('sp', x1_s[tsl, :], x1t[b][:], ['p4_x1_%d_0' % b, 'p4_x1_%d_1' % b], [('x1', t)], 'p4_st%d' % b)

        for t in range(NT):
            p4_front(t)
            if t >= 1:
                p4_back(t - 1)
        p4_back(NT - 1)
    T.barrier()
    with ExitStack() as es4b:
        pTt = [psum(es4b, "p4b_pT%d" % i, [128, 8, 128], BF16) for i in range(2)]
        norm_transpose(es4b, "p4b", lambda t: x1_s[t * 128:(t + 1) * 128, :], 1, h2T, NT, pTt, dst_tag='h2T_t')
    T.barrier()
    if stop_after == '4':
        return finish(kb, es, es_all, out_d)

    es5 = ExitStack()
    memT = sbuf(es5, "memT", [128, 8, MEM], BF16)
    with ExitStack() as es5a:
        pTt = [psum(es5a, "p5a_pT%d" % i, [128, 8, 128], BF16) for i in range(2)]
        norm_transpose(es5a, "p5a", lambda t: mem_d[t * 128:(t + 1) * 128, :], 2, memT, 2, pTt, dst_tag='memT_t')
    T.barrier()
    with ExitStack() as es5b:
        wx = {}
        for nm, wd in (('q', wxq_d), ('k', wxk_d), ('v', wxv_d), ('o', wxo_d)):
            wx[nm] = sbuf(es5b, "p5_w" + nm, [128, 8, D], BF16)
            for kc in range(8):
                kb.dma('pool', wx[nm][:, kc, :], wd[kc * 128:(kc + 1) * 128, :], [], ['p5_w' + nm], 'p5_wl' + nm)
        KT = sbuf(es5b, "p5_KT", [128, 8, MEM], BF16)
        Vx = sbuf(es5b, "p5_Vx", [128, 2, D], BF16)
        pA = [psum(es5b, "p5_pA%d" % i, [128, 512], F32) for i in range(2)]
        pS = [psum(es5b, "p5_pS%d" % i, [128, 512], F32) for i in range(2)]
        pZ = psum(es5b, "p5_pZ", [128, 512], F32)
        pO = [psum(es5b, "p5_pO%d" % i, [128, 512], F32) for i in range(2)]
        ia = 0
        for c in range(8):
            p_, pn = pA[ia % 2], 'ps:p5_pA%d' % (ia % 2)
            for kc in range(8):
                kb.op('pe', lambda e: e.matmul(p_[:, 0:MEM], lhsT=wx['k'][:, kc, c * 128:(c + 1) * 128], rhs=memT[:, kc, :], start=(kc == 0), stop=(kc == 7)),
                      ['p5_wk'], [pn])
            kb.op('act', lambda e: e.copy(out=KT[:, c, :], in_=p_[:, 0:MEM]), [pn], ['p5_KT'])
            ia += 1
        for kt in range(2):
            for hf in range(2):
                p_, pn = pA[ia % 2], 'ps:p5_pA%d' % (ia % 2)
                for kc in range(8):
                    kb.op('pe', lambda e: e.matmul(p_[:], lhsT=memT[:, kc, kt * 128:(kt + 1) * 128], rhs=wx['v'][:, kc, hf * 512:(hf + 1) * 512], start=(kc == 0), stop=(kc == 7)),
                          ['p5_wv'], [pn], attach='p5_wv')
                kb.op('act', lambda e: e.copy(out=Vx[:, kt, hf * 512:(hf + 1) * 512], in_=p_[:]), [pn], ['p5_Vx'])
                ia += 1
        qTx = [sbuf(es5b, "p5_qT%d" % i, [128, 8, 512], BF16) for i in range(2)]
        PTx = [sbuf(es5b, "p5_PT%d" % i, [128, 2, 512], BF16) for i in range(2)]
        rZ = [sbuf(es5b, "p5_rZ%d" % i, [128, 512], F32) for i in range(2)]
        oTx = [sbuf(es5b, "p5_oT%d" % i, [128, 8, 512], BF16) for i in range(2)]
        x1t5 = [sbuf(es5b, "p5_x1_%d" % i, [128, D], F32) for i in range(2)]
        x2t5 = [sbuf(es5b, "p5_x2_%d" % i, [128, D], F32) for i in range(2)]
        hi_ = 0
        ti_ = 0
        for blk in range(NB):
            bs = blk % 2
            bsl = slice(blk * 512, (blk + 1) * 512)
            for c in range(8):
                p_, pn = pA[ia % 2], 'ps:p5_pA%d' % (ia % 2)
                for kc in range(8):
                    kb.op('pe', lambda e: e.matmul(p_[:], lhsT=wx['q'][:, kc, c * 128:(c + 1) * 128], rhs=h2T[:, kc, bsl], start=(kc == 0), stop=(kc == 7)),
                          ['p5_wq'], [pn])
                kb.op('act', lambda e: e.copy(out=qTx[bs][:, c, :], in_=p_[:]), [pn], ['p5_qT%d_%d' % (bs, c)])
                ia += 1
            for h in range(4):
                hs = hi_ % 2
                for kt in range(2):
                    p_, pn = pS[kt], 'ps:p5_pS%d' % kt
                    for dc in range(2):
                        kb.op('pe', lambda e: e.matmul(p_[:], lhsT=KT[:, 2 * h + dc, kt * 128:(kt + 1) * 128], rhs=qTx[bs][:, 2 * h + dc, :], start=(dc == 0), stop=(dc == 1)),
                              ['p5_KT', 'p5_qT%d_%d' % (bs, 2 * h + dc)], [pn])
                    kb.op('act', lambda e: e.activation(out=PTx[hs][:, kt, :], in_=p_[:], func=AF.Exp, scale=1.0 / 16.0), [pn], ['p5_PT%d_%d' % (hs, kt)])
                for kt in range(2):
                    kb.op('pe', lambda e: e.matmul(pZ[:], lhsT=ones_bb[:], rhs=PTx[hs][:, kt, :], start=(kt == 0), stop=(kt == 1)),
                          ['ones_bb', 'p5_PT%d_%d' % (hs, kt)], ['ps:p5_pZ'])
                kb.op('dve', lambda e: e.reciprocal(out=rZ[hs][:], in_=pZ[:]), ['ps:p5_pZ'], ['p5_rZ%d' % hs])
                for dc in range(2):
                    p_, pn = pO[dc], 'ps:p5_pO%d' % dc
                    for kt in range(2):
                        kb.op('pe', lambda e: e.matmul(p_[:], lhsT=Vx[:, kt, h * 256 + dc * 128:h * 256 + (dc + 1) * 128], rhs=PTx[hs][:, kt, :], start=(kt == 0), stop=(kt == 1)),
                              ['p5_Vx', 'p5_PT%d_%d' % (hs, kt)], [pn])
                    kb.op('dve', lambda e: e.tensor_tensor(out=oTx[bs][:, 2 * h + dc, :], in0=p_[:], in1=rZ[hs][:], op=ALU.mult),
                          [pn, 'p5_rZ%d' % hs], ['p5_oT%d_%d' % (bs, 2 * h + dc)])
                hi_ += 1
            for sub in range(4):
                t = blk * 4 + sub
                ts_ = ti_ % 2
                tsl = slice(t * 128, (t + 1) * 128)
                kb.dma('sp', x1t5[ts_][:], x1_s[tsl, :], [], ['p5_x1_%d' % ts_], 'p5_xl%d' % ts_)
                for hf in range(2):
                    p_, pn = pA[ia % 2], 'ps:p5_pA%d' % (ia % 2)
                    for kc in range(8):
                        kb.op('pe', lambda e: e.matmul(p_[:], lhsT=oTx[bs][:, kc, sub * 128:(sub + 1) * 128], rhs=wx['o'][:, kc, hf * 512:(hf + 1) * 512], start=(kc == 0), stop=(kc == 7)),
                              ['p5_oT%d_%d' % (bs, kc), 'p5_wo'], [pn])
                    kb.op('dve', lambda e: e.tensor_tensor(out=x2t5[ts_][:, hf * 512:(hf + 1) * 512], in0=p_[:], in1=x1t5[ts_][:, hf * 512:(hf + 1) * 512], op=ALU.add),
                          [pn, 'p5_x1_%d' % ts_], ['p5_x2_%d_%d' % (ts_, hf)])
                    ia += 1
                kb.dma('sp', x2_s[tsl, :], x2t5[ts_][:], ['p5_x2_%d_0' % ts_, 'p5_x2_%d_1' % ts_], [('x2', t)], 'p5_st%d' % ts_)
                ti_ += 1
    es5.close()
    es4.close()
    T.barrier()
    if stop_after == '5':
        return finish(kb, es, es_all, out_d)

    with ExitStack() as es5c:
        gbc = sbuf(es5c, "p5c_gbc", [128, D], F32)
        bbc = sbuf(es5c, "p5c_bbc", [128, 36], F32)
        eoff = sbuf(es5c, "p5c_eoff", [128, NEXP], F32)
        trisf = sbuf(es5c, "p5c_trisf", [128, 128], F32)
        trisb = sbuf(es5c, "p5c_trisb", [128, 128], BF16)
        wr = sbuf(es5c, "p5c_wr", [128, 8, 36], BF16)
        tokid = sbuf(es5c, "p5c_tokid", [128, NT], I32)
        macc = sbuf(es5c, "p5c_macc", [128, NEXP], BF16)
        kb.dma('sp', gbc[:], gffn_d.partition_broadcast(128), [], ['gbc'], 'p5c_c0')
        kb.dma('sp', bbc[:], br_d.partition_broadcast(128), [], ['bbc'], 'p5c_c1')
        kb.dma('sp', eoff[:], eoff_d.partition_broadcast(128), [], ['eoff'], 'p5c_c2')
        kb.dma('sp', trisf[:], tris_d[:, :], [], ['trisf'], 'p5c_c3')
        kb.dma('sp', tokid[:], tokid_d[:, :], [], ['tokid'], 'p5c_c4')
        for kc in range(8):
            kb.dma('pool', wr[:, kc, :], wr_d[kc * 128:(kc + 1) * 128, :], [], ['wr'], 'p5c_c5')
        kb.op('dve', lambda e: e.tensor_copy(out=trisb[:], in_=trisf[:]), ['trisf'], ['trisb'])
        kb.op('pool', lambda e: e.memset(macc[:], 0.0), [], ['macc'])
        x2t = [sbuf(es5c, "p5c_x2_%d" % i, [128, D], F32) for i in range(2)]
        junkc = sbuf(es5c, "p5c_junk", [128, D], BF16)
        ssq = [sbuf(es5c, "p5c_ssq%d" % i, [128, 1], F32) for i in range(2)]
        h3 = [sbuf(es5c, "p5c_h3_%d" % i, [128, D], BF16) for i in range(2)]
        h3T = [sbuf(es5c, "p5c_h3T%d" % i, [128, 8, 128], BF16) for i in range(2)]
        pT5 = [psum(es5c, "p5c_pT%d" % i, [128, 8, 128], BF16) for i in range(2)]
        pL = [psum(es5c, "p5c_pL%d" % i, [128, 512], F32) for i in range(2)]
        pPos = [psum(es5c, "p5c_pPos%d" % i, [128, 512], F32) for i in range(2)]
        lg = sbuf(es5c, "p5c_lg", [128, 36], F32)
        sm = sbuf(es5c, "p5c_sm", [128, 16], F32)
        ge = sbuf(es5c, "p5c_ge", [128, 4], F32)
        oh = sbuf(es5c, "p5c_oh", [128, 4], F32)
        lm = sbuf(es5c, "p5c_lm", [128, 4, 8], F32)
        m8 = sbuf(es5c, "p5c_m8", [128, 8], F32)
        m1 = sbuf(es5c, "p5c_m1", [128, NEXP], F32)
        m2 = sbuf(es5c, "p5c_m2", [128, NEXP], F32)
        maskb5 = [sbuf(es5c, "p5c_mask%d" % i, [128, NEXP], BF16) for i in range(2)]
        sl = sbuf(es5c, "p5c_sl", [128, NEXP], F32)
        junk32 = sbuf(es5c, "p5c_junk32", [128, NEXP], F32)
        sf = sbuf(es5c, "p5c_sf", [128, 2], F32)
        lmf = lm[:].rearrange("p g e -> p (g e)")
        for t in range(NT):
            b = t % 2
            tsl = slice(t * 128, (t + 1) * 128)
            kb.dma('sp', x2t[b][:], x2_s[tsl, :], [], ['x2t%d' % b], 'p5c_xl%d' % b)
            kb.op('act', lambda e: e.activation(out=junkc[:], in_=x2t[b][:], func=AF.Square, accum_out=ssq[b][:]), ['x2t%d' % b], ['junkc', 'ssq%d' % b])
            kb.op('act', lambda e: e.activation(out=ssq[b][:], in_=ssq[b][:], func=AF.Ln, scale=1.0 / D, bias=EPS), ['ssq%d' % b], ['ssq%d' % b])
            kb.op('act', lambda e: e.activation(out=ssq[b][:], in_=ssq[b][:], func=AF.Exp, scale=-0.5), ['ssq%d' % b], ['ssq%d' % b])
            kb.op('dve', lambda e: e.scalar_tensor_tensor(out=h3[b][:], in0=x2t[b][:], scalar=ssq[b][:, 0:1], in1=gbc[:], op0=ALU.mult, op1=ALU.mult),
                  ['x2t%d' % b, 'ssq%d' % b, 'gbc'], ['h3_%d' % b])
            for c in range(8):
                kb.op('pe', lambda e: e.transpose(out=pT5[b][:, c, :], in_=h3[b][:, c * 128:(c + 1) * 128], identity=ident_b[:]), ['h3_%d' % b, 'ident_b'], ['ps:p5c_pT%d' % b])
            kb.op('act', lambda e: e.copy(out=h3T[b][:], in_=pT5[b][:]), ['ps:p5c_pT%d' % b], ['h3T%d' % b])
            for kc in range(8):
                kb.op('pe', lambda e: e.matmul(pL[b][:, 0:36], lhsT=h3T[b][:, kc, :], rhs=wr[:, kc, :], start=(kc == 0), stop=(kc == 7)), ['h3T%d' % b, 'wr'], ['ps:p5c_pL%d' % b])
            kb.op('dve', lambda e: e.tensor_tensor(out=lg[:], in0=pL[b][:, 0:36], in1=bbc[:], op=ALU.add), ['ps:p5c_pL%d' % b, 'bbc'], ['lg'])
            kb.op('dve', lambda e: e.reduce_max(out=sm[:, 0:1], in_=lg[:, 0:4], axis=AX.X), ['lg'], ['sm0'])
            kb.op('dve', lambda e: e.tensor_scalar(out=sm[:, 1:2], in0=sm[:, 0:1], scalar1=-1.0, scalar2=None, op0=ALU.mult), ['sm0'], ['sm1'])
            kb.op('act', lambda e: e.activation(out=ge[:], in_=lg[:, 0:4], func=AF.Exp, bias=sm[:, 1:2], accum_out=sm[:, 2:3]), ['lg', 'sm1'], ['ge', 'sm2'])
            kb.op('dve', lambda e: e.reciprocal(out=sm[:, 3:4], in_=sm[:, 2:3]), ['sm2'], ['sm3'])
            kb.op('dve', lambda e: e.tensor_scalar(out=oh[:], in0=lg[:, 0:4], scalar1=sm[:, 0:1], scalar2=None, op0=ALU.is_equal), ['lg', 'sm0'], ['oh'])
            kb.op('dve', lambda e: e.tensor_scalar(out=oh[:], in0=oh[:], scalar1=-1.0, scalar2=1.0e9, op0=ALU.add, op1=ALU.mult), ['oh'], ['oh'])
            kb.op('dve', lambda e: e.tensor_tensor(out=lm[:], in0=lg[:, 4:36].rearrange("p (g e) -> p g e", g=4), in1=oh[:].unsqueeze(2).to_broadcast([128, 4, 8]), op=ALU.add),
                  ['lg', 'oh'], ['lm'])
            kb.op('dve', lambda e: e.max(out=m8[:], in_=lmf), ['lm'], ['m8'])
            kb.op('dve', lambda e: e.tensor_scalar(out=sm[:, 4:5], in0=m8[:, 0:1], scalar1=-1.0, scalar2=None, op0=ALU.mult), ['m8'], ['sm4'])
            kb.op('act', lambda e: e.activation(out=sm[:, 5:6], in_=m8[:, 1:2], func=AF.Exp, bias=sm[:, 4:5]), ['m8', 'sm4'], ['sm5'])
            kb.op('dve', lambda e: e.tensor_scalar(out=sm[:, 6:7], in0=sm[:, 5:6], scalar1=1.0, scalar2=None, op0=ALU.add), ['sm5'], ['sm6'])
            kb.op('dve', lambda e: e.reciprocal(out=sm[:, 7:8], in_=sm[:, 6:7]), ['sm6'], ['sm7'])
            kb.op('dve', lambda e: e.tensor_tensor(out=comb_w[:, t, 0:1], in0=sm[:, 7:8], in1=sm[:, 3:4], op=ALU.mult), ['sm7', 'sm3'], [('cw', t)])
            kb.op('dve', lambda e: e.tensor_tensor(out=comb_w[:, t, 1:2], in0=comb_w[:, t, 0:1], in1=sm[:, 5:6], op=ALU.mult), [('cw', t), 'sm5'], [('cw2', t)])
            kb.op('dve', lambda e: e.tensor_scalar(out=m1[:], in0=lmf, scalar1=m8[:, 0:1], scalar2=None, op0=ALU.is_equal), ['lm', 'm8'], ['m1'])
            kb.op('dve', lambda e: e.tensor_scalar(out=m2[:], in0=lmf, scalar1=m8[:, 1:2], scalar2=None, op0=ALU.is_equal), ['lm', 'm8'], ['m2'])
            kb.op('dve', lambda e: e.tensor_tensor(out=maskb5[b][:], in0=m1[:], in1=m2[:], op=ALU.add), ['m1', 'm2'], ['mask%d' % b])
            kb.op('pe', lambda e: e.matmul(pPos[b][:, 0:NEXP], lhsT=trisb[:], rhs=maskb5[b][:], start=True, stop=False), ['trisb', 'mask%d' % b], ['ps:p5c_pPos%d' % b])
            kb.op('pe', lambda e: e.matmul(pPos[b][:, 0:NEXP], lhsT=ones_bb[:], rhs=macc[:], start=False, stop=True), ['ones_bb', 'macc'], ['ps:p5c_pPos%d' % b], attach='macc')
            kb.op('dve', lambda e: e.tensor_tensor(out=macc[:], in0=macc[:], in1=maskb5[b][:], op=ALU.add), ['macc', 'mask%d' % b], ['macc'])
            kb.op('dve', lambda e: e.scalar_tensor_tensor(out=sl[:], in0=pPos[b][:, 0:NEXP], scalar=float(CAP - 1), in1=eoff[:], op0=ALU.min, op1=ALU.add),
                  ['ps:p5c_pPos%d' % b, 'eoff'], ['sl'])
            kb.op('dve', lambda e: e.scalar_tensor_tensor(out=junk32[:], in0=sl[:], scalar=1.0, in1=m1[:], op0=ALU.mult, op1=ALU.mult, accum_out=sf[:, 0:1]), ['sl', 'm1'], ['junk32', 'sf0'])
            kb.op('dve', lambda e: e.scalar_tensor_tensor(out=junk32[:], in0=sl[:], scalar=1.0, in1=m2[:], op0=ALU.mult, op1=ALU.mult, accum_out=sf[:, 1:2]), ['sl', 'm2'], ['junk32', 'sf1'])
            kb.op('dve', lambda e: e.tensor_copy(out=slot_i[:, t, :], in_=sf[:]), ['sf0', 'sf1'], [('slot', t)])
            for k2 in range(2):
                T.op('pool', lambda e: e.indirect_dma_start(out=Xs_d[:, :], out_offset=bass.IndirectOffsetOnAxis(ap=slot_i[:, t, k2:k2 + 1], axis=0),
                                                            in_=h3[b][:], in_offset=None),
                     reads=['h3_%d' % b, ('slot', t)], writes=[('Xs', t, k2)], lane='p5c_sc%d_%d' % (b, k2))
    T.barrier()
    if stop_after == '5b':
        return finish(kb, es, es_all, out_d)

    with ExitStack() as es6:
        w1b = [sbuf(es6, "p6_w1_%d" % i, [128, 8, DEXP], BF16) for i in range(2)]
        w3b = [sbuf(es6, "p6_w3_%d" % i, [128, 8, DEXP], BF16) for i in range(2)]
        w2b = [sbuf(es6, "p6_w2_%d" % i, [128, 4, D], BF16) for i in range(2)]
        NXB = 2 * CHB
        xb = [sbuf(es6, "p6_xb%d" % i, [128, D], BF16) for i in range(NXB)]
        CHN = CHB * 128
        XT = [sbuf(es6, "p6_XT%d" % i, [128, 8, CHN], BF16) for i in range(2)]
        s1 = [sbuf(es6, "p6_s1_%d" % i, [128, CHN], BF16) for i in range(2)]
        gT6 = [sbuf(es6, "p6_gT%d" % i, [128, 4, CHN], BF16) for i in range(2)]
        ysb = [sbuf(es6, "p6_y%d" % i, [128, D], BF16) for i in range(2)]
        pT6 = [psum(es6, "p6_pT%d" % i, [128, 8, 128], BF16) for i in range(2)]
        p1 = [psum(es6, "p6_p1_%d" % i, [128, 512], F32) for i in range(2)]
        p3 = [psum(es6, "p6_p3_%d" % i, [128, 512], F32) for i in range(2)]
        py = [psum(es6, "p6_py%d" % i, [128, 512], F32) for i in range(2)]
        chunks = [(ex, hb) for ex in range(NEXP) for hb in range(CAPB // CHB)]
        cnt6 = {'mi': 0, 'yi': 0}

        def load_w(ex):
            ws = ex % 2
            kb.dma('pool', w1b[ws][:], w1_d[ex].rearrange("(c p) n -> p c n", p=128), [], ['w1_%d' % ws], 'p6_w1l%d' % ws)
            kb.dma('pool', w3b[ws][:], w3_d[ex].rearrange("(c p) n -> p c n", p=128), [], ['w3_%d' % ws], 'p6_w3l%d' % ws)
            kb.dma('pool', w2b[ws][:], w2_d[ex].rearrange("(c p) n -> p c n", p=128), [], ['w2_%d' % ws], 'p6_w2l%d' % ws)

        def load_x(i):
            ex, hb = chunks[i]
            row0 = ex * CAP + hb * CHN
            for j in range(CHB):
                xs_ = (i * CHB + j) % NXB
                kb.dma('sp', xb[xs_][:], Xs_d[row0 + j * 128:row0 + (j + 1) * 128, :], [], ['xb%d' % xs_], 'p6_xl%d' % xs_)

        def stage_T(i):
            cs = i % 2
            for j in range(CHB):
                xi_ = i * CHB + j
                xs_ = xi_ % NXB
                pt_ = xi_ % 2
                for c in range(8):
                    kb.op('pe', lambda e: e.transpose(out=pT6[pt_][:, c, :], in_=xb[xs_][:, c * 128:(c + 1) * 128], identity=ident_b[:]), ['xb%d' % xs_, 'ident_b'], ['ps:p6_pT%d' % pt_])
                if xi_ % 2 == 0:
                    kb.op('act', lambda e: e.copy(out=XT[cs][:, :, j * 128:(j + 1) * 128], in_=pT6[pt_][:]), ['ps:p6_pT%d' % pt_], ['XT%d_%d' % (cs, j)])
                else:
                    kb.op('dve', lambda e: e.tensor_copy(out=XT[cs][:, :, j * 128:(j + 1) * 128], in_=pT6[pt_][:]), ['ps:p6_pT%d' % pt_], ['XT%d_%d' % (cs, j)])

        def stage_A(i):
            ex, hb = chunks[i]
            ws = ex % 2
            cs = i % 2
            xtn = ['XT%d_%d' % (cs, j) for j in range(CHB)]
            for m in range(4):
                ms = cnt6['mi'] % 2
                for kc in range(8):
                    kb.op('pe', lambda e: e.matmul(p1[ms][:, 0:CHN], lhsT=w1b[ws][:, kc, m * 128:(m + 1) * 128], rhs=XT[cs][:, kc, :], start=(kc == 0), stop=(kc == 7)),
                          ['w1_%d' % ws] + xtn, ['ps:p6_p1_%d' % ms])
                for kc in range(8):
                    kb.op('pe', lambda e: e.matmul(p3[ms][:, 0:CHN], lhsT=w3b[ws][:, kc, m * 128:(m + 1) * 128], rhs=XT[cs][:, kc, :], start=(kc == 0), stop=(kc == 7)),
                          ['w3_%d' % ws] + xtn, ['ps:p6_p3_%d' % ms])
                kb.op('act', lambda e: e.activation(out=s1[ms][:], in_=p1[ms][:, 0:CHN], func=AF.Silu), ['ps:p6_p1_%d' % ms], ['s1_%d' % ms])
                kb.op('dve', lambda e: e.tensor_tensor(out=gT6[cs][:, m, :], in0=p3[ms][:, 0:CHN], in1=s1[ms][:], op=ALU.mult), ['ps:p6_p3_%d' % ms, 's1_%d' % ms], ['gT%d_%d' % (cs, m)])
                cnt6['mi'] += 1

        def stage_Y(i):
            ex, hb = chunks[i]
            ws = ex % 2
            cs = i % 2
            row0 = ex * CAP + hb * CHN
            gtn = ['gT%d_%d' % (cs, m) for m in range(4)]
            for j in range(CHB):
                ys_ = cnt6['yi'] % 2
                for hf in range(2):
                    for kc in range(4):
                        kb.op('pe', lambda e: e.matmul(py[hf][:], lhsT=gT6[cs][:, kc, j * 128:(j + 1) * 128], rhs=w2b[ws][:, kc, hf * 512:(hf + 1) * 512], start=(kc == 0), stop=(kc == 3)),
                              gtn + ['w2_%d' % ws], ['ps:p6_py%d' % hf])
                    if hf == 0:
                        kb.op('act', lambda e: e.copy(out=ysb[ys_][:, 0:512], in_=py[0][:]), ['ps:p6_py0'], ['ysb%d_0' % ys_])
                    else:
                        kb.op('dve', lambda e: e.tensor_copy(out=ysb[ys_][:, 512:1024], in_=py[1][:]), ['ps:p6_py1'], ['ysb%d_1' % ys_])
                kb.dma('sp', Y_d[row0 + j * 128:row0 + (j + 1) * 128, :], ysb[ys_][:], ['ysb%d_0' % ys_, 'ysb%d_1' % ys_], [('Y', ex, hb, j)], 'p6_st%d' % ys_)
                cnt6['yi'] += 1

        nch = len(chunks)
        load_w(0)
        load_x(0)
        stage_T(0)
        for i in range(nch):
            ex, hb = chunks[i]
            if hb == 0 and ex + 1 < NEXP:
                load_w(ex + 1)
            if i + 1 < nch:
                load_x(i + 1)
            stage_A(i)
            if i + 1 < nch:
                stage_T(i + 1)
            stage_Y(i)
    T.barrier()
    if stop_after == '6':
        return finish(kb, es, es_all, out_d)

    with ExitStack() as es7:
        gfb = sbuf(es7, "p7_gfb", [128, D], F32)
        kb.dma('sp', gfb[:], gfin_d.partition_broadcast(128), [], ['gfb'], 'p7_c0')
        x2t7 = [sbuf(es7, "p7_x2_%d" % i, [128, D], F32) for i in range(2)]
        y1 = [sbuf(es7, "p7_y1_%d" % i, [128, D], BF16) for i in range(2)]
        y2 = [sbuf(es7, "p7_y2_%d" % i, [128, D], BF16) for i in range(2)]
        x3 = [sbuf(es7, "p7_x3_%d" % i, [128, D], F32) for i in range(2)]
        junk7 = sbuf(es7, "p7_junk", [128, D], BF16)
        ssq7 = [sbuf(es7, "p7_ssq%d" % i, [128, 1], F32) for i in range(2)]
        o7 = [sbuf(es7, "p7_o%d" % i, [128, D], F32) for i in range(2)]
        for t in range(NT):
            b = t % 2
            tsl = slice(t * 128, (t + 1) * 128)
            kb.dma('sp', x2t7[b][:], x2_s[tsl, :], [], ['x2t%d' % b], 'p7_xl%d' % b)
            for k2, yy in ((0, y1), (1, y2)):
                T.op('pool', lambda e: e.indirect_dma_start(out=yy[b][:], out_offset=None, in_=Y_d[:, :],
                                                            in_offset=bass.IndirectOffsetOnAxis(ap=slot_i[:, t, k2:k2 + 1], axis=0)),
                     reads=[], writes=['y%d_%d' % (k2, b)], lane='p7_g%d_%d' % (k2, b))
            kb.op('dve', lambda e: e.scalar_tensor_tensor(out=x3[b][:], in0=y1[b][:], scalar=comb_w[:, t, 0:1], in1=x2t7[b][:], op0=ALU.mult, op1=ALU.add),
                  ['y0_%d' % b, 'x2t%d' % b], ['x3_%d' % b])
            kb.op('dve', lambda e: e.scalar_tensor_tensor(out=x3[b][:], in0=y2[b][:], scalar=comb_w[:, t, 1:2], in1=x3[b][:], op0=ALU.mult, op1=ALU.add),
                  ['y1_%d' % b, 'x3_%d' % b], ['x3_%d' % b])
            kb.op('act', lambda e: e.activation(out=junk7[:], in_=x3[b][:], func=AF.Square, accum_out=ssq7[b][:]), ['x3_%d' % b], ['junk7', 'ssq7_%d' % b])
            kb.op('act', lambda e: e.activation(out=ssq7[b][:], in_=ssq7[b][:], func=AF.Ln, scale=1.0 / D, bias=EPS), ['ssq7_%d' % b], ['ssq7_%d' % b])
            kb.op('act', lambda e: e.activation(out=ssq7[b][:], in_=ssq7[b][:], func=AF.Exp, scale=-0.5), ['ssq7_%d' % b], ['ssq7_%d' % b])
            kb.op('dve', lambda e: e.scalar_tensor_tensor(out=o7[b][:], in0=x3[b][:], scalar=ssq7[b][:, 0:1], in1=gfb[:], op0=ALU.mult, op1=ALU.mult),
                  ['x3_%d' % b, 'ssq7_%d' % b, 'gfb'], ['o7_%d' % b])
            kb.dma('sp', out_d[tsl, :], o7[b][:], ['o7_%d' % b], [('out', t)], 'p7_st%d' % b)
    return finish(kb, es, es_all, out_d)


def finish(kb, es, es_all, out_d):
    kb.T.barrier()
    return kb


def prep_inputs(inputs, b):
    f = lambda k: np.ascontiguousarray(np.asarray(inputs[k], dtype=np.float32))
    c = host_consts()
    m = {}
    m['x'] = f('x')[b]
    m['mem'] = f('mem')[b]
    perm = win_perm()
    m['w_in_ext'] = np.ascontiguousarray(f('w_in')[0][:, perm])
    m['w_out'] = f('w_out')[0]

    def g128(v):
        return v.reshape(8, 128).T
    m['gT'] = np.ascontiguousarray(np.concatenate([g128(f('norm_mix_g')[0]), g128(f('norm_x_g')[0]),
                                                   g128(f('norm_mem_g')[0]), g128(f('norm_ffn_g')[0])], axis=1))
    m['g_final'] = f('norm_final_g')
    m['g_ffn_row'] = f('norm_ffn_g')[0]
    kk = np.arange(128)[:, None]
    qq = np.arange(128)[None, :]
    m['tri_strict'] = (kk < qq).astype(np.float32)
    m['eoff'] = (np.arange(NEXP) * CAP).astype(np.float32)
    m['tokid'] = (np.arange(NT)[None, :] * 128 + np.arange(128)[:, None]).astype(np.int32)
    m['cosT'] = c['cosT']; m['sinT'] = c['sinT']; m['ident_f'] = c['ident_f']; m['maskb'] = c['maskb']; m['tri_incl'] = c['tri_incl']
    m['da_lambda'] = f('da_lambda')[0].reshape(256)
    m['da_subln_g'] = f('da_subln_g')[0]
    cw = f('ml_conv_w')[0][:, 0, :]
    cb = f('ml_conv_b')[0]
    convT = np.zeros((128, 40), np.float32)
    for gidx in range(8):
        cols = slice(gidx * 128, (gidx + 1) * 128)
        convT[:, gidx * 5:gidx * 5 + 4] = cw[:, cols].T
        convT[:, gidx * 5 + 4] = cb[cols]
    m['convT'] = convT
    m['ml_gate_b'] = f('ml_gate_b')[0].reshape(8)
    m['ml_norm_g'] = f('ml_norm_g')[0]
    for k in ('w_xq', 'w_xk', 'w_xv', 'w_xo'):
        m[k] = f(k)[0]
    m['w_router'] = np.ascontiguousarray(np.concatenate([f('w_router_group')[0], f('w_router_expert')[0]], axis=1))
    m['b_router'] = np.concatenate([f('b_router_group')[0], f('b_router_expert')[0]])
    m['w1'] = f('w1')[0]; m['w3'] = f('w3')[0]; m['w2'] = f('w2')[0]
    return m


_CACHE = {}


def kernel(**inputs):
    if 'kb' not in _CACHE:
        _CACHE['kb'] = build()
    kb = _CACHE['kb']
    n = 8
    maps = []
    for b in range(n):
        m = prep_inputs(inputs, b)
        maps.append({k: v for k, v in m.items() if k in kb.inp})
    res = run_bass_kernel_spmd(kb.nc, maps, core_ids=list(range(n)))
    return np.stack([np.asarray(r["out"], dtype=np.float32) for r in res.results], axis=0)
```

```python
import numpy as np
from contextlib import ExitStack
import concourse.bass as bass
import concourse.mybir as mybir
from concourse.bass_utils import run_bass_kernel_spmd

F32 = mybir.dt.float32
BF16 = mybir.dt.bfloat16
I32 = mybir.dt.int32
AF = mybir.ActivationFunctionType
ALU = mybir.AluOpType
AX = mybir.AxisListType

S = 4096
D = 1024
NT = S // 128
NB = S // 512
EPS = 1e-6
MEM = 256
NEXP = 32
DEXP = 512
CAPB = 6
CHB = 3
CAP = CAPB * 128
LAM_INIT = 0.2
NEG = -30000.0
STRICT = False

FM_COLS = 24 * 128
TM_COLS = 512 * 3 + 8
WIN_COLS = FM_COLS + TM_COLS


class Trk:
    def __init__(self, nc, needed=None):
        self.nc = nc
        self.needed_in = needed
        self.needed = {}
        self.phys = {}
        self.pcnt = {}
        self.eng = {'pe': nc.tensor, 'act': nc.scalar, 'dve': nc.vector, 'pool': nc.gpsimd, 'sp': nc.sync}
        self.sem = {}
        self.cnt = {}
        self.seen = {e: {} for e in self.eng}
        self.lastw = {}
        self.reads = {}
        self.stack = ExitStack()
        self.phase = 0
        self.nsem = 0
        self.pool = {'sw': [], 'hw': []}
        self.kind = {}

    def lane(self, name, eng='sp'):
        if name not in self.sem:
            kind = 'sw' if eng == 'pool' else 'hw'
            self.kind[name] = kind
            if self.pool[kind] and not name.startswith('eng_'):
                sh, c = self.pool[kind].pop()
                self.sem[name] = sh
                self.cnt[name] = c
            else:
                self.nsem += 1
                s = self.stack.enter_context(self.nc.semaphore("s%d" % self.nsem))
                self.sem[name] = s
                self.cnt[name] = 0
        return name

    def elane(self, eng):
        return "eng_%s" % eng

    def pval(self, ln, v):
        if not ln.startswith("eng_"):
            return v
        self.needed.setdefault(ln, set()).add(v)
        if self.needed_in is None:
            return v
        return self.phys[ln][v]

    def wait(self, eng, ticket):
        ln, v = ticket
        if self.seen[eng].get(ln, 0) < v:
            self.eng[eng].wait_ge(self.sem[ln], self.pval(ln, v))
            self.seen[eng][ln] = v

    def op(self, eng, fn, reads=(), writes=(), lane=None, inc=None, attach=None):
        deps = {}
        own = self.elane(eng)
        psr = [r for r in reads if isinstance(r, str) and r.startswith('ps:')]
        if psr:
            reads = [r for r in reads if r not in psr]
            writes = list(writes) + [r for r in psr if r not in writes]
        for r in reads:
            t = self.lastw.get(r)
            if t is not None:
                deps[t[0]] = max(deps.get(t[0], 0), t[1])
        for w in writes:
            t = self.lastw.get(w)
            if t is not None and (STRICT or t[0] != own):
                deps[t[0]] = max(deps.get(t[0], 0), t[1])
            for t in self.reads.get(w, ()):
                if STRICT or t[0] != own:
                    deps[t[0]] = max(deps.get(t[0], 0), t[1])
        for ln, v in deps.items():
            self.wait(eng, (ln, v))
        ins = fn(self.eng[eng])
        if attach is not None:
            t = self.lastw.get(attach)
            if t is not None:
                ins._wait_ge(self.sem[t[0]], self.pval(t[0], t[1]))
        if lane is None:
            lane = own
            inc = 1
        elif inc is None:
            inc = 16
        self.lane(lane, eng)
        self.cnt[lane] += inc
        t = (lane, self.cnt[lane])
        if lane.startswith("eng_"):
            if self.needed_in is None or t[1] in self.needed_in.get(lane, ()):
                ins.then_inc(self.sem[lane], 1)
                self.pcnt[lane] = self.pcnt.get(lane, 0) + 1
                self.phys.setdefault(lane, {})[t[1]] = self.pcnt[lane]
        else:
            ins.then_inc(self.sem[lane], inc)
        for r in reads:
            self.reads.setdefault(r, []).append(t)
        for w in writes:
            self.lastw[w] = t
            self.reads[w] = []
        return t

    def barrier(self):
        for e in self.eng:
            for ln, v in self.cnt.items():
                if v > 0:
                    self.wait(e, (ln, v))
        self.lastw = {}
        self.reads = {}
        self.phase += 1
        for ln in list(self.sem.keys()):
            if not ln.startswith("eng_"):
                self.pool[self.kind.pop(ln)].append((self.sem.pop(ln), self.cnt.pop(ln)))
                for e in self.eng:
                    self.seen[e].pop(ln, None)


def host_consts():
    c = {}
    c['ident_f'] = np.eye(128, dtype=np.float32)
    k = np.arange(128)[:, None]
    q = np.arange(128)[None, :]
    c['maskb'] = np.where(k <= q, 0.0, NEG).astype(np.float32)
    c['tri_incl'] = (k <= q).astype(np.float32)
    inv_freq = (10000.0 ** (-np.arange(0, 64, 2, dtype=np.float32) / np.float32(64))).astype(np.float32)
    pos = np.arange(S, dtype=np.float32)
    ang = (pos[:, None] * inv_freq[None, :]).astype(np.float32)
    cs = np.cos(ang).astype(np.float32).T
    sn = np.sin(ang).astype(np.float32).T
    cosT = np.zeros((128, S), np.float32)
    sinT = np.zeros((128, S), np.float32)
    for p in range(128):
        d = p % 64
        cosT[p] = cs[d % 32]
        sinT[p] = -sn[d % 32] if d < 32 else sn[d % 32]
    c['cosT'] = cosT
    c['sinT'] = sinT
    return c


def win_perm():
    idx = []
    off_q, off_k, off_v = 0, 512, 1024
    off_mq, off_mk, off_mv, off_mo, off_mi, off_mf = 1536, 2048, 2560, 3072, 3584, 3588

    def sw(base):
        out = []
        for m in range(2):
            b = base + m * 64
            out += list(range(b + 32, b + 64)) + list(range(b, b + 32))
        return out
    for h in range(4):
        idx += list(range(off_q + h * 128, off_q + (h + 1) * 128))
        idx += sw(off_q + h * 128)
        idx += list(range(off_k + h * 128, off_k + (h + 1) * 128))
        idx += sw(off_k + h * 128)
    idx += list(range(off_mq, off_mq + 512))
    idx += list(range(off_mk, off_mk + 512))
    idx += list(range(off_v, off_v + 512))
    idx += list(range(off_mv, off_mv + 512))
    idx += list(range(off_mo, off_mo + 512))
    idx += list(range(off_mi, off_mi + 4)) + list(range(off_mf, off_mf + 4))
    assert len(idx) == WIN_COLS
    return np.array(idx)


class K:
    def __init__(self, debug=None, needed=None):
        self.debug = debug or ()
        nc = bass.Bass("TRN2", target_bir_lowering=False)
        self.nc = nc
        self.T = Trk(nc, needed)
        self.inp = {}
        self.scr = {}
        self.dmaq = 0

    def din(self, name, shape, dt=F32):
        kb = self

        class Lazy:
            def _get(s_):
                if name not in kb.inp:
                    kb.inp[name] = kb.nc.dram_tensor(name, list(shape), dt, kind="ExternalInput").ap()
                return kb.inp[name]

            def __getitem__(s_, k):
                return s_._get()[k]

            def __getattr__(s_, a):
                return getattr(s_._get(), a)
        return Lazy()

    def dscr(self, name, shape, dt):
        kind = "ExternalOutput" if name in self.debug else "Internal"
        self.scr[name] = self.nc.dram_tensor(name, list(shape), dt, kind=kind).ap()
        return self.scr[name]

    def dma(self, eng, out, in_, reads, writes, lane, **kw):
        return self.T.op(eng, lambda e: e.dma_start(out=out, in_=in_, **kw), reads=reads, writes=writes, lane=lane)

    def op(self, eng, fn, reads=(), writes=(), attach=None):
        if eng == 'pe' and attach is None and reads:
            attach = reads[0]
        return self.T.op(eng, fn, reads=reads, writes=writes, attach=attach)


def build(debug=None, stop_after=None, skip12=False, p3_tiles=NT):
    rec = _build(debug, stop_after, skip12, p3_tiles, None)
    return _build(debug, stop_after, skip12, p3_tiles, rec.T.needed)


def _build(debug, stop_after, skip12, p3_tiles, needed):
    kb = K(debug, needed)
    nc, T = kb.nc, kb.T
    din, dscr = kb.din, kb.dscr
    x_d = din("x", [S, D])
    mem_d = din("mem", [MEM, D])
    win_d = din("w_in_ext", [D, WIN_COLS])
    wout_d = din("w_out", [D, D])
    gT_d = din("gT", [128, 4 * 8])
    gfin_d = din("g_final", [D])
    gffn_d = din("g_ffn_row", [D])
    tris_d = din("tri_strict", [128, 128])
    eoff_d = din("eoff", [NEXP])
    tokid_d = din("tokid", [128, NT], I32)
    cos_d = din("cosT", [128, S])
    sin_d = din("sinT", [128, S])
    ident_d = din("ident_f", [128, 128])
    maskb_d = din("maskb", [128, 128])
    tri_d = din("tri_incl", [128, 128])
    lam_d = din("da_lambda", [256])
    subln_d = din("da_subln_g", [128])
    convw_d = din("convT", [128, 8 * 5])
    gateb_d = din("ml_gate_b", [8])
    mlg_d = din("ml_norm_g", [512])
    wxq_d = din("w_xq", [D, D]); wxk_d = din("w_xk", [D, D]); wxv_d = din("w_xv", [D, D]); wxo_d = din("w_xo", [D, D])
    wr_d = din("w_router", [D, 36])
    br_d = din("b_router", [36])
    w1_d = din("w1", [NEXP, D, DEXP]); w3_d = din("w3", [NEXP, D, DEXP]); w2_d = din("w2", [NEXP, DEXP, D])
    out_d = nc.dram_tensor("out", [S, D], F32, kind="ExternalOutput").ap()

    qT_s = dscr("qT_s", [4, 128, S], BF16)
    kT_s = dscr("kT_s", [4, 128, S], BF16)
    mqT_s = dscr("mqT_s", [4, 128, S], BF16)
    mkT_s = dscr("mkT_s", [4, 128, S], BF16)
    tmb_s = dscr("tmb_s", [S, 1536], BF16)
    gate_s = dscr("gate_s", [128, NT, 8], F32)
    y_s = dscr("y_s", [S, D], BF16)
    x1_s = dscr("x1_s", [S, D], F32)
    x2_s = dscr("x2_s", [S, D], F32)
    Xs_d = dscr("Xs_d", [NEXP * CAP, D], BF16)
    Y_d = dscr("Y_d", [NEXP * CAP, D], BF16)

    es_all = ExitStack()

    def sbuf(es, name, shape, dt):
        return es.enter_context(nc.sbuf_tensor("sb_" + name, list(shape), dt))

    def psum(es, name, shape, dt):
        return es.enter_context(nc.psum_tensor("ps_" + name, list(shape), dt))

    ident_f = sbuf(es_all, "ident_f", [128, 128], F32)
    ident_b = sbuf(es_all, "ident_b", [128, 128], BF16)
    gT = sbuf(es_all, "gT", [128, 32], F32)
    kb.dma('sp', ident_f[:], ident_d[:, :], [], ['ident_f'], 'c0')
    kb.dma('sp', gT[:], gT_d[:, :], [], ['gT'], 'c1')
    kb.dma('pool', ident_b[:], ident_d[:, :], [], ['ident_b'], 'c2')
    slot_i = sbuf(es_all, "slot_i", [128, NT, 2], I32)
    comb_w = sbuf(es_all, "comb_w", [128, NT, 2], F32)
    ones_bb = sbuf(es_all, "ones_bb", [128, 128], BF16)
    kb.op('pool', lambda e: e.memset(ones_bb[:], 1.0), [], ['ones_bb'])
    es = ExitStack()
    if True:
        zt = sbuf(es, "zt", [128, 8192], BF16)
        kb.op('pool', lambda e: e.memset(zt[:], 0.0), [], ['zt'])
        for e_ in range(NEXP):
            kb.dma('pool', Xs_d[e_ * CAP:(e_ + 1) * CAP, :].rearrange("(p r) c -> p (r c)", p=128), zt[:, 0:CAP * D // 128], ['zt'], [('Xs0', e_)], 'zX%d' % (e_ % 4))

    hT = sbuf(es, "hT", [128, 8, S], BF16)
    cosT = sbuf(es, "cosT", [128, S], F32)
    sinT = sbuf(es, "sinT", [128, S], F32)
    convT = sbuf(es, "convT", [128, 40], F32)
    kb.dma('sp', cosT[:], cos_d[:, :], [], ['cosT'], 'c3')
    kb.dma('sp', sinT[:], sin_d[:, :], [], ['sinT'], 'c4')
    kb.dma('sp', convT[:], convw_d[:, :], [], ['convT'], 'c5')

    def norm_transpose(es_, tagp, x_src_tiles, gcol, dstT, ntiles, pT_tiles, dst_tag='hT_t', sb_src=None, emit_only=False):
        xt = [sbuf(es_, "%s_xt%d" % (tagp, i), [128, D], F32) for i in range(3)] if sb_src is None else None
        junk = sbuf(es_, tagp + "_junk", [128, D], BF16)
        ssq = [sbuf(es_, "%s_ssq%d" % (tagp, i), [128, 1], F32) for i in range(2)]
        rstd = [sbuf(es_, "%s_rstd%d" % (tagp, i), [128, 1], F32) for i in range(2)]
        xs = [sbuf(es_, "%s_xs%d" % (tagp, i), [128, D], BF16) for i in range(2)]
        def emit(t):
            a, b = t % 3, t % 2
            if sb_src is None:
                kb.dma('sp', xt[a][:], x_src_tiles(t), [], [tagp + 'xt%d' % a], tagp + 'ld%d' % a)
                x_ap, x_res = xt[a][:], [tagp + 'xt%d' % a]
            else:
                x_ap, x_res = sb_src(t)
            kb.op('act', lambda e: e.activation(out=junk[:], in_=x_ap, func=AF.Square, accum_out=ssq[b][:]),
                  x_res, [tagp + 'junk', tagp + 'ssq%d' % b])
            kb.op('act', lambda e: e.activation(out=rstd[b][:], in_=ssq[b][:], func=AF.Sqrt, scale=1.0 / D, bias=EPS),
                  [tagp + 'ssq%d' % b], [tagp + 'rstd%d' % b])
            kb.op('dve', lambda e: e.reciprocal(out=rstd[b][:], in_=rstd[b][:]),
                  [tagp + 'rstd%d' % b], [tagp + 'rstd%d' % b])
            kb.op('dve', lambda e: e.tensor_scalar(out=xs[b][:], in0=x_ap, scalar1=rstd[b][:], scalar2=None, op0=ALU.mult),
                  x_res + [tagp + 'rstd%d' % b], [tagp + 'xs%d' % b])
            pT = pT_tiles[b]
            for c in range(8):
                kb.op('pe', lambda e: e.transpose(out=pT[:, c, :], in_=xs[b][:, c * 128:(c + 1) * 128], identity=ident_b[:]),
                      [tagp + 'xs%d' % b, 'ident_b'], ['ps:' + tagp + 'pT%d' % b])
            kb.op('dve', lambda e: e.tensor_tensor(out=dstT[:, :, t * 128:(t + 1) * 128], in0=pT[:],
                                                    in1=gT[:, gcol * 8:(gcol + 1) * 8].unsqueeze(2).to_broadcast([128, 8, 128]),
                                                    op=ALU.mult),
                  ['ps:' + tagp + 'pT%d' % b, 'gT'], [dst_tag + '%d' % t])

        if emit_only:
            return emit
        for t in range(ntiles):
            emit(t)

    NT1 = 0 if skip12 else NT
    NB1 = 0 if skip12 else NB
    with ExitStack() as es1a:
        pTt = [psum(es1a, "p1a_pT%d" % i, [128, 8, 128], BF16) for i in range(2)]
        norm_transpose(es1a, "p1a", lambda t: x_d[t * 128:(t + 1) * 128, :], 0, hT, NT1, pTt)
    T.barrier()

    with ExitStack() as es1b:
        wq = [sbuf(es1b, "p1b_w%d" % i, [128, 8, 256], BF16) for i in range(2)]
        pq = [psum(es1b, "p1b_pq%d" % i, [128, 512], F32) for i in range(4)]
        r1 = [sbuf(es1b, "p1b_r1_%d" % i, [128, 512], F32) for i in range(2)]
        r2 = [sbuf(es1b, "p1b_r2_%d" % i, [128, 512], F32) for i in range(2)]
        ro = [sbuf(es1b, "p1b_ro%d" % i, [128, 512], BF16) for i in range(2)]
        it = 0
        for pair in range(0 if skip12 else 8):
            h, isk = pair // 2, pair % 2
            wbuf = wq[pair % 2]
            c0 = pair * 256
            kb.dma('pool', wbuf[:], win_d[:, c0:c0 + 256].rearrange("(c p) n -> p c n", p=128), [], ['p1b_w%d' % (pair % 2)], 'p1b_wl%d' % (pair % 2))
            dst = (kT_s if isk else qT_s)
            for blk in range(NB):
                pa, pb = pq[(it % 2) * 2], pq[(it % 2) * 2 + 1]
                na, nb_ = 'ps:p1b_pq%d' % ((it % 2) * 2), 'ps:p1b_pq%d' % ((it % 2) * 2 + 1)
                for kc in range(8):
                    kb.op('pe', lambda e: e.matmul(pa[:], lhsT=wbuf[:, kc, 0:128], rhs=hT[:, kc, blk * 512:(blk + 1) * 512], start=(kc == 0), stop=(kc == 7)),
                          ['p1b_w%d' % (pair % 2)] + ['hT_t%d' % (blk * 4 + j) for j in range(4)], [na])
                for kc in range(8):
                    kb.op('pe', lambda e: e.matmul(pb[:], lhsT=wbuf[:, kc, 128:256], rhs=hT[:, kc, blk * 512:(blk + 1) * 512], start=(kc == 0), stop=(kc == 7)),
                          ['p1b_w%d' % (pair % 2)] + ['hT_t%d' % (blk * 4 + j) for j in range(4)], [nb_])
                s_ = it % 2
                kb.op('dve', lambda e: e.tensor_tensor(out=r1[s_][:], in0=pa[:], in1=cosT[:, blk * 512:(blk + 1) * 512], op=ALU.mult),
                      [na, 'cosT'], ['p1b_r1_%d' % s_])
                kb.op('dve', lambda e: e.tensor_tensor(out=r2[s_][:], in0=pb[:], in1=sinT[:, blk * 512:(blk + 1) * 512], op=ALU.mult),
                      [nb_, 'sinT'], ['p1b_r2_%d' % s_])
                kb.op('pool', lambda e: e.tensor_tensor(out=ro[s_][:], in0=r1[s_][:], in1=r2[s_][:], op=ALU.add),
                      ['p1b_r1_%d' % s_, 'p1b_r2_%d' % s_], ['p1b_ro%d' % s_])
                kb.dma('sp', dst[h, :, blk * 512:(blk + 1) * 512], ro[s_][:], ['p1b_ro%d' % s_], [('qk', pair, blk)], 'p1b_st%d' % s_)
                it += 1

    if stop_after == '1b':
        return finish(kb, es, es_all, out_d)
    T.barrier()
    with ExitStack() as es1c:
        wm = [sbuf(es1c, "p1c_w%d" % i, [128, 8, 128], BF16) for i in range(2)]
        pm = [psum(es1c, "p1c_pm%d" % i, [128, 512], F32) for i in range(2)]
        ub = [sbuf(es1c, "p1c_ub%d" % i, [128, 515], F32) for i in range(2)]
        ca = [sbuf(es1c, "p1c_ca%d" % i, [128, 512], F32) for i in range(2)]
        co = [sbuf(es1c, "p1c_co%d" % i, [128, 512], BF16) for i in range(2)]
        it = 0
        for g in range(0 if skip12 else 8):
            wbuf = wm[g % 2]
            wn = 'p1c_w%d' % (g % 2)
            c0 = 16 * 128 + g * 128
            kb.dma('pool', wbuf[:], win_d[:, c0:c0 + 128].rearrange("(c p) n -> p c n", p=128), [], [wn], 'p1c_wl%d' % (g % 2))
            dst = (mqT_s if g < 4 else mkT_s)
            h = g % 4
            cw = lambda i: convT[:, g * 5 + i:g * 5 + i + 1]
            for blk in range(NB):
                s_ = it % 2
                p_, pn = pm[s_], 'ps:p1c_pm%d' % s_
                u_, un = ub[s_], 'p1c_ub%d' % s_
                for kc in range(8):
                    kb.op('pe', lambda e: e.matmul(p_[:], lhsT=wbuf[:, kc, :], rhs=hT[:, kc, blk * 512:(blk + 1) * 512], start=(kc == 0), stop=(kc == 7)),
                          [wn], [pn])
                if blk == 0:
                    kb.op('pool', lambda e: e.memset(u_[:, 0:3], 0.0), [], [un + 'h'])
                else:
                    up = ub[1 - s_]
                    kb.op('act', lambda e: e.copy(out=u_[:, 0:3], in_=up[:, 512:515]), ['p1c_ub%d' % (1 - s_)], [un + 'h'])
                kb.op('act', lambda e: e.copy(out=u_[:, 3:515], in_=p_[:]), [pn], [un])
                a_, an = ca[s_], 'p1c_ca%d' % s_
                kb.op('dve', lambda e: e.tensor_scalar(out=a_[:], in0=u_[:, 0:512], scalar1=cw(0), scalar2=None, op0=ALU.mult),
                      [un, un + 'h', 'convT'], [an])
                for i in (1, 2, 3):
                    kb.op('dve', lambda e: e.scalar_tensor_tensor(out=a_[:], in0=u_[:, i:i + 512], scalar=cw(i), in1=a_[:], op0=ALU.mult, op1=ALU.add),
                          [un, un + 'h', an, 'convT'], [an])
                o_, on = co[s_], 'p1c_co%d' % s_
                kb.op('act', lambda e: e.activation(out=o_[:], in_=a_[:], func=AF.Silu, bias=cw(4)), [an, 'convT'], [on])
                kb.dma('sp', dst[h, :, blk * 512:(blk + 1) * 512], o_[:], [on], [('mqk', g, blk)], 'p1c_st%d' % s_)
                it += 1
    T.barrier()
    with ExitStack() as es1d:
        wt = sbuf(es1d, "p1d_w", [128, 8, TM_COLS], BF16)
        for kc in range(8):
            kb.dma('pool', wt[:, kc, :], win_d[kc * 128:(kc + 1) * 128, FM_COLS:WIN_COLS], [], ['p1d_w'], 'p1d_wl')
        pt = [[psum(es1d, "p1d_p%d_%d" % (i, j), [128, 512], F32) for j in range(4)] for i in range(2)]
        ot = [sbuf(es1d, "p1d_o%d" % i, [128, 1536], BF16) for i in range(2)]
        og = [sbuf(es1d, "p1d_g%d" % i, [128, 8], F32) for i in range(2)]
        for t in range(NT1):
            s_ = t % 2
            for j in range(4):
                n0, n1 = (j * 512, (j + 1) * 512) if j < 3 else (1536, 1544)
                for kc in range(8):
                    kb.op('pe', lambda e: e.matmul(pt[s_][j][:, 0:n1 - n0], lhsT=hT[:, kc, t * 128:(t + 1) * 128], rhs=wt[:, kc, n0:n1], start=(kc == 0), stop=(kc == 7)),
                          ['p1d_w'], ['ps:p1d_p%d_%d' % (s_, j)], attach='p1d_w')
            for j in range(3):
                eng = 'act' if j != 1 else 'dve'
                if eng == 'act':
                    kb.op('act', lambda e: e.copy(out=ot[s_][:, j * 512:(j + 1) * 512], in_=pt[s_][j][:]), ['ps:p1d_p%d_%d' % (s_, j)], ['p1d_o%d_%d' % (s_, j)])
                else:
                    kb.op('dve', lambda e: e.tensor_copy(out=ot[s_][:, j * 512:(j + 1) * 512], in_=pt[s_][j][:]), ['ps:p1d_p%d_%d' % (s_, j)], ['p1d_o%d_%d' % (s_, j)])
            kb.op('dve', lambda e: e.tensor_copy(out=og[s_][:], in_=pt[s_][3][:, 0:8]), ['ps:p1d_p%d_3' % s_], ['p1d_g%d' % s_])
            kb.dma('sp', tmb_s[t * 128:(t + 1) * 128, :], ot[s_][:], ['p1d_o%d_%d' % (s_, j) for j in range(3)], [('tmb', t)], 'p1d_st%d' % s_)
            kb.dma('sp', gate_s[:, t, :], og[s_][:], ['p1d_g%d' % s_], [('gate', t)], 'p1d_sg%d' % s_)
    es.close()
    T.barrier()
    if stop_after == '1':
        return finish(kb, es, es_all, out_d)

    with ExitStack() as es2:
        lamb = sbuf(es2, "p2_lamb", [128, 256], F32)
        lj = sbuf(es2, "p2_lj", [128, 64], F32)
        ls = sbuf(es2, "p2_ls", [128, 2], F32)
        nlam = sbuf(es2, "p2_nlam", [128, 1], F32)
        sg = sbuf(es2, "p2_sg", [128, 128], F32)
        maskf = sbuf(es2, "p2_maskf", [128, 128], F32)
        maskb = sbuf(es2, "p2_maskb", [128, 128], BF16)
        kb.dma('sp', lamb[:], lam_d.partition_broadcast(128), [], ['lamb'], 'p2_c0')
        kb.dma('sp', sg[:], subln_d.partition_broadcast(128), [], ['sg'], 'p2_c1')
        kb.dma('sp', maskf[:], maskb_d[:, :], [], ['maskf'], 'p2_c2')
        kb.op('dve', lambda e: e.tensor_copy(out=maskb[:], in_=maskf[:]), ['maskf'], ['maskb'])
        for i in range(2):
            kb.op('dve', lambda e: e.scalar_tensor_tensor(out=lj[:], in0=lamb[:, i * 128:i * 128 + 64], scalar=1.0, in1=lamb[:, i * 128 + 64:i * 128 + 128],
                                                          op0=ALU.mult, op1=ALU.mult, accum_out=ls[:, i:i + 1]), ['lamb'], ['lj', 'ls'])
        kb.op('act', lambda e: e.activation(out=ls[:], in_=ls[:], func=AF.Exp), ['ls'], ['ls'])
        kb.op('dve', lambda e: e.tensor_tensor(out=nlam[:], in0=ls[:, 1:2], in1=ls[:, 0:1], op=ALU.subtract), ['ls'], ['nlam'])
        kb.op('dve', lambda e: e.tensor_scalar(out=nlam[:], in0=nlam[:], scalar1=-LAM_INIT, scalar2=None, op0=ALU.add), ['nlam'], ['nlam'])
        kb.op('dve', lambda e: e.tensor_scalar(out=sg[:], in0=sg[:], scalar1=1.0 - LAM_INIT, scalar2=None, op0=ALU.mult), ['sg'], ['sg'])

        kTh = [sbuf(es2, "p2_kT%d" % i, [128, S], BF16) for i in range(2)]
        Vh = [sbuf(es2, "p2_V%d" % i, [128, NT, 129], BF16) for i in range(2)]
        qTb = [sbuf(es2, "p2_q%d" % i, [128, 512], BF16) for i in range(2)]
        PT = [sbuf(es2, "p2_PT%d" % i, [128, 512], BF16) for i in range(3)]
        ps_s = [psum(es2, "p2_ps%d" % i, [128, 512], F32) for i in range(2)]
        accA_b = [psum(es2, "p2_accA%d" % i, [128, 512], F32) for i in range(2)]
        accB_b = [psum(es2, "p2_accB%d" % i, [128, 512], F32) for i in range(2)]
        accA = [b_[:, 0:387].rearrange("p (q c) -> p q c", c=129) for b_ in accA_b]
        accB = [b_[:, 0:129].rearrange("p (q c) -> p q c", c=129) for b_ in accB_b]
        rz = sbuf(es2, "p2_rz", [128, 2, 4], F32)
        o0 = [sbuf(es2, "p2_o0_%d" % i, [128, 4, 128], F32) for i in range(2)]
        oo = [sbuf(es2, "p2_oo_%d" % i, [128, 4, 128], F32) for i in range(2)]
        junk = sbuf(es2, "p2_junk", [128, 128], F32)
        ss = sbuf(es2, "p2_ss", [128, 4], F32)
        yo = [sbuf(es2, "p2_yo%d" % i, [128, 4, 128], BF16) for i in range(2)]
        for i in range(2):
            kb.op('pool', lambda e: e.memset(Vh[i][:, :, 128:129], 1.0), [], ['V%dones' % i])
        sc_it = 0
        hq = 0
        for h in range(0 if skip12 else 4):
            hs = h % 2
            kb.dma('sp', kTh[hs][:], kT_s[h, :, :], [], ['kT%d' % hs], 'p2_kl%d' % hs)
            kb.dma('sp', Vh[hs][:, :, 0:128], tmb_s[:, h * 128:(h + 1) * 128].rearrange("(t p) c -> p t c", p=128), [], ['V%d' % hs], 'p2_vl%d' % hs)
            for qb in range(NB):
                qs_ = hq % 2
                kb.dma('sp', qTb[qs_][:], qT_s[h, :, qb * 512:(qb + 1) * 512], [], ['q%d' % qs_], 'p2_ql%d' % qs_)
                nkt = 4 * qb + 4
                for m in range(2):
                    a_i = (hq * 2 + m) % 2
                    aA, aB = accA[a_i], accB[a_i]
                    an = 'ps:acc%d' % a_i
                    rows = slice(m * 64, (m + 1) * 64)
                    state = {'startedA': False}

                    def emit_scores(kt, sc):
                        j = kt - 4 * qb
                        c0 = max(j, 0) * 128
                        p_s, pn = ps_s[sc % 2], 'ps:s%d' % (sc % 2)
                        P_, Pn = PT[sc % 3], 'PT%d' % (sc % 3)
                        kb.op('pe', lambda e: e.matmul(p_s[:, c0:512], lhsT=kTh[hs][rows, kt * 128:(kt + 1) * 128], rhs=qTb[qs_][rows, c0:512],
                                                        start=True, stop=(j < 0)), ['kT%d' % hs, 'q%d' % qs_], [pn])
                        if j >= 0:
                            kb.op('pe', lambda e: e.matmul(p_s[:, c0:c0 + 128], lhsT=ident_b[:], rhs=maskb[:], start=False, stop=True),
                                  ['ident_b', 'maskb'], [pn])
                        kb.op('act', lambda e: e.activation(out=P_[:, c0:512], in_=p_s[:, c0:512], func=AF.Exp, scale=0.125), [pn], [Pn])

                    def emit_pv(kt, sc):
                        j = kt - 4 * qb
                        P_, Pn = PT[sc % 3], 'PT%d' % (sc % 3)
                        for qs in range(max(j, 0), 4):
                            last = (kt == 4 * qb + qs)
                            if qs < 3:
                                o_ap = aA[:, qs, :]
                                st = not state['startedA']
                                state['startedA'] = True
                            else:
                                o_ap = aB[:, 0, :]
                                st = (kt == 0)
                            kb.op('pe', lambda e: e.matmul(o_ap, lhsT=P_[:, qs * 128:(qs + 1) * 128], rhs=Vh[hs][:, kt, :], start=st, stop=last, skip_group_check=True),
                                  [Pn, 'V%d' % hs, 'V%dones' % hs], [an])

                    prev = None
                    for kt in range(nkt):
                        emit_scores(kt, sc_it)
                        if prev is not None:
                            emit_pv(*prev)
                        prev = (kt, sc_it)
                        sc_it += 1
                    emit_pv(*prev)
                    pq_ = hq % 2
                    kb.op('dve', lambda e: e.reciprocal(out=rz[:, m, 0:3], in_=aA[:, :, 128]), [an], ['rz%d' % m])
                    kb.op('dve', lambda e: e.reciprocal(out=rz[:, m, 3:4], in_=aB[:, :, 128]), [an], ['rz%d' % m])
                    if m == 0:
                        for qs in range(4):
                            src = aA[:, qs, 0:128] if qs < 3 else aB[:, 0, 0:128]
                            kb.op('dve', lambda e: e.tensor_scalar(out=o0[pq_][:, qs, :], in0=src, scalar1=rz[:, 0, qs:qs + 1], scalar2=None, op0=ALU.mult),
                                  [an, 'rz0'], ['o0_%d' % pq_])
                    else:
                        kb.op('dve', lambda e: e.tensor_scalar(out=rz[:, 1, :], in0=rz[:, 1, :], scalar1=nlam[:, 0:1], scalar2=None, op0=ALU.mult),
                              ['rz1', 'nlam'], ['rz1'])
                        for qs in range(4):
                            src = aA[:, qs, 0:128] if qs < 3 else aB[:, 0, 0:128]
                            kb.op('dve', lambda e: e.scalar_tensor_tensor(out=oo[pq_][:, qs, :], in0=src, scalar=rz[:, 1, qs:qs + 1], in1=o0[pq_][:, qs, :],
                                                                          op0=ALU.mult, op1=ALU.add), [an, 'rz1', 'o0_%d' % pq_], ['oo_%d' % pq_])
                pq_ = hq % 2
                for qs in range(4):
                    kb.op('dve', lambda e: e.scalar_tensor_tensor(out=junk[:], in0=oo[pq_][:, qs, :], scalar=1.0, in1=oo[pq_][:, qs, :], op0=ALU.mult, op1=ALU.mult,
                                                                  accum_out=ss[:, qs:qs + 1]), ['oo_%d' % pq_], ['junk', 'ss'])
                kb.op('act', lambda e: e.activation(out=ss[:], in_=ss[:], func=AF.Ln, scale=1.0 / 128, bias=EPS), ['ss'], ['ss'])
                kb.op('act', lambda e: e.activation(out=ss[:], in_=ss[:], func=AF.Exp, scale=-0.5), ['ss'], ['ss'])
                for qs in range(4):
                    kb.op('dve', lambda e: e.scalar_tensor_tensor(out=yo[pq_][:, qs, :], in0=oo[pq_][:, qs, :], scalar=ss[:, qs:qs + 1], in1=sg[:], op0=ALU.mult, op1=ALU.mult),
                          ['oo_%d' % pq_, 'ss', 'sg'], ['yo%d' % pq_])
                kb.dma('sp', y_s[qb * 512:(qb + 1) * 512, h * 128:(h + 1) * 128].rearrange("(qs p) c -> p qs c", p=128), yo[pq_][:],
                       ['yo%d' % pq_], [('yda', h, qb)], 'p2_st%d' % pq_)
                hq += 1
    T.barrier()
    if stop_after == '2':
        return finish(kb, es, es_all, out_d)

    with ExitStack() as es3:
        DH = 128
        KS = float(DH) ** -0.5
        tri = sbuf(es3, "p3_tri", [128, 128], F32)
        maskf4 = sbuf(es3, "p3_maskf4", [128, 4, 128], F32)
        ones_f = sbuf(es3, "p3_ones", [128, 128], F32)
        gb = sbuf(es3, "p3_gb", [128, 8], F32)
        mlg = sbuf(es3, "p3_mlg", [128, 512], F32)
        G = sbuf(es3, "p3_G", [128, NT, 8], F32)
        ig = sbuf(es3, "p3_ig", [128, NT, 4], F32)
        lf = sbuf(es3, "p3_lf", [128, NT, 4], F32)
        bcol = sbuf(es3, "p3_bcol", [128, NT, 4], F32)
        ea = sbuf(es3, "p3_ea", [128, NT, 4], F32)
        wcol = sbuf(es3, "p3_wcol", [128, NT, 4], F32)
        eaL = sbuf(es3, "p3_eaL", [128, NT, 4], F32)
        mqT = sbuf(es3, "p3_mqT", [128, 4, S], BF16)
        mkT = sbuf(es3, "p3_mkT", [128, 4, S], BF16)
        MV = sbuf(es3, "p3_MV", [128, NT, 4, 129], BF16)
        kb.dma('sp', tri[:], tri_d[:, :], [], ['tri'], 'p3_c0')
        for hh in range(4):
            kb.dma('sp', maskf4[:, hh, :], maskb_d[:, :], [], ['maskf4'], 'p3_c1')
        kb.dma('sp', gb[:], gateb_d.partition_broadcast(128), [], ['gb'], 'p3_c2')
        kb.dma('sp', mlg[:], mlg_d.partition_broadcast(128), [], ['mlg'], 'p3_c3')
        kb.dma('sp', G[:], gate_s, [], ['G'], 'p3_c4')
        for hh in range(4):
            kb.dma('sp', mqT[:, hh, :], mqT_s[hh, :, :], [], ['mqT'], 'p3_c5')
            kb.dma('sp', mkT[:, hh, :], mkT_s[hh, :, :], [], ['mkT'], 'p3_c6')
        kb.op('pool', lambda e: e.memset(MV[:, :, :, 128:129], 1.0), [], ['MVones'])
        for hh in range(4):
            kb.dma('sp', MV[:, :, hh, 0:128], tmb_s[:, 512 + hh * 128:512 + (hh + 1) * 128].rearrange("(t p) c -> p t c", p=128), [], ['MV'], 'p3_c7')
        kb.op('pool', lambda e: e.memset(ones_f[:], 1.0), [], ['ones_f'])
        if stop_after == '3a':
            T.barrier()
            return finish(kb, es, es_all, out_d)
        kb.op('dve', lambda e: e.tensor_tensor(out=ig[:], in0=G[:, :, 0:4], in1=gb[:, 0:4].unsqueeze(1).to_broadcast([128, NT, 4]), op=ALU.add), ['G', 'gb'], ['ig'])
        kb.op('dve', lambda e: e.tensor_tensor(out=lf[:], in0=G[:, :, 4:8], in1=gb[:, 4:8].unsqueeze(1).to_broadcast([128, NT, 4]), op=ALU.add), ['G', 'gb'], ['lf'])
        kb.op('act', lambda e: e.activation(out=lf[:], in_=lf[:], func=AF.Exp, scale=-1.0), ['lf'], ['lf'])
        kb.op('act', lambda e: e.activation(out=lf[:], in_=lf[:], func=AF.Ln, bias=1.0), ['lf'], ['lf'])
        kb.op('dve', lambda e: e.tensor_scalar(out=lf[:], in0=lf[:], scalar1=-1.0, scalar2=None, op0=ALU.mult), ['lf'], ['lf'])
        if stop_after == '3b':
            T.barrier()
            return finish(kb, es, es_all, out_d)
        pg_b = psum(es3, "p3_pg", [128, 512], F32)
        pg = pg_b[:, 0:256].rearrange("p (a b) -> p a b", a=2)
        tri_b = sbuf(es3, "p3_tri_b", [128, 128], BF16)
        ones_b = sbuf(es3, "p3_ones_b", [128, 128], BF16)
        maskb4 = sbuf(es3, "p3_maskb4", [128, 4, 128], BF16)
        lf_hi = sbuf(es3, "p3_lf_hi", [128, NT, 4], BF16)
        lf_lo = sbuf(es3, "p3_lf_lo", [128, NT, 4], BF16)
        kb.op('dve', lambda e: e.tensor_copy(out=tri_b[:], in_=tri[:]), ['tri'], ['tri_b'])
        kb.op('dve', lambda e: e.tensor_copy(out=ones_b[:], in_=ones_f[:]), ['ones_f'], ['ones_b'])
        kb.op('dve', lambda e: e.tensor_copy(out=maskb4[:], in_=maskf4[:]), ['maskf4'], ['maskb4'])
        kb.op('dve', lambda e: e.tensor_copy(out=lf_hi[:], in_=lf[:]), ['lf'], ['lf_hi'])
        kb.op('dve', lambda e: e.tensor_tensor(out=lf_lo[:], in0=lf[:], in1=lf_hi[:], op=ALU.subtract), ['lf', 'lf_hi'], ['lf_lo'])
        if stop_after == '3b1':
            T.barrier()
            return finish(kb, es, es_all, out_d)
        lfh2 = lf_hi[:].rearrange("p t h -> p (t h)")
        lfl2 = lf_lo[:].rearrange("p t h -> p (t h)")
        kb.op('pe', lambda e: e.matmul(pg[:, 0, :], lhsT=tri_b[:], rhs=lfh2, start=True, stop=False), ['tri_b', 'lf_hi'], ['ps:pg'])
        kb.op('pe', lambda e: e.matmul(pg[:, 0, :], lhsT=tri_b[:], rhs=lfl2, start=False, stop=True), ['tri_b', 'lf_lo'], ['ps:pg'])
        kb.op('pe', lambda e: e.matmul(pg[:, 1, :], lhsT=ones_b[:], rhs=lfh2, start=True, stop=False), ['ones_b', 'lf_hi'], ['ps:pg'])
        kb.op('pe', lambda e: e.matmul(pg[:, 1, :], lhsT=ones_b[:], rhs=lfl2, start=False, stop=True), ['ones_b', 'lf_lo'], ['ps:pg'])
        if stop_after == '3b2':
            T.barrier()
            return finish(kb, es, es_all, out_d)
        f2 = lambda t_: t_[:].rearrange("p t h -> p (t h)")
        kb.op('dve', lambda e: e.scalar_tensor_tensor(out=f2(bcol), in0=pg[:, 0, :], scalar=-1.0, in1=f2(ig), op0=ALU.mult, op1=ALU.add), ['ig', 'ps:pg'], ['bcol'])
        kb.op('act', lambda e: e.activation(out=f2(ea), in_=pg[:, 0, :], func=AF.Exp), ['ps:pg'], ['ea'])
        if stop_after == '3b3':
            T.barrier()
            return finish(kb, es, es_all, out_d)
        kb.op('dve', lambda e: e.tensor_tensor(out=f2(wcol), in0=pg[:, 1, :], in1=f2(bcol), op=ALU.add), ['bcol', 'ps:pg'], ['wcol'])
        kb.op('act', lambda e: e.activation(out=f2(wcol), in_=f2(wcol), func=AF.Exp), ['wcol'], ['wcol'])
        kb.op('act', lambda e: e.activation(out=f2(eaL), in_=pg[:, 1, :], func=AF.Exp), ['ps:pg'], ['eaL'])

        if stop_after == '3c':
            T.barrier()
            return finish(kb, es, es_all, out_d)
        pX = [psum(es3, "p3_pX%d" % i, [128, 4, 128], F32) for i in range(2)]
        pP = [psum(es3, "p3_pP%d" % i, [128, 512], F32) for i in range(4)]
        pTk_b = psum(es3, "p3_pTk", [128, 1024], BF16)
        pTk = pTk_b[:, 0:512].rearrange("p (a b) -> p a b", a=4)
        Rc = [sbuf(es3, "p3_Rc%d" % i, [128, 4, 128], BF16) for i in range(2)]
        Rl = [sbuf(es3, "p3_Rl%d" % i, [128, 4, 128], BF16) for i in range(2)]
        DT = [sbuf(es3, "p3_DT%d" % i, [128, 4, 128], F32) for i in range(2)]
        SD = [sbuf(es3, "p3_SD%d" % i, [128, 128], BF16) for i in range(4)]
        KW = [sbuf(es3, "p3_KW%d" % i, [128, 128], BF16) for i in range(4)]
        Cf = [sbuf(es3, "p3_Cf%d" % i, [128, 129], F32) for i in range(4)]
        Cb = [sbuf(es3, "p3_Cb%d" % i, [128, 129], BF16) for i in range(4)]
        intra_sb = [sbuf(es3, "p3_intra%d" % i, [128, 129], F32) for i in range(4)]
        Usb = [sbuf(es3, "p3_Usb%d" % i, [128, 129], F32) for i in range(4)]
        num = [sbuf(es3, "p3_num%d" % i, [128, 4, 129], F32) for i in range(2)]
        rdn = sbuf(es3, "p3_rdn", [128, 4], F32)
        hsc = [sbuf(es3, "p3_hsc%d" % i, [128, 4, 128], F32) for i in range(2)]
        stats = sbuf(es3, "p3_stats", [128, 4, 6], F32)
        mvv = sbuf(es3, "p3_mvv", [128, 4, 2], F32)
        rstd3 = sbuf(es3, "p3_rstd", [128, 4], F32)
        mo_t = [sbuf(es3, "p3_mo%d" % i, [128, 512], BF16) for i in range(2)]
        gg = [sbuf(es3, "p3_gg%d" % i, [128, 512], F32) for i in range(2)]
        yo3 = [sbuf(es3, "p3_yo%d" % i, [128, 512], BF16) for i in range(2)]
        for hh in range(4):
            kb.op('pool', lambda e: e.memset(Cf[hh][:], 0.0), [], ['Cf%d' % hh])
            kb.op('pool', lambda e: e.memset(Cb[hh][:], 0.0), [], ['Cb%d' % hh])
        u_it = 0
        for c in range(p3_tiles):
            cs = c % 2
            tsl = slice(c * 128, (c + 1) * 128)
            kb.dma('sp', mo_t[cs][:], tmb_s[tsl, 1024:1536], [], ['mo%d' % cs], 'p3_mol%d' % cs)
            kb.op('act', lambda e: e.activation(out=gg[cs][:], in_=mo_t[cs][:], func=AF.Exp, scale=-1.0), ['mo%d' % cs], ['gg%d' % cs])
            kb.op('pool', lambda e: e.tensor_scalar(out=gg[cs][:], in0=gg[cs][:], scalar1=1.0, scalar2=None, op0=ALU.add), ['gg%d' % cs], ['gg%d' % cs])
            kb.op('dve', lambda e: e.reciprocal(out=gg[cs][:], in_=gg[cs][:]), ['gg%d' % cs], ['gg%d' % cs])
            kb.op('pool', lambda e: e.tensor_tensor(out=gg[cs][:], in0=gg[cs][:], in1=mlg[:], op=ALU.mult), ['gg%d' % cs, 'mlg'], ['gg%d' % cs])
            kb.op('dve', lambda e: e.tensor_tensor(out=Rc[cs][:], in0=tri_b[:].unsqueeze(1).to_broadcast([128, 4, 128]),
                                                    in1=lf_hi[:, c, :].unsqueeze(2).to_broadcast([128, 4, 128]), op=ALU.mult), ['tri_b', 'lf_hi'], ['Rc%d' % cs])
            kb.op('dve', lambda e: e.tensor_tensor(out=Rl[cs][:], in0=tri_b[:].unsqueeze(1).to_broadcast([128, 4, 128]),
                                                    in1=lf_lo[:, c, :].unsqueeze(2).to_broadcast([128, 4, 128]), op=ALU.mult), ['tri_b', 'lf_lo'], ['Rl%d' % cs])
            pXf = pX[cs][:].rearrange("p h j -> p (h j)")
            kb.op('pe', lambda e: e.matmul(pXf, lhsT=ones_b[:], rhs=Rc[cs][:].rearrange("p h j -> p (h j)"), start=True, stop=False),
                  ['ones_b', 'Rc%d' % cs], ['ps:pX%d' % cs])
            kb.op('pe', lambda e: e.matmul(pXf, lhsT=ones_b[:], rhs=Rl[cs][:].rearrange("p h j -> p (h j)"), start=False, stop=False),
                  ['ones_b', 'Rl%d' % cs], ['ps:pX%d' % cs])
            kb.op('pe', lambda e: e.matmul(pXf, lhsT=ident_b[:], rhs=maskb4[:].rearrange("p h j -> p (h j)"), start=False, stop=True),
                  ['ident_b', 'maskb4'], ['ps:pX%d' % cs])
            for hh in range(4):
                kb.op('act', lambda e: e.activation(out=DT[cs][:, hh, :], in_=pX[cs][:, hh, :], func=AF.Exp, bias=bcol[:, c, hh:hh + 1]),
                      ['ps:pX%d' % cs, 'bcol'], ['DT%d_%d' % (cs, hh)])
            q_t = lambda hh: mqT[:, hh, tsl]
            k_t = lambda hh: mkT[:, hh, tsl]
            Pn = lambda hh: 'ps:pP%d' % hh
            for hh in range(4):
                kb.op('pe', lambda e: e.matmul(pP[hh][:, 0:128], lhsT=k_t(hh), rhs=q_t(hh), start=True, stop=True), ['mkT', 'mqT'], [Pn(hh)])
            for hh in range(4):
                kb.op('pe', lambda e: e.transpose(out=pTk[:, hh, :], in_=k_t(hh), identity=ident_b[:]), ['mkT', 'ident_b'], ['ps:pTk'])
            for hh in range(4):
                kb.op('dve', lambda e: e.scalar_tensor_tensor(out=SD[hh][:], in0=pP[hh][:, 0:128], scalar=KS, in1=DT[cs][:, hh, :], op0=ALU.mult, op1=ALU.mult),
                      [Pn(hh), 'DT%d_%d' % (cs, hh)], ['SD%d' % hh])
            for hh in range(4):
                kb.op('dve', lambda e: e.tensor_scalar(out=KW[hh][:], in0=pTk[:, hh, :], scalar1=wcol[:, c, hh:hh + 1], scalar2=KS, op0=ALU.mult, op1=ALU.mult),
                      ['ps:pTk', 'wcol'], ['KW%d' % hh])
            for hh in range(4):
                kb.op('pe', lambda e: e.matmul(pP[hh][:, 129:258], lhsT=SD[hh][:], rhs=MV[:, c, hh, :], start=True, stop=True), ['SD%d' % hh, 'MV', 'MVones'], [Pn(hh)])
                kb.op('pe', lambda e: e.matmul(pP[hh][:, 258:387], lhsT=q_t(hh), rhs=Cb[hh][:], start=True, stop=True), ['mqT', 'Cb%d' % hh], [Pn(hh)], attach='Cb%d' % hh)
            for hh in range(4):
                kb.op('act', lambda e: e.copy(out=intra_sb[hh][:], in_=pP[hh][:, 129:258]), [Pn(hh)], ['intra%d' % hh])
                kb.op('dve', lambda e: e.scalar_tensor_tensor(out=num[cs][:, hh, :], in0=pP[hh][:, 258:387], scalar=ea[:, c, hh:hh + 1], in1=intra_sb[hh][:], op0=ALU.mult, op1=ALU.add),
                      [Pn(hh), 'ea', 'intra%d' % hh], ['num%d_%d' % (cs, hh)])
            for hh in range(4):
                kb.op('pe', lambda e: e.matmul(pP[hh][:, 0:129], lhsT=KW[hh][:], rhs=MV[:, c, hh, :], start=True, stop=True), ['KW%d' % hh, 'MV', 'MVones'], [Pn(hh)])
            for hh in range(4):
                kb.op('act', lambda e: e.copy(out=Usb[hh][:], in_=pP[hh][:, 0:129]), [Pn(hh)], ['Usb%d' % hh])
            for hh in range(4):
                kb.op('dve', lambda e: e.scalar_tensor_tensor(out=Cf[hh][:], in0=Cf[hh][:], scalar=eaL[:, c, hh:hh + 1], in1=Usb[hh][:], op0=ALU.mult, op1=ALU.add),
                      ['Cf%d' % hh, 'eaL', 'Usb%d' % hh], ['Cf%d' % hh])
                kb.op('act', lambda e: e.copy(out=Cb[hh][:], in_=Cf[hh][:]), ['Cf%d' % hh], ['Cb%d' % hh])
            nn = ['num%d_%d' % (cs, hh) for hh in range(4)]
            kb.op('dve', lambda e: e.tensor_tensor(out=rdn[:], in0=num[cs][:, :, 128], in1=num[cs][:, :, 128], op=ALU.mult), nn, ['rdn'])
            kb.op('dve', lambda e: e.tensor_scalar(out=rdn[:], in0=rdn[:], scalar1=1.0, scalar2=None, op0=ALU.max), ['rdn'], ['rdn'])
            kb.op('act', lambda e: e.activation(out=rdn[:], in_=rdn[:], func=AF.Ln), ['rdn'], ['rdn'])
            kb.op('act', lambda e: e.activation(out=rdn[:], in_=rdn[:], func=AF.Exp, scale=-0.5), ['rdn'], ['rdn'])
            kb.op('dve', lambda e: e.tensor_tensor(out=hsc[cs][:], in0=num[cs][:, :, 0:128], in1=rdn[:].unsqueeze(2).to_broadcast([128, 4, 128]), op=ALU.mult),
                  nn + ['rdn'], ['hsc%d' % cs])
            for hh in range(4):
                kb.op('dve', lambda e: e.bn_stats(out=stats[:, hh, :], in_=hsc[cs][:, hh, :]), ['hsc%d' % cs], ['stats'])
                kb.op('dve', lambda e: e.bn_aggr(out=mvv[:, hh, :], in_=stats[:, hh, :]), ['stats'], ['mvv'])
            kb.op('act', lambda e: e.activation(out=rstd3[:], in_=mvv[:, :, 1], func=AF.Ln, bias=EPS), ['mvv'], ['rstd3'])
            kb.op('act', lambda e: e.activation(out=rstd3[:], in_=rstd3[:], func=AF.Exp, scale=-0.5), ['rstd3'], ['rstd3'])
            for hh in range(4):
                kb.op('dve', lambda e: e.tensor_scalar(out=hsc[cs][:, hh, :], in0=hsc[cs][:, hh, :], scalar1=mvv[:, hh, 0:1], scalar2=rstd3[:, hh:hh + 1], op0=ALU.subtract, op1=ALU.mult),
                      ['hsc%d' % cs, 'mvv', 'rstd3'], ['hsc%d' % cs])
            kb.op('dve', lambda e: e.tensor_tensor(out=yo3[cs][:], in0=hsc[cs][:].rearrange("p h d -> p (h d)"), in1=gg[cs][:], op=ALU.mult),
                  ['hsc%d' % cs, 'gg%d' % cs], ['yo3_%d' % cs])
            kb.dma('sp', y_s[tsl, 512:1024], yo3[cs][:], ['yo3_%d' % cs], [('yml', c)], 'p3_st%d' % cs)
    T.barrier()
    if stop_after == '3':
        return finish(kb, es, es_all, out_d)

    es4 = ExitStack()
    h2T = sbuf(es4, "h2T", [128, 8, S], BF16)
    with ExitStack() as es4a:
        wo = sbuf(es4a, "p4_wo", [128, 8, D], BF16)
        for kc in range(8):
            kb.dma('pool', wo[:, kc, :], wout_d[kc * 128:(kc + 1) * 128, :], [], ['p4_wo'], 'p4_wl')
        yt = [sbuf(es4a, "p4_yt%d" % i, [128, D], BF16) for i in range(2)]
        xt4 = [sbuf(es4a, "p4_xt%d" % i, [128, D], F32) for i in range(2)]
        yT = [sbuf(es4a, "p4_yT%d" % i, [128, 8, 128], BF16) for i in range(2)]
        x1t = [sbuf(es4a, "p4_x1_%d" % i, [128, D], F32) for i in range(2)]
        pTy = [psum(es4a, "p4_pT%d" % i, [128, 8, 128], BF16) for i in range(2)]
        po4 = [[psum(es4a, "p4_po%d_%d" % (i, j), [128, 512], F32) for j in range(2)] for i in range(2)]
        pTn = [psum(es4a, "p4n_pT%d" % i, [128, 8, 128], BF16) for i in range(2)]
        emit_norm4 = norm_transpose(es4a, "p4n", None, 1, h2T, NT, pTn, dst_tag='h2T_t',
                                    sb_src=lambda t: (x1t[t % 2][:], ['p4_x1_%d_0' % (t % 2), 'p4_x1_%d_1' % (t % 2)]), emit_only=True)
        def p4_front(t):
            b = t % 2
            tsl = slice(t * 128, (t + 1) * 128)
            kb.dma('sp', yt[b][:], y_s[tsl, :], [], ['p4_yt%d' % b], 'p4_yl%d' % b)
            kb.dma('sp', xt4[b][:], x_d[tsl, :], [], ['p4_xt%d' % b], 'p4_xl%d' % b)
            for c in range(8):
                kb.op('pe', lambda e: e.transpose(out=pTy[b][:, c, :], in_=yt[b][:, c * 128:(c + 1) * 128], identity=ident_b[:]),
                      ['p4_yt%d' % b, 'ident_b'], ['ps:p4_pT%d' % b])
            kb.op('act', lambda e: e.copy(out=yT[b][:], in_=pTy[b][:]), ['ps:p4_pT%d' % b], ['p4_yT%d' % b])

        def p4_back(t):
            b = t % 2
            tsl = slice(t * 128, (t + 1) * 128)
            for hf in range(2):
                for kc in range(8):
                    kb.op('pe', lambda e: e.matmul(po4[b][hf][:], lhsT=yT[b][:, kc, :], rhs=wo[:, kc, hf * 512:(hf + 1) * 512], start=(kc == 0), stop=(kc == 7)),
                          ['p4_yT%d' % b, 'p4_wo'], ['ps:p4_po%d_%d' % (b, hf)])
                kb.op('dve', lambda e: e.tensor_tensor(out=x1t[b][:, hf * 512:(hf + 1) * 512], in0=po4[b][hf][:], in1=xt4[b][:, hf * 512:(hf + 1) * 512], op=ALU.add),
                      ['ps:p4_po%d_%d' % (b, hf), 'p4_xt%d' % b], ['p4_x1_%d_%d' % (b, hf)])
            kb.dma('sp', x1_s[tsl, :], x1t[b][:], ['p4_x1_%d_0' % b, 'p4_x1_%d_1' % b], [('x1', t)], 'p4_st%d' % b)
            emit_norm4(t)

        for t in range(NT):
            p4_front(t)
            if t >= 1:
                p4_back(t - 1)
        p4_back(NT - 1)
    T.barrier()
    if stop_after == '4':
        return finish(kb, es, es_all, out_d)

    es5 = ExitStack()
    memT = sbuf(es5, "memT", [128, 8, MEM], BF16)
    with ExitStack() as es5a:
        pTt = [psum(es5a, "p5a_pT%d" % i, [128, 8, 128], BF16) for i in range(2)]
        norm_transpose(es5a, "p5a", lambda t: mem_d[t * 128:(t + 1) * 128, :], 2, memT, 2, pTt, dst_tag='memT_t')
    T.barrier()
    with ExitStack() as es5b:
        wx = {}
        for nm, wd in (('q', wxq_d), ('k', wxk_d), ('v', wxv_d), ('o', wxo_d)):
            wx[nm] = sbuf(es5b, "p5_w" + nm, [128, 8, D], BF16)
            for kc in range(8):
                kb.dma('pool', wx[nm][:, kc, :], wd[kc * 128:(kc + 1) * 128, :], [], ['p5_w' + nm], 'p5_wl' + nm)
        KT = sbuf(es5b, "p5_KT", [128, 8, MEM], BF16)
        Vx = sbuf(es5b, "p5_Vx", [128, 2, D], BF16)
        pA = [psum(es5b, "p5_pA%d" % i, [128, 512], F32) for i in range(2)]
        pS = [psum(es5b, "p5_pS%d" % i, [128, 512], F32) for i in range(2)]
        pZ = psum(es5b, "p5_pZ", [128, 512], F32)
        pO = [psum(es5b, "p5_pO%d" % i, [128, 512], F32) for i in range(2)]
        ia = 0
        for c in range(8):
            p_, pn = pA[ia % 2], 'ps:p5_pA%d' % (ia % 2)
            for kc in range(8):
                kb.op('pe', lambda e: e.matmul(p_[:, 0:MEM], lhsT=wx['k'][:, kc, c * 128:(c + 1) * 128], rhs=memT[:, kc, :], start=(kc == 0), stop=(kc == 7)),
                      ['p5_wk'], [pn])
            kb.op('act', lambda e: e.copy(out=KT[:, c, :], in_=p_[:, 0:MEM]), [pn], ['p5_KT'])
            ia += 1
        for kt in range(2):
            for hf in range(2):
                p_, pn = pA[ia % 2], 'ps:p5_pA%d' % (ia % 2)
                for kc in range(8):
                    kb.op('pe', lambda e: e.matmul(p_[:], lhsT=memT[:, kc, kt * 128:(kt + 1) * 128], rhs=wx['v'][:, kc, hf * 512:(hf + 1) * 512], start=(kc == 0), stop=(kc == 7)),
                          ['p5_wv'], [pn], attach='p5_wv')
                kb.op('act', lambda e: e.copy(out=Vx[:, kt, hf * 512:(hf + 1) * 512], in_=p_[:]), [pn], ['p5_Vx'])
                ia += 1
        qTx = [sbuf(es5b, "p5_qT%d" % i, [128, 8, 512], BF16) for i in range(2)]
        PTx = [sbuf(es5b, "p5_PT%d" % i, [128, 2, 512], BF16) for i in range(2)]
        rZ = [sbuf(es5b, "p5_rZ%d" % i, [128, 512], F32) for i in range(2)]
        oTx = [sbuf(es5b, "p5_oT%d" % i, [128, 8, 512], BF16) for i in range(2)]
        x1t5 = [sbuf(es5b, "p5_x1_%d" % i, [128, D], F32) for i in range(2)]
        x2t5 = [sbuf(es5b, "p5_x2_%d" % i, [128, D], F32) for i in range(2)]
        hi_ = 0
        ti_ = 0
        for blk in range(NB):
            bs = blk % 2
            bsl = slice(blk * 512, (blk + 1) * 512)
            for c in range(8):
                p_, pn = pA[ia % 2], 'ps:p5_pA%d' % (ia % 2)
                for kc in range(8):
                    kb.op('pe', lambda e: e.matmul(p_[:], lhsT=wx['q'][:, kc, c * 128:(c + 1) * 128], rhs=h2T[:, kc, bsl], start=(kc == 0), stop=(kc == 7)),
                          ['p5_wq'], [pn])
                kb.op('act', lambda e: e.copy(out=qTx[bs][:, c, :], in_=p_[:]), [pn], ['p5_qT%d_%d' % (bs, c)])
                ia += 1
            for h in range(4):
                hs = hi_ % 2
                for kt in range(2):
                    p_, pn = pS[kt], 'ps:p5_pS%d' % kt
                    for dc in range(2):
                        kb.op('pe', lambda e: e.matmul(p_[:], lhsT=KT[:, 2 * h + dc, kt * 128:(kt + 1) * 128], rhs=qTx[bs][:, 2 * h + dc, :], start=(dc == 0), stop=(dc == 1)),
                              ['p5_KT', 'p5_qT%d_%d' % (bs, 2 * h + dc)], [pn])
                    kb.op('act', lambda e: e.activation(out=PTx[hs][:, kt, :], in_=p_[:], func=AF.Exp, scale=1.0 / 16.0), [pn], ['p5_PT%d_%d' % (hs, kt)])
                for kt in range(2):
                    kb.op('pe', lambda e: e.matmul(pZ[:], lhsT=ones_bb[:], rhs=PTx[hs][:, kt, :], start=(kt == 0), stop=(kt == 1)),
                          ['ones_bb', 'p5_PT%d_%d' % (hs, kt)], ['ps:p5_pZ'])
                kb.op('dve', lambda e: e.reciprocal(out=rZ[hs][:], in_=pZ[:]), ['ps:p5_pZ'], ['p5_rZ%d' % hs])
                for dc in range(2):
                    p_, pn = pO[dc], 'ps:p5_pO%d' % dc
                    for kt in range(2):
                        kb.op('pe', lambda e: e.matmul(p_[:], lhsT=Vx[:, kt, h * 256 + dc * 128:h * 256 + (dc + 1) * 128], rhs=PTx[hs][:, kt, :], start=(kt == 0), stop=(kt == 1)),
                              ['p5_Vx', 'p5_PT%d_%d' % (hs, kt)], [pn])
                    kb.op('dve', lambda e: e.tensor_tensor(out=oTx[bs][:, 2 * h + dc, :], in0=p_[:], in1=rZ[hs][:], op=ALU.mult),
                          [pn, 'p5_rZ%d' % hs], ['p5_oT%d_%d' % (bs, 2 * h + dc)])
                hi_ += 1
            for sub in range(4):
                t = blk * 4 + sub
                ts_ = ti_ % 2
                tsl = slice(t * 128, (t + 1) * 128)
                kb.dma('sp', x1t5[ts_][:], x1_s[tsl, :], [], ['p5_x1_%d' % ts_], 'p5_xl%d' % ts_)
                for hf in range(2):
                    p_, pn = pA[ia % 2], 'ps:p5_pA%d' % (ia % 2)
                    for kc in range(8):
                        kb.op('pe', lambda e: e.matmul(p_[:], lhsT=oTx[bs][:, kc, sub * 128:(sub + 1) * 128], rhs=wx['o'][:, kc, hf * 512:(hf + 1) * 512], start=(kc == 0), stop=(kc == 7)),
                              ['p5_oT%d_%d' % (bs, kc), 'p5_wo'], [pn])
                    kb.op('dve', lambda e: e.tensor_tensor(out=x2t5[ts_][:, hf * 512:(hf + 1) * 512], in0=p_[:], in1=x1t5[ts_][:, hf * 512:(hf + 1) * 512], op=ALU.add),
                          [pn, 'p5_x1_%d' % ts_], ['p5_x2_%d_%d' % (ts_, hf)])
                    ia += 1
                kb.dma('sp', x2_s[tsl, :], x2t5[ts_][:], ['p5_x2_%d_0' % ts_, 'p5_x2_%d_1' % ts_], [('x2', t)], 'p5_st%d' % ts_)
                ti_ += 1
    es5.close()
    es4.close()
    T.barrier()
    if stop_after == '5':
        return finish(kb, es, es_all, out_d)

    with ExitStack() as es5c:
        gbc = sbuf(es5c, "p5c_gbc", [128, D], F32)
        bbc = sbuf(es5c, "p5c_bbc", [128, 36], F32)
        eoff = sbuf(es5c, "p5c_eoff", [128, NEXP], F32)
        trisf = sbuf(es5c, "p5c_trisf", [128, 128], F32)
        trisb = sbuf(es5c, "p5c_trisb", [128, 128], BF16)
        wr = sbuf(es5c, "p5c_wr", [128, 8, 36], BF16)
        tokid = sbuf(es5c, "p5c_tokid", [128, NT], I32)
        macc = sbuf(es5c, "p5c_macc", [128, NEXP], BF16)
        kb.dma('sp', gbc[:], gffn_d.partition_broadcast(128), [], ['gbc'], 'p5c_c0')
        kb.dma('sp', bbc[:], br_d.partition_broadcast(128), [], ['bbc'], 'p5c_c1')
        kb.dma('sp', eoff[:], eoff_d.partition_broadcast(128), [], ['eoff'], 'p5c_c2')
        kb.dma('sp', trisf[:], tris_d[:, :], [], ['trisf'], 'p5c_c3')
        kb.dma('sp', tokid[:], tokid_d[:, :], [], ['tokid'], 'p5c_c4')
        for kc in range(8):
            kb.dma('pool', wr[:, kc, :], wr_d[kc * 128:(kc + 1) * 128, :], [], ['wr'], 'p5c_c5')
        kb.op('dve', lambda e: e.tensor_copy(out=trisb[:], in_=trisf[:]), ['trisf'], ['trisb'])
        kb.op('pool', lambda e: e.memset(macc[:], 0.0), [], ['macc'])
        x2t = [sbuf(es5c, "p5c_x2_%d" % i, [128, D], F32) for i in range(2)]
        junkc = sbuf(es5c, "p5c_junk", [128, D], BF16)
        ssq = [sbuf(es5c, "p5c_ssq%d" % i, [128, 1], F32) for i in range(2)]
        h3 = [sbuf(es5c, "p5c_h3_%d" % i, [128, D], BF16) for i in range(2)]
        h3T = [sbuf(es5c, "p5c_h3T%d" % i, [128, 8, 128], BF16) for i in range(2)]
        pT5 = [psum(es5c, "p5c_pT%d" % i, [128, 8, 128], BF16) for i in range(2)]
        pL = [psum(es5c, "p5c_pL%d" % i, [128, 512], F32) for i in range(2)]
        pPos = [psum(es5c, "p5c_pPos%d" % i, [128, 512], F32) for i in range(2)]
        lg = sbuf(es5c, "p5c_lg", [128, 36], F32)
        sm = sbuf(es5c, "p5c_sm", [128, 16], F32)
        ge = sbuf(es5c, "p5c_ge", [128, 4], F32)
        oh = sbuf(es5c, "p5c_oh", [128, 4], F32)
        lm = sbuf(es5c, "p5c_lm", [128, 4, 8], F32)
        m8 = sbuf(es5c, "p5c_m8", [128, 8], F32)
        m1 = sbuf(es5c, "p5c_m1", [128, NEXP], F32)
        m2 = sbuf(es5c, "p5c_m2", [128, NEXP], F32)
        maskb5 = [sbuf(es5c, "p5c_mask%d" % i, [128, NEXP], BF16) for i in range(2)]
        sl = sbuf(es5c, "p5c_sl", [128, NEXP], F32)
        junk32 = sbuf(es5c, "p5c_junk32", [128, NEXP], F32)
        sf = sbuf(es5c, "p5c_sf", [128, 2], F32)
        lmf = lm[:].rearrange("p g e -> p (g e)")
        for t in range(NT):
            b = t % 2
            tsl = slice(t * 128, (t + 1) * 128)
            kb.dma('sp', x2t[b][:], x2_s[tsl, :], [], ['x2t%d' % b], 'p5c_xl%d' % b)
            kb.op('act', lambda e: e.activation(out=junkc[:], in_=x2t[b][:], func=AF.Square, accum_out=ssq[b][:]), ['x2t%d' % b], ['junkc', 'ssq%d' % b])
            kb.op('act', lambda e: e.activation(out=ssq[b][:], in_=ssq[b][:], func=AF.Ln, scale=1.0 / D, bias=EPS), ['ssq%d' % b], ['ssq%d' % b])
            kb.op('act', lambda e: e.activation(out=ssq[b][:], in_=ssq[b][:], func=AF.Exp, scale=-0.5), ['ssq%d' % b], ['ssq%d' % b])
            kb.op('dve', lambda e: e.scalar_tensor_tensor(out=h3[b][:], in0=x2t[b][:], scalar=ssq[b][:, 0:1], in1=gbc[:], op0=ALU.mult, op1=ALU.mult),
                  ['x2t%d' % b, 'ssq%d' % b, 'gbc'], ['h3_%d' % b])
            for c in range(8):
                kb.op('pe', lambda e: e.transpose(out=pT5[b][:, c, :], in_=h3[b][:, c * 128:(c + 1) * 128], identity=ident_b[:]), ['h3_%d' % b, 'ident_b'], ['ps:p5c_pT%d' % b])
            kb.op('act', lambda e: e.copy(out=h3T[b][:], in_=pT5[b][:]), ['ps:p5c_pT%d' % b], ['h3T%d' % b])
            for kc in range(8):
                kb.op('pe', lambda e: e.matmul(pL[b][:, 0:36], lhsT=h3T[b][:, kc, :], rhs=wr[:, kc, :], start=(kc == 0), stop=(kc == 7)), ['h3T%d' % b, 'wr'], ['ps:p5c_pL%d' % b])
            kb.op('dve', lambda e: e.tensor_tensor(out=lg[:], in0=pL[b][:, 0:36], in1=bbc[:], op=ALU.add), ['ps:p5c_pL%d' % b, 'bbc'], ['lg'])
            kb.op('dve', lambda e: e.reduce_max(out=sm[:, 0:1], in_=lg[:, 0:4], axis=AX.X), ['lg'], ['sm0'])
            kb.op('dve', lambda e: e.tensor_scalar(out=sm[:, 1:2], in0=sm[:, 0:1], scalar1=-1.0, scalar2=None, op0=ALU.mult), ['sm0'], ['sm1'])
            kb.op('act', lambda e: e.activation(out=ge[:], in_=lg[:, 0:4], func=AF.Exp, bias=sm[:, 1:2], accum_out=sm[:, 2:3]), ['lg', 'sm1'], ['ge', 'sm2'])
            kb.op('dve', lambda e: e.reciprocal(out=sm[:, 3:4], in_=sm[:, 2:3]), ['sm2'], ['sm3'])
            kb.op('dve', lambda e: e.tensor_scalar(out=oh[:], in0=lg[:, 0:4], scalar1=sm[:, 0:1], scalar2=None, op0=ALU.is_equal), ['lg', 'sm0'], ['oh'])
            kb.op('dve', lambda e: e.tensor_scalar(out=oh[:], in0=oh[:], scalar1=-1.0, scalar2=1.0e9, op0=ALU.add, op1=ALU.mult), ['oh'], ['oh'])
            kb.op('dve', lambda e: e.tensor_tensor(out=lm[:], in0=lg[:, 4:36].rearrange("p (g e) -> p g e", g=4), in1=oh[:].unsqueeze(2).to_broadcast([128, 4, 8]), op=ALU.add),
                  ['lg', 'oh'], ['lm'])
            kb.op('dve', lambda e: e.max(out=m8[:], in_=lmf), ['lm'], ['m8'])
            kb.op('dve', lambda e: e.tensor_scalar(out=sm[:, 4:5], in0=m8[:, 0:1], scalar1=-1.0, scalar2=None, op0=ALU.mult), ['m8'], ['sm4'])
            kb.op('act', lambda e: e.activation(out=sm[:, 5:6], in_=m8[:, 1:2], func=AF.Exp, bias=sm[:, 4:5]), ['m8', 'sm4'], ['sm5'])
            kb.op('dve', lambda e: e.tensor_scalar(out=sm[:, 6:7], in0=sm[:, 5:6], scalar1=1.0, scalar2=None, op0=ALU.add), ['sm5'], ['sm6'])
            kb.op('dve', lambda e: e.reciprocal(out=sm[:, 7:8], in_=sm[:, 6:7]), ['sm6'], ['sm7'])
            kb.op('dve', lambda e: e.tensor_tensor(out=comb_w[:, t, 0:1], in0=sm[:, 7:8], in1=sm[:, 3:4], op=ALU.mult), ['sm7', 'sm3'], [('cw', t)])
            kb.op('dve', lambda e: e.tensor_tensor(out=comb_w[:, t, 1:2], in0=comb_w[:, t, 0:1], in1=sm[:, 5:6], op=ALU.mult), [('cw', t), 'sm5'], [('cw2', t)])
            kb.op('dve', lambda e: e.tensor_scalar(out=m1[:], in0=lmf, scalar1=m8[:, 0:1], scalar2=None, op0=ALU.is_equal), ['lm', 'm8'], ['m1'])
            kb.op('dve', lambda e: e.tensor_scalar(out=m2[:], in0=lmf, scalar1=m8[:, 1:2], scalar2=None, op0=ALU.is_equal), ['lm', 'm8'], ['m2'])
            kb.op('dve', lambda e: e.tensor_tensor(out=maskb5[b][:], in0=m1[:], in1=m2[:], op=ALU.add), ['m1', 'm2'], ['mask%d' % b])
            kb.op('pe', lambda e: e.matmul(pPos[b][:, 0:NEXP], lhsT=trisb[:], rhs=maskb5[b][:], start=True, stop=False), ['trisb', 'mask%d' % b], ['ps:p5c_pPos%d' % b])
            kb.op('pe', lambda e: e.matmul(pPos[b][:, 0:NEXP], lhsT=ones_bb[:], rhs=macc[:], start=False, stop=True), ['ones_bb', 'macc'], ['ps:p5c_pPos%d' % b], attach='macc')
            kb.op('dve', lambda e: e.tensor_tensor(out=macc[:], in0=macc[:], in1=maskb5[b][:], op=ALU.add), ['macc', 'mask%d' % b], ['macc'])
            kb.op('dve', lambda e: e.scalar_tensor_tensor(out=sl[:], in0=pPos[b][:, 0:NEXP], scalar=float(CAP - 1), in1=eoff[:], op0=ALU.min, op1=ALU.add),
                  ['ps:p5c_pPos%d' % b, 'eoff'], ['sl'])
            kb.op('dve', lambda e: e.scalar_tensor_tensor(out=junk32[:], in0=sl[:], scalar=1.0, in1=m1[:], op0=ALU.mult, op1=ALU.mult, accum_out=sf[:, 0:1]), ['sl', 'm1'], ['junk32', 'sf0'])
            kb.op('dve', lambda e: e.scalar_tensor_tensor(out=junk32[:], in0=sl[:], scalar=1.0, in1=m2[:], op0=ALU.mult, op1=ALU.mult, accum_out=sf[:, 1:2]), ['sl', 'm2'], ['junk32', 'sf1'])
            kb.op('dve', lambda e: e.tensor_copy(out=slot_i[:, t, :], in_=sf[:]), ['sf0', 'sf1'], [('slot', t)])
            for k2 in range(2):
                T.op('pool', lambda e: e.indirect_dma_start(out=Xs_d[:, :], out_offset=bass.IndirectOffsetOnAxis(ap=slot_i[:, t, k2:k2 + 1], axis=0),
                                                            in_=h3[b][:], in_offset=None),
                     reads=['h3_%d' % b, ('slot', t)], writes=[('Xs', t, k2)], lane='p5c_sc%d_%d' % (b, k2))
    T.barrier()
    if stop_after == '5b':
        return finish(kb, es, es_all, out_d)

    with ExitStack() as es6:
        w1b = [sbuf(es6, "p6_w1_%d" % i, [128, 8, DEXP], BF16) for i in range(2)]
        w3b = [sbuf(es6, "p6_w3_%d" % i, [128, 8, DEXP], BF16) for i in range(2)]
        w2b = [sbuf(es6, "p6_w2_%d" % i, [128, 4, D], BF16) for i in range(2)]
        NXB = 2 * CHB
        xb = [sbuf(es6, "p6_xb%d" % i, [128, D], BF16) for i in range(NXB)]
        CHN = CHB * 128
        XT = [sbuf(es6, "p6_XT%d" % i, [128, 8, CHN], BF16) for i in range(2)]
        s1 = [sbuf(es6, "p6_s1_%d" % i, [128, CHN], BF16) for i in range(2)]
        gT6 = [sbuf(es6, "p6_gT%d" % i, [128, 4, CHN], BF16) for i in range(2)]
        ysb = [sbuf(es6, "p6_y%d" % i, [128, D], BF16) for i in range(2)]
        pT6 = [psum(es6, "p6_pT%d" % i, [128, 8, 128], BF16) for i in range(2)]
        p1 = [psum(es6, "p6_p1_%d" % i, [128, 512], F32) for i in range(2)]
        p3 = [psum(es6, "p6_p3_%d" % i, [128, 512], F32) for i in range(2)]
        py = [psum(es6, "p6_py%d" % i, [128, 512], F32) for i in range(2)]
        chunks = [(ex, hb) for ex in range(NEXP) for hb in range(CAPB // CHB)]
        cnt6 = {'mi': 0, 'yi': 0}

        def load_w(ex):
            ws = ex % 2
            kb.dma('pool', w1b[ws][:], w1_d[ex].rearrange("(c p) n -> p c n", p=128), [], ['w1_%d' % ws], 'p6_w1l%d' % ws)
            kb.dma('pool', w3b[ws][:], w3_d[ex].rearrange("(c p) n -> p c n", p=128), [], ['w3_%d' % ws], 'p6_w3l%d' % ws)
            kb.dma('pool', w2b[ws][:], w2_d[ex].rearrange("(c p) n -> p c n", p=128), [], ['w2_%d' % ws], 'p6_w2l%d' % ws)

        def load_x(i):
            ex, hb = chunks[i]
            row0 = ex * CAP + hb * CHN
            for j in range(CHB):
                xs_ = (i * CHB + j) % NXB
                kb.dma('sp', xb[xs_][:], Xs_d[row0 + j * 128:row0 + (j + 1) * 128, :], [], ['xb%d' % xs_], 'p6_xl%d' % xs_)

        def stage_T(i):
            cs = i % 2
            for j in range(CHB):
                xi_ = i * CHB + j
                xs_ = xi_ % NXB
                pt_ = xi_ % 2
                for c in range(8):
                    kb.op('pe', lambda e: e.transpose(out=pT6[pt_][:, c, :], in_=xb[xs_][:, c * 128:(c + 1) * 128], identity=ident_b[:]), ['xb%d' % xs_, 'ident_b'], ['ps:p6_pT%d' % pt_])
                if xi_ % 2 == 0:
                    kb.op('act', lambda e: e.copy(out=XT[cs][:, :, j * 128:(j + 1) * 128], in_=pT6[pt_][:]), ['ps:p6_pT%d' % pt_], ['XT%d_%d' % (cs, j)])
                else:
                    kb.op('dve', lambda e: e.tensor_copy(out=XT[cs][:, :, j * 128:(j + 1) * 128], in_=pT6[pt_][:]), ['ps:p6_pT%d' % pt_], ['XT%d_%d' % (cs, j)])

        def stage_A(i):
            ex, hb = chunks[i]
            ws = ex % 2
            cs = i % 2
            xtn = ['XT%d_%d' % (cs, j) for j in range(CHB)]
            for m in range(4):
                ms = cnt6['mi'] % 2
                for kc in range(8):
                    kb.op('pe', lambda e: e.matmul(p1[ms][:, 0:CHN], lhsT=w1b[ws][:, kc, m * 128:(m + 1) * 128], rhs=XT[cs][:, kc, :], start=(kc == 0), stop=(kc == 7)),
                          ['w1_%d' % ws] + xtn, ['ps:p6_p1_%d' % ms])
                for kc in range(8):
                    kb.op('pe', lambda e: e.matmul(p3[ms][:, 0:CHN], lhsT=w3b[ws][:, kc, m * 128:(m + 1) * 128], rhs=XT[cs][:, kc, :], start=(kc == 0), stop=(kc == 7)),
                          ['w3_%d' % ws] + xtn, ['ps:p6_p3_%d' % ms])
                kb.op('act', lambda e: e.activation(out=s1[ms][:], in_=p1[ms][:, 0:CHN], func=AF.Silu), ['ps:p6_p1_%d' % ms], ['s1_%d' % ms])
                kb.op('dve', lambda e: e.tensor_tensor(out=gT6[cs][:, m, :], in0=p3[ms][:, 0:CHN], in1=s1[ms][:], op=ALU.mult), ['ps:p6_p3_%d' % ms, 's1_%d' % ms], ['gT%d_%d' % (cs, m)])
                cnt6['mi'] += 1

        def stage_Y(i):
            ex, hb = chunks[i]
            ws = ex % 2
            cs = i % 2
            row0 = ex * CAP + hb * CHN
            gtn = ['gT%d_%d' % (cs, m) for m in range(4)]
            for j in range(CHB):
                ys_ = cnt6['yi'] % 2
                for hf in range(2):
                    for kc in range(4):
                        kb.op('pe', lambda e: e.matmul(py[hf][:], lhsT=gT6[cs][:, kc, j * 128:(j + 1) * 128], rhs=w2b[ws][:, kc, hf * 512:(hf + 1) * 512], start=(kc == 0), stop=(kc == 3)),
                              gtn + ['w2_%d' % ws], ['ps:p6_py%d' % hf])
                    if hf == 0:
                        kb.op('act', lambda e: e.copy(out=ysb[ys_][:, 0:512], in_=py[0][:]), ['ps:p6_py0'], ['ysb%d_0' % ys_])
                    else:
                        kb.op('dve', lambda e: e.tensor_copy(out=ysb[ys_][:, 512:1024], in_=py[1][:]), ['ps:p6_py1'], ['ysb%d_1' % ys_])
                kb.dma('sp', Y_d[row0 + j * 128:row0 + (j + 1) * 128, :], ysb[ys_][:], ['ysb%d_0' % ys_, 'ysb%d_1' % ys_], [('Y', ex, hb, j)], 'p6_st%d' % ys_)
                cnt6['yi'] += 1

        nch = len(chunks)
        load_w(0)
        load_x(0)
        stage_T(0)
        for i in range(nch):
            ex, hb = chunks[i]
            if hb == 0 and ex + 1 < NEXP:
                load_w(ex + 1)
            if i + 1 < nch:
                load_x(i + 1)
            stage_A(i)
            if i + 1 < nch:
                stage_T(i + 1)
            stage_Y(i)
    T.barrier()
    if stop_after == '6':
        return finish(kb, es, es_all, out_d)

    with ExitStack() as es7:
        gfb = sbuf(es7, "p7_gfb", [128, D], F32)
        kb.dma('sp', gfb[:], gfin_d.partition_broadcast(128), [], ['gfb'], 'p7_c0')
        x2t7 = [sbuf(es7, "p7_x2_%d" % i, [128, D], F32) for i in range(2)]
        y1 = [sbuf(es7, "p7_y1_%d" % i, [128, D], BF16) for i in range(2)]
        y2 = [sbuf(es7, "p7_y2_%d" % i, [128, D], BF16) for i in range(2)]
        x3 = [sbuf(es7, "p7_x3_%d" % i, [128, D], F32) for i in range(2)]
        junk7 = sbuf(es7, "p7_junk", [128, D], BF16)
        ssq7 = [sbuf(es7, "p7_ssq%d" % i, [128, 1], F32) for i in range(2)]
        o7 = [sbuf(es7, "p7_o%d" % i, [128, D], F32) for i in range(2)]
        for t in range(NT):
            b = t % 2
            tsl = slice(t * 128, (t + 1) * 128)
            kb.dma('sp', x2t7[b][:], x2_s[tsl, :], [], ['x2t%d' % b], 'p7_xl%d' % b)
            for k2, yy in ((0, y1), (1, y2)):
                T.op('pool', lambda e: e.indirect_dma_start(out=yy[b][:], out_offset=None, in_=Y_d[:, :],
                                                            in_offset=bass.IndirectOffsetOnAxis(ap=slot_i[:, t, k2:k2 + 1], axis=0)),
                     reads=[], writes=['y%d_%d' % (k2, b)], lane='p7_g%d_%d' % (k2, b))
            kb.op('dve', lambda e: e.scalar_tensor_tensor(out=x3[b][:], in0=y1[b][:], scalar=comb_w[:, t, 0:1], in1=x2t7[b][:], op0=ALU.mult, op1=ALU.add),
                  ['y0_%d' % b, 'x2t%d' % b], ['x3_%d' % b])
            kb.op('dve', lambda e: e.scalar_tensor_tensor(out=x3[b][:], in0=y2[b][:], scalar=comb_w[:, t, 1:2], in1=x3[b][:], op0=ALU.mult, op1=ALU.add),
                  ['y1_%d' % b, 'x3_%d' % b], ['x3_%d' % b])
            kb.op('act', lambda e: e.activation(out=junk7[:], in_=x3[b][:], func=AF.Square, accum_out=ssq7[b][:]), ['x3_%d' % b], ['junk7', 'ssq7_%d' % b])
            kb.op('act', lambda e: e.activation(out=ssq7[b][:], in_=ssq7[b][:], func=AF.Ln, scale=1.0 / D, bias=EPS), ['ssq7_%d' % b], ['ssq7_%d' % b])
            kb.op('act', lambda e: e.activation(out=ssq7[b][:], in_=ssq7[b][:], func=AF.Exp, scale=-0.5), ['ssq7_%d' % b], ['ssq7_%d' % b])
            kb.op('dve', lambda e: e.scalar_tensor_tensor(out=o7[b][:], in0=x3[b][:], scalar=ssq7[b][:, 0:1], in1=gfb[:], op0=ALU.mult, op1=ALU.mult),
                  ['x3_%d' % b, 'ssq7_%d' % b, 'gfb'], ['o7_%d' % b])
            kb.dma('sp', out_d[tsl, :], o7[b][:], ['o7_%d' % b], [('out', t)], 'p7_st%d' % b)
    return finish(kb, es, es_all, out_d)


def finish(kb, es, es_all, out_d):
    kb.T.barrier()
    return kb


def prep_inputs(inputs, b):
    f = lambda k: np.ascontiguousarray(np.asarray(inputs[k], dtype=np.float32))
    c = host_consts()
    m = {}
    m['x'] = f('x')[b]
    m['mem'] = f('mem')[b]
    perm = win_perm()
    m['w_in_ext'] = np.ascontiguousarray(f('w_in')[0][:, perm])
    m['w_out'] = f('w_out')[0]

    def g128(v):
        return v.reshape(8, 128).T
    m['gT'] = np.ascontiguousarray(np.concatenate([g128(f('norm_mix_g')[0]), g128(f('norm_x_g')[0]),
                                                   g128(f('norm_mem_g')[0]), g128(f('norm_ffn_g')[0])], axis=1))
    m['g_final'] = f('norm_final_g')
    m['g_ffn_row'] = f('norm_ffn_g')[0]
    kk = np.arange(128)[:, None]
    qq = np.arange(128)[None, :]
    m['tri_strict'] = (kk < qq).astype(np.float32)
    m['eoff'] = (np.arange(NEXP) * CAP).astype(np.float32)
    m['tokid'] = (np.arange(NT)[None, :] * 128 + np.arange(128)[:, None]).astype(np.int32)
    m['cosT'] = c['cosT']; m['sinT'] = c['sinT']; m['ident_f'] = c['ident_f']; m['maskb'] = c['maskb']; m['tri_incl'] = c['tri_incl']
    m['da_lambda'] = f('da_lambda')[0].reshape(256)
    m['da_subln_g'] = f('da_subln_g')[0]
    cw = f('ml_conv_w')[0][:, 0, :]
    cb = f('ml_conv_b')[0]
    convT = np.zeros((128, 40), np.float32)
    for gidx in range(8):
        cols = slice(gidx * 128, (gidx + 1) * 128)
        convT[:, gidx * 5:gidx * 5 + 4] = cw[:, cols].T
        convT[:, gidx * 5 + 4] = cb[cols]
    m['convT'] = convT
    m['ml_gate_b'] = f('ml_gate_b')[0].reshape(8)
    m['ml_norm_g'] = f('ml_norm_g')[0]
    for k in ('w_xq', 'w_xk', 'w_xv', 'w_xo'):
        m[k] = f(k)[0]
    m['w_router'] = np.ascontiguousarray(np.concatenate([f('w_router_group')[0], f('w_router_expert')[0]], axis=1))
    m['b_router'] = np.concatenate([f('b_router_group')[0], f('b_router_expert')[0]])
    m['w1'] = f('w1')[0]; m['w3'] = f('w3')[0]; m['w2'] = f('w2')[0]
    return m


_CACHE = {}


def kernel(**inputs):
    if 'kb' not in _CACHE:
        _CACHE['kb'] = build()
    kb = _CACHE['kb']
    n = 8
    maps = []
    for b in range(n):
        m = prep_inputs(inputs, b)
        maps.append({k: v for k, v in m.items() if k in kb.inp})
    res = run_bass_kernel_spmd(kb.nc, maps, core_ids=list(range(n)))
    return np.stack([np.asarray(r["out"], dtype=np.float32) for r in res.results], axis=0)
```

```python
import numpy as np
from contextlib import ExitStack
import concourse.bass as bass
import concourse.mybir as mybir
from concourse.bass_utils import run_bass_kernel_spmd

F32 = mybir.dt.float32
BF16 = mybir.dt.bfloat16
I32 = mybir.dt.int32
AF = mybir.ActivationFunctionType
ALU = mybir.AluOpType
AX = mybir.AxisListType

S = 4096
D = 1024
NT = S // 128
NB = S // 512
EPS = 1e-6
MEM = 256
NEXP = 32
DEXP = 512
CAPB = 6
CHB = 3
CAP = CAPB * 128
LAM_INIT = 0.2
NEG = -30000.0
STRICT = False

FM_COLS = 24 * 128
TM_COLS = 512 * 3 + 8
WIN_COLS = FM_COLS + TM_COLS


class Trk:
    def __init__(self, nc, needed=None):
        self.nc = nc
        self.needed_in = needed
        self.needed = {}
        self.phys = {}
        self.pcnt = {}
        self.eng = {'pe': nc.tensor, 'act': nc.scalar, 'dve': nc.vector, 'pool': nc.gpsimd, 'sp': nc.sync}
        self.sem = {}
        self.cnt = {}
        self.seen = {e: {} for e in self.eng}
        self.lastw = {}
        self.reads = {}
        self.stack = ExitStack()
        self.phase = 0
        self.nsem = 0
        self.pool = {'sw': [], 'hw': []}
        self.kind = {}

    def lane(self, name, eng='sp'):
        if name not in self.sem:
            kind = 'sw' if eng == 'pool' else 'hw'
            self.kind[name] = kind
            if self.pool[kind] and not name.startswith('eng_'):
                sh, c = self.pool[kind].pop()
                self.sem[name] = sh
                self.cnt[name] = c
            else:
                self.nsem += 1
                s = self.stack.enter_context(self.nc.semaphore("s%d" % self.nsem))
                self.sem[name] = s
                self.cnt[name] = 0
        return name

    def elane(self, eng):
        return "eng_%s" % eng

    def pval(self, ln, v):
        if not ln.startswith("eng_"):
            return v
        self.needed.setdefault(ln, set()).add(v)
        if self.needed_in is None:
            return v
        return self.phys[ln][v]

    def wait(self, eng, ticket):
        ln, v = ticket
        if self.seen[eng].get(ln, 0) < v:
            self.eng[eng].wait_ge(self.sem[ln], self.pval(ln, v))
            self.seen[eng][ln] = v

    def op(self, eng, fn, reads=(), writes=(), lane=None, inc=None, attach=None):
        deps = {}
        own = self.elane(eng)
        psr = [r for r in reads if isinstance(r, str) and r.startswith('ps:')]
        if psr:
            reads = [r for r in reads if r not in psr]
            writes = list(writes) + [r for r in psr if r not in writes]
        for r in reads:
            t = self.lastw.get(r)
            if t is not None:
                deps[t[0]] = max(deps.get(t[0], 0), t[1])
        for w in writes:
            t = self.lastw.get(w)
            if t is not None and (STRICT or t[0] != own):
                deps[t[0]] = max(deps.get(t[0], 0), t[1])
            for t in self.reads.get(w, ()):
                if STRICT or t[0] != own:
                    deps[t[0]] = max(deps.get(t[0], 0), t[1])
        for ln, v in deps.items():
            self.wait(eng, (ln, v))
        ins = fn(self.eng[eng])
        if attach is not None:
            t = self.lastw.get(attach)
            if t is not None:
                ins._wait_ge(self.sem[t[0]], self.pval(t[0], t[1]))
        if lane is None:
            lane = own
            inc = 1
        elif inc is None:
            inc = 16
        self.lane(lane, eng)
        self.cnt[lane] += inc
        t = (lane, self.cnt[lane])
        if lane.startswith("eng_"):
            if self.needed_in is None or t[1] in self.needed_in.get(lane, ()):
                ins.then_inc(self.sem[lane], 1)
                self.pcnt[lane] = self.pcnt.get(lane, 0) + 1
                self.phys.setdefault(lane, {})[t[1]] = self.pcnt[lane]
        else:
            ins.then_inc(self.sem[lane], inc)
        for r in reads:
            self.reads.setdefault(r, []).append(t)
        for w in writes:
            self.lastw[w] = t
            self.reads[w] = []
        return t

    def barrier(self):
        for e in self.eng:
            for ln, v in self.cnt.items():
                if v > 0:
                    self.wait(e, (ln, v))
        self.lastw = {}
        self.reads = {}
        self.phase += 1
        for ln in list(self.sem.keys()):
            if not ln.startswith("eng_"):
                self.pool[self.kind.pop(ln)].append((self.sem.pop(ln), self.cnt.pop(ln)))
                for e in self.eng:
                    self.seen[e].pop(ln, None)


def host_consts():
    c = {}
    c['ident_f'] = np.eye(128, dtype=np.float32)
    k = np.arange(128)[:, None]
    q = np.arange(128)[None, :]
    c['maskb'] = np.where(k <= q, 0.0, NEG).astype(np.float32)
    c['tri_incl'] = (k <= q).astype(np.float32)
    inv_freq = (10000.0 ** (-np.arange(0, 64, 2, dtype=np.float32) / np.float32(64))).astype(np.float32)
    pos = np.arange(S, dtype=np.float32)
    ang = (pos[:, None] * inv_freq[None, :]).astype(np.float32)
    cs = np.cos(ang).astype(np.float32).T
    sn = np.sin(ang).astype(np.float32).T
    cosT = np.zeros((128, S), np.float32)
    sinT = np.zeros((128, S), np.float32)
    for p in range(128):
        d = p % 64
        cosT[p] = cs[d % 32]
        sinT[p] = -sn[d % 32] if d < 32 else sn[d % 32]
    c['cosT'] = cosT
    c['sinT'] = sinT
    return c


def win_perm():
    idx = []
    off_q, off_k, off_v = 0, 512, 1024
    off_mq, off_mk, off_mv, off_mo, off_mi, off_mf = 1536, 2048, 2560, 3072, 3584, 3588

    def sw(base):
        out = []
        for m in range(2):
            b = base + m * 64
            out += list(range(b + 32, b + 64)) + list(range(b, b + 32))
        return out
    for h in range(4):
        idx += list(range(off_q + h * 128, off_q + (h + 1) * 128))
        idx += sw(off_q + h * 128)
        idx += list(range(off_k + h * 128, off_k + (h + 1) * 128))
        idx += sw(off_k + h * 128)
    idx += list(range(off_mq, off_mq + 512))
    idx += list(range(off_mk, off_mk + 512))
    idx += list(range(off_v, off_v + 512))
    idx += list(range(off_mv, off_mv + 512))
    idx += list(range(off_mo, off_mo + 512))
    idx += list(range(off_mi, off_mi + 4)) + list(range(off_mf, off_mf + 4))
    assert len(idx) == WIN_COLS
    return np.array(idx)


class K:
    def __init__(self, debug=None, needed=None):
        self.debug = debug or ()
        nc = bass.Bass("TRN2", target_bir_lowering=False)
        self.nc = nc
        self.T = Trk(nc, needed)
        self.inp = {}
        self.scr = {}
        self.dmaq = 0

    def din(self, name, shape, dt=F32):
        kb = self

        class Lazy:
            def _get(s_):
                if name not in kb.inp:
                    kb.inp[name] = kb.nc.dram_tensor(name, list(shape), dt, kind="ExternalInput").ap()
                return kb.inp[name]

            def __getitem__(s_, k):
                return s_._get()[k]

            def __getattr__(s_, a):
                return getattr(s_._get(), a)
        return Lazy()

    def dscr(self, name, shape, dt):
        kind = "ExternalOutput" if name in self.debug else "Internal"
        self.scr[name] = self.nc.dram_tensor(name, list(shape), dt, kind=kind).ap()
        return self.scr[name]

    def dma(self, eng, out, in_, reads, writes, lane, **kw):
        return self.T.op(eng, lambda e: e.dma_start(out=out, in_=in_, **kw), reads=reads, writes=writes, lane=lane)

    def op(self, eng, fn, reads=(), writes=(), attach=None):
        if eng == 'pe' and attach is None and reads:
            attach = reads[0]
        return self.T.op(eng, fn, reads=reads, writes=writes, attach=attach)


def build(debug=None, stop_after=None, skip12=False, p3_tiles=NT):
    rec = _build(debug, stop_after, skip12, p3_tiles, None)
    return _build(debug, stop_after, skip12, p3_tiles, rec.T.needed)


def _build(debug, stop_after, skip12, p3_tiles, needed):
    kb = K(debug, needed)
    nc, T = kb.nc, kb.T
    din, dscr = kb.din, kb.dscr
    x_d = din("x", [S, D])
    mem_d = din("mem", [MEM, D])
    win_d = din("w_in_ext", [D, WIN_COLS])
    wout_d = din("w_out", [D, D])
    gT_d = din("gT", [128, 4 * 8])
    gfin_d = din("g_final", [D])
    gffn_d = din("g_ffn_row", [D])
    tris_d = din("tri_strict", [128, 128])
    eoff_d = din("eoff", [NEXP])
    tokid_d = din("tokid", [128, NT], I32)
    cos_d = din("cosT", [128, S])
    sin_d = din("sinT", [128, S])
    ident_d = din("ident_f", [128, 128])
    maskb_d = din("maskb", [128, 128])
    tri_d = din("tri_incl", [128, 128])
    lam_d = din("da_lambda", [256])
    subln_d = din("da_subln_g", [128])
    convw_d = din("convT", [128, 8 * 5])
    gateb_d = din("ml_gate_b", [8])
    mlg_d = din("ml_norm_g", [512])
    wxq_d = din("w_xq", [D, D]); wxk_d = din("w_xk", [D, D]); wxv_d = din("w_xv", [D, D]); wxo_d = din("w_xo", [D, D])
    wr_d = din("w_router", [D, 36])
    br_d = din("b_router", [36])
    w1_d = din("w1", [NEXP, D, DEXP]); w3_d = din("w3", [NEXP, D, DEXP]); w2_d = din("w2", [NEXP, DEXP, D])
    out_d = nc.dram_tensor("out", [S, D], F32, kind="ExternalOutput").ap()

    qT_s = dscr("qT_s", [4, 128, S], BF16)
    kT_s = dscr("kT_s", [4, 128, S], BF16)
    mqT_s = dscr("mqT_s", [4, 128, S], BF16)
    mkT_s = dscr("mkT_s", [4, 128, S], BF16)
    tmb_s = dscr("tmb_s", [S, 1536], BF16)
    gate_s = dscr("gate_s", [128, NT, 8], F32)
    y_s = dscr("y_s", [S, D], BF16)
    x1_s = dscr("x1_s", [S, D], F32)
    x2_s = dscr("x2_s", [S, D], F32)
    Xs_d = dscr("Xs_d", [NEXP * CAP, D], BF16)
    Y_d = dscr("Y_d", [NEXP * CAP, D], BF16)

    es_all = ExitStack()

    def sbuf(es, name, shape, dt):
        return es.enter_context(nc.sbuf_tensor("sb_" + name, list(shape), dt))

    def psum(es, name, shape, dt):
        return es.enter_context(nc.psum_tensor("ps_" + name, list(shape), dt))

    ident_f = sbuf(es_all, "ident_f", [128, 128], F32)
    ident_b = sbuf(es_all, "ident_b", [128, 128], BF16)
    gT = sbuf(es_all, "gT", [128, 32], F32)
    kb.dma('sp', ident_f[:], ident_d[:, :], [], ['ident_f'], 'c0')
    kb.dma('sp', gT[:], gT_d[:, :], [], ['gT'], 'c1')
    kb.dma('pool', ident_b[:], ident_d[:, :], [], ['ident_b'], 'c2')
    slot_i = sbuf(es_all, "slot_i", [128, NT, 2], I32)
    comb_w = sbuf(es_all, "comb_w", [128, NT, 2], F32)
    ones_bb = sbuf(es_all, "ones_bb", [128, 128], BF16)
    kb.op('pool', lambda e: e.memset(ones_bb[:], 1.0), [], ['ones_bb'])
    es = ExitStack()
    if True:
        zt = sbuf(es, "zt", [128, 8192], BF16)
        kb.op('pool', lambda e: e.memset(zt[:], 0.0), [], ['zt'])
        for e_ in range(NEXP):
            kb.dma('pool', Xs_d[e_ * CAP:(e_ + 1) * CAP, :].rearrange("(p r) c -> p (r c)", p=128), zt[:, 0:CAP * D // 128], ['zt'], [('Xs0', e_)], 'zX%d' % (e_ % 4))

    hT = sbuf(es, "hT", [128, 8, S], BF16)
    cosT = sbuf(es, "cosT", [128, S], F32)
    sinT = sbuf(es, "sinT", [128, S], F32)
    convT = sbuf(es, "convT", [128, 40], F32)
    kb.dma('sp', cosT[:], cos_d[:, :], [], ['cosT'], 'c3')
    kb.dma('sp', sinT[:], sin_d[:, :], [], ['sinT'], 'c4')
    kb.dma('sp', convT[:], convw_d[:, :], [], ['convT'], 'c5')

    def norm_transpose(es_, tagp, x_src_tiles, gcol, dstT, ntiles, pT_tiles, dst_tag='hT_t', sb_src=None, emit_only=False):
        xt = [sbuf(es_, "%s_xt%d" % (tagp, i), [128, D], F32) for i in range(3)] if sb_src is None else None
        junk = sbuf(es_, tagp + "_junk", [128, D], BF16)
        ssq = [sbuf(es_, "%s_ssq%d" % (tagp, i), [128, 1], F32) for i in range(2)]
        rstd = [sbuf(es_, "%s_rstd%d" % (tagp, i), [128, 1], F32) for i in range(2)]
        xs = [sbuf(es_, "%s_xs%d" % (tagp, i), [128, D], BF16) for i in range(2)]
        def emit(t):
            a, b = t % 3, t % 2
            if sb_src is None:
                kb.dma('sp', xt[a][:], x_src_tiles(t), [], [tagp + 'xt%d' % a], tagp + 'ld%d' % a)
                x_ap, x_res = xt[a][:], [tagp + 'xt%d' % a]
            else:
                x_ap, x_res = sb_src(t)
            kb.op('act', lambda e: e.activation(out=junk[:], in_=x_ap, func=AF.Square, accum_out=ssq[b][:]),
                  x_res, [tagp + 'junk', tagp + 'ssq%d' % b])
            kb.op('act', lambda e: e.activation(out=rstd[b][:], in_=ssq[b][:], func=AF.Sqrt, scale=1.0 / D, bias=EPS),
                  [tagp + 'ssq%d' % b], [tagp + 'rstd%d' % b])
            kb.op('dve', lambda e: e.reciprocal(out=rstd[b][:], in_=rstd[b][:]),
                  [tagp + 'rstd%d' % b], [tagp + 'rstd%d' % b])
            kb.op('dve', lambda e: e.tensor_scalar(out=xs[b][:], in0=x_ap, scalar1=rstd[b][:], scalar2=None, op0=ALU.mult),
                  x_res + [tagp + 'rstd%d' % b], [tagp + 'xs%d' % b])
            pT = pT_tiles[b]
            for c in range(8):
                kb.op('pe', lambda e: e.transpose(out=pT[:, c, :], in_=xs[b][:, c * 128:(c + 1) * 128], identity=ident_b[:]),
                      [tagp + 'xs%d' % b, 'ident_b'], ['ps:' + tagp + 'pT%d' % b])
            kb.op('dve', lambda e: e.tensor_tensor(out=dstT[:, :, t * 128:(t + 1) * 128], in0=pT[:],
                                                    in1=gT[:, gcol * 8:(gcol + 1) * 8].unsqueeze(2).to_broadcast([128, 8, 128]),
                                                    op=ALU.mult),
                  ['ps:' + tagp + 'pT%d' % b, 'gT'], [dst_tag + '%d' % t])

        if emit_only:
            return emit
        for t in range(ntiles):
            emit(t)

    NT1 = 0 if skip12 else NT
    NB1 = 0 if skip12 else NB
    with ExitStack() as es1a:
        pTt = [psum(es1a, "p1a_pT%d" % i, [128, 8, 128], BF16) for i in range(2)]
        norm_transpose(es1a, "p1a", lambda t: x_d[t * 128:(t + 1) * 128, :], 0, hT, NT1, pTt)
    T.barrier()

    with ExitStack() as es1b:
        wq = [sbuf(es1b, "p1b_w%d" % i, [128, 8, 256], BF16) for i in range(2)]
        pq = [psum(es1b, "p1b_pq%d" % i, [128, 512], F32) for i in range(4)]
        r1 = [sbuf(es1b, "p1b_r1_%d" % i, [128, 512], F32) for i in range(2)]
        r2 = [sbuf(es1b, "p1b_r2_%d" % i, [128, 512], F32) for i in range(2)]
        ro = [sbuf(es1b, "p1b_ro%d" % i, [128, 512], BF16) for i in range(2)]
        it = 0
        for pair in range(0 if skip12 else 8):
            h, isk = pair // 2, pair % 2
            wbuf = wq[pair % 2]
            c0 = pair * 256
            kb.dma('pool', wbuf[:], win_d[:, c0:c0 + 256].rearrange("(c p) n -> p c n", p=128), [], ['p1b_w%d' % (pair % 2)], 'p1b_wl%d' % (pair % 2))
            dst = (kT_s if isk else qT_s)
            for blk in range(NB):
                pa, pb = pq[(it % 2) * 2], pq[(it % 2) * 2 + 1]
                na, nb_ = 'ps:p1b_pq%d' % ((it % 2) * 2), 'ps:p1b_pq%d' % ((it % 2) * 2 + 1)
                for kc in range(8):
                    kb.op('pe', lambda e: e.matmul(pa[:], lhsT=wbuf[:, kc, 0:128], rhs=hT[:, kc, blk * 512:(blk + 1) * 512], start=(kc == 0), stop=(kc == 7)),
                          ['p1b_w%d' % (pair % 2)] + ['hT_t%d' % (blk * 4 + j) for j in range(4)], [na])
                for kc in range(8):
                    kb.op('pe', lambda e: e.matmul(pb[:], lhsT=wbuf[:, kc, 128:256], rhs=hT[:, kc, blk * 512:(blk + 1) * 512], start=(kc == 0), stop=(kc == 7)),
                          ['p1b_w%d' % (pair % 2)] + ['hT_t%d' % (blk * 4 + j) for j in range(4)], [nb_])
                s_ = it % 2
                kb.op('dve', lambda e: e.tensor_tensor(out=r1[s_][:], in0=pa[:], in1=cosT[:, blk * 512:(blk + 1) * 512], op=ALU.mult),
                      [na, 'cosT'], ['p1b_r1_%d' % s_])
                kb.op('dve', lambda e: e.tensor_tensor(out=r2[s_][:], in0=pb[:], in1=sinT[:, blk * 512:(blk + 1) * 512], op=ALU.mult),
                      [nb_, 'sinT'], ['p1b_r2_%d' % s_])
                kb.op('pool', lambda e: e.tensor_tensor(out=ro[s_][:], in0=r1[s_][:], in1=r2[s_][:], op=ALU.add),
                      ['p1b_r1_%d' % s_, 'p1b_r2_%d' % s_], ['p1b_ro%d' % s_])
                kb.dma('sp', dst[h, :, blk * 512:(blk + 1) * 512], ro[s_][:], ['p1b_ro%d' % s_], [('qk', pair, blk)], 'p1b_st%d' % s_)
                it += 1

    if stop_after == '1b':
        return finish(kb, es, es_all, out_d)
    T.barrier()
    with ExitStack() as es1c:
        wm = [sbuf(es1c, "p1c_w%d" % i, [128, 8, 128], BF16) for i in range(2)]
        pm = [psum(es1c, "p1c_pm%d" % i, [128, 512], F32) for i in range(2)]
        ub = [sbuf(es1c, "p1c_ub%d" % i, [128, 515], F32) for i in range(2)]
        ca = [sbuf(es1c, "p1c_ca%d" % i, [128, 512], F32) for i in range(2)]
        co = [sbuf(es1c, "p1c_co%d" % i, [128, 512], BF16) for i in range(2)]
        it = 0
        for g in range(0 if skip12 else 8):
            wbuf = wm[g % 2]
            wn = 'p1c_w%d' % (g % 2)
            c0 = 16 * 128 + g * 128
            kb.dma('pool', wbuf[:], win_d[:, c0:c0 + 128].rearrange("(c p) n -> p c n", p=128), [], [wn], 'p1c_wl%d' % (g % 2))
            dst = (mqT_s if g < 4 else mkT_s)
            h = g % 4
            cw = lambda i: convT[:, g * 5 + i:g * 5 + i + 1]
            for blk in range(NB):
                s_ = it % 2
                p_, pn = pm[s_], 'ps:p1c_pm%d' % s_
                u_, un = ub[s_], 'p1c_ub%d' % s_
                for kc in range(8):
                    kb.op('pe', lambda e: e.matmul(p_[:], lhsT=wbuf[:, kc, :], rhs=hT[:, kc, blk * 512:(blk + 1) * 512], start=(kc == 0), stop=(kc == 7)),
                          [wn], [pn])
                if blk == 0:
                    kb.op('pool', lambda e: e.memset(u_[:, 0:3], 0.0), [], [un + 'h'])
                else:
                    up = ub[1 - s_]
                    kb.op('act', lambda e: e.copy(out=u_[:, 0:3], in_=up[:, 512:515]), ['p1c_ub%d' % (1 - s_)], [un + 'h'])
                kb.op('act', lambda e: e.copy(out=u_[:, 3:515], in_=p_[:]), [pn], [un])
                a_, an = ca[s_], 'p1c_ca%d' % s_
                kb.op('dve', lambda e: e.tensor_scalar(out=a_[:], in0=u_[:, 0:512], scalar1=cw(0), scalar2=None, op0=ALU.mult),
                      [un, un + 'h', 'convT'], [an])
                for i in (1, 2, 3):
                    kb.op('dve', lambda e: e.scalar_tensor_tensor(out=a_[:], in0=u_[:, i:i + 512], scalar=cw(i), in1=a_[:], op0=ALU.mult, op1=ALU.add),
                          [un, un + 'h', an, 'convT'], [an])
                o_, on = co[s_], 'p1c_co%d' % s_
                kb.op('act', lambda e: e.activation(out=o_[:], in_=a_[:], func=AF.Silu, bias=cw(4)), [an, 'convT'], [on])
                kb.dma('sp', dst[h, :, blk * 512:(blk + 1) * 512], o_[:], [on], [('mqk', g, blk)], 'p1c_st%d' % s_)
                it += 1
    T.barrier()
    with ExitStack() as es1d:
        wt = sbuf(es1d, "p1d_w", [128, 8, TM_COLS], BF16)
        for kc in range(8):
            kb.dma('pool', wt[:, kc, :], win_d[kc * 128:(kc + 1) * 128, FM_COLS:WIN_COLS], [], ['p1d_w'], 'p1d_wl')
        pt = [[psum(es1d, "p1d_p%d_%d" % (i, j), [128, 512], F32) for j in range(4)] for i in range(2)]
        ot = [sbuf(es1d, "p1d_o%d" % i, [128, 1536], BF16) for i in range(2)]
        og = [sbuf(es1d, "p1d_g%d" % i, [128, 8], F32) for i in range(2)]
        for t in range(NT1):
            s_ = t % 2
            for j in range(4):
                n0, n1 = (j * 512, (j + 1) * 512) if j < 3 else (1536, 1544)
                for kc in range(8):
                    kb.op('pe', lambda e: e.matmul(pt[s_][j][:, 0:n1 - n0], lhsT=hT[:, kc, t * 128:(t + 1) * 128], rhs=wt[:, kc, n0:n1], start=(kc == 0), stop=(kc == 7)),
                          ['p1d_w'], ['ps:p1d_p%d_%d' % (s_, j)], attach='p1d_w')
            for j in range(3):
                eng = 'act' if j != 1 else 'dve'
                if eng == 'act':
                    kb.op('act', lambda e: e.copy(out=ot[s_][:, j * 512:(j + 1) * 512], in_=pt[s_][j][:]), ['ps:p1d_p%d_%d' % (s_, j)], ['p1d_o%d_%d' % (s_, j)])
                else:
                    kb.op('dve', lambda e: e.tensor_copy(out=ot[s_][:, j * 512:(j + 1) * 512], in_=pt[s_][j][:]), ['ps:p1d_p%d_%d' % (s_, j)], ['p1d_o%d_%d' % (s_, j)])
            kb.op('dve', lambda e: e.tensor_copy(out=og[s_][:], in_=pt[s_][3][:, 0:8]), ['ps:p1d_p%d_3' % s_], ['p1d_g%d' % s_])
            kb.dma('sp', tmb_s[t * 128:(t + 1) * 128, :], ot[s_][:], ['p1d_o%d_%d' % (s_, j) for j in range(3)], [('tmb', t)], 'p1d_st%d' % s_)
            kb.dma('sp', gate_s[:, t, :], og[s_][:], ['p1d_g%d' % s_], [('gate', t)], 'p1d_sg%d' % s_)
    es.close()
    T.barrier()
    if stop_after == '1':
        return finish(kb, es, es_all, out_d)

    with ExitStack() as es2:
        lamb = sbuf(es2, "p2_lamb", [128, 256], F32)
        lj = sbuf(es2, "p2_lj", [128, 64], F32)
        ls = sbuf(es2, "p2_ls", [128, 2], F32)
        nlam = sbuf(es2, "p2_nlam", [128, 1], F32)
        sg = sbuf(es2, "p2_sg", [128, 128], F32)
        maskf = sbuf(es2, "p2_maskf", [128, 128], F32)
        maskb = sbuf(es2, "p2_maskb", [128, 128], BF16)
        kb.dma('sp', lamb[:], lam_d.partition_broadcast(128), [], ['lamb'], 'p2_c0')
        kb.dma('sp', sg[:], subln_d.partition_broadcast(128), [], ['sg'], 'p2_c1')
        kb.dma('sp', maskf[:], maskb_d[:, :], [], ['maskf'], 'p2_c2')
        kb.op('dve', lambda e: e.tensor_copy(out=maskb[:], in_=maskf[:]), ['maskf'], ['maskb'])
        for i in range(2):
            kb.op('dve', lambda e: e.scalar_tensor_tensor(out=lj[:], in0=lamb[:, i * 128:i * 128 + 64], scalar=1.0, in1=lamb[:, i * 128 + 64:i * 128 + 128],
                                                          op0=ALU.mult, op1=ALU.mult, accum_out=ls[:, i:i + 1]), ['lamb'], ['lj', 'ls'])
        kb.op('act', lambda e: e.activation(out=ls[:], in_=ls[:], func=AF.Exp), ['ls'], ['ls'])
        kb.op('dve', lambda e: e.tensor_tensor(out=nlam[:], in0=ls[:, 1:2], in1=ls[:, 0:1], op=ALU.subtract), ['ls'], ['nlam'])
        kb.op('dve', lambda e: e.tensor_scalar(out=nlam[:], in0=nlam[:], scalar1=-LAM_INIT, scalar2=None, op0=ALU.add), ['nlam'], ['nlam'])
        kb.op('dve', lambda e: e.tensor_scalar(out=sg[:], in0=sg[:], scalar1=1.0 - LAM_INIT, scalar2=None, op0=ALU.mult), ['sg'], ['sg'])

        kTh = [sbuf(es2, "p2_kT%d" % i, [128, S], BF16) for i in range(2)]
        Vh = [sbuf(es2, "p2_V%d" % i, [128, NT, 129], BF16) for i in range(2)]
        qTb = [sbuf(es2, "p2_q%d" % i, [128, 512], BF16) for i in range(2)]
        PT = [sbuf(es2, "p2_PT%d" % i, [128, 512], BF16) for i in range(3)]
        ps_s = [psum(es2, "p2_ps%d" % i, [128, 512], F32) for i in range(2)]
        accA_b = [psum(es2, "p2_accA%d" % i, [128, 512], F32) for i in range(2)]
        accB_b = [psum(es2, "p2_accB%d" % i, [128, 512], F32) for i in range(2)]
        accA = [b_[:, 0:387].rearrange("p (q c) -> p q c", c=129) for b_ in accA_b]
        accB = [b_[:, 0:129].rearrange("p (q c) -> p q c", c=129) for b_ in accB_b]
        rz = sbuf(es2, "p2_rz", [128, 2, 4], F32)
        o0 = [sbuf(es2, "p2_o0_%d" % i, [128, 4, 128], F32) for i in range(2)]
        oo = [sbuf(es2, "p2_oo_%d" % i, [128, 4, 128], F32) for i in range(2)]
        junk = sbuf(es2, "p2_junk", [128, 128], F32)
        ss = sbuf(es2, "p2_ss", [128, 4], F32)
        yo = [sbuf(es2, "p2_yo%d" % i, [128, 4, 128], BF16) for i in range(2)]
        for i in range(2):
            kb.op('pool', lambda e: e.memset(Vh[i][:, :, 128:129], 1.0), [], ['V%dones' % i])
        sc_it = 0
        hq = 0
        for h in range(0 if skip12 else 4):
            hs = h % 2
            kb.dma('sp', kTh[hs][:], kT_s[h, :, :], [], ['kT%d' % hs], 'p2_kl%d' % hs)
            kb.dma('sp', Vh[hs][:, :, 0:128], tmb_s[:, h * 128:(h + 1) * 128].rearrange("(t p) c -> p t c", p=128), [], ['V%d' % hs], 'p2_vl%d' % hs)
            for qb in range(NB):
                qs_ = hq % 2
                kb.dma('sp', qTb[qs_][:], qT_s[h, :, qb * 512:(qb + 1) * 512], [], ['q%d' % qs_], 'p2_ql%d' % qs_)
                nkt = 4 * qb + 4
                for m in range(2):
                    a_i = (hq * 2 + m) % 2
                    aA, aB = accA[a_i], accB[a_i]
                    an = 'ps:acc%d' % a_i
                    rows = slice(m * 64, (m + 1) * 64)
                    state = {'startedA': False}

                    def emit_scores(kt, sc):
                        j = kt - 4 * qb
                        c0 = max(j, 0) * 128
                        p_s, pn = ps_s[sc % 2], 'ps:s%d' % (sc % 2)
                        P_, Pn = PT[sc % 3], 'PT%d' % (sc % 3)
                        kb.op('pe', lambda e: e.matmul(p_s[:, c0:512], lhsT=kTh[hs][rows, kt * 128:(kt + 1) * 128], rhs=qTb[qs_][rows, c0:512],
                                                        start=True, stop=(j < 0)), ['kT%d' % hs, 'q%d' % qs_], [pn])
                        if j >= 0:
                            kb.op('pe', lambda e: e.matmul(p_s[:, c0:c0 + 128], lhsT=ident_b[:], rhs=maskb[:], start=False, stop=True),
                                  ['ident_b', 'maskb'], [pn])
                        kb.op('act', lambda e: e.activation(out=P_[:, c0:512], in_=p_s[:, c0:512], func=AF.Exp, scale=0.125), [pn], [Pn])

                    def emit_pv(kt, sc):
                        j = kt - 4 * qb
                        P_, Pn = PT[sc % 3], 'PT%d' % (sc % 3)
                        for qs in range(max(j, 0), 4):
                            last = (kt == 4 * qb + qs)
                            if qs < 3:
                                o_ap = aA[:, qs, :]
                                st = not state['startedA']
                                state['startedA'] = True
                            else:
                                o_ap = aB[:, 0, :]
                                st = (kt == 0)
                            kb.op('pe', lambda e: e.matmul(o_ap, lhsT=P_[:, qs * 128:(qs + 1) * 128], rhs=Vh[hs][:, kt, :], start=st, stop=last, skip_group_check=True),
                                  [Pn, 'V%d' % hs, 'V%dones' % hs], [an])

                    prev = None
                    for kt in range(nkt):
                        emit_scores(kt, sc_it)
                        if prev is not None:
                            emit_pv(*prev)
                        prev = (kt, sc_it)
                        sc_it += 1
                    emit_pv(*prev)
                    pq_ = hq % 2
                    kb.op('dve', lambda e: e.reciprocal(out=rz[:, m, 0:3], in_=aA[:, :, 128]), [an], ['rz%d' % m])
                    kb.op('dve', lambda e: e.reciprocal(out=rz[:, m, 3:4], in_=aB[:, :, 128]), [an], ['rz%d' % m])
                    if m == 0:
                        for qs in range(4):
                            src = aA[:, qs, 0:128] if qs < 3 else aB[:, 0, 0:128]
                            kb.op('dve', lambda e: e.tensor_scalar(out=o0[pq_][:, qs, :], in0=src, scalar1=rz[:, 0, qs:qs + 1], scalar2=None, op0=ALU.mult),
                                  [an, 'rz0'], ['o0_%d' % pq_])
                    else:
                        kb.op('dve', lambda e: e.tensor_scalar(out=rz[:, 1, :], in0=rz[:, 1, :], scalar1=nlam[:, 0:1], scalar2=None, op0=ALU.mult),
                              ['rz1', 'nlam'], ['rz1'])
                        for qs in range(4):
                            src = aA[:, qs, 0:128] if qs < 3 else aB[:, 0, 0:128]
                            kb.op('dve', lambda e: e.scalar_tensor_tensor(out=oo[pq_][:, qs, :], in0=src, scalar=rz[:, 1, qs:qs + 1], in1=o0[pq_][:, qs, :],
                                                                          op0=ALU.mult, op1=ALU.add), [an, 'rz1', 'o0_%d' % pq_], ['oo_%d' % pq_])
                pq_ = hq % 2
                for qs in range(4):
                    kb.op('dve', lambda e: e.scalar_tensor_tensor(out=junk[:], in0=oo[pq_][:, qs, :], scalar=1.0, in1=oo[pq_][:, qs, :], op0=ALU.mult, op1=ALU.mult,
                                                                  accum_out=ss[:, qs:qs + 1]), ['oo_%d' % pq_], ['junk', 'ss'])
                kb.op('act', lambda e: e.activation(out=ss[:], in_=ss[:], func=AF.Ln, scale=1.0 / 128, bias=EPS), ['ss'], ['ss'])
                kb.op('act', lambda e: e.activation(out=ss[:], in_=ss[:], func=AF.Exp, scale=-0.5), ['ss'], ['ss'])
                for qs in range(4):
                    kb.op('dve', lambda e: e.scalar_tensor_tensor(out=yo[pq_][:, qs, :], in0=oo[pq_][:, qs, :], scalar=ss[:, qs:qs + 1], in1=sg[:], op0=ALU.mult, op1=ALU.mult),
                          ['oo_%d' % pq_, 'ss', 'sg'], ['yo%d' % pq_])
                kb.dma('sp', y_s[qb * 512:(qb + 1) * 512, h * 128:(h + 1) * 128].rearrange("(qs p) c -> p qs c", p=128), yo[pq_][:],
                       ['yo%d' % pq_], [('yda', h, qb)], 'p2_st%d' % pq_)
                hq += 1
    T.barrier()
    if stop_after == '2':
        return finish(kb, es, es_all, out_d)

    with ExitStack() as es3:
        DH = 128
        KS = float(DH) ** -0.5
        tri = sbuf(es3, "p3_tri", [128, 128], F32)
        maskf4 = sbuf(es3, "p3_maskf4", [128, 4, 128], F32)
        ones_f = sbuf(es3, "p3_ones", [128, 128], F32)
        gb = sbuf(es3, "p3_gb", [128, 8], F32)
        mlg = sbuf(es3, "p3_mlg", [128, 512], F32)
        G = sbuf(es3, "p3_G", [128, NT, 8], F32)
        ig = sbuf(es3, "p3_ig", [128, NT, 4], F32)
        lf = sbuf(es3, "p3_lf", [128, NT, 4], F32)
        bcol = sbuf(es3, "p3_bcol", [128, NT, 4], F32)
        ea = sbuf(es3, "p3_ea", [128, NT, 4], F32)
        wcol = sbuf(es3, "p3_wcol", [128, NT, 4], F32)
        eaL = sbuf(es3, "p3_eaL", [128, NT, 4], F32)
        mqT = sbuf(es3, "p3_mqT", [128, 4, S], BF16)
        mkT = sbuf(es3, "p3_mkT", [128, 4, S], BF16)
        MV = sbuf(es3, "p3_MV", [128, NT, 4, 129], BF16)
        kb.dma('sp', tri[:], tri_d[:, :], [], ['tri'], 'p3_c0')
        for hh in range(4):
            kb.dma('sp', maskf4[:, hh, :], maskb_d[:, :], [], ['maskf4'], 'p3_c1')
        kb.dma('sp', gb[:], gateb_d.partition_broadcast(128), [], ['gb'], 'p3_c2')
        kb.dma('sp', mlg[:], mlg_d.partition_broadcast(128), [], ['mlg'], 'p3_c3')
        kb.dma('sp', G[:], gate_s, [], ['G'], 'p3_c4')
        for hh in range(4):
            kb.dma('sp', mqT[:, hh, :], mqT_s[hh, :, :], [], ['mqT'], 'p3_c5')
            kb.dma('sp', mkT[:, hh, :], mkT_s[hh, :, :], [], ['mkT'], 'p3_c6')
        kb.op('pool', lambda e: e.memset(MV[:, :, :, 128:129], 1.0), [], ['MVones'])
        for hh in range(4):
            kb.dma('sp', MV[:, :, hh, 0:128], tmb_s[:, 512 + hh * 128:512 + (hh + 1) * 128].rearrange("(t p) c -> p t c", p=128), [], ['MV'], 'p3_c7')
        kb.op('pool', lambda e: e.memset(ones_f[:], 1.0), [], ['ones_f'])
        if stop_after == '3a':
            T.barrier()
            return finish(kb, es, es_all, out_d)
        kb.op('dve', lambda e: e.tensor_tensor(out=ig[:], in0=G[:, :, 0:4], in1=gb[:, 0:4].unsqueeze(1).to_broadcast([128, NT, 4]), op=ALU.add), ['G', 'gb'], ['ig'])
        kb.op('dve', lambda e: e.tensor_tensor(out=lf[:], in0=G[:, :, 4:8], in1=gb[:, 4:8].unsqueeze(1).to_broadcast([128, NT, 4]), op=ALU.add), ['G', 'gb'], ['lf'])
        kb.op('act', lambda e: e.activation(out=lf[:], in_=lf[:], func=AF.Exp, scale=-1.0), ['lf'], ['lf'])
        kb.op('act', lambda e: e.activation(out=lf[:], in_=lf[:], func=AF.Ln, bias=1.0), ['lf'], ['lf'])
        kb.op('dve', lambda e: e.tensor_scalar(out=lf[:], in0=lf[:], scalar1=-1.0, scalar2=None, op0=ALU.mult), ['lf'], ['lf'])
        if stop_after == '3b':
            T.barrier()
            return finish(kb, es, es_all, out_d)
        pg_b = psum(es3, "p3_pg", [128, 512], F32)
        pg = pg_b[:, 0:256].rearrange("p (a b) -> p a b", a=2)
        tri_b = sbuf(es3, "p3_tri_b", [128, 128], BF16)
        ones_b = sbuf(es3, "p3_ones_b", [128, 128], BF16)
        maskb4 = sbuf(es3, "p3_maskb4", [128, 4, 128], BF16)
        lf_hi = sbuf(es3, "p3_lf_hi", [128, NT, 4], BF16)
        lf_lo = sbuf(es3, "p3_lf_lo", [128, NT, 4], BF16)
        kb.op('dve', lambda e: e.tensor_copy(out=tri_b[:], in_=tri[:]), ['tri'], ['tri_b'])
        kb.op('dve', lambda e: e.tensor_copy(out=ones_b[:], in_=ones_f[:]), ['ones_f'], ['ones_b'])
        kb.op('dve', lambda e: e.tensor_copy(out=maskb4[:], in_=maskf4[:]), ['maskf4'], ['maskb4'])
        kb.op('dve', lambda e: e.tensor_copy(out=lf_hi[:], in_=lf[:]), ['lf'], ['lf_hi'])
        kb.op('dve', lambda e: e.tensor_tensor(out=lf_lo[:], in0=lf[:], in1=lf_hi[:], op=ALU.subtract), ['lf', 'lf_hi'], ['lf_lo'])
        if stop_after == '3b1':
            T.barrier()
            return finish(kb, es, es_all, out_d)
        lfh2 = lf_hi[:].rearrange("p t h -> p (t h)")
        lfl2 = lf_lo[:].rearrange("p t h -> p (t h)")
        kb.op('pe', lambda e: e.matmul(pg[:, 0, :], lhsT=tri_b[:], rhs=lfh2, start=True, stop=False), ['tri_b', 'lf_hi'], ['ps:pg'])
        kb.op('pe', lambda e: e.matmul(pg[:, 0, :], lhsT=tri_b[:], rhs=lfl2, start=False, stop=True), ['tri_b', 'lf_lo'], ['ps:pg'])
        kb.op('pe', lambda e: e.matmul(pg[:, 1, :], lhsT=ones_b[:], rhs=lfh2, start=True, stop=False), ['ones_b', 'lf_hi'], ['ps:pg'])
        kb.op('pe', lambda e: e.matmul(pg[:, 1, :], lhsT=ones_b[:], rhs=lfl2, start=False, stop=True), ['ones_b', 'lf_lo'], ['ps:pg'])
        if stop_after == '3b2':
            T.barrier()
            return finish(kb, es, es_all, out_d)
        f2 = lambda t_: t_[:].rearrange("p t h -> p (t h)")
        kb.op('dve', lambda e: e.scalar_tensor_tensor(out=f2(bcol), in0=pg[:, 0, :], scalar=-1.0, in1=f2(ig), op0=ALU.mult, op1=ALU.add), ['ig', 'ps:pg'], ['bcol'])
        kb.op('act', lambda e: e.activation(out=f2(ea), in_=pg[:, 0, :], func=AF.Exp), ['ps:pg'], ['ea'])
        if stop_after == '3b3':
            T.barrier()
            return finish(kb, es, es_all, out_d)
        kb.op('dve', lambda e: e.tensor_tensor(out=f2(wcol), in0=pg[:, 1, :], in1=f2(bcol), op=ALU.add), ['bcol', 'ps:pg'], ['wcol'])
        kb.op('act', lambda e: e.activation(out=f2(wcol), in_=f2(wcol), func=AF.Exp), ['wcol'], ['wcol'])
        kb.op('act', lambda e: e.activation(out=f2(eaL), in_=pg[:, 1, :], func=AF.Exp), ['ps:pg'], ['eaL'])

        if stop_after == '3c':
            T.barrier()
            return finish(kb, es, es_all, out_d)
        pX = [psum(es3, "p3_pX%d" % i, [128, 4, 128], F32) for i in range(2)]
        pP = [psum(es3, "p3_pP%d" % i, [128, 512], F32) for i in range(4)]
        pTk_b = psum(es3, "p3_pTk", [128, 1024], BF16)
        pTk = pTk_b[:, 0:512].rearrange("p (a b) -> p a b", a=4)
        Rc = [sbuf(es3, "p3_Rc%d" % i, [128, 4, 128], BF16) for i in range(2)]
        Rl = [sbuf(es3, "p3_Rl%d" % i, [128, 4, 128], BF16) for i in range(2)]
        DT = [sbuf(es3, "p3_DT%d" % i, [128, 4, 128], F32) for i in range(2)]
        SD = [sbuf(es3, "p3_SD%d" % i, [128, 128], BF16) for i in range(4)]
        KW = [sbuf(es3, "p3_KW%d" % i, [128, 128], BF16) for i in range(4)]
        Cf = [sbuf(es3, "p3_Cf%d" % i, [128, 129], F32) for i in range(4)]
        Cb = [sbuf(es3, "p3_Cb%d" % i, [128, 129], BF16) for i in range(4)]
        intra_sb = [sbuf(es3, "p3_intra%d" % i, [128, 129], F32) for i in range(4)]
        Usb = [sbuf(es3, "p3_Usb%d" % i, [128, 129], F32) for i in range(4)]
        num = [sbuf(es3, "p3_num%d" % i, [128, 4, 129], F32) for i in range(2)]
        rdn = sbuf(es3, "p3_rdn", [128, 4], F32)
        hsc = [sbuf(es3, "p3_hsc%d" % i, [128, 4, 128], F32) for i in range(2)]
        stats = sbuf(es3, "p3_stats", [128, 4, 6], F32)
        mvv = sbuf(es3, "p3_mvv", [128, 4, 2], F32)
        rstd3 = sbuf(es3, "p3_rstd", [128, 4], F32)
        mo_t = [sbuf(es3, "p3_mo%d" % i, [128, 512], BF16) for i in range(2)]
        gg = [sbuf(es3, "p3_gg%d" % i, [128, 512], F32) for i in range(2)]
        yo3 = [sbuf(es3, "p3_yo%d" % i, [128, 512], BF16) for i in range(2)]
        for hh in range(4):
            kb.op('pool', lambda e: e.memset(Cf[hh][:], 0.0), [], ['Cf%d' % hh])
            kb.op('pool', lambda e: e.memset(Cb[hh][:], 0.0), [], ['Cb%d' % hh])
        u_it = 0
        for c in range(p3_tiles):
            cs = c % 2
            tsl = slice(c * 128, (c + 1) * 128)
            kb.dma('sp', mo_t[cs][:], tmb_s[tsl, 1024:1536], [], ['mo%d' % cs], 'p3_mol%d' % cs)
            kb.op('act', lambda e: e.activation(out=gg[cs][:], in_=mo_t[cs][:], func=AF.Exp, scale=-1.0), ['mo%d' % cs], ['gg%d' % cs])
            kb.op('pool', lambda e: e.tensor_scalar(out=gg[cs][:], in0=gg[cs][:], scalar1=1.0, scalar2=None, op0=ALU.add), ['gg%d' % cs], ['gg%d' % cs])
            kb.op('dve', lambda e: e.reciprocal(out=gg[cs][:], in_=gg[cs][:]), ['gg%d' % cs], ['gg%d' % cs])
            kb.op('pool', lambda e: e.tensor_tensor(out=gg[cs][:], in0=gg[cs][:], in1=mlg[:], op=ALU.mult), ['gg%d' % cs, 'mlg'], ['gg%d' % cs])
            kb.op('dve', lambda e: e.tensor_tensor(out=Rc[cs][:], in0=tri_b[:].unsqueeze(1).to_broadcast([128, 4, 128]),
                                                    in1=lf_hi[:, c, :].unsqueeze(2).to_broadcast([128, 4, 128]), op=ALU.mult), ['tri_b', 'lf_hi'], ['Rc%d' % cs])
            kb.op('dve', lambda e: e.tensor_tensor(out=Rl[cs][:], in0=tri_b[:].unsqueeze(1).to_broadcast([128, 4, 128]),
                                                    in1=lf_lo[:, c, :].unsqueeze(2).to_broadcast([128, 4, 128]), op=ALU.mult), ['tri_b', 'lf_lo'], ['Rl%d' % cs])
            pXf = pX[cs][:].rearrange("p h j -> p (h j)")
            kb.op('pe', lambda e: e.matmul(pXf, lhsT=ones_b[:], rhs=Rc[cs][:].rearrange("p h j -> p (h j)"), start=True, stop=False),
                  ['ones_b', 'Rc%d' % cs], ['ps:pX%d' % cs])
            kb.op('pe', lambda e: e.matmul(pXf, lhsT=ones_b[:], rhs=Rl[cs][:].rearrange("p h j -> p (h j)"), start=False, stop=False),
                  ['ones_b', 'Rl%d' % cs], ['ps:pX%d' % cs])
            kb.op('pe', lambda e: e.matmul(pXf, lhsT=ident_b[:], rhs=maskb4[:].rearrange("p h j -> p (h j)"), start=False, stop=True),
                  ['ident_b', 'maskb4'], ['ps:pX%d' % cs])
            for hh in range(4):
                kb.op('act', lambda e: e.activation(out=DT[cs][:, hh, :], in_=pX[cs][:, hh, :], func=AF.Exp, bias=bcol[:, c, hh:hh + 1]),
                      ['ps:pX%d' % cs, 'bcol'], ['DT%d_%d' % (cs, hh)])
            q_t = lambda hh: mqT[:, hh, tsl]
            k_t = lambda hh: mkT[:, hh, tsl]
            Pn = lambda hh: 'ps:pP%d' % hh
            for hh in range(4):
                kb.op('pe', lambda e: e.matmul(pP[hh][:, 0:128], lhsT=k_t(hh), rhs=q_t(hh), start=True, stop=True), ['mkT', 'mqT'], [Pn(hh)])
            for hh in range(4):
                kb.op('pe', lambda e: e.transpose(out=pTk[:, hh, :], in_=k_t(hh), identity=ident_b[:]), ['mkT', 'ident_b'], ['ps:pTk'])
            for hh in range(4):
                kb.op('dve', lambda e: e.scalar_tensor_tensor(out=SD[hh][:], in0=pP[hh][:, 0:128], scalar=KS, in1=DT[cs][:, hh, :], op0=ALU.mult, op1=ALU.mult),
                      [Pn(hh), 'DT%d_%d' % (cs, hh)], ['SD%d' % hh])
            for hh in range(4):
                kb.op('dve', lambda e: e.tensor_scalar(out=KW[hh][:], in0=pTk[:, hh, :], scalar1=wcol[:, c, hh:hh + 1], scalar2=KS, op0=ALU.mult, op1=ALU.mult),
                      ['ps:pTk', 'wcol'], ['KW%d' % hh])
            for hh in range(4):
                kb.op('pe', lambda e: e.matmul(pP[hh][:, 129:258], lhsT=SD[hh][:], rhs=MV[:, c, hh, :], start=True, stop=True), ['SD%d' % hh, 'MV', 'MVones'], [Pn(hh)])
                kb.op('pe', lambda e: e.matmul(pP[hh][:, 258:387], lhsT=q_t(hh), rhs=Cb[hh][:], start=True, stop=True), ['mqT', 'Cb%d' % hh], [Pn(hh)], attach='Cb%d' % hh)
            for hh in range(4):
                kb.op('act', lambda e: e.copy(out=intra_sb[hh][:], in_=pP[hh][:, 129:258]), [Pn(hh)], ['intra%d' % hh])
                kb.op('dve', lambda e: e.scalar_tensor_tensor(out=num[cs][:, hh, :], in0=pP[hh][:, 258:387], scalar=ea[:, c, hh:hh + 1], in1=intra_sb[hh][:], op0=ALU.mult, op1=ALU.add),
                      [Pn(hh), 'ea', 'intra%d' % hh], ['num%d_%d' % (cs, hh)])
            for hh in range(4):
                kb.op('pe', lambda e: e.matmul(pP[hh][:, 0:129], lhsT=KW[hh][:], rhs=MV[:, c, hh, :], start=True, stop=True), ['KW%d' % hh, 'MV', 'MVones'], [Pn(hh)])
            for hh in range(4):
                kb.op('act', lambda e: e.copy(out=Usb[hh][:], in_=pP[hh][:, 0:129]), [Pn(hh)], ['Usb%d' % hh])
            for hh in range(4):
                kb.op('dve', lambda e: e.scalar_tensor_tensor(out=Cf[hh][:], in0=Cf[hh][:], scalar=eaL[:, c, hh:hh + 1], in1=Usb[hh][:], op0=ALU.mult, op1=ALU.add),
                      ['Cf%d' % hh, 'eaL', 'Usb%d' % hh], ['Cf%d' % hh])
                kb.op('act', lambda e: e.copy(out=Cb[hh][:], in_=Cf[hh][:]), ['Cf%d' % hh], ['Cb%d' % hh])
            nn = ['num%d_%d' % (cs, hh) for hh in range(4)]
            kb.op('dve', lambda e: e.tensor_tensor(out=rdn[:], in0=num[cs][:, :, 128], in1=num[cs][:, :, 128], op=ALU.mult), nn, ['rdn'])
            kb.op('dve', lambda e: e.tensor_scalar(out=rdn[:], in0=rdn[:], scalar1=1.0, scalar2=None, op0=ALU.max), ['rdn'], ['rdn'])
            kb.op('act', lambda e: e.activation(out=rdn[:], in_=rdn[:], func=AF.Ln), ['rdn'], ['rdn'])
            kb.op('act', lambda e: e.activation(out=rdn[:], in_=rdn[:], func=AF.Exp, scale=-0.5), ['rdn'], ['rdn'])
            kb.op('dve', lambda e: e.tensor_tensor(out=hsc[cs][:], in0=num[cs][:, :, 0:128], in1=rdn[:].unsqueeze(2).to_broadcast([128, 4, 128]), op=ALU.mult),
                  nn + ['rdn'], ['hsc%d' % cs])
            for hh in range(4):
                kb.op('dve', lambda e: e.bn_stats(out=stats[:, hh, :], in_=hsc[cs][:, hh, :]), ['hsc%d' % cs], ['stats'])
                kb.op('dve', lambda e: e.bn_aggr(out=mvv[:, hh, :], in_=stats[:, hh, :]), ['stats'], ['mvv'])
            kb.op('act', lambda e: e.activation(out=rstd3[:], in_=mvv[:, :, 1], func=AF.Ln, bias=EPS), ['mvv'], ['rstd3'])
            kb.op('act', lambda e: e.activation(out=rstd3[:], in_=rstd3[:], func=AF.Exp, scale=-0.5), ['rstd3'], ['rstd3'])
            for hh in range(4):
                kb.op('dve', lambda e: e.tensor_scalar(out=hsc[cs][:, hh, :], in0=hsc[cs][:, hh, :], scalar1=mvv[:, hh, 0:1], scalar2=rstd3[:, hh:hh + 1], op0=ALU.subtract, op1=ALU.mult),
                      ['hsc%d' % cs, 'mvv', 'rstd3'], ['hsc%d' % cs])
            kb.op('dve', lambda e: e.tensor_tensor(out=yo3[cs][:], in0=hsc[cs][:].rearrange("p h d -> p (h d)"), in1=gg[cs][:], op=ALU.mult),
                  ['hsc%d' % cs, 'gg%d' % cs], ['yo3_%d' % cs])
            kb.dma('sp', y_s[tsl, 512:1024], yo3[cs][:], ['yo3_%d' % cs], [('yml', c)], 'p3_st%d' % cs)
    T.barrier()
    if stop_after == '3':
        return finish(kb, es, es_all, out_d)

    es4 = ExitStack()
    h2T = sbuf(es4, "h2T", [128, 8, S], BF16)
    wx = {}
    for nm, wd in (('q', wxq_d), ('k', wxk_d), ('v', wxv_d), ('o', wxo_d)):
        wx[nm] = sbuf(es4, "p5_w" + nm, [128, 8, D], BF16)
    with ExitStack() as es4a:
        wo = sbuf(es4a, "p4_wo", [128, 8, D], BF16)
        for kc in range(8):
            kb.dma('pool', wo[:, kc, :], wout_d[kc * 128:(kc + 1) * 128, :], [], ['p4_wo'], 'p4_wl')
        yt = [sbuf(es4a, "p4_yt%d" % i, [128, D], BF16) for i in range(2)]
        xt4 = [sbuf(es4a, "p4_xt%d" % i, [128, D], F32) for i in range(2)]
        yT = [sbuf(es4a, "p4_yT%d" % i, [128, 8, 128], BF16) for i in range(2)]
        x1t = [sbuf(es4a, "p4_x1_%d" % i, [128, D], F32) for i in range(2)]
        pTy = [psum(es4a, "p4_pT%d" % i, [128, 8, 128], BF16) for i in range(2)]
        po4 = [[psum(es4a, "p4_po%d_%d" % (i, j), [128, 512], F32) for j in range(2)] for i in range(2)]
        pTn = [psum(es4a, "p4n_pT%d" % i, [128, 8, 128], BF16) for i in range(2)]
        for nm, wd in (('k', wxk_d), ('v', wxv_d), ('q', wxq_d), ('o', wxo_d)):
            for kc in range(8):
                kb.dma('pool', wx[nm][:, kc, :], wd[kc * 128:(kc + 1) * 128, :], [], ['p5_w' + nm], 'p5_wl' + nm)
        emit_norm4 = norm_transpose(es4a, "p4n", None, 1, h2T, NT, pTn, dst_tag='h2T_t',
                                    sb_src=lambda t: (x1t[t % 2][:], ['p4_x1_%d_0' % (t % 2), 'p4_x1_%d_1' % (t % 2)]), emit_only=True)
        def p4_front(t):
            b = t % 2
            tsl = slice(t * 128, (t + 1) * 128)
            kb.dma('sp', yt[b][:], y_s[tsl, :], [], ['p4_yt%d' % b], 'p4_yl%d' % b)
            kb.dma('sp', xt4[b][:], x_d[tsl, :], [], ['p4_xt%d' % b], 'p4_xl%d' % b)
            for c in range(8):
                kb.op('pe', lambda e: e.transpose(out=pTy[b][:, c, :], in_=yt[b][:, c * 128:(c + 1) * 128], identity=ident_b[:]),
                      ['p4_yt%d' % b, 'ident_b'], ['ps:p4_pT%d' % b])
            kb.op('act', lambda e: e.copy(out=yT[b][:], in_=pTy[b][:]), ['ps:p4_pT%d' % b], ['p4_yT%d' % b])

        def p4_back(t):
            b = t % 2
            tsl = slice(t * 128, (t + 1) * 128)
            for hf in range(2):
                for kc in range(8):
                    kb.op('pe', lambda e: e.matmul(po4[b][hf][:], lhsT=yT[b][:, kc, :], rhs=wo[:, kc, hf * 512:(hf + 1) * 512], start=(kc == 0), stop=(kc == 7)),
                          ['p4_yT%d' % b, 'p4_wo'], ['ps:p4_po%d_%d' % (b, hf)])
                kb.op('dve', lambda e: e.tensor_tensor(out=x1t[b][:, hf * 512:(hf + 1) * 512], in0=po4[b][hf][:], in1=xt4[b][:, hf * 512:(hf + 1) * 512], op=ALU.add),
                      ['ps:p4_po%d_%d' % (b, hf), 'p4_xt%d' % b], ['p4_x1_%d_%d' % (b, hf)])
            kb.dma('sp', x1_s[tsl, :], x1t[b][:], ['p4_x1_%d_0' % b, 'p4_x1_%d_1' % b], [('x1', t)], 'p4_st%d' % b)
            emit_norm4(t)

        for t in range(NT):
            p4_front(t)
            if t >= 1:
                p4_back(t - 1)
        p4_back(NT - 1)
    T.barrier()
    if stop_after == '4':
        return finish(kb, es, es_all, out_d)

    es5 = ExitStack()
    memT = sbuf(es5, "memT", [128, 8, MEM], BF16)
    with ExitStack() as es5a:
        pTt = [psum(es5a, "p5a_pT%d" % i, [128, 8, 128], BF16) for i in range(2)]
        norm_transpose(es5a, "p5a", lambda t: mem_d[t * 128:(t + 1) * 128, :], 2, memT, 2, pTt, dst_tag='memT_t')
    T.barrier()
    with ExitStack() as es5b:
        KT = sbuf(es5b, "p5_KT", [128, 8, MEM], BF16)
        Vx = sbuf(es5b, "p5_Vx", [128, 2, D], BF16)
        pA = [psum(es5b, "p5_pA%d" % i, [128, 512], F32) for i in range(2)]
        pS = [psum(es5b, "p5_pS%d" % i, [128, 512], F32) for i in range(2)]
        pZ = psum(es5b, "p5_pZ", [128, 512], F32)
        pO = [psum(es5b, "p5_pO%d" % i, [128, 512], F32) for i in range(2)]
        ia = 0
        for c in range(8):
            p_, pn = pA[ia % 2], 'ps:p5_pA%d' % (ia % 2)
            for kc in range(8):
                kb.op('pe', lambda e: e.matmul(p_[:, 0:MEM], lhsT=wx['k'][:, kc, c * 128:(c + 1) * 128], rhs=memT[:, kc, :], start=(kc == 0), stop=(kc == 7)),
                      ['p5_wk'], [pn])
            kb.op('act', lambda e: e.copy(out=KT[:, c, :], in_=p_[:, 0:MEM]), [pn], ['p5_KT'])
            ia += 1
        for kt in range(2):
            for hf in range(2):
                p_, pn = pA[ia % 2], 'ps:p5_pA%d' % (ia % 2)
                for kc in range(8):
                    kb.op('pe', lambda e: e.matmul(p_[:], lhsT=memT[:, kc, kt * 128:(kt + 1) * 128], rhs=wx['v'][:, kc, hf * 512:(hf + 1) * 512], start=(kc == 0), stop=(kc == 7)),
                          ['p5_wv'], [pn], attach='p5_wv')
                kb.op('act', lambda e: e.copy(out=Vx[:, kt, hf * 512:(hf + 1) * 512], in_=p_[:]), [pn], ['p5_Vx'])
                ia += 1
        qTx = [sbuf(es5b, "p5_qT%d" % i, [128, 8, 512], BF16) for i in range(2)]
        PTx = [sbuf(es5b, "p5_PT%d" % i, [128, 2, 512], BF16) for i in range(2)]
        rZ = [sbuf(es5b, "p5_rZ%d" % i, [128, 512], F32) for i in range(2)]
        oTx = [sbuf(es5b, "p5_oT%d" % i, [128, 8, 512], BF16) for i in range(2)]
        x1t5 = [sbuf(es5b, "p5_x1_%d" % i, [128, D], F32) for i in range(2)]
        x2t5 = [sbuf(es5b, "p5_x2_%d" % i, [128, D], F32) for i in range(2)]
        hi_ = 0
        ti_ = 0
        for blk in range(NB):
            bs = blk % 2
            bsl = slice(blk * 512, (blk + 1) * 512)
            for c in range(8):
                p_, pn = pA[ia % 2], 'ps:p5_pA%d' % (ia % 2)
                for kc in range(8):
                    kb.op('pe', lambda e: e.matmul(p_[:], lhsT=wx['q'][:, kc, c * 128:(c + 1) * 128], rhs=h2T[:, kc, bsl], start=(kc == 0), stop=(kc == 7)),
                          ['p5_wq'], [pn])
                kb.op('act', lambda e: e.copy(out=qTx[bs][:, c, :], in_=p_[:]), [pn], ['p5_qT%d_%d' % (bs, c)])
                ia += 1
            for h in range(4):
                hs = hi_ % 2
                for kt in range(2):
                    p_, pn = pS[kt], 'ps:p5_pS%d' % kt
                    for dc in range(2):
                        kb.op('pe', lambda e: e.matmul(p_[:], lhsT=KT[:, 2 * h + dc, kt * 128:(kt + 1) * 128], rhs=qTx[bs][:, 2 * h + dc, :], start=(dc == 0), stop=(dc == 1)),
                              ['p5_KT', 'p5_qT%d_%d' % (bs, 2 * h + dc)], [pn])
                    kb.op('act', lambda e: e.activation(out=PTx[hs][:, kt, :], in_=p_[:], func=AF.Exp, scale=1.0 / 16.0), [pn], ['p5_PT%d_%d' % (hs, kt)])
                for kt in range(2):
                    kb.op('pe', lambda e: e.matmul(pZ[:], lhsT=ones_bb[:], rhs=PTx[hs][:, kt, :], start=(kt == 0), stop=(kt == 1)),
                          ['ones_bb', 'p5_PT%d_%d' % (hs, kt)], ['ps:p5_pZ'])
                kb.op('dve', lambda e: e.reciprocal(out=rZ[hs][:], in_=pZ[:]), ['ps:p5_pZ'], ['p5_rZ%d' % hs])
                for dc in range(2):
                    p_, pn = pO[dc], 'ps:p5_pO%d' % dc
                    for kt in range(2):
                        kb.op('pe', lambda e: e.matmul(p_[:], lhsT=Vx[:, kt, h * 256 + dc * 128:h * 256 + (dc + 1) * 128], rhs=PTx[hs][:, kt, :], start=(kt == 0), stop=(kt == 1)),
                              ['p5_Vx', 'p5_PT%d_%d' % (hs, kt)], [pn])
                    kb.op('dve', lambda e: e.tensor_tensor(out=oTx[bs][:, 2 * h + dc, :], in0=p_[:], in1=rZ[hs][:], op=ALU.mult),
                          [pn, 'p5_rZ%d' % hs], ['p5_oT%d_%d' % (bs, 2 * h + dc)])
                hi_ += 1
            for sub in range(4):
                t = blk * 4 + sub
                ts_ = ti_ % 2
                tsl = slice(t * 128, (t + 1) * 128)
                kb.dma('sp', x1t5[ts_][:], x1_s[tsl, :], [], ['p5_x1_%d' % ts_], 'p5_xl%d' % ts_)
                for hf in range(2):
                    p_, pn = pA[ia % 2], 'ps:p5_pA%d' % (ia % 2)
                    for kc in range(8):
                        kb.op('pe', lambda e: e.matmul(p_[:], lhsT=oTx[bs][:, kc, sub * 128:(sub + 1) * 128], rhs=wx['o'][:, kc, hf * 512:(hf + 1) * 512], start=(kc == 0), stop=(kc == 7)),
                              ['p5_oT%d_%d' % (bs, kc), 'p5_wo'], [pn])
                    kb.op('dve', lambda e: e.tensor_tensor(out=x2t5[ts_][:, hf * 512:(hf + 1) * 512], in0=p_[:], in1=x1t5[ts_][:, hf * 512:(hf + 1) * 512], op=ALU.add),
                          [pn, 'p5_x1_%d' % ts_], ['p5_x2_%d_%d' % (ts_, hf)])
                    ia += 1
                kb.dma('sp', x2_s[tsl, :], x2t5[ts_][:], ['p5_x2_%d_0' % ts_, 'p5_x2_%d_1' % ts_], [('x2', t)], 'p5_st%d' % ts_)
                ti_ += 1
    es5.close()
    es4.close()
    T.barrier()
    if stop_after == '5':
        return finish(kb, es, es_all, out_d)

    es6w = ExitStack()
    w1b = [sbuf(es6w, "p6_w1_%d" % i, [128, 8, DEXP], BF16) for i in range(2)]
    w3b = [sbuf(es6w, "p6_w3_%d" % i, [128, 8, DEXP], BF16) for i in range(2)]
    w2b = [sbuf(es6w, "p6_w2_%d" % i, [128, 4, D], BF16) for i in range(2)]
    kb.dma('pool', w1b[0][:], w1_d[0].rearrange("(c p) n -> p c n", p=128), [], ['w1_0'], 'p6_w1l0')
    kb.dma('pool', w3b[0][:], w3_d[0].rearrange("(c p) n -> p c n", p=128), [], ['w3_0'], 'p6_w3l0')
    kb.dma('pool', w2b[0][:], w2_d[0].rearrange("(c p) n -> p c n", p=128), [], ['w2_0'], 'p6_w2l0')
    with ExitStack() as es5c:
        gbc = sbuf(es5c, "p5c_gbc", [128, D], F32)
        bbc = sbuf(es5c, "p5c_bbc", [128, 36], F32)
        eoff = sbuf(es5c, "p5c_eoff", [128, NEXP], F32)
        trisf = sbuf(es5c, "p5c_trisf", [128, 128], F32)
        trisb = sbuf(es5c, "p5c_trisb", [128, 128], BF16)
        wr = sbuf(es5c, "p5c_wr", [128, 8, 36], BF16)
        tokid = sbuf(es5c, "p5c_tokid", [128, NT], I32)
        macc = sbuf(es5c, "p5c_macc", [128, NEXP], BF16)
        kb.dma('sp', gbc[:], gffn_d.partition_broadcast(128), [], ['gbc'], 'p5c_c0')
        kb.dma('sp', bbc[:], br_d.partition_broadcast(128), [], ['bbc'], 'p5c_c1')
        kb.dma('sp', eoff[:], eoff_d.partition_broadcast(128), [], ['eoff'], 'p5c_c2')
        kb.dma('sp', trisf[:], tris_d[:, :], [], ['trisf'], 'p5c_c3')
        kb.dma('sp', tokid[:], tokid_d[:, :], [], ['tokid'], 'p5c_c4')
        for kc in range(8):
            kb.dma('pool', wr[:, kc, :], wr_d[kc * 128:(kc + 1) * 128, :], [], ['wr'], 'p5c_c5')
        kb.op('dve', lambda e: e.tensor_copy(out=trisb[:], in_=trisf[:]), ['trisf'], ['trisb'])
        kb.op('pool', lambda e: e.memset(macc[:], 0.0), [], ['macc'])
        x2t = [sbuf(es5c, "p5c_x2_%d" % i, [128, D], F32) for i in range(2)]
        junkc = sbuf(es5c, "p5c_junk", [128, D], BF16)
        ssq = [sbuf(es5c, "p5c_ssq%d" % i, [128, 1], F32) for i in range(2)]
        h3 = [sbuf(es5c, "p5c_h3_%d" % i, [128, D], BF16) for i in range(2)]
        h3T = [sbuf(es5c, "p5c_h3T%d" % i, [128, 8, 128], BF16) for i in range(2)]
        pT5 = [psum(es5c, "p5c_pT%d" % i, [128, 8, 128], BF16) for i in range(2)]
        pL = [psum(es5c, "p5c_pL%d" % i, [128, 512], F32) for i in range(2)]
        pPos = [psum(es5c, "p5c_pPos%d" % i, [128, 512], F32) for i in range(2)]
        lg = sbuf(es5c, "p5c_lg", [128, 36], F32)
        sm = sbuf(es5c, "p5c_sm", [128, 16], F32)
        ge = sbuf(es5c, "p5c_ge", [128, 4], F32)
        oh = sbuf(es5c, "p5c_oh", [128, 4], F32)
        lm = sbuf(es5c, "p5c_lm", [128, 4, 8], F32)
        m8 = sbuf(es5c, "p5c_m8", [128, 8], F32)
        m1 = sbuf(es5c, "p5c_m1", [128, NEXP], F32)
        m2 = sbuf(es5c, "p5c_m2", [128, NEXP], F32)
        maskb5 = [sbuf(es5c, "p5c_mask%d" % i, [128, NEXP], BF16) for i in range(2)]
        sl = sbuf(es5c, "p5c_sl", [128, NEXP], F32)
        junk32 = sbuf(es5c, "p5c_junk32", [128, NEXP], F32)
        sf = sbuf(es5c, "p5c_sf", [128, 2], F32)
        lmf = lm[:].rearrange("p g e -> p (g e)")
        for t in range(NT):
            b = t % 2
            tsl = slice(t * 128, (t + 1) * 128)
            kb.dma('sp', x2t[b][:], x2_s[tsl, :], [], ['x2t%d' % b], 'p5c_xl%d' % b)
            kb.op('act', lambda e: e.activation(out=junkc[:], in_=x2t[b][:], func=AF.Square, accum_out=ssq[b][:]), ['x2t%d' % b], ['junkc', 'ssq%d' % b])
            kb.op('act', lambda e: e.activation(out=ssq[b][:], in_=ssq[b][:], func=AF.Ln, scale=1.0 / D, bias=EPS), ['ssq%d' % b], ['ssq%d' % b])
            kb.op('act', lambda e: e.activation(out=ssq[b][:], in_=ssq[b][:], func=AF.Exp, scale=-0.5), ['ssq%d' % b], ['ssq%d' % b])
            kb.op('dve', lambda e: e.scalar_tensor_tensor(out=h3[b][:], in0=x2t[b][:], scalar=ssq[b][:, 0:1], in1=gbc[:], op0=ALU.mult, op1=ALU.mult),
                  ['x2t%d' % b, 'ssq%d' % b, 'gbc'], ['h3_%d' % b])
            for c in range(8):
                kb.op('pe', lambda e: e.transpose(out=pT5[b][:, c, :], in_=h3[b][:, c * 128:(c + 1) * 128], identity=ident_b[:]), ['h3_%d' % b, 'ident_b'], ['ps:p5c_pT%d' % b])
            kb.op('act', lambda e: e.copy(out=h3T[b][:], in_=pT5[b][:]), ['ps:p5c_pT%d' % b], ['h3T%d' % b])
            for kc in range(8):
                kb.op('pe', lambda e: e.matmul(pL[b][:, 0:36], lhsT=h3T[b][:, kc, :], rhs=wr[:, kc, :], start=(kc == 0), stop=(kc == 7)), ['h3T%d' % b, 'wr'], ['ps:p5c_pL%d' % b])
            kb.op('dve', lambda e: e.tensor_tensor(out=lg[:], in0=pL[b][:, 0:36], in1=bbc[:], op=ALU.add), ['ps:p5c_pL%d' % b, 'bbc'], ['lg'])
            kb.op('dve', lambda e: e.reduce_max(out=sm[:, 0:1], in_=lg[:, 0:4], axis=AX.X), ['lg'], ['sm0'])
            kb.op('dve', lambda e: e.tensor_scalar(out=sm[:, 1:2], in0=sm[:, 0:1], scalar1=-1.0, scalar2=None, op0=ALU.mult), ['sm0'], ['sm1'])
            kb.op('act', lambda e: e.activation(out=ge[:], in_=lg[:, 0:4], func=AF.Exp, bias=sm[:, 1:2], accum_out=sm[:, 2:3]), ['lg', 'sm1'], ['ge', 'sm2'])
            kb.op('dve', lambda e: e.reciprocal(out=sm[:, 3:4], in_=sm[:, 2:3]), ['sm2'], ['sm3'])
            kb.op('dve', lambda e: e.tensor_scalar(out=oh[:], in0=lg[:, 0:4], scalar1=sm[:, 0:1], scalar2=None, op0=ALU.is_equal), ['lg', 'sm0'], ['oh'])
            kb.op('dve', lambda e: e.tensor_scalar(out=oh[:], in0=oh[:], scalar1=-1.0, scalar2=1.0e9, op0=ALU.add, op1=ALU.mult), ['oh'], ['oh'])
            kb.op('dve', lambda e: e.tensor_tensor(out=lm[:], in0=lg[:, 4:36].rearrange("p (g e) -> p g e", g=4), in1=oh[:].unsqueeze(2).to_broadcast([128, 4, 8]), op=ALU.add),
                  ['lg', 'oh'], ['lm'])
            kb.op('dve', lambda e: e.max(out=m8[:], in_=lmf), ['lm'], ['m8'])
            kb.op('dve', lambda e: e.tensor_scalar(out=sm[:, 4:5], in0=m8[:, 0:1], scalar1=-1.0, scalar2=None, op0=ALU.mult), ['m8'], ['sm4'])
            kb.op('act', lambda e: e.activation(out=sm[:, 5:6], in_=m8[:, 1:2], func=AF.Exp, bias=sm[:, 4:5]), ['m8', 'sm4'], ['sm5'])
            kb.op('dve', lambda e: e.tensor_scalar(out=sm[:, 6:7], in0=sm[:, 5:6], scalar1=1.0, scalar2=None, op0=ALU.add), ['sm5'], ['sm6'])
            kb.op('dve', lambda e: e.reciprocal(out=sm[:, 7:8], in_=sm[:, 6:7]), ['sm6'], ['sm7'])
            kb.op('dve', lambda e: e.tensor_tensor(out=comb_w[:, t, 0:1], in0=sm[:, 7:8], in1=sm[:, 3:4], op=ALU.mult), ['sm7', 'sm3'], [('cw', t)])
            kb.op('dve', lambda e: e.tensor_tensor(out=comb_w[:, t, 1:2], in0=comb_w[:, t, 0:1], in1=sm[:, 5:6], op=ALU.mult), [('cw', t), 'sm5'], [('cw2', t)])
            kb.op('dve', lambda e: e.tensor_scalar(out=m1[:], in0=lmf, scalar1=m8[:, 0:1], scalar2=None, op0=ALU.is_equal), ['lm', 'm8'], ['m1'])
            kb.op('dve', lambda e: e.tensor_scalar(out=m2[:], in0=lmf, scalar1=m8[:, 1:2], scalar2=None, op0=ALU.is_equal), ['lm', 'm8'], ['m2'])
            kb.op('dve', lambda e: e.tensor_tensor(out=maskb5[b][:], in0=m1[:], in1=m2[:], op=ALU.add), ['m1', 'm2'], ['mask%d' % b])
            kb.op('pe', lambda e: e.matmul(pPos[b][:, 0:NEXP], lhsT=trisb[:], rhs=maskb5[b][:], start=True, stop=False), ['trisb', 'mask%d' % b], ['ps:p5c_pPos%d' % b])
            kb.op('pe', lambda e: e.matmul(pPos[b][:, 0:NEXP], lhsT=ones_bb[:], rhs=macc[:], start=False, stop=True), ['ones_bb', 'macc'], ['ps:p5c_pPos%d' % b], attach='macc')
            kb.op('dve', lambda e: e.tensor_tensor(out=macc[:], in0=macc[:], in1=maskb5[b][:], op=ALU.add), ['macc', 'mask%d' % b], ['macc'])
            kb.op('dve', lambda e: e.scalar_tensor_tensor(out=sl[:], in0=pPos[b][:, 0:NEXP], scalar=float(CAP - 1), in1=eoff[:], op0=ALU.min, op1=ALU.add),
                  ['ps:p5c_pPos%d' % b, 'eoff'], ['sl'])
            kb.op('dve', lambda e: e.scalar_tensor_tensor(out=junk32[:], in0=sl[:], scalar=1.0, in1=m1[:], op0=ALU.mult, op1=ALU.mult, accum_out=sf[:, 0:1]), ['sl', 'm1'], ['junk32', 'sf0'])
            kb.op('dve', lambda e: e.scalar_tensor_tensor(out=junk32[:], in0=sl[:], scalar=1.0, in1=m2[:], op0=ALU.mult, op1=ALU.mult, accum_out=sf[:, 1:2]), ['sl', 'm2'], ['junk32', 'sf1'])
            kb.op('dve', lambda e: e.tensor_copy(out=slot_i[:, t, :], in_=sf[:]), ['sf0', 'sf1'], [('slot', t)])
            for k2 in range(2):
                T.op('pool', lambda e: e.indirect_dma_start(out=Xs_d[:, :], out_offset=bass.IndirectOffsetOnAxis(ap=slot_i[:, t, k2:k2 + 1], axis=0),
                                                            in_=h3[b][:], in_offset=None),
                     reads=['h3_%d' % b, ('slot', t)], writes=[('Xs', t, k2)], lane='p5c_sc%d_%d' % (b, k2))
    T.barrier()
    if stop_after == '5b':
        return finish(kb, es, es_all, out_d)

    with ExitStack() as es6:
        NXB = 2 * CHB
        xb = [sbuf(es6, "p6_xb%d" % i, [128, D], BF16) for i in range(NXB)]
        CHN = CHB * 128
        XT = [sbuf(es6, "p6_XT%d" % i, [128, 8, CHN], BF16) for i in range(2)]
        s1 = [sbuf(es6, "p6_s1_%d" % i, [128, CHN], BF16) for i in range(2)]
        gT6 = [sbuf(es6, "p6_gT%d" % i, [128, 4, CHN], BF16) for i in range(2)]
        ysb = [sbuf(es6, "p6_y%d" % i, [128, D], BF16) for i in range(2)]
        pT6 = [psum(es6, "p6_pT%d" % i, [128, 8, 128], BF16) for i in range(2)]
        p1 = [psum(es6, "p6_p1_%d" % i, [128, 512], F32) for i in range(2)]
        p3 = [psum(es6, "p6_p3_%d" % i, [128, 512], F32) for i in range(2)]
        py = [psum(es6, "p6_py%d" % i, [128, 512], F32) for i in range(2)]
        chunks = [(ex, hb) for ex in range(NEXP) for hb in range(CAPB // CHB)]
        cnt6 = {'mi': 0, 'yi': 0}

        def load_w(ex):
            ws = ex % 2
            kb.dma('pool', w1b[ws][:], w1_d[ex].rearrange("(c p) n -> p c n", p=128), [], ['w1_%d' % ws], 'p6_w1l%d' % ws)
            kb.dma('pool', w3b[ws][:], w3_d[ex].rearrange("(c p) n -> p c n", p=128), [], ['w3_%d' % ws], 'p6_w3l%d' % ws)
            kb.dma('pool', w2b[ws][:], w2_d[ex].rearrange("(c p) n -> p c n", p=128), [], ['w2_%d' % ws], 'p6_w2l%d' % ws)

        def load_x(i):
            ex, hb = chunks[i]
            row0 = ex * CAP + hb * CHN
            for j in range(CHB):
                xs_ = (i * CHB + j) % NXB
                kb.dma('sp', xb[xs_][:], Xs_d[row0 + j * 128:row0 + (j + 1) * 128, :], [], ['xb%d' % xs_], 'p6_xl%d' % xs_)

        def stage_T(i):
            cs = i % 2
            for j in range(CHB):
                xi_ = i * CHB + j
                xs_ = xi_ % NXB
                pt_ = xi_ % 2
                for c in range(8):
                    kb.op('pe', lambda e: e.transpose(out=pT6[pt_][:, c, :], in_=xb[xs_][:, c * 128:(c + 1) * 128], identity=ident_b[:]), ['xb%d' % xs_, 'ident_b'], ['ps:p6_pT%d' % pt_])
                if xi_ % 2 == 0:
                    kb.op('act', lambda e: e.copy(out=XT[cs][:, :, j * 128:(j + 1) * 128], in_=pT6[pt_][:]), ['ps:p6_pT%d' % pt_], ['XT%d_%d' % (cs, j)])
                else:
                    kb.op('dve', lambda e: e.tensor_copy(out=XT[cs][:, :, j * 128:(j + 1) * 128], in_=pT6[pt_][:]), ['ps:p6_pT%d' % pt_], ['XT%d_%d' % (cs, j)])

        def stage_A(i):
            ex, hb = chunks[i]
            ws = ex % 2
            cs = i % 2
            xtn = ['XT%d_%d' % (cs, j) for j in range(CHB)]
            for m in range(4):
                ms = cnt6['mi'] % 2
                for kc in range(8):
                    kb.op('pe', lambda e: e.matmul(p1[ms][:, 0:CHN], lhsT=w1b[ws][:, kc, m * 128:(m + 1) * 128], rhs=XT[cs][:, kc, :], start=(kc == 0), stop=(kc == 7)),
                          ['w1_%d' % ws] + xtn, ['ps:p6_p1_%d' % ms])
                for kc in range(8):
                    kb.op('pe', lambda e: e.matmul(p3[ms][:, 0:CHN], lhsT=w3b[ws][:, kc, m * 128:(m + 1) * 128], rhs=XT[cs][:, kc, :], start=(kc == 0), stop=(kc == 7)),
                          ['w3_%d' % ws] + xtn, ['ps:p6_p3_%d' % ms])
                kb.op('act', lambda e: e.activation(out=s1[ms][:], in_=p1[ms][:, 0:CHN], func=AF.Silu), ['ps:p6_p1_%d' % ms], ['s1_%d' % ms])
                kb.op('dve', lambda e: e.tensor_tensor(out=gT6[cs][:, m, :], in0=p3[ms][:, 0:CHN], in1=s1[ms][:], op=ALU.mult), ['ps:p6_p3_%d' % ms, 's1_%d' % ms], ['gT%d_%d' % (cs, m)])
                cnt6['mi'] += 1

        def stage_Y(i):
            ex, hb = chunks[i]
            ws = ex % 2
            cs = i % 2
            row0 = ex * CAP + hb * CHN
            gtn = ['gT%d_%d' % (cs, m) for m in range(4)]
            for j in range(CHB):
                ys_ = cnt6['yi'] % 2
                for hf in range(2):
                    for kc in range(4):
                        kb.op('pe', lambda e: e.matmul(py[hf][:], lhsT=gT6[cs][:, kc, j * 128:(j + 1) * 128], rhs=w2b[ws][:, kc, hf * 512:(hf + 1) * 512], start=(kc == 0), stop=(kc == 3)),
                              gtn + ['w2_%d' % ws], ['ps:p6_py%d' % hf])
                    if hf == 0:
                        kb.op('act', lambda e: e.copy(out=ysb[ys_][:, 0:512], in_=py[0][:]), ['ps:p6_py0'], ['ysb%d_0' % ys_])
                    else:
                        kb.op('dve', lambda e: e.tensor_copy(out=ysb[ys_][:, 512:1024], in_=py[1][:]), ['ps:p6_py1'], ['ysb%d_1' % ys_])
                kb.dma('sp', Y_d[row0 + j * 128:row0 + (j + 1) * 128, :], ysb[ys_][:], ['ysb%d_0' % ys_, 'ysb%d_1' % ys_], [('Y', ex, hb, j)], 'p6_st%d' % ys_)
                cnt6['yi'] += 1

        nch = len(chunks)
        load_x(0)
        stage_T(0)
        for i in range(nch):
            ex, hb = chunks[i]
            if hb == 0 and ex + 1 < NEXP:
                load_w(ex + 1)
            if i + 1 < nch:
                load_x(i + 1)
            stage_A(i)
            if i + 1 < nch:
                stage_T(i + 1)
            stage_Y(i)
    es6w.close()
    T.barrier()
    if stop_after == '6':
        return finish(kb, es, es_all, out_d)

    with ExitStack() as es7:
        gfb = sbuf(es7, "p7_gfb", [128, D], F32)
        kb.dma('sp', gfb[:], gfin_d.partition_broadcast(128), [], ['gfb'], 'p7_c0')
        x2t7 = [sbuf(es7, "p7_x2_%d" % i, [128, D], F32) for i in range(2)]
        y1 = [sbuf(es7, "p7_y1_%d" % i, [128, D], BF16) for i in range(2)]
        y2 = [sbuf(es7, "p7_y2_%d" % i, [128, D], BF16) for i in range(2)]
        x3 = [sbuf(es7, "p7_x3_%d" % i, [128, D], F32) for i in range(2)]
        junk7 = sbuf(es7, "p7_junk", [128, D], BF16)
        ssq7 = [sbuf(es7, "p7_ssq%d" % i, [128, 1], F32) for i in range(2)]
        o7 = [sbuf(es7, "p7_o%d" % i, [128, D], F32) for i in range(2)]
        for t in range(NT):
            b = t % 2
            tsl = slice(t * 128, (t + 1) * 128)
            kb.dma('sp', x2t7[b][:], x2_s[tsl, :], [], ['x2t%d' % b], 'p7_xl%d' % b)
            for k2, yy in ((0, y1), (1, y2)):
                T.op('pool', lambda e: e.indirect_dma_start(out=yy[b][:], out_offset=None, in_=Y_d[:, :],
                                                            in_offset=bass.IndirectOffsetOnAxis(ap=slot_i[:, t, k2:k2 + 1], axis=0)),
                     reads=[], writes=['y%d_%d' % (k2, b)], lane='p7_g%d_%d' % (k2, b))
            kb.op('dve', lambda e: e.scalar_tensor_tensor(out=x3[b][:], in0=y1[b][:], scalar=comb_w[:, t, 0:1], in1=x2t7[b][:], op0=ALU.mult, op1=ALU.add),
                  ['y0_%d' % b, 'x2t%d' % b], ['x3_%d' % b])
            kb.op('dve', lambda e: e.scalar_tensor_tensor(out=x3[b][:], in0=y2[b][:], scalar=comb_w[:, t, 1:2], in1=x3[b][:], op0=ALU.mult, op1=ALU.add),
                  ['y1_%d' % b, 'x3_%d' % b], ['x3_%d' % b])
            kb.op('act', lambda e: e.activation(out=junk7[:], in_=x3[b][:], func=AF.Square, accum_out=ssq7[b][:]), ['x3_%d' % b], ['junk7', 'ssq7_%d' % b])
            kb.op('act', lambda e: e.activation(out=ssq7[b][:], in_=ssq7[b][:], func=AF.Ln, scale=1.0 / D, bias=EPS), ['ssq7_%d' % b], ['ssq7_%d' % b])
            kb.op('act', lambda e: e.activation(out=ssq7[b][:], in_=ssq7[b][:], func=AF.Exp, scale=-0.5), ['ssq7_%d' % b], ['ssq7_%d' % b])
            kb.op('dve', lambda e: e.scalar_tensor_tensor(out=o7[b][:], in0=x3[b][:], scalar=ssq7[b][:, 0:1], in1=gfb[:], op0=ALU.mult, op1=ALU.mult),
                  ['x3_%d' % b, 'ssq7_%d' % b, 'gfb'], ['o7_%d' % b])
            kb.dma('sp', out_d[tsl, :], o7[b][:], ['o7_%d' % b], [('out', t)], 'p7_st%d' % b)
    return finish(kb, es, es_all, out_d)


def finish(kb, es, es_all, out_d):
    kb.T.barrier()
    return kb


def prep_inputs(inputs, b):
    f = lambda k: np.ascontiguousarray(np.asarray(inputs[k], dtype=np.float32))
    c = host_consts()
    m = {}
    m['x'] = f('x')[b]
    m['mem'] = f('mem')[b]
    perm = win_perm()
    m['w_in_ext'] = np.ascontiguousarray(f('w_in')[0][:, perm])
    m['w_out'] = f('w_out')[0]

    def g128(v):
        return v.reshape(8, 128).T
    m['gT'] = np.ascontiguousarray(np.concatenate([g128(f('norm_mix_g')[0]), g128(f('norm_x_g')[0]),
                                                   g128(f('norm_mem_g')[0]), g128(f('norm_ffn_g')[0])], axis=1))
    m['g_final'] = f('norm_final_g')
    m['g_ffn_row'] = f('norm_ffn_g')[0]
    kk = np.arange(128)[:, None]
    qq = np.arange(128)[None, :]
    m['tri_strict'] = (kk < qq).astype(np.float32)
    m['eoff'] = (np.arange(NEXP) * CAP).astype(np.float32)
    m['tokid'] = (np.arange(NT)[None, :] * 128 + np.arange(128)[:, None]).astype(np.int32)
    m['cosT'] = c['cosT']; m['sinT'] = c['sinT']; m['ident_f'] = c['ident_f']; m['maskb'] = c['maskb']; m['tri_incl'] = c['tri_incl']
    m['da_lambda'] = f('da_lambda')[0].reshape(256)
    m['da_subln_g'] = f('da_subln_g')[0]
    cw = f('ml_conv_w')[0][:, 0, :]
    cb = f('ml_conv_b')[0]
    convT = np.zeros((128, 40), np.float32)
    for gidx in range(8):
        cols = slice(gidx * 128, (gidx + 1) * 128)
        convT[:, gidx * 5:gidx * 5 + 4] = cw[:, cols].T
        convT[:, gidx * 5 + 4] = cb[cols]
    m['convT'] = convT
    m['ml_gate_b'] = f('ml_gate_b')[0].reshape(8)
    m['ml_norm_g'] = f('ml_norm_g')[0]
    for k in ('w_xq', 'w_xk', 'w_xv', 'w_xo'):
        m[k] = f(k)[0]
    m['w_router'] = np.ascontiguousarray(np.concatenate([f('w_router_group')[0], f('w_router_expert')[0]], axis=1))
    m['b_router'] = np.concatenate([f('b_router_group')[0], f('b_router_expert')[0]])
    m['w1'] = f('w1')[0]; m['w3'] = f('w3')[0]; m['w2'] = f('w2')[0]
    return m


_CACHE = {}


def kernel(**inputs):
    if 'kb' not in _CACHE:
        _CACHE['kb'] = build()
    kb = _CACHE['kb']
    n = 8
    maps = []
    for b in range(n):
        m = prep_inputs(inputs, b)
        maps.append({k: v for k, v in m.items() if k in kb.inp})
    res = run_bass_kernel_spmd(kb.nc, maps, core_ids=list(range(n)))
    return np.stack([np.asarray(r["out"], dtype=np.float32) for r in res.results], axis=0)
```
